# Optimizing a Trainium2 kernel written in Bass

```python
import numpy as np
import jax
import jax.numpy as jnp
from jax import lax

D_MODEL = 1024
BATCH = 16
SEQ = 2048
DEPTH = 2

HEAD_DIM = 64
ROT_DIM = HEAD_DIM // 4
ROPE_THETA = 500000.0
NORM_EPS = 1e-6
Q_BLOCK = 128
D_FF = 2816
NEG_INF = -1e30

A_GROUPS = 4
A_CHUNK = 128
A_WIDTH = A_GROUPS * HEAD_DIM
B_HEADS = 12
B_KV_HEADS = 3
B_WIDTH = B_HEADS * HEAD_DIM
B_KV_WIDTH = B_KV_HEADS * HEAD_DIM
CMP_LEN = 32
CMP_STRIDE = 16
CMP_HIDDEN = 256
SLC_BLOCK = 64
SLC_TOPN = 8
WINDOW = 512
N_BRANCH = 3
FORCE_SCORE = 1e4
C_HEADS = 8
C_WIDTH = C_HEADS * HEAD_DIM
IDX_HEADS = 4
IDX_DIM = 32
IDX_ROT = IDX_DIM // 4
DSA_TOPK = 256
D_HEADS = 8
D_WIDTH = D_HEADS * HEAD_DIM
MOBA_BLOCK = 256
MOBA_TOPK = 3

EVEN_SPLITS = (2 * A_WIDTH, B_WIDTH) + (B_KV_WIDTH,) * 6 + (B_HEADS * N_BRANCH,)
ODD_SPLITS = (C_WIDTH, HEAD_DIM, HEAD_DIM, IDX_HEADS * IDX_DIM, IDX_DIM, IDX_HEADS, D_WIDTH, D_WIDTH, D_WIDTH)
EVEN_IN = sum(EVEN_SPLITS)
ODD_IN = sum(ODD_SPLITS)
MIX_OUT = A_WIDTH + B_WIDTH
N_EVEN = (DEPTH + 1) // 2
N_ODD = DEPTH // 2

kernel_name = 'hybrid_gmlp_nsa_dsa_moba_macaron'


def rmsnorm(x, g):
    xf = x.astype(jnp.float32)
    y = xf * lax.rsqrt(jnp.mean(xf * xf, axis=-1, keepdims=True) + NORM_EPS)
    return (y * g.astype(jnp.float32)).astype(x.dtype)


def rope_tables(positions, rot_dim):
    inv_freq = ROPE_THETA ** (-jnp.arange(0, rot_dim, 2, dtype=jnp.float32) / rot_dim)
    ang = positions.astype(jnp.float32)[..., None] * inv_freq
    return jnp.cos(ang), jnp.sin(ang)


def partial_rope(x, cos, sin):
    half = cos.shape[-1]
    bshape = cos.shape[:2] + (1,) * (x.ndim - 3) + (half,)
    c = cos.reshape(bshape).astype(x.dtype)
    s = sin.reshape(bshape).astype(x.dtype)
    x1, x2, rest = x[..., :half], x[..., half:2 * half], x[..., 2 * half:]
    return jnp.concatenate([x1 * c - x2 * s, x2 * c + x1 * s, rest], axis=-1)


def masked_softmax(s, mask):
    s = jnp.where(mask, s.astype(jnp.float32), NEG_INF)
    return jnp.where(mask, jax.nn.softmax(s, axis=-1), 0.0)


def swiglu_ffn(x, g, w_gate, w_up, w_down):
    h = rmsnorm(x, g)
    return (jax.nn.silu(h @ w_gate) * (h @ w_up)) @ w_down


def split_cols(z, sizes):
    return jnp.split(z, [int(c) for c in np.cumsum(sizes)[:-1]], axis=-1)


def rows(a, b, q0):
    return lax.dynamic_slice_in_dim(a[b], q0, Q_BLOCK, axis=0)


def map_query_blocks(fn, batch, seq):
    n_qb = seq // Q_BLOCK

    def step(i):
        return fn(i // n_qb, (i % n_qb) * Q_BLOCK)

    out = lax.map(step, jnp.arange(batch * n_qb, dtype=jnp.int32))
    return out.reshape((batch, seq) + out.shape[2:])


def chunked_gmlp(z, sgu_norm, sgu_w, sgu_b):
    bsz, seq, _ = z.shape
    u, v = jnp.split(jax.nn.gelu(z), 2, axis=-1)
    v = rmsnorm(v, sgu_norm).reshape(bsz, seq // A_CHUNK, A_CHUNK, A_GROUPS, HEAD_DIM)
    causal = jnp.tril(jnp.ones((A_CHUNK, A_CHUNK), dtype=bool))
    w = jnp.where(causal, sgu_w, jnp.zeros_like(sgu_w))
    mixed = jnp.einsum('gts,bcsgd->bctgd', w, v) + sgu_b.T[:, :, None]
    return u * mixed.reshape(bsz, seq, A_WIDTH)


def compress_blocks(t, pos_emb, w1, w2):
    bsz, seq = t.shape[:2]
    n_cmp = (seq - CMP_LEN) // CMP_STRIDE + 1
    idx = jnp.arange(n_cmp)[:, None] * CMP_STRIDE + jnp.arange(CMP_LEN)[None, :]
    blk = t[:, idx] + pos_emb[:, None, :]
    blk = jnp.moveaxis(blk, 3, 2).reshape(bsz, n_cmp, B_KV_HEADS, CMP_LEN * HEAD_DIM)
    return jax.nn.gelu(blk @ w1) @ w2


def cmp_to_slc_overlap(n_cmp, n_slc):
    c0 = np.arange(n_cmp) * CMP_STRIDE
    s0 = np.arange(n_slc) * SLC_BLOCK
    m = (c0[:, None] < s0[None, :] + SLC_BLOCK) & (c0[:, None] + CMP_LEN > s0[None, :])
    return jnp.asarray(m, dtype=jnp.float32)


def nsa_attention(q_rot, q_raw, k_cmp, v_cmp, k_slc, v_slc, k_win, v_win, gates):
    bsz, seq = q_rot.shape[:2]
    grp = B_HEADS // B_KV_HEADS
    scale = HEAD_DIM ** -0.5
    n_cmp = k_cmp.shape[1]
    n_slc = seq // SLC_BLOCK
    top_n = min(SLC_TOPN, n_slc)
    overlap = cmp_to_slc_overlap(n_cmp, n_slc)
    cmp_end = jnp.arange(n_cmp) * CMP_STRIDE + CMP_LEN - 1
    blk_id = jnp.arange(n_slc)
    k_blk = k_slc.reshape(bsz, n_slc, SLC_BLOCK, B_KV_HEADS, HEAD_DIM).transpose(0, 3, 1, 2, 4)
    v_blk = v_slc.reshape(bsz, n_slc, SLC_BLOCK, B_KV_HEADS, HEAD_DIM).transpose(0, 3, 1, 2, 4)
    pad = jnp.zeros((bsz, WINDOW, B_KV_HEADS, HEAD_DIM), k_win.dtype)
    k_win_p = jnp.concatenate([pad, k_win], axis=1)
    v_win_p = jnp.concatenate([pad, v_win], axis=1)
    g_idx = jnp.arange(B_KV_HEADS)[:, None, None]
    win_off = jnp.arange(Q_BLOCK + WINDOW) - WINDOW

    def block(b, q0):
        t = q0 + jnp.arange(Q_BLOCK)
        qr = rows(q_rot, b, q0).reshape(Q_BLOCK, B_KV_HEADS, grp, HEAD_DIM)
        qn = rows(q_raw, b, q0).reshape(Q_BLOCK, B_KV_HEADS, grp, HEAD_DIM)
        s_c = jnp.einsum('qgrd,ngd->grqn', qn, k_cmp[b]) * scale
        p_c = masked_softmax(s_c, cmp_end[None, :] <= t[:, None])
        o_c = jnp.einsum('grqn,ngd->qgrd', p_c.astype(v_cmp.dtype), v_cmp[b])
        imp = jnp.einsum('grqn,nj->gqj', p_c, overlap)
        admissible = blk_id[None, :] * SLC_BLOCK <= t[:, None]
        forced = (blk_id[None, :] == 0) | (blk_id[None, :] == t[:, None] // SLC_BLOCK)
        imp = jnp.where(admissible, jnp.where(forced, FORCE_SCORE, imp), NEG_INF)
        _, sel = lax.top_k(imp, top_n)
        sel_ok = jnp.take_along_axis(jnp.broadcast_to(admissible, imp.shape), sel, axis=-1)
        ks = k_blk[b][g_idx, sel].reshape(B_KV_HEADS, Q_BLOCK, top_n * SLC_BLOCK, HEAD_DIM)
        vs = v_blk[b][g_idx, sel].reshape(B_KV_HEADS, Q_BLOCK, top_n * SLC_BLOCK, HEAD_DIM)
        tok = (sel[..., None] * SLC_BLOCK + jnp.arange(SLC_BLOCK)).reshape(B_KV_HEADS, Q_BLOCK, top_n * SLC_BLOCK)
        m_s = (tok <= t[None, :, None]) & jnp.repeat(sel_ok, SLC_BLOCK, axis=-1)
        s_s = jnp.einsum('qgrd,gqkd->grqk', qr, ks) * scale
        p_s = masked_softmax(s_s, m_s[:, None])
        o_s = jnp.einsum('grqk,gqkd->qgrd', p_s.astype(vs.dtype), vs)
        kw = lax.dynamic_slice_in_dim(k_win_p[b], q0, Q_BLOCK + WINDOW, axis=0)
        vw = lax.dynamic_slice_in_dim(v_win_p[b], q0, Q_BLOCK + WINDOW, axis=0)
        s_pos = q0 + win_off
        m_w = (s_pos[None, :] <= t[:, None]) & (s_pos[None, :] > t[:, None] - WINDOW) & (s_pos[None, :] >= 0)
        s_w = jnp.einsum('qgrd,kgd->grqk', qr, kw) * scale
        p_w = masked_softmax(s_w, m_w)
        o_w = jnp.einsum('grqk,kgd->qgrd', p_w.astype(vw.dtype), vw)
        g = rows(gates, b, q0).reshape(Q_BLOCK, B_KV_HEADS, grp, N_BRANCH)
        o = g[..., 0:1] * o_c + g[..., 1:2] * o_s + g[..., 2:3] * o_w
        return o.reshape(Q_BLOCK, B_WIDTH)

    return map_query_blocks(block, bsz, seq)


def dsa_attention(q, k, v, q_idx, k_idx, w_idx):
    bsz, seq = q.shape[:2]
    top_k = min(DSA_TOPK, seq // 4)
    scale = HEAD_DIM ** -0.5
    key_pos = jnp.arange(seq)

    def block(b, q0):
        t = q0 + jnp.arange(Q_BLOCK)
        qb = rows(q, b, q0)
        logits = jnp.einsum('qhd,sd->qhs', rows(q_idx, b, q0), k_idx[b]).astype(jnp.float32) * IDX_DIM ** -0.5
        score = jnp.einsum('qh,qhs->qs', rows(w_idx, b, q0).astype(jnp.float32), jax.nn.relu(logits)) * IDX_HEADS ** -0.5
        score = jnp.where(key_pos[None, :] <= t[:, None], score, NEG_INF)
        _, sel = lax.top_k(score, top_k)
        ok = sel <= t[:, None]
        ks = k[b][sel]
        vs = v[b][sel]
        s = jnp.einsum('qhd,qkd->hqk', qb, ks) * scale
        p = masked_softmax(s, ok[None])
        o = jnp.einsum('hqk,qkd->qhd', p.astype(vs.dtype), vs)
        return o.reshape(Q_BLOCK, C_WIDTH)

    return map_query_blocks(block, bsz, seq)


def moba_attention(q, k, v):
    bsz, seq = q.shape[:2]
    n_blk = -(-seq // MOBA_BLOCK)
    pad = n_blk * MOBA_BLOCK - seq
    scale = HEAD_DIM ** -0.5
    k_blk = jnp.pad(k, ((0, 0), (0, pad), (0, 0), (0, 0))).reshape(bsz, n_blk, MOBA_BLOCK, D_HEADS, HEAD_DIM).transpose(0, 3, 1, 2, 4)
    v_blk = jnp.pad(v, ((0, 0), (0, pad), (0, 0), (0, 0))).reshape(bsz, n_blk, MOBA_BLOCK, D_HEADS, HEAD_DIM).transpose(0, 3, 1, 2, 4)
    k_mean = k_blk.mean(axis=3)
    top_k = min(MOBA_TOPK, n_blk - 1)
    blk_id = jnp.arange(n_blk)
    h_idx = jnp.arange(D_HEADS)[:, None, None]

    def block(b, q0):
        t = q0 + jnp.arange(Q_BLOCK)
        qb = rows(q, b, q0)
        own = q0 // MOBA_BLOCK
        k_own = lax.dynamic_index_in_dim(k_blk[b], own, axis=1, keepdims=False)
        v_own = lax.dynamic_index_in_dim(v_blk[b], own, axis=1, keepdims=False)
        own_pos = own * MOBA_BLOCK + jnp.arange(MOBA_BLOCK)
        s_own = jnp.einsum('qhd,hkd->hqk', qb, k_own) * scale
        m_own = jnp.broadcast_to(own_pos[None, :] <= t[:, None], s_own.shape)
        if top_k == 0:
            p = masked_softmax(s_own, m_own)
            o = jnp.einsum('hqk,hkd->qhd', p.astype(v_own.dtype), v_own)
        else:
            gate = jnp.einsum('qhd,hjd->hqj', qb, k_mean[b]).astype(jnp.float32)
            gate = jnp.where(blk_id < own, gate, NEG_INF)
            _, sel = lax.top_k(gate, top_k)
            ok = sel < own
            n_sel = top_k * MOBA_BLOCK
            ks = k_blk[b][h_idx, sel].reshape(D_HEADS, Q_BLOCK, n_sel, HEAD_DIM)
            vs = v_blk[b][h_idx, sel].reshape(D_HEADS, Q_BLOCK, n_sel, HEAD_DIM)
            s_sel = jnp.einsum('qhd,hqkd->hqk', qb, ks) * scale
            m_sel = jnp.repeat(ok, MOBA_BLOCK, axis=-1)
            p = masked_softmax(jnp.concatenate([s_sel, s_own], axis=-1), jnp.concatenate([m_sel, m_own], axis=-1))
            o = (jnp.einsum('hqk,hqkd->qhd', p[..., :n_sel].astype(vs.dtype), vs)
                 + jnp.einsum('hqk,hkd->qhd', p[..., n_sel:].astype(v_own.dtype), v_own))
        return o.reshape(Q_BLOCK, D_WIDTH)

    return map_query_blocks(block, bsz, seq)


def even_mixer(h, cos, sin, w_in, sgu_norm, sgu_w, sgu_b, cmp_pos_k, cmp_w1_k, cmp_w2_k,
               cmp_pos_v, cmp_w1_v, cmp_w2_v, w_out):
    bsz, seq, _ = h.shape
    a_in, q, kc, vc, ksl, vsl, kw, vw, gl = split_cols(h @ w_in, EVEN_SPLITS)

    def kv(t):
        return t.reshape(bsz, seq, B_KV_HEADS, HEAD_DIM)

    q_raw = q.reshape(bsz, seq, B_HEADS, HEAD_DIM)
    a_out = chunked_gmlp(a_in, sgu_norm, sgu_w, sgu_b)
    b_out = nsa_attention(partial_rope(q_raw, cos, sin), q_raw,
                          compress_blocks(kv(kc), cmp_pos_k, cmp_w1_k, cmp_w2_k),
                          compress_blocks(kv(vc), cmp_pos_v, cmp_w1_v, cmp_w2_v),
                          partial_rope(kv(ksl), cos, sin), kv(vsl),
                          partial_rope(kv(kw), cos, sin), kv(vw),
                          jax.nn.sigmoid(gl).reshape(bsz, seq, B_HEADS, N_BRANCH))
    return jnp.concatenate([a_out, b_out], axis=-1) @ w_out


def odd_mixer(h, cos, sin, cos_i, sin_i, w_in, w_out):
    bsz, seq, _ = h.shape
    qc, kc, vc, qi, ki, wi, qd, kd, vd = split_cols(h @ w_in, ODD_SPLITS)
    c_out = dsa_attention(partial_rope(qc.reshape(bsz, seq, C_HEADS, HEAD_DIM), cos, sin),
                          partial_rope(kc, cos, sin), vc,
                          partial_rope(qi.reshape(bsz, seq, IDX_HEADS, IDX_DIM), cos_i, sin_i),
                          partial_rope(ki, cos_i, sin_i), wi)
    d_out = moba_attention(partial_rope(qd.reshape(bsz, seq, D_HEADS, HEAD_DIM), cos, sin),
                           partial_rope(kd.reshape(bsz, seq, D_HEADS, HEAD_DIM), cos, sin),
                           vd.reshape(bsz, seq, D_HEADS, HEAD_DIM))
    return jnp.concatenate([c_out, d_out], axis=-1) @ w_out


def setup_inputs(seed: int = 0) -> dict:
    key = jax.random.key(seed)
    keys = iter(jax.random.split(key, 40))

    def nrm(shape, scale):
        return jax.random.normal(next(keys), shape, jnp.float32) * scale

    def gain(shape):
        return 1.0 + nrm(shape, 0.1)

    x = nrm((BATCH, SEQ, D_MODEL), 1.0)
    offset = jax.random.randint(next(keys), (BATCH, 1), 0, 4096, dtype=jnp.int32)
    positions = offset + jnp.arange(SEQ, dtype=jnp.int32)[None, :]
    return {
        'x': x,
        'positions': positions,
        'ffn1_norm': gain((DEPTH, D_MODEL)),
        'ffn1_w_gate': nrm((DEPTH, D_MODEL, D_FF), D_MODEL ** -0.5),
        'ffn1_w_up': nrm((DEPTH, D_MODEL, D_FF), D_MODEL ** -0.5),
        'ffn1_w_down': nrm((DEPTH, D_FF, D_MODEL), D_FF ** -0.5),
        'mix_norm': gain((DEPTH, D_MODEL)),
        'ffn2_norm': gain((DEPTH, D_MODEL)),
        'ffn2_w_gate': nrm((DEPTH, D_MODEL, D_FF), D_MODEL ** -0.5),
        'ffn2_w_up': nrm((DEPTH, D_MODEL, D_FF), D_MODEL ** -0.5),
        'ffn2_w_down': nrm((DEPTH, D_FF, D_MODEL), D_FF ** -0.5),
        'ev_w_in': nrm((N_EVEN, D_MODEL, EVEN_IN), D_MODEL ** -0.5),
        'ev_sgu_norm': gain((N_EVEN, A_WIDTH)),
        'ev_sgu_w': nrm((N_EVEN, A_GROUPS, A_CHUNK, A_CHUNK), A_CHUNK ** -0.5),
        'ev_sgu_b': gain((N_EVEN, A_GROUPS, A_CHUNK)),
        'ev_cmp_pos_k': nrm((N_EVEN, CMP_LEN, HEAD_DIM), 0.1),
        'ev_cmp_w1_k': nrm((N_EVEN, CMP_LEN * HEAD_DIM, CMP_HIDDEN), (CMP_LEN * HEAD_DIM) ** -0.5),
        'ev_cmp_w2_k': nrm((N_EVEN, CMP_HIDDEN, HEAD_DIM), CMP_HIDDEN ** -0.5),
        'ev_cmp_pos_v': nrm((N_EVEN, CMP_LEN, HEAD_DIM), 0.1),
        'ev_cmp_w1_v': nrm((N_EVEN, CMP_LEN * HEAD_DIM, CMP_HIDDEN), (CMP_LEN * HEAD_DIM) ** -0.5),
        'ev_cmp_w2_v': nrm((N_EVEN, CMP_HIDDEN, HEAD_DIM), CMP_HIDDEN ** -0.5),
        'ev_w_out': nrm((N_EVEN, MIX_OUT, D_MODEL), MIX_OUT ** -0.5),
        'od_w_in': nrm((N_ODD, D_MODEL, ODD_IN), D_MODEL ** -0.5),
        'od_w_out': nrm((N_ODD, MIX_OUT, D_MODEL), MIX_OUT ** -0.5),
        'final_norm': gain((D_MODEL,)),
    }


def reference(x, positions, ffn1_norm, ffn1_w_gate, ffn1_w_up, ffn1_w_down, mix_norm,
              ffn2_norm, ffn2_w_gate, ffn2_w_up, ffn2_w_down, ev_w_in, ev_sgu_norm, ev_sgu_w,
              ev_sgu_b, ev_cmp_pos_k, ev_cmp_w1_k, ev_cmp_w2_k, ev_cmp_pos_v, ev_cmp_w1_v,
              ev_cmp_w2_v, ev_w_out, od_w_in, od_w_out, final_norm):
    cos, sin = rope_tables(positions, ROT_DIM)
    cos_i, sin_i = rope_tables(positions, IDX_ROT)
    for i in range(DEPTH):
        x = x + 0.5 * swiglu_ffn(x, ffn1_norm[i], ffn1_w_gate[i], ffn1_w_up[i], ffn1_w_down[i])
        h = rmsnorm(x, mix_norm[i])
        if i % 2 == 0:
            e = i // 2
            x = x + even_mixer(h, cos, sin, ev_w_in[e], ev_sgu_norm[e], ev_sgu_w[e], ev_sgu_b[e],
                               ev_cmp_pos_k[e], ev_cmp_w1_k[e], ev_cmp_w2_k[e],
                               ev_cmp_pos_v[e], ev_cmp_w1_v[e], ev_cmp_w2_v[e], ev_w_out[e])
        else:
            o = i // 2
            x = x + odd_mixer(h, cos, sin, cos_i, sin_i, od_w_in[o], od_w_out[o])
        x = x + 0.5 * swiglu_ffn(x, ffn2_norm[i], ffn2_w_gate[i], ffn2_w_up[i], ffn2_w_down[i])
    return rmsnorm(x, final_norm)
```

```python
from contextlib import ExitStack
import numpy as np
import concourse.bass as bass
import concourse.mybir as mybir
from concourse.bass_utils import run_bass_kernel_spmd

F32 = mybir.dt.float32
BF16 = mybir.dt.bfloat16
I32 = mybir.dt.int32
AF = mybir.ActivationFunctionType
ALU = mybir.AluOpType
AX = mybir.AxisListType

ENGS = ['pe', 'act', 'dve', 'pool', 'sp']
NDSEM = 80


class T:
    _n = 0

    def __init__(self, t, name=None):
        self.t = t
        T._n += 1
        self.id = T._n
        self.name = name

    def __getitem__(self, idx):
        return self.t[idx]


class Op:
    __slots__ = ('eng', 'fn', 'deps', 'isdma', 'sem', 'cnt', 'signal', 'waits', 'vc')


class Sched:
    def __init__(self, nc):
        self.nc = nc
        self.esem = {e: nc.alloc_semaphore(name=f'es_{e}') for e in ENGS}
        self.ecnt = {e: 0 for e in ENGS}
        self.free_dsems = [nc.alloc_semaphore(name=f'ds_{i}') for i in range(NDSEM)]
        self.dcnt = {}
        self.n_inst = 0
        self._reset()

    def _reset(self):
        self.ops = []
        self.state = {}
        self.buf_dsem = {}
        self.stack = ExitStack()

    def sb(self, shape, dtype, name=None):
        t = self.stack.enter_context(self.nc.sbuf_tensor(f'{name or "sb"}_{T._n}', list(shape), dtype))
        return T(t, name)

    def ps(self, shape, dtype, name=None):
        t = self.stack.enter_context(self.nc.psum_tensor(f'{name or "ps"}_{T._n}', list(shape), dtype))
        return T(t, name)

    @staticmethod
    def _norm(item):
        if isinstance(item, T):
            return item.id, None
        return item[0].id, item[1]

    def _track(self, r, w, opi):
        deps = {}
        for item in r:
            tid, key = self._norm(item)
            st = self.state.setdefault(tid, {})
            for k, ent in st.items():
                if k == key or k is None or key is None:
                    if ent[0] is not None:
                        deps[ent[0]] = True
            st.setdefault(key, [None, []])[1].append(opi)
        for item in w:
            tid, key = self._norm(item)
            st = self.state.setdefault(tid, {})
            for k, ent in st.items():
                if k == key or k is None or key is None:
                    if ent[0] is not None:
                        deps.setdefault(ent[0], False)
                    for x in ent[1]:
                        deps.setdefault(x, False)
            if key is None:
                st.clear()
            st[key] = [opi, []]
        deps.pop(opi, None)
        return deps

    @staticmethod
    def _skip(p, o, raw):
        if p.isdma or o.isdma or p.eng != o.eng:
            return False
        return p.eng == 'pe' or not raw

    def op(self, eng, fn, r=(), w=()):
        o = Op()
        o.eng, o.fn, o.isdma, o.signal = eng, fn, False, False
        o.sem, o.cnt, o.waits, o.vc = None, 0, None, None
        o.deps = self._track(r, w, len(self.ops))
        self.ops.append(o)
        return o

    def dma(self, eng, out_ap, in_ap, r, w, **kw):
        o = self.op(eng, lambda e: e.dma_start(out=out_ap, in_=in_ap, **kw), r, w)
        o.isdma = True
        tid, _ = self._norm(w[0])
        if tid not in self.buf_dsem:
            self.buf_dsem[tid] = self.free_dsems.pop()
        o.sem = self.buf_dsem[tid]
        self.dcnt[o.sem] = self.dcnt.get(o.sem, 0) + 16
        o.cnt = self.dcnt[o.sem]
        return o

    def pe(self, fn, r=(), w=()):
        return self.op('pe', fn, r, w)

    def act(self, fn, r=(), w=()):
        return self.op('act', fn, r, w)

    def dve(self, fn, r=(), w=()):
        return self.op('dve', fn, r, w)

    def pool(self, fn, r=(), w=()):
        return self.op('pool', fn, r, w)

    def flush(self):
        nc, ops = self.nc, self.ops
        for o in ops:
            for d, raw in o.deps.items():
                p = ops[d]
                if p.isdma or self._skip(p, o, raw):
                    continue
                p.signal = True
        for o in ops:
            if not o.isdma and o.signal:
                self.ecnt[o.eng] += 1
                o.cnt = self.ecnt[o.eng]
                o.sem = self.esem[o.eng]
        known = {e: {} for e in ENGS}
        for o in ops:
            kn = known[o.eng]
            waits = {}
            for d in sorted(o.deps, reverse=True):
                p = ops[d]
                if self._skip(p, o, o.deps[d]):
                    continue
                if kn.get(p.sem, 0) >= p.cnt:
                    continue
                if waits.get(p.sem, 0) < p.cnt:
                    waits[p.sem] = p.cnt
                for s, c in p.vc.items():
                    if kn.get(s, 0) < c:
                        kn[s] = c
                kn[p.sem] = p.cnt
            o.waits = list(waits.items())
            if o.isdma or o.signal:
                o.vc = dict(kn)
        by = {e: [o for o in ops if o.eng == e] for e in ENGS}
        final_d = [(s, self.dcnt[s]) for s in set(self.buf_dsem.values())]
        self.n_inst += len(ops)

        def emit(e, lst):
            for o in lst:
                for s, c in o.waits:
                    e.wait_ge(s, c)
                ins = o.fn(e)
                if o.isdma:
                    ins.then_inc(o.sem, 16)
                elif o.signal:
                    ins.then_inc(o.sem, 1)

        with nc.Block() as block:
            @block.tensor
            def _(e):
                emit(e, by['pe'])

            @block.scalar
            def _(e):
                emit(e, by['act'])

            @block.vector
            def _(e):
                emit(e, by['dve'])

            @block.gpsimd
            def _(e):
                emit(e, by['pool'])

            @block.sync
            def _(e):
                emit(e, by['sp'])
                for s, c in final_d:
                    e.wait_ge(s, c)
        for s in self.buf_dsem.values():
            self.free_dsems.append(s)
        self.stack.close()
        self._reset()


D = 1024
DFF = 2816
NFC = DFF // 128
SEQ = 2048
EPS = 1e-6


def load_consts(S, cpack):
    c = {}
    idf = S.sb([128, 128], F32, 'idf')
    S.dma('sp', idf[:], cpack['ident'], r=[], w=[idf])
    idb = S.sb([128, 128], BF16, 'idb')
    S.dve(lambda e: e.tensor_copy(out=idb[:], in_=idf[:]), r=[idf], w=[idb])
    c['ident'] = idb
    return c


def rms_rstd(S, xt, junk, ss, rstd, width, key=None):
    S.act(lambda e: e.activation(out=junk[:], in_=xt[:], func=AF.Square, accum_out=ss[:]),
          r=[xt], w=[junk, ss])
    S.act(lambda e: e.activation(out=rstd[:], in_=ss[:], func=AF.Sqrt, bias=EPS, scale=1.0 / width),
          r=[ss], w=[rstd])
    S.dve(lambda e: e.reciprocal(out=rstd[:], in_=rstd[:]), r=[rstd], w=[rstd])


def ffn_stage(S, x_in, x_out, g_ap, wg, wu, wd, ntok, cpack, final_g=None):
    CH = 1024
    NT = CH // 128
    consts = load_consts(S, cpack)
    ident = consts['ident']
    gb = S.sb([128, D], F32, 'gb')
    S.dma('sp', gb[:], g_ap.partition_broadcast(128), r=[], w=[gb])
    if final_g is not None:
        fgb = S.sb([128, D], F32, 'fgb')
        S.dma('sp', fgb[:], final_g.partition_broadcast(128), r=[], w=[fgb])
    hT = S.sb([128, 8, CH], BF16, 'hT')
    actT = S.sb([128, NFC, CH], BF16, 'actT')
    wdb = S.sb([128, NFC, D], BF16, 'wdb')
    xt = [S.sb([128, D], F32, f'xt{i}') for i in range(2)]
    hb = [S.sb([128, D], BF16, f'hb{i}') for i in range(2)]
    junk = S.sb([128, D], F32, 'junk')
    ss = [S.sb([128, 1], F32, f'ss{i}') for i in range(2)]
    rstd = [S.sb([128, 1], F32, f'rstd{i}') for i in range(2)]
    FB = 256
    NB = DFF // FB
    wgs = [S.sb([128, 8, FB], F32, f'wgs{i}') for i in range(2)]
    wus = [S.sb([128, 8, FB], F32, f'wus{i}') for i in range(2)]
    wgb = [S.sb([128, 8, FB], BF16, f'wgb{i}') for i in range(2)]
    wub = [S.sb([128, 8, FB], BF16, f'wub{i}') for i in range(2)]
    wds = [S.sb([128, D], F32, f'wds{i}') for i in range(3)]
    sg = [S.sb([128, 512], F32, f'sg{i}') for i in range(2)]
    ot = [S.sb([128, D], F32, f'ot{i}') for i in range(2)]
    ptr = [S.ps([128, 8, 128], BF16, f'ptr{i}') for i in range(2)]
    pg = [S.ps([128, 512], F32, f'pg{i}') for i in range(2)]
    pu = [S.ps([128, 512], F32, f'pu{i}') for i in range(2)]
    py = [S.ps([128, 512], F32, f'py{i}') for i in range(2)]
    wg_v = wg.rearrange("(c p) f -> p c f", p=128)
    wu_v = wu.rearrange("(c p) f -> p c f", p=128)
    wd_v = wd.rearrange("(c p) m -> p c m", p=128)

    for ch in range(ntok // CH):
        t0 = ch * CH
        for i in range(NT):
            b = i % 2
            x_t, h_b = xt[b], hb[b]
            S.dma('sp', x_t[:], x_in[t0 + i * 128:t0 + (i + 1) * 128, :], r=[], w=[x_t])
            rms_rstd(S, x_t, junk, ss[b], rstd[b], D)
            S.dve(lambda e, x_t=x_t, h_b=h_b, b=b: e.scalar_tensor_tensor(
                out=h_b[:], in0=x_t[:], scalar=rstd[b][:, 0:1], in1=gb[:], op0=ALU.mult, op1=ALU.mult),
                r=[x_t, rstd[b], gb], w=[h_b])
            p = ptr[b]
            for c in range(8):
                S.pe(lambda e, p=p, h_b=h_b, c=c: e.transpose(out=p[:, c, :], in_=h_b[:, c * 128:(c + 1) * 128],
                                                               identity=ident[:]),
                     r=[h_b, ident], w=[(p, c)])
            S.act(lambda e, p=p, i=i: e.copy(out=hT[:, :, i * 128:(i + 1) * 128], in_=p[:]),
                  r=[p], w=[(hT, i // 4)])
        def load_wd(fc):
            s = wds[fc % 3]
            S.dma('sp', s[:], wd_v[:, fc, :], r=[], w=[s])
            S.pool(lambda e, s=s, fc=fc: e.tensor_copy(out=wdb[:, fc, :], in_=s[:]), r=[s], w=[(wdb, fc)])
        for fb in range(NB):
            b = fb % 2
            S.dma('sp', wgs[b][:], wg_v[:, :, fb * FB:(fb + 1) * FB], r=[], w=[wgs[b]])
            S.dma('sp', wus[b][:], wu_v[:, :, fb * FB:(fb + 1) * FB], r=[], w=[wus[b]])
            S.pool(lambda e, b=b: e.tensor_copy(out=wgb[b][:], in_=wgs[b][:]), r=[wgs[b]], w=[wgb[b]])
            S.pool(lambda e, b=b: e.tensor_copy(out=wub[b][:], in_=wus[b][:]), r=[wus[b]], w=[wub[b]])
            load_wd(2 * fb)
            load_wd(2 * fb + 1)
            for fs in range(FB // 128):
                fc = fb * (FB // 128) + fs
                for tb in range(CH // 512):
                    q = (fc * 2 + tb) % 2
                    for c in range(8):
                        S.pe(lambda e, q=q, b=b, c=c, fs=fs, tb=tb: e.matmul(
                            pg[q][:], lhsT=wgb[b][:, c, fs * 128:(fs + 1) * 128],
                            rhs=hT[:, c, tb * 512:(tb + 1) * 512], start=(c == 0), stop=(c == 7)),
                            r=[wgb[b], (hT, tb)], w=[pg[q]])
                    for c in range(8):
                        S.pe(lambda e, q=q, b=b, c=c, fs=fs, tb=tb: e.matmul(
                            pu[q][:], lhsT=wub[b][:, c, fs * 128:(fs + 1) * 128],
                            rhs=hT[:, c, tb * 512:(tb + 1) * 512], start=(c == 0), stop=(c == 7)),
                            r=[wub[b], (hT, tb)], w=[pu[q]])
                    S.act(lambda e, q=q: e.activation(out=sg[q][:], in_=pg[q][:], func=AF.Silu),
                          r=[pg[q]], w=[sg[q]])
                    S.dve(lambda e, q=q, fc=fc, tb=tb: e.tensor_tensor(
                        out=actT[:, fc, tb * 512:(tb + 1) * 512], in0=pu[q][:], in1=sg[q][:], op=ALU.mult),
                        r=[pu[q], sg[q]], w=[(actT, (fc, tb))])
        for i in range(NT):
            b = i % 2
            x_t, o_t = xt[b], ot[b]
            S.dma('sp', x_t[:], x_in[t0 + i * 128:t0 + (i + 1) * 128, :], r=[], w=[x_t])
            for mh in range(2):
                p = py[mh]
                for fc in range(NFC):
                    S.pe(lambda e, p=p, fc=fc, i=i, mh=mh: e.matmul(
                        p[:], lhsT=actT[:, fc, i * 128:(i + 1) * 128], rhs=wdb[:, fc, mh * 512:(mh + 1) * 512],
                        start=(fc == 0), stop=(fc == NFC - 1)),
                        r=[(actT, (fc, i // 4)), (wdb, fc)], w=[p])
                S.dve(lambda e, p=p, x_t=x_t, o_t=o_t, mh=mh: e.scalar_tensor_tensor(
                    out=o_t[:, mh * 512:(mh + 1) * 512], in0=p[:], scalar=0.5, in1=x_t[:, mh * 512:(mh + 1) * 512],
                    op0=ALU.mult, op1=ALU.add), r=[p, x_t], w=[(o_t, mh)])
            if final_g is not None:
                rms_rstd(S, o_t, junk, ss[b], rstd[b], D)
                S.dve(lambda e, o_t=o_t, b=b: e.scalar_tensor_tensor(
                    out=o_t[:], in0=o_t[:], scalar=rstd[b][:, 0:1], in1=fgb[:], op0=ALU.mult, op1=ALU.mult),
                    r=[o_t, rstd[b], fgb], w=[o_t])
            S.dma('sp', x_out[t0 + i * 128:t0 + (i + 1) * 128, :], o_t[:], r=[o_t], w=[(x_out_T(S, x_out), t0 // 128 + i)])
    S.flush()


_dram_T = {}
_dkey = [0]


def x_out_T(S, ap):
    k = ap.name
    if k not in _dram_T:
        _dram_T[k] = T(None, k)
    return _dram_T[k]


def DW(S, ap):
    _dkey[0] += 1
    return (x_out_T(S, ap), _dkey[0])


THETA = 500000.0
NEG = -30000.0


def _cpack_layout():
    items = [('ident', 128), ('tri_ge', 128), ('band_lt', 128), ('tril_st', 128), ('ones', 128),
             ('invf64', 1), ('nsgn64', 1), ('P64', 128), ('invf32', 1), ('nsgn32', 1), ('P32', 128),
             ('cmpbias', 2048), ('overlap', 32), ('E32', 2048), ('E8', 2048),
             ('keep', 512), ('addm', 512), ('adm', 512), ('tri_qs', 128), ('mbias', 128), ('mpast', 128), ('mown', 128)]
    off, o = {}, 0
    for k, n in items:
        off[k] = (o, n)
        o += n
    return off, o


CP_OFF, CP_N = _cpack_layout()


def host_consts():
    cp = np.zeros((128, CP_N), np.float32)

    def put(k, a):
        o, n = CP_OFF[k]
        a = np.asarray(a, np.float32)
        cp[:a.shape[0], o:o + a.shape[1]] = a
    p = np.arange(128)
    put('ident', np.eye(128))
    kk, qq = p[:, None], p[None, :]
    put('tri_ge', np.where(qq >= kk, 0.0, NEG))
    put('band_lt', np.where(qq < kk, 0.0, NEG))
    put('tril_st', (kk <= qq).astype(np.float32))
    put('tri_qs', np.where(qq <= kk, 0.0, -1e30))
    put('ones', np.ones((128, 128)))
    m64 = p % 64
    put('invf64', np.where(m64 < 16, THETA ** (-(2.0 * (m64 % 8)) / 16.0), 0.0)[:, None])
    put('nsgn64', np.where(m64 < 8, -1.0, np.where(m64 < 16, 1.0, 0.0))[:, None])
    P = np.zeros((128, 128))
    for m in range(128):
        if m % 64 < 8:
            P[m + 8, m] = 1
        elif m % 64 < 16:
            P[m - 8, m] = 1
    put('P64', P)
    m32 = p % 32
    put('invf32', np.where(m32 < 8, THETA ** (-(2.0 * (m32 % 4)) / 8.0), 0.0)[:, None])
    put('nsgn32', np.where(m32 < 4, -1.0, np.where(m32 < 8, 1.0, 0.0))[:, None])
    P = np.zeros((128, 128))
    for m in range(128):
        if m % 32 < 4:
            P[m + 4, m] = 1
        elif m % 32 < 8:
            P[m - 4, m] = 1
    put('P32', P)
    n = np.arange(127)
    t = np.arange(2048)
    put('cmpbias', np.where(16 * n[:, None] + 31 <= t[None, :], 0.0, NEG))
    c0 = n * 16
    s0 = np.arange(32) * 64
    put('overlap', ((c0[:, None] < s0[None, :] + 64) & (c0[:, None] + 32 > s0[None, :])).astype(np.float32))
    put('E32', (t[None, :] // 64 == np.arange(32)[:, None]).astype(np.float32))
    put('E8', (t[None, :] // 256 == np.arange(8)[:, None]).astype(np.float32))
    tt = (np.arange(16)[None, :, None] * 128 + p[:, None, None])
    j = np.arange(32)[None, None, :]
    adm = j * 64 <= tt
    forced = (j == 0) | (j == tt // 64)
    put('keep', (adm & ~forced).astype(np.float32).reshape(128, 512))
    put('addm', np.where(adm, np.where(forced, 1e4, 0.0), -1e30).reshape(128, 512))
    put('adm', adm.astype(np.float32).reshape(128, 512))
    own = (np.arange(16)[None, :, None] * 128 + p[:, None, None]) // 256
    j8 = np.arange(8)[None, None, :]
    put('mbias', np.where(j8 < own, 0.0, -1e30).reshape(128, 128))
    put('mpast', (j8 < own).astype(np.float32).reshape(128, 128))
    put('mown', (j8 == own).astype(np.float32).reshape(128, 128))
    return cp


class Consts:
    def __init__(self, S, cpack_ap):
        self.S, self.ap, self.cache = S, cpack_ap, {}

    def get(self, k, dtype=F32, rows=128):
        key = (k, dtype)
        if key in self.cache:
            return self.cache[key]
        S = self.S
        o, n = CP_OFF[k]
        if dtype == F32:
            f = S.sb([128, n], F32, 'c_' + k)
            S.dma('sp', f[:], self.ap[:, o:o + n], r=[], w=[f], allow_slow_non_contiguous=(n == 1))
            self.cache[key] = f
            return f
        if not hasattr(self, 'stg'):
            self.stg = S.sb([128, 2048], F32, 'c_stg')
        f = self.stg
        S.dma('sp', f[:, 0:n], self.ap[:, o:o + n], r=[], w=[f])
        b = S.sb([128, n], dtype, 'cb_' + k)
        S.pool(lambda e: e.tensor_copy(out=b[:], in_=f[:, 0:n]), r=[f], w=[b])
        self.cache[key] = b
        return b


def rope_tables(S, C, pos_ap, ntok, invk, sgnk, tmp):
    invf = C.get(invk)
    nsg = C.get(sgnk)
    if 'pi' not in tmp:
        tmp['pi'] = S.sb([128, 1024], I32, 'pos_i')
        tmp['ang'] = S.sb([128, 1024], F32, 'ang')
        tmp['kf'] = S.sb([128, 1024], F32, 'kf')
        tmp['ki'] = S.sb([128, 1024], I32, 'ki')
    pi_, ang, kf, ki = tmp['pi'], tmp['ang'], tmp['kf'], tmp['ki']
    ct = S.sb([128, ntok], F32, 'ropeC')
    st = S.sb([128, ntok], F32, 'ropeS')
    TWO_PI = 2.0 * np.pi
    for c0 in range(0, ntok, 1024):
        S.dma('sp', pi_[:], pos_ap[c0:c0 + 1024].partition_broadcast(128), r=[], w=[pi_])
        S.dve(lambda e: e.tensor_copy(out=ang[:], in_=pi_[:]), r=[pi_], w=[ang])
        S.dve(lambda e: e.tensor_scalar(out=ang[:], in0=ang[:], scalar1=invf[:, 0:1], scalar2=None, op0=ALU.mult),
              r=[ang, invf], w=[ang])

        def reduce_sin(dst, shift, post, c0=c0):
            S.dve(lambda e: e.tensor_scalar(out=kf[:], in0=ang[:], scalar1=shift, scalar2=1.0 / TWO_PI,
                                            op0=ALU.add, op1=ALU.mult), r=[ang], w=[kf])
            S.dve(lambda e: e.tensor_copy(out=ki[:], in_=kf[:]), r=[kf], w=[ki])
            S.dve(lambda e: e.tensor_copy(out=kf[:], in_=ki[:]), r=[ki], w=[kf])
            S.dve(lambda e: e.scalar_tensor_tensor(out=kf[:], in0=kf[:], scalar=-TWO_PI, in1=ang[:],
                                                   op0=ALU.mult, op1=ALU.add), r=[kf, ang], w=[kf])
            S.dve(lambda e: e.tensor_scalar(out=kf[:], in0=kf[:], scalar1=shift, scalar2=3.14159, op0=ALU.add,
                                            op1=ALU.min), r=[kf], w=[kf])
            S.dve(lambda e: e.tensor_scalar(out=kf[:], in0=kf[:], scalar1=-3.14159, scalar2=None, op0=ALU.max),
                  r=[kf], w=[kf])
            S.act(lambda e: e.activation(out=dst[:, c0:c0 + 1024], in_=kf[:], func=AF.Sin), r=[kf], w=[(dst, c0)])
            if post is not None:
                S.dve(lambda e: e.tensor_scalar(out=dst[:, c0:c0 + 1024], in0=dst[:, c0:c0 + 1024],
                                                scalar1=post[:, 0:1], scalar2=None, op0=ALU.mult),
                      r=[(dst, c0), post], w=[(dst, c0)])
        reduce_sin(ct, np.pi / 2.0, None)
        reduce_sin(st, 0.0, nsg)
    return ct, st


def proj_stage(S, x, w_in, nin, g_ap, pos, ntok, cpack, fm_specs, tm_groups, tm_post, use_idx=False):
    C = Consts(S, cpack)
    ident = C.get('ident', BF16)
    gb = S.sb([128, D], F32, 'gb')
    S.dma('sp', gb[:], g_ap.partition_broadcast(128), r=[], w=[gb])
    winb = S.sb([128, 8, nin], BF16, 'winb')
    wv = w_in.rearrange("(c p) f -> p c f", p=128)
    wst = [S.sb([128, 8, 256], F32, f'wst{i}') for i in range(2)]
    for bi, c0 in enumerate(range(0, nin, 256)):
        n = min(256, nin - c0)
        st = wst[bi % 2]
        S.dma('sp', st[:, :, 0:n], wv[:, :, c0:c0 + n], r=[], w=[st])
        S.pool(lambda e, st=st, c0=c0, n=n: e.tensor_copy(out=winb[:, :, c0:c0 + n], in_=st[:, :, 0:n]),
               r=[st], w=[(winb, bi)])
    ropes = {}
    rtmp = {}
    if any(sp[3] == 'r64' for sp in fm_specs):
        ct, sn = rope_tables(S, C, pos, ntok, 'invf64', 'nsgn64', rtmp)
        ropes['r64'] = (ct, sn, C.get('P64', BF16))
    if use_idx:
        ct, sn = rope_tables(S, C, pos, ntok, 'invf32', 'nsgn32', rtmp)
        ropes['r32'] = (ct, sn, C.get('P32', BF16))
    xt = [S.sb([128, D], F32, f'xt{i}') for i in range(2)]
    hb = [S.sb([128, D], BF16, f'hb{i}') for i in range(2)]
    junk = S.sb([128, D], F32, 'junk')
    ss = [S.sb([128, 1], F32, f'ss{i}') for i in range(2)]
    rstd = [S.sb([128, 1], F32, f'rstd{i}') for i in range(2)]
    hT = [S.sb([128, 8, 512], BF16, f'hT{i}') for i in range(2)]
    xh = [S.sb([128, 512], BF16, f'xh{i}') for i in range(2)]
    t1 = [S.sb([128, 512], F32, f't1{i}') for i in range(2)]
    t2 = [S.sb([128, 512], F32, f't2{i}') for i in range(2)]
    xr = [S.sb([128, 512], BF16, f'xr{i}') for i in range(2)]
    ptr = [S.ps([128, 8, 128], BF16, f'ptr{i}') for i in range(2)]
    pf = [S.ps([128, 512], F32, f'pf{i}') for i in range(2)]
    p2 = [S.ps([128, 512], F32, f'p2{i}') for i in range(2)]
    pt = [S.ps([128, 512], F32, f'pt{i}') for i in range(2)]
    nfm = 0
    ntm = 0
    for blk in range(ntok // 512):
        t0 = blk * 512
        hTb = hT[blk % 2]
        for i in range(4):
            b = i % 2
            x_t, h_b = xt[b], hb[b]
            S.dma('sp', x_t[:], x[t0 + i * 128:t0 + (i + 1) * 128, :], r=[], w=[x_t])
            rms_rstd(S, x_t, junk, ss[b], rstd[b], D)
            S.dve(lambda e, x_t=x_t, h_b=h_b, b=b: e.scalar_tensor_tensor(
                out=h_b[:], in0=x_t[:], scalar=rstd[b][:, 0:1], in1=gb[:], op0=ALU.mult, op1=ALU.mult),
                r=[x_t, rstd[b], gb], w=[h_b])
            p = ptr[b]
            for c in range(8):
                S.pe(lambda e, p=p, h_b=h_b, c=c: e.transpose(out=p[:, c, :], in_=h_b[:, c * 128:(c + 1) * 128],
                                                               identity=ident[:]), r=[h_b, ident], w=[(p, c)])
            S.act(lambda e, p=p, i=i, hTb=hTb: e.copy(out=hTb[:, :, i * 128:(i + 1) * 128], in_=p[:]),
                  r=[p], w=[(hTb, i)])
        for (col0, M, scale, rope, raw_dst, rot_dst) in fm_specs:
            q = nfm % 2
            nfm += 1
            pp = pf[q]
            for c in range(8):
                S.pe(lambda e, pp=pp, c=c, col0=col0, M=M, hTb=hTb: e.matmul(
                    pp[0:M, :], lhsT=winb[:, c, col0:col0 + M], rhs=hTb[:, c, :], start=(c == 0), stop=(c == 7)),
                    r=[winb, hTb], w=[pp])
            xq = xh[q]
            S.act(lambda e, xq=xq, pp=pp, M=M, scale=scale: e.mul(out=xq[0:M, :], in_=pp[0:M, :], mul=scale),
                  r=[pp], w=[xq])
            if raw_dst is not None:
                S.dma('pool', raw_dst[:, t0:t0 + 512], xq[0:M, :], r=[xq], w=[DW(S, raw_dst)])
            if rope is not None:
                ct, sn, Pm = ropes[rope]
                pq = p2[q]
                S.pe(lambda e, pq=pq, xq=xq, M=M, Pm=Pm: e.matmul(pq[0:M, :], lhsT=Pm[0:M, 0:M], rhs=xq[0:M, :],
                                                                  start=True, stop=True), r=[xq, Pm], w=[pq])
                S.dve(lambda e, q=q, xq=xq, M=M, ct=ct, t0=t0: e.tensor_tensor(
                    out=t1[q][0:M, :], in0=xq[0:M, :], in1=ct[0:M, t0:t0 + 512], op=ALU.mult),
                    r=[xq, ct], w=[t1[q]])
                S.dve(lambda e, q=q, pq=pq, M=M, sn=sn, t0=t0: e.tensor_tensor(
                    out=t2[q][0:M, :], in0=pq[0:M, :], in1=sn[0:M, t0:t0 + 512], op=ALU.mult),
                    r=[pq, sn], w=[t2[q]])
                S.pool(lambda e, q=q, M=M: e.tensor_tensor(out=xr[q][0:M, :], in0=t1[q][0:M, :], in1=t2[q][0:M, :],
                                                          op=ALU.add), r=[t1[q], t2[q]], w=[xr[q]])
                S.dma('pool', rot_dst[:, t0:t0 + 512], xr[q][0:M, :], r=[xr[q]], w=[DW(S, rot_dst)])
        for i in range(4):
            for gi, (col0, N) in enumerate(tm_groups):
                q = ntm % 2
                ntm += 1
                pp = pt[q]
                for c in range(8):
                    S.pe(lambda e, pp=pp, c=c, col0=col0, N=N, i=i, hTb=hTb: e.matmul(
                        pp[:, 0:N], lhsT=hTb[:, c, i * 128:(i + 1) * 128], rhs=winb[:, c, col0:col0 + N],
                        start=(c == 0), stop=(c == 7)), r=[winb, (hTb, i)], w=[pp])
                tm_post(gi, pp, t0 + i * 128, ntm)
    S.flush()


def even_proj(S, x, prm, dr, ntok, cpack):
    at = [S.sb([128, 512], BF16, f'at{i}') for i in range(2)]
    g1 = [S.sb([128, 512], F32, f'g1{i}') for i in range(2)]
    vb = [S.sb([128, 192], BF16, f'vb{i}') for i in range(2)]
    vb2 = [S.sb([128, 192], BF16, f'vb2{i}') for i in range(2)]
    gt = [S.sb([128, 36], F32, f'gt{i}') for i in range(2)]
    sgb = S.sb([128, 256], F32, 'sgb')
    S.dma('sp', sgb[:], prm['sgu_norm'].partition_broadcast(128), r=[], w=[sgb])
    junk = S.sb([128, 256], F32, 'junk2')
    ss = [S.sb([128, 1], F32, f'ssv{i}') for i in range(2)]
    rs = [S.sb([128, 1], F32, f'rsv{i}') for i in range(2)]
    cnt = [0]

    def tm_post(gi, pp, tok0, n):
        if gi == 0:
            b = cnt[0] % 2
            cnt[0] += 1
            g, a = g1[b], at[b]
            S.act(lambda e: e.activation(out=g[:], in_=pp[:], func=AF.Gelu_apprx_tanh), r=[pp], w=[g])
            S.pool(lambda e: e.tensor_copy(out=a[:, 0:256], in_=g[:, 0:256]), r=[g], w=[(a, 0)])
            S.act(lambda e: e.activation(out=junk[:], in_=g[:, 256:512], func=AF.Square, accum_out=ss[b][:]),
                  r=[g], w=[junk, ss[b]])
            S.act(lambda e: e.activation(out=rs[b][:], in_=ss[b][:], func=AF.Sqrt, bias=EPS, scale=1.0 / 256),
                  r=[ss[b]], w=[rs[b]])
            S.dve(lambda e: e.reciprocal(out=rs[b][:], in_=rs[b][:]), r=[rs[b]], w=[rs[b]])
            S.dve(lambda e: e.scalar_tensor_tensor(out=a[:, 256:512], in0=g[:, 256:512], scalar=rs[b][:, 0:1],
                                                   in1=sgb[:], op0=ALU.mult, op1=ALU.mult),
                  r=[g, rs[b], sgb], w=[(a, 1)])
            S.dma('pool', dr['a_tok'][tok0:tok0 + 128, :], a[:], r=[a], w=[DW(S, dr['a_tok'])])
        elif gi == 1:
            v = vb[(tok0 // 128) % 2]
            S.act(lambda e: e.copy(out=v[:], in_=pp[:, 0:192]), r=[pp], w=[v])
            S.dma('pool', dr['vsl'][tok0:tok0 + 128, :], v[:], r=[v], w=[DW(S, dr['vsl'])])
        else:
            v = vb2[(tok0 // 128) % 2]
            g = gt[(tok0 // 128) % 2]
            S.act(lambda e: e.copy(out=v[:], in_=pp[:, 0:192]), r=[pp], w=[v])
            S.act(lambda e: e.activation(out=g[:], in_=pp[:, 192:228], func=AF.Sigmoid), r=[pp], w=[g])
            S.dma('pool', dr['vw'][tok0:tok0 + 128, :], v[:], r=[v], w=[DW(S, dr['vw'])])
            S.dma('pool', dr['gate'][tok0:tok0 + 128, :], g[:], r=[g], w=[DW(S, dr['gate'])])
    fm = []
    for i in range(6):
        fm.append((512 + 128 * i, 128, 0.125, 'r64', dr['qraw'][128 * i:128 * (i + 1), :],
                   dr['qrot'][128 * i:128 * (i + 1), :]))
    for nm, c0, rope in (('kc', 1280, None), ('vc', 1472, None), ('ksl', 1664, 'r64'), ('kw', 2048, 'r64')):
        for (o, M) in ((0, 128), (128, 64)):
            dst = dr[nm][o:o + M, :]
            fm.append((c0 + o, M, 1.0, rope, dst if rope is None else None, dst if rope else None))
    proj_stage(S, x, prm['w_in'], 2468, prm['mix_norm'], prm['pos'], ntok, cpack, fm,
               [(0, 512), (1856, 192), (2240, 228)], tm_post)


def attn_chunk(S, c, tiles, qT, kT, extra, vext, ident, STps, PT, Oacc, finalize, tag, qdep=None):
    cover = {}
    for n, (kt, lo, hi, bt, blo) in enumerate(tiles):
        for j in range(lo // 128, hi // 128):
            cover.setdefault(j, []).append(n)

    qd_ = qdep if qdep is not None else qT

    def qk(n):
        kt, lo, hi, bt, blo = tiles[n]
        ps = STps[n % 2]
        nterm = 1 + (1 if extra else 0) + (1 if bt is not None else 0)
        S.pe(lambda e: e.matmul(ps[:, lo:hi], lhsT=kT[:, kt * 128:(kt + 1) * 128],
                                rhs=qT[:, c * 512 + lo:c * 512 + hi], start=True, stop=(nterm == 1)),
             r=[kT, qd_], w=[ps])
        k = 1
        if extra:
            k += 1
            S.pe(lambda e: e.matmul(ps[:, lo:hi], lhsT=extra[0](kt), rhs=extra[1](kt, c, lo, hi),
                                    start=False, stop=(k == nterm), skip_group_check=True),
                 r=list(extra[2]), w=[ps])
        if bt is not None:
            S.pe(lambda e: e.matmul(ps[:, blo:blo + 128], lhsT=ident[:], rhs=bt[:], start=False, stop=True,
                                    skip_group_check=True), r=[ident, bt], w=[ps])

    qk(0)
    for n, (kt, lo, hi, bt, blo) in enumerate(tiles):
        if n + 1 < len(tiles):
            qk(n + 1)
        ps, p = STps[n % 2], PT[n % 2]
        S.act(lambda e, ps=ps, p=p, lo=lo, hi=hi: e.activation(out=p[:, lo:hi], in_=ps[:, lo:hi], func=AF.Exp),
              r=[ps], w=[p])
        for j in range(lo // 128, hi // 128):
            S.pe(lambda e, p=p, j=j, kt=kt, n=n: e.matmul(
                Oacc[j][:, 0:65], lhsT=p[:, j * 128:(j + 1) * 128], rhs=vext[:, kt, :],
                start=(cover[j][0] == n), stop=(cover[j][-1] == n), skip_group_check=True),
                r=[p, vext], w=[Oacc[j]])
            if cover[j][-1] == n:
                finalize(c, j, Oacc[j])


def causal_tiles(c, tri):
    out = []
    for kt in range(4 * c + 4):
        if kt < 4 * c:
            out.append((kt, 0, 512, None, 0))
        else:
            lo = (kt - 4 * c) * 128
            out.append((kt, lo, 512, tri, lo))
    return out


def window_tiles(c, tri, band):
    out = []
    if c >= 1:
        out.append((4 * c - 1, 0, 512, band, 384))
        for i in range(3):
            out.append((4 * c - 4 + i, 0, 128 * (i + 1), band, 128 * i))
    for i in range(4):
        out.append((4 * c + i, 128 * i, 512, tri, 128 * i))
    return out


def nsa_stage(S, prm, dr, nseq, cpack):
    C = Consts(S, cpack)
    ident = C.get('ident', BF16)
    tri = C.get('tri_ge', BF16)
    band = C.get('band_lt', BF16)
    ones = C.get('ones', BF16)
    cmpb = C.get('cmpbias', BF16)
    ovl = C.get('overlap', F32)
    E32 = C.get('E32', BF16)
    keep, addm, adm = C.get('keep'), C.get('addm'), C.get('adm')
    tril = C.get('tril_st', F32)
    wTf = S.sb([128, 4, 128], F32, 'wTf')
    S.dma('sp', wTf[:], prm['sgu_wT'].rearrange("g s t -> s g t"), r=[], w=[wTf])
    wTb = S.sb([128, 4, 128], BF16, 'wTb')
    for g in range(4):
        S.dve(lambda e, g=g: e.tensor_tensor(out=wTb[:, g, :], in0=wTf[:, g, :], in1=tril[:], op=ALU.mult),
              r=[wTf, tril], w=[(wTb, g)])
    bT = S.sb([128, 4], F32, 'bT')
    S.dma('sp', bT[:], prm['sgu_bT'], r=[], w=[bT])
    cw = {}
    stg = [S.sb([64, 4, 256], F32, f'w1s{i}') for i in range(2)]
    w2s = S.sb([128, 2, 64], F32, 'w2s')
    pst = S.sb([64, 32], F32, 'pst')
    pm0 = S.ps([128, 512], F32, 'pm0')
    pmisc = [pm0, pm0]
    ptb = S.ps([128, 1024], BF16, 'ptb')
    k = 0
    for kv in ('k', 'v'):
        w1b = S.sb([64, 32, 256], BF16, 'w1b' + kv)
        w1v = prm['cmp_w1_' + kv].rearrange("(l e) c -> e l c", e=64)
        for q4 in range(8):
            st = stg[k % 2]
            k += 1
            S.dma('sp', st[:], w1v[:, q4 * 4:(q4 + 1) * 4, :], r=[], w=[st])
            S.pool(lambda e, st=st, w1b=w1b, q4=q4: e.tensor_copy(out=w1b[:, q4 * 4:(q4 + 1) * 4, :], in_=st[:]),
                   r=[st], w=[(w1b, q4)])
        w2b = S.sb([128, 2, 64], BF16, 'w2b' + kv)
        S.dma('sp', w2s[:], prm['cmp_w2_' + kv].rearrange("(c p) e -> p c e", p=128), r=[], w=[w2s])
        S.dve(lambda e, w2b=w2b: e.tensor_copy(out=w2b[:], in_=w2s[:]), r=[w2s], w=[w2b])
        posb = S.sb([64, 32], BF16, 'posb' + kv)
        S.dma('sp', pst[:], prm['cmp_posT_' + kv], r=[], w=[pst])
        S.dve(lambda e, posb=posb: e.tensor_copy(out=posb[:], in_=pst[:]), r=[pst], w=[posb])
        pbias = S.sb([128, 2], F32, 'pbias' + kv)
        for cc in range(2):
            pp = pmisc[cc]
            for l in range(32):
                S.pe(lambda e, pp=pp, w1b=w1b, posb=posb, l=l, cc=cc: e.matmul(
                    pp[:, 0:1], lhsT=w1b[:, l, cc * 128:(cc + 1) * 128], rhs=posb[:, l:l + 1],
                    start=(l == 0), stop=(l == 31)), r=[w1b, posb], w=[pp])
            S.dve(lambda e, pp=pp, pbias=pbias, cc=cc: e.tensor_copy(out=pbias[:, cc:cc + 1], in_=pp[:, 0:1]),
                  r=[pp], w=[(pbias, cc)])
        cw[kv] = (w1b, w2b, pbias)
    atok = S.sb([128, 16, 512], BF16, 'atok')
    gates = S.sb([128, 16, 36], F32, 'gates')
    otok = S.sb([128, 16, 1024], BF16, 'otok')
    accg = S.sb([128, 16, 256], F32, 'accg')
    kcT = S.sb([64, SEQ], BF16, 'kcT')
    vcT = S.sb([64, SEQ], BF16, 'vcT')
    kslT = S.sb([64, SEQ], BF16, 'kslT')
    kwT = S.sb([64, SEQ], BF16, 'kwT')
    vsl = S.sb([128, 16, 65], BF16, 'vslx')
    vw = S.sb([128, 16, 65], BF16, 'vwx')
    S.pool(lambda e: e.memset(vsl[:], 1.0), r=[], w=[vsl])
    S.pool(lambda e: e.memset(vw[:], 1.0), r=[], w=[vw])
    qraw = S.sb([64, SEQ], BF16, 'qrawT')
    qrot = S.sb([64, SEQ], BF16, 'qrotT')
    ghT = S.sb([128, 2, 128], BF16, 'ghT')
    kcmpT = S.sb([64, 128], BF16, 'kcmpT')
    vcmp = S.sb([128, 64], BF16, 'vcmp')
    psumT = S.sb([128, SEQ], F32, 'psumT')
    negselT = S.sb([32, SEQ], BF16, 'negselT')
    pcT = [S.sb([128, 512], BF16, f'pcT{i}') for i in range(2)]
    rinv = [S.sb([128, 512], F32, f'rinv{i}') for i in range(2)]
    pnf = [S.sb([128, 512], F32, f'pnf{i}') for i in range(2)]
    pnb = [S.sb([128, 512], BF16, f'pnb{i}') for i in range(2)]
    PT = [S.sb([128, 512], BF16, f'PT{i}') for i in range(2)]
    gm = [S.sb([128, 256], F32, f'gm{i}') for i in range(2)]
    sel = [S.sb([128, 32], F32, f'sel{i}') for i in range(2)]
    m8 = [S.sb([128, 8], F32, f'm8{i}') for i in range(2)]
    nsel = [S.sb([128, 32], BF16, f'nsel{i}') for i in range(2)]
    fsc = [S.sb([128, 1], F32, f'fsc{i}') for i in range(4)]
    STps = [S.ps([128, 512], F32, f'st{i}') for i in range(2)]
    Oacc = [S.ps([128, 512], F32, f'oa{i}') for i in range(4)]
    fcount = [0]

    def make_finalize(r, gidx, first):
        def fin(c, j, O):
            i = 4 * c + j
            f = fsc[fcount[0] % 4]
            fcount[0] += 1
            S.dve(lambda e: e.tensor_scalar(out=f[:], in0=O[:, 64:65], scalar1=1e-30, scalar2=None, op0=ALU.max),
                  r=[O], w=[f])
            S.dve(lambda e: e.reciprocal(out=f[:], in_=f[:]), r=[f], w=[f])
            S.dve(lambda e: e.tensor_tensor(out=f[:], in0=f[:], in1=gates[:, i, gidx:gidx + 1], op=ALU.mult),
                  r=[f, gates], w=[f])
            S.dve(lambda e: e.scalar_tensor_tensor(
                out=accg[:, i, r * 64:(r + 1) * 64], in0=O[:, 0:64], scalar=f[:, 0:1],
                in1=accg[:, i, r * 64:(r + 1) * 64], op0=ALU.mult, op1=ALU.add),
                r=[O, f, (accg, (i, r))], w=[(accg, (i, r))])
        return fin

    for sq in range(nseq):
        tb = sq * SEQ
        S.dma('sp', atok[:], dr['a_tok'][tb:tb + SEQ, :].rearrange("(n p) c -> p n c", p=128), r=[], w=[atok])
        S.dma('sp', gates[:], dr['gate'][tb:tb + SEQ, :].rearrange("(n p) c -> p n c", p=128), r=[], w=[gates])
        for i in range(16):
            pp = pmisc[i % 2]
            g_ = gm[i % 2]
            for g in range(4):
                S.pe(lambda e, pp=pp, g=g, i=i: e.matmul(pp[:, g * 64:(g + 1) * 64], lhsT=wTb[:, g, :],
                                                          rhs=atok[:, i, 256 + g * 64:256 + (g + 1) * 64],
                                                          start=True, stop=True), r=[wTb, atok], w=[pp])
                S.dve(lambda e, pp=pp, g_=g_, g=g: e.tensor_scalar(
                    out=g_[:, g * 64:(g + 1) * 64], in0=pp[:, g * 64:(g + 1) * 64], scalar1=bT[:, g:g + 1],
                    scalar2=None, op0=ALU.add), r=[pp, bT], w=[(g_, g)])
            S.dve(lambda e, g_=g_, i=i: e.tensor_tensor(out=otok[:, i, 0:256], in0=g_[:], in1=atok[:, i, 0:256],
                                                         op=ALU.mult), r=[g_, atok], w=[(otok, (i, 0))])
        for g in range(3):
            for nm, tl in (('kc', kcT), ('vc', vcT), ('ksl', kslT), ('kw', kwT)):
                S.dma('sp', tl[:], dr[nm][g * 64:(g + 1) * 64, tb:tb + SEQ], r=[], w=[tl])
            for nm, tl in (('vsl', vsl), ('vw', vw)):
                S.dma('sp', tl[:, :, 0:64], dr[nm][tb:tb + SEQ, g * 64:(g + 1) * 64].rearrange(
                    "(n p) c -> p n c", p=128), r=[], w=[tl])
            for kv, src in (('k', kcT), ('v', vcT)):
                w1b, w2b, pbias = cw[kv]
                for cc in range(2):
                    pp = pmisc[cc]
                    for l in range(32):
                        S.pe(lambda e, pp=pp, w1b=w1b, src=src, l=l, cc=cc: e.matmul(
                            pp[:, 0:127], lhsT=w1b[:, l, cc * 128:(cc + 1) * 128], rhs=src[:, l:l + 2017:16],
                            start=(l == 0), stop=(l == 31)), r=[w1b, src], w=[pp])
                    S.act(lambda e, pp=pp, cc=cc, pbias=pbias: e.activation(
                        out=ghT[:, cc, 0:127], in_=pp[:, 0:127], func=AF.Gelu_apprx_tanh, bias=pbias[:, cc:cc + 1]),
                        r=[pp, pbias], w=[(ghT, cc)])
                pp = pmisc[0]
                if kv == 'k':
                    for cc in range(2):
                        S.pe(lambda e, pp=pp, cc=cc, w2b=w2b: e.matmul(pp[0:64, 0:127], lhsT=w2b[:, cc, :],
                                                                     rhs=ghT[:, cc, 0:127], start=(cc == 0),
                                                                     stop=(cc == 1)), r=[w2b, ghT], w=[pp])
                    S.act(lambda e, pp=pp: e.copy(out=kcmpT[:, 0:127], in_=pp[0:64, 0:127]), r=[pp], w=[kcmpT])
                else:
                    for cc in range(2):
                        S.pe(lambda e, pp=pp, cc=cc, w2b=w2b: e.matmul(pp[0:127, 0:64], lhsT=ghT[:, cc, 0:127],
                                                                     rhs=w2b[:, cc, :], start=(cc == 0),
                                                                     stop=(cc == 1)), r=[w2b, ghT], w=[pp])
                    S.act(lambda e, pp=pp: e.copy(out=vcmp[0:127, :], in_=pp[0:127, 0:64]), r=[pp], w=[vcmp])
            for r in range(4):
                h = 4 * g + r
                S.dma('sp', qraw[:], dr['qraw'][h * 64:(h + 1) * 64, tb:tb + SEQ], r=[], w=[qraw])
                for c in range(4):
                    b = c % 2
                    ps = STps[b]
                    S.pe(lambda e, ps=ps, c=c: e.matmul(ps[0:127, :], lhsT=kcmpT[:, 0:127],
                                                        rhs=qraw[:, c * 512:(c + 1) * 512], start=True, stop=False),
                         r=[kcmpT, qraw], w=[ps])
                    S.pe(lambda e, ps=ps, c=c: e.matmul(ps[0:127, :], lhsT=ident[0:127, 0:127],
                                                        rhs=cmpb[0:127, c * 512:(c + 1) * 512], start=False, stop=True),
                         r=[ident, cmpb], w=[ps])
                    S.act(lambda e, ps=ps, b=b: e.activation(out=pcT[b][0:127, :], in_=ps[0:127, :], func=AF.Exp),
                          r=[ps], w=[pcT[b]])
                    pd = pmisc[b]
                    S.pe(lambda e, pd=pd, b=b: e.matmul(pd[0:127, :], lhsT=ones[0:127, 0:127], rhs=pcT[b][0:127, :],
                                                        start=True, stop=True), r=[ones, pcT[b]], w=[pd])
                    S.dve(lambda e, pd=pd, b=b: e.tensor_scalar(out=rinv[b][0:127, :], in0=pd[0:127, :], scalar1=1e-30,
                                                                scalar2=None, op0=ALU.max), r=[pd], w=[rinv[b]])
                    S.dve(lambda e, b=b: e.reciprocal(out=rinv[b][0:127, :], in_=rinv[b][0:127, :]),
                          r=[rinv[b]], w=[rinv[b]])
                    S.dve(lambda e, b=b: e.tensor_tensor(out=pnf[b][0:127, :], in0=pcT[b][0:127, :],
                                                         in1=rinv[b][0:127, :], op=ALU.mult),
                          r=[pcT[b], rinv[b]], w=[pnf[b]])
                    S.pool(lambda e, b=b: e.tensor_copy(out=pnb[b][0:127, :], in_=pnf[b][0:127, :]),
                           r=[pnf[b]], w=[pnb[b]])
                    if r == 0:
                        S.pool(lambda e, b=b, c=c: e.tensor_copy(out=psumT[0:127, c * 512:(c + 1) * 512],
                                                                 in_=pnf[b][0:127, :]), r=[pnf[b]], w=[(psumT, c)])
                    else:
                        S.pool(lambda e, b=b, c=c: e.tensor_tensor(
                            out=psumT[0:127, c * 512:(c + 1) * 512], in0=psumT[0:127, c * 512:(c + 1) * 512],
                            in1=pnf[b][0:127, :], op=ALU.add), r=[pnf[b], (psumT, c)], w=[(psumT, c)])
                    for j in range(4):
                        i = 4 * c + j
                        O = Oacc[j]
                        S.pe(lambda e, O=O, b=b, j=j: e.matmul(O[:, 0:64], lhsT=pnb[b][0:127, j * 128:(j + 1) * 128],
                                                               rhs=vcmp[0:127, :], start=True, stop=True),
                             r=[pnb[b], vcmp], w=[O])
                        S.dve(lambda e, O=O, i=i, r=r, h=h: e.tensor_scalar(
                            out=accg[:, i, r * 64:(r + 1) * 64], in0=O[:, 0:64], scalar1=gates[:, i, 3 * h:3 * h + 1],
                            scalar2=None, op0=ALU.mult), r=[O, gates], w=[(accg, (i, r))])
            for i in range(16):
                b = i % 2
                pp = pmisc[b]
                S.pe(lambda e, pp=pp, i=i: e.matmul(pp[:, 0:32], lhsT=psumT[0:127, i * 128:(i + 1) * 128],
                                                    rhs=ovl[0:127, 0:32], start=True, stop=True),
                     r=[psumT, ovl], w=[pp])
                sl = sel[b]
                S.dve(lambda e, pp=pp, sl=sl, i=i: e.tensor_tensor(out=sl[:], in0=pp[:, 0:32],
                                                                  in1=keep[:, i * 32:(i + 1) * 32], op=ALU.mult),
                      r=[pp, keep], w=[sl])
                S.dve(lambda e, sl=sl, i=i: e.tensor_tensor(out=sl[:], in0=sl[:], in1=addm[:, i * 32:(i + 1) * 32],
                                                           op=ALU.add), r=[sl, addm], w=[sl])
                S.dve(lambda e, sl=sl, b=b: e.max(out=m8[b][:], in_=sl[:]), r=[sl], w=[m8[b]])
                S.dve(lambda e, sl=sl, b=b: e.tensor_scalar(out=sl[:], in0=sl[:], scalar1=m8[b][:, 7:8], scalar2=None,
                                                            op0=ALU.is_ge), r=[sl, m8[b]], w=[sl])
                S.dve(lambda e, sl=sl, i=i: e.tensor_tensor(out=sl[:], in0=sl[:], in1=adm[:, i * 32:(i + 1) * 32],
                                                           op=ALU.mult), r=[sl, adm], w=[sl])
                S.dve(lambda e, sl=sl, b=b: e.tensor_scalar(out=nsel[b][:], in0=sl[:], scalar1=-NEG, scalar2=NEG,
                                                            op0=ALU.mult, op1=ALU.add), r=[sl], w=[nsel[b]])
                S.pe(lambda e, b=b: e.transpose(out=ptb[0:32, b * 128:(b + 1) * 128], in_=nsel[b][:],
                                                identity=ident[:]), r=[nsel[b], ident], w=[(ptb, b)])
                S.act(lambda e, i=i, b=b: e.copy(out=negselT[:, i * 128:(i + 1) * 128],
                                                 in_=ptb[0:32, b * 128:(b + 1) * 128]),
                      r=[(ptb, b)], w=[(negselT, i // 4)])
            for r in range(4):
                h = 4 * g + r
                S.dma('sp', qrot[:], dr['qrot'][h * 64:(h + 1) * 64, tb:tb + SEQ], r=[], w=[qrot])
                for c in range(4):
                    attn_chunk(S, c, causal_tiles(c, tri), qrot, kslT,
                               (lambda kt: E32[0:32, kt * 128:(kt + 1) * 128],
                                lambda kt, c, lo, hi: negselT[:, c * 512 + lo:c * 512 + hi], [E32, negselT]),
                               vsl, ident, STps, PT, Oacc, make_finalize(r, 3 * h + 1, False), 'slc')
                for c in range(4):
                    attn_chunk(S, c, window_tiles(c, tri, band), qrot, kwT, None,
                               vw, ident, STps, PT, Oacc, make_finalize(r, 3 * h + 2, False), 'win')
            S.act(lambda e, g=g: e.copy(out=otok[:, :, 256 + 256 * g:256 + 256 * (g + 1)], in_=accg[:]),
                  r=[accg], w=[(otok, ('g', g))])
        S.dma('sp', dr['o_tok'][tb:tb + SEQ, :].rearrange("(n p) c -> p n c", p=128), otok[:],
              r=[otok], w=[DW(S, dr['o_tok'])])
    S.flush()


def outproj_stage(S, x_in, x_out, o_tok, w_out, ntok, cpack):
    C = Consts(S, cpack)
    ident = C.get('ident', BF16)
    wob = S.sb([128, 8, D], BF16, 'wob')
    wst = [S.sb([128, D], F32, f'wos{i}') for i in range(2)]
    wv = w_out.rearrange("(c p) m -> p c m", p=128)
    for c in range(8):
        st = wst[c % 2]
        S.dma('sp', st[:], wv[:, c, :], r=[], w=[st])
        S.pool(lambda e, st=st, c=c: e.tensor_copy(out=wob[:, c, :], in_=st[:]), r=[st], w=[(wob, c)])
    ot = [S.sb([128, D], BF16, f'oo{i}') for i in range(2)]
    oT = [S.sb([128, 8, 128], BF16, f'oT{i}') for i in range(2)]
    xt = [S.sb([128, D], F32, f'xo{i}') for i in range(2)]
    xo = [S.sb([128, D], F32, f'xn{i}') for i in range(2)]
    ptr = [S.ps([128, 8, 128], BF16, f'ptr{i}') for i in range(2)]
    py = [S.ps([128, 512], F32, f'py{i}') for i in range(4)]
    for i in range(ntok // 128):
        b = i % 2
        S.dma('sp', ot[b][:], o_tok[i * 128:(i + 1) * 128, :], r=[], w=[ot[b]])
        S.dma('sp', xt[b][:], x_in[i * 128:(i + 1) * 128, :], r=[], w=[xt[b]])
        p = ptr[b]
        for c in range(8):
            S.pe(lambda e, p=p, b=b, c=c: e.transpose(out=p[:, c, :], in_=ot[b][:, c * 128:(c + 1) * 128],
                                                      identity=ident[:]), r=[ot[b], ident], w=[(p, c)])
        S.act(lambda e, p=p, b=b: e.copy(out=oT[b][:], in_=p[:]), r=[p], w=[oT[b]])
        for mh in range(2):
            pp = py[b * 2 + mh]
            for c in range(8):
                S.pe(lambda e, pp=pp, b=b, c=c, mh=mh: e.matmul(pp[:], lhsT=oT[b][:, c, :],
                                                                rhs=wob[:, c, mh * 512:(mh + 1) * 512],
                                                                start=(c == 0), stop=(c == 7)),
                     r=[oT[b], wob], w=[pp])
            S.dve(lambda e, pp=pp, b=b, mh=mh: e.tensor_tensor(out=xo[b][:, mh * 512:(mh + 1) * 512], in0=pp[:],
                                                               in1=xt[b][:, mh * 512:(mh + 1) * 512], op=ALU.add),
                  r=[pp, xt[b]], w=[(xo[b], mh)])
        S.dma('pool', x_out[i * 128:(i + 1) * 128, :], xo[b][:], r=[xo[b]], w=[DW(S, x_out)])
    S.flush()


def odd_proj(S, x, prm, dr, ntok, cpack):
    vb = [S.sb([128, 64], BF16, f'vb{i}') for i in range(2)]
    wb = [S.sb([128, 4], F32, f'wb{i}') for i in range(2)]
    vd = [S.sb([128, 512], BF16, f'vd{i}') for i in range(2)]

    def tm_post(gi, pp, tok0, n):
        b = (tok0 // 128) % 2
        if gi == 0:
            S.act(lambda e: e.copy(out=vb[b][:], in_=pp[:, 0:64]), r=[pp], w=[vb[b]])
            S.dma('pool', dr['vcd'][tok0:tok0 + 128, :], vb[b][:], r=[vb[b]], w=[DW(S, dr['vcd'])])
        elif gi == 1:
            S.act(lambda e: e.copy(out=wb[b][:], in_=pp[:, 0:4]), r=[pp], w=[wb[b]])
            S.dma('pool', dr['wi'][tok0:tok0 + 128, :], wb[b][:], r=[wb[b]], w=[DW(S, dr['wi'])])
        else:
            S.act(lambda e: e.copy(out=vd[b][:], in_=pp[:, 0:512]), r=[pp], w=[vd[b]])
            S.dma('pool', dr['vdd'][tok0:tok0 + 128, :], vd[b][:], r=[vd[b]], w=[DW(S, dr['vdd'])])
    fm = []
    for i in range(4):
        fm.append((128 * i, 128, 0.125, 'r64', None, dr['qc'][128 * i:128 * (i + 1), :]))
    fm.append((512, 64, 1.0, 'r64', None, dr['kcd'][:, :]))
    fm.append((640, 128, 1.0, 'r32', None, dr['qi'][:, :]))
    fm.append((768, 32, 1.0, 'r32', None, dr['ki'][:, :]))
    for i in range(4):
        fm.append((804 + 128 * i, 128, 0.125, 'r64', None, dr['qd'][128 * i:128 * (i + 1), :]))
    for i in range(4):
        fm.append((1316 + 128 * i, 128, 1.0, 'r64', None, dr['kd'][128 * i:128 * (i + 1), :]))
    proj_stage(S, x, prm['w_in'], 2340, prm['mix_norm'], prm['pos'], ntok, cpack, fm,
               [(576, 64), (800, 4), (1828, 512)], tm_post, use_idx=True)


NBIS = 14


def dsa_moba_stage(S, prm, dr, nseq, cpack):
    C = Consts(S, cpack)
    ident = C.get('ident', BF16)
    tri = C.get('tri_ge', BF16)
    E8 = C.get('E8', BF16)
    triqs = C.get('tri_qs', F32)
    mbias, mpast, mown = C.get('mbias'), C.get('mpast'), C.get('mown')
    qi = [S.sb([32, SEQ], BF16, f'qi{h}') for h in range(4)]
    ki = S.sb([32, SEQ], BF16, 'ki')
    wi = S.sb([128, 16, 4], F32, 'wi')
    absw = S.sb([128, 16, 4], F32, 'absw')
    sgnw = S.sb([128, 16, 4], F32, 'sgnw')
    score = S.sb([128, SEQ], F32, 'score')
    rl = [S.sb([128, 512], F32, f'rl{i}') for i in range(2)]
    junkb = S.sb([128, SEQ], BF16, 'junkb')
    nmask = S.sb([128, SEQ], BF16, 'nmask')
    nmT = S.sb([128, 16, 512], BF16, 'nmT')
    kcT = S.sb([64, SEQ], BF16, 'kcT')
    vcx = S.sb([128, 16, 65], BF16, 'vcx')
    vdx = S.sb([128, 16, 65], BF16, 'vdx')
    S.pool(lambda e: e.memset(vcx[:], 1.0), r=[], w=[vcx])
    S.pool(lambda e: e.memset(vdx[:], 1.0), r=[], w=[vdx])
    qcall = S.sb([64, 8, SEQ], BF16, 'qcall')
    qd = S.sb([64, SEQ], BF16, 'qd')
    kd = S.sb([64, SEQ], BF16, 'kd')
    kmf = S.sb([64, 8], F32, 'kmf')
    kmb = S.sb([64, 8], BF16, 'kmb')
    negsel8 = S.sb([8, SEQ], BF16, 'negsel8')
    otok = S.sb([128, 16, 1024], BF16, 'otok')
    PT = [S.sb([128, 512], BF16, f'PT{i}') for i in range(2)]
    st5 = [S.sb([128, 8], F32, f'st5{i}') for i in range(2)]
    gs = [S.sb([128, 8], F32, f'gs{i}') for i in range(2)]
    m8 = [S.sb([128, 8], F32, f'm8{i}') for i in range(2)]
    ns8 = [S.sb([128, 8], BF16, f'ns8{i}') for i in range(2)]
    fsc = [S.sb([128, 1], F32, f'fsc{i}') for i in range(4)]
    STps = [S.ps([128, 512], F32, f'st{i}') for i in range(2)]
    Oacc = [S.ps([128, 512], F32, f'oa{i}') for i in range(4)]
    ptb = S.ps([128, 8, 128], BF16, 'ptb')
    pl = S.ps([128, 512], F32, 'pl')
    fcount = [0]

    def make_fin(col0):
        def fin(c, j, O):
            i = 4 * c + j
            f = fsc[fcount[0] % 4]
            fcount[0] += 1
            S.dve(lambda e: e.tensor_scalar(out=f[:], in0=O[:, 64:65], scalar1=1e-30, scalar2=None, op0=ALU.max),
                  r=[O], w=[f])
            S.dve(lambda e: e.reciprocal(out=f[:], in_=f[:]), r=[f], w=[f])
            S.dve(lambda e: e.tensor_scalar(out=otok[:, i, col0:col0 + 64], in0=O[:, 0:64], scalar1=f[:, 0:1],
                                            scalar2=None, op0=ALU.mult), r=[O, f], w=[(otok, (i, col0))])
        return fin

    for sq in range(nseq):
        tb = sq * SEQ
        for h in range(4):
            S.dma('sp', qi[h][:], dr['qi'][h * 32:(h + 1) * 32, tb:tb + SEQ], r=[], w=[qi[h]])
        S.dma('sp', ki[:], dr['ki'][:, tb:tb + SEQ], r=[], w=[ki])
        S.dma('sp', wi[:], dr['wi'][tb:tb + SEQ, :].rearrange("(n p) c -> p n c", p=128), r=[], w=[wi])
        S.act(lambda e: e.activation(out=absw[:], in_=wi[:], func=AF.Abs), r=[wi], w=[absw])
        S.act(lambda e: e.activation(out=sgnw[:], in_=wi[:], func=AF.Sign), r=[wi], w=[sgnw])
        S.dma('sp', kcT[:], dr['kcd'][:, tb:tb + SEQ], r=[], w=[kcT])
        S.dma('sp', vcx[:, :, 0:64], dr['vcd'][tb:tb + SEQ, :].rearrange("(n p) c -> p n c", p=128), r=[], w=[vcx])
        for h in range(8):
            S.dma('sp', qcall[:, h, :], dr['qc'][h * 64:(h + 1) * 64, tb:tb + SEQ], r=[], w=[(qcall, h)])
        for c in range(4):
            for j in range(4):
                i = 4 * c + j
                W = 128 * (i + 1)
                if i < 2:
                    for st_ in range(i + 1):
                        if st_ == i:
                            S.pool(lambda e, st_=st_, j=j: e.tensor_copy(out=nmT[:, st_, j * 128:(j + 1) * 128],
                                                                         in_=tri[:]), r=[tri], w=[(nmT, (st_, j))])
                        else:
                            S.pool(lambda e, st_=st_, j=j: e.memset(nmT[:, st_, j * 128:(j + 1) * 128], 0.0),
                                   r=[], w=[(nmT, (st_, j))])
                    continue
                for h in range(4):
                    for sc in range((W + 511) // 512):
                        n = min(512, W - 512 * sc)
                        rb = rl[(h * 4 + sc) % 2]
                        S.pe(lambda e, h=h, sc=sc, n=n, i=i: e.matmul(pl[:, 0:n], lhsT=qi[h][:, i * 128:(i + 1) * 128],
                                                                       rhs=ki[:, sc * 512:sc * 512 + n], start=True,
                                                                       stop=True), r=[qi[h], ki], w=[pl])
                        S.act(lambda e, rb=rb, n=n, i=i, h=h: e.activation(out=rb[:, 0:n], in_=pl[:, 0:n], func=AF.Relu,
                                                                           scale=absw[:, i, h:h + 1]),
                              r=[pl, absw], w=[rb])
                        if h == 0:
                            S.dve(lambda e, rb=rb, n=n, sc=sc, i=i, h=h: e.tensor_scalar(
                                out=score[:, sc * 512:sc * 512 + n], in0=rb[:, 0:n], scalar1=sgnw[:, i, h:h + 1],
                                scalar2=None, op0=ALU.mult), r=[rb, sgnw], w=[(score, sc)])
                        else:
                            S.dve(lambda e, rb=rb, n=n, sc=sc, i=i, h=h: e.scalar_tensor_tensor(
                                out=score[:, sc * 512:sc * 512 + n], in0=rb[:, 0:n], scalar=sgnw[:, i, h:h + 1],
                                in1=score[:, sc * 512:sc * 512 + n], op0=ALU.mult, op1=ALU.add),
                                r=[rb, sgnw, (score, sc)], w=[(score, sc)])
                s5 = st5[i % 2]
                S.dve(lambda e, s5=s5, W=W: e.tensor_reduce(out=s5[:, 5:6], in_=score[:, 0:W], axis=AX.X, op=ALU.max),
                      r=[score], w=[(s5, 5)])
                S.dve(lambda e, s5=s5, W=W: e.tensor_reduce(out=s5[:, 0:1], in_=score[:, 0:W], axis=AX.X, op=ALU.min),
                      r=[score], w=[(s5, 0)])
                S.dve(lambda e, s5=s5: e.tensor_tensor(out=s5[:, 1:2], in0=s5[:, 5:6], in1=s5[:, 0:1], op=ALU.subtract),
                      r=[(s5, 5), (s5, 0)], w=[(s5, 1)])
                S.dve(lambda e, i=i: e.tensor_tensor(out=score[:, i * 128:(i + 1) * 128],
                                                     in0=score[:, i * 128:(i + 1) * 128], in1=triqs[:], op=ALU.add),
                      r=[score, triqs], w=[score])
                for it in range(NBIS):
                    S.dve(lambda e, s5=s5: e.tensor_scalar(out=s5[:, 1:2], in0=s5[:, 1:2], scalar1=0.5, scalar2=None,
                                                           op0=ALU.mult), r=[(s5, 1)], w=[(s5, 1)])
                    S.dve(lambda e, s5=s5: e.tensor_tensor(out=s5[:, 2:3], in0=s5[:, 0:1], in1=s5[:, 1:2], op=ALU.add),
                          r=[(s5, 0), (s5, 1)], w=[(s5, 2)])
                    S.dve(lambda e, s5=s5, W=W: e.tensor_scalar(out=junkb[:, 0:W], in0=score[:, 0:W],
                                                                scalar1=s5[:, 2:3], scalar2=0.0, op0=ALU.is_ge,
                                                                op1=ALU.add, accum_out=s5[:, 3:4]),
                          r=[score, (s5, 2)], w=[junkb, (s5, 3)])
                    S.dve(lambda e, s5=s5: e.tensor_scalar(out=s5[:, 4:5], in0=s5[:, 3:4], scalar1=255.5, scalar2=None,
                                                           op0=ALU.is_ge), r=[(s5, 3)], w=[(s5, 4)])
                    S.dve(lambda e, s5=s5: e.scalar_tensor_tensor(out=s5[:, 0:1], in0=s5[:, 1:2], scalar=s5[:, 4:5],
                                                                  in1=s5[:, 0:1], op0=ALU.mult, op1=ALU.add),
                          r=[(s5, 1), (s5, 4), (s5, 0)], w=[(s5, 0)])
                S.dve(lambda e, s5=s5, W=W: e.tensor_scalar(out=nmask[:, 0:W], in0=score[:, 0:W], scalar1=s5[:, 0:1],
                                                            scalar2=NEG, op0=ALU.is_lt, op1=ALU.mult),
                      r=[score, (s5, 0)], w=[nmask])
                for s0 in range(0, i + 1, 8):
                    n = min(8, i + 1 - s0)
                    for k in range(n):
                        S.pe(lambda e, s0=s0, k=k: e.transpose(out=ptb[:, k, :],
                                                               in_=nmask[:, (s0 + k) * 128:(s0 + k + 1) * 128],
                                                               identity=ident[:]), r=[nmask, ident], w=[(ptb, k)])
                    S.act(lambda e, s0=s0, n=n, j=j: e.copy(out=nmT[:, s0:s0 + n, j * 128:(j + 1) * 128],
                                                            in_=ptb[:, 0:n, :]), r=[ptb], w=[(nmT, ('b', s0, j))])
            for h in range(8):
                attn_chunk(S, c, causal_tiles(c, None), qcall[:, h, :], kcT,
                           (lambda kt: ident[:], lambda kt, c, lo, hi: nmT[:, kt, lo:hi], [ident, nmT]),
                           vcx, ident, STps, PT, Oacc, make_fin(64 * h), 'dsa', qdep=(qcall, h))
        for h in range(8):
            S.dma('sp', qd[:], dr['qd'][h * 64:(h + 1) * 64, tb:tb + SEQ], r=[], w=[qd])
            S.dma('sp', kd[:], dr['kd'][h * 64:(h + 1) * 64, tb:tb + SEQ], r=[], w=[kd])
            S.dma('sp', vdx[:, :, 0:64], dr['vdd'][tb:tb + SEQ, h * 64:(h + 1) * 64].rearrange(
                "(n p) c -> p n c", p=128), r=[], w=[vdx])
            S.dve(lambda e: e.tensor_reduce(out=kmf[:], in_=kd[:].rearrange("p (j k) -> p j k", k=256), axis=AX.X,
                                            op=ALU.add), r=[kd], w=[kmf])
            S.dve(lambda e: e.tensor_scalar(out=kmb[:], in0=kmf[:], scalar1=1.0 / 256, scalar2=None, op0=ALU.mult),
                  r=[kmf], w=[kmb])
            for i in range(16):
                b = i % 2
                S.pe(lambda e, i=i: e.matmul(pl[:, 0:8], lhsT=qd[:, i * 128:(i + 1) * 128], rhs=kmb[:], start=True,
                                             stop=True), r=[qd, kmb], w=[pl])
                g_ = gs[b]
                S.dve(lambda e, g_=g_, i=i: e.tensor_tensor(out=g_[:], in0=pl[:, 0:8], in1=mbias[:, i * 8:(i + 1) * 8],
                                                           op=ALU.add), r=[pl, mbias], w=[g_])
                S.dve(lambda e, g_=g_, b=b: e.max(out=m8[b][:], in_=g_[:]), r=[g_], w=[m8[b]])
                S.dve(lambda e, g_=g_, b=b: e.tensor_scalar(out=g_[:], in0=g_[:], scalar1=m8[b][:, 2:3], scalar2=None,
                                                            op0=ALU.is_ge), r=[g_, m8[b]], w=[g_])
                S.dve(lambda e, g_=g_, i=i: e.tensor_tensor(out=g_[:], in0=g_[:], in1=mpast[:, i * 8:(i + 1) * 8],
                                                           op=ALU.mult), r=[g_, mpast], w=[g_])
                S.dve(lambda e, g_=g_, i=i: e.tensor_tensor(out=g_[:], in0=g_[:], in1=mown[:, i * 8:(i + 1) * 8],
                                                           op=ALU.add), r=[g_, mown], w=[g_])
                S.dve(lambda e, g_=g_, b=b: e.tensor_scalar(out=ns8[b][:], in0=g_[:], scalar1=-NEG, scalar2=NEG,
                                                            op0=ALU.mult, op1=ALU.add), r=[g_], w=[ns8[b]])
                S.pe(lambda e, b=b: e.transpose(out=ptb[0:8, b, :], in_=ns8[b][:], identity=ident[:]),
                     r=[ns8[b], ident], w=[(ptb, b)])
                S.act(lambda e, b=b, i=i: e.copy(out=negsel8[:, i * 128:(i + 1) * 128], in_=ptb[0:8, b, :]),
                      r=[(ptb, b)], w=[(negsel8, i // 4)])
            for c in range(4):
                attn_chunk(S, c, causal_tiles(c, tri), qd, kd,
                           (lambda kt: E8[0:8, kt * 128:(kt + 1) * 128], lambda kt, c, lo, hi: negsel8[:, c * 512 + lo:c * 512 + hi],
                            [E8, negsel8]),
                           vdx, ident, STps, PT, Oacc, make_fin(512 + 64 * h), 'moba')
        S.dma('sp', dr['o_tok'][tb:tb + SEQ, :].rearrange("(n p) c -> p n c", p=128), otok[:],
              r=[otok], w=[DW(S, dr['o_tok'])])
    S.flush()


NCORES = 8
TPC = 2 * SEQ


def build_program(stages=None):
    nc = bass.Bass("TRN2", target_bir_lowering=False)
    ins = {}

    def din(name, shape, dt=F32):
        ins[name] = nc.dram_tensor(name, list(shape), dt, kind="ExternalInput").ap()
        return ins[name]

    def scr(name, shape, dt=BF16):
        return nc.dram_tensor(name, list(shape), dt, kind="Internal").ap()

    x = din('x', [TPC, D])
    pos = din('pos', [TPC], I32)
    cp = din('cpack', [128, CP_N])
    P = {}
    for L in range(2):
        for f in ('ffn1', 'ffn2'):
            P[f'{f}_norm{L}'] = din(f'{f}_norm{L}', [D])
            P[f'{f}_wg{L}'] = din(f'{f}_wg{L}', [D, DFF])
            P[f'{f}_wu{L}'] = din(f'{f}_wu{L}', [D, DFF])
            P[f'{f}_wd{L}'] = din(f'{f}_wd{L}', [DFF, D])
        P[f'mix_norm{L}'] = din(f'mix_norm{L}', [D])
    ev = {'w_in': din('ev_w_in', [D, 2468]), 'mix_norm': P['mix_norm0'], 'pos': pos,
          'sgu_norm': din('ev_sgu_norm', [256]), 'sgu_wT': din('ev_sgu_wT', [4, 128, 128]),
          'sgu_bT': din('ev_sgu_bT', [128, 4]),
          'cmp_w1_k': din('ev_w1k', [2048, 256]), 'cmp_w2_k': din('ev_w2k', [256, 64]),
          'cmp_posT_k': din('ev_pk', [64, 32]),
          'cmp_w1_v': din('ev_w1v', [2048, 256]), 'cmp_w2_v': din('ev_w2v', [256, 64]),
          'cmp_posT_v': din('ev_pv', [64, 32])}
    ev_w_out = din('ev_w_out', [D, D])
    od = {'w_in': din('od_w_in', [D, 2340]), 'mix_norm': P['mix_norm1'], 'pos': pos}
    od_w_out = din('od_w_out', [D, D])
    fin_g = din('final_norm', [D])
    y = nc.dram_tensor('y', [TPC, D], F32, kind="ExternalOutput").ap()
    xa = scr('xa', [TPC, D], F32)
    xb = scr('xb', [TPC, D], F32)
    T_ = TPC
    dre = {'a_tok': scr('a_tok', [T_, 512]), 'qraw': scr('qraw', [768, T_]), 'qrot': scr('qrot', [768, T_]),
           'kc': scr('kc', [192, T_]), 'vc': scr('vc', [192, T_]), 'ksl': scr('ksl', [192, T_]),
           'kw': scr('kw', [192, T_]), 'vsl': scr('vsl', [T_, 192]), 'vw': scr('vw', [T_, 192]),
           'gate': scr('gate', [T_, 36], F32), 'o_tok': scr('o_tok0', [T_, 1024])}
    dro = {'qc': scr('qc', [512, T_]), 'kcd': scr('kcd', [64, T_]), 'vcd': scr('vcd', [T_, 64]),
           'qi': scr('qi', [128, T_]), 'ki': scr('ki', [32, T_]), 'wi': scr('wi', [T_, 4], F32),
           'qd': scr('qd', [512, T_]), 'kd': scr('kd', [512, T_]), 'vdd': scr('vdd', [T_, 512]),
           'o_tok': scr('o_tok1', [T_, 1024])}
    S = Sched(nc)

    def ffn(xi, xo, f, L, fg=None):
        ffn_stage(S, xi, xo, P[f'{f}_norm{L}'], P[f'{f}_wg{L}'], P[f'{f}_wu{L}'], P[f'{f}_wd{L}'], TPC, cp_d, fg)
    cp_d = {'ident': cp[:, CP_OFF['ident'][0]:CP_OFF['ident'][0] + 128]}
    ffn(x, xa, 'ffn1', 0)
    even_proj(S, xa, ev, dre, TPC, cp)
    nsa_stage(S, ev, dre, 2, cp)
    outproj_stage(S, xa, xb, dre['o_tok'], ev_w_out, TPC, cp)
    ffn(xb, xa, 'ffn2', 0)
    ffn(xa, xb, 'ffn1', 1)
    odd_proj(S, xb, od, dro, TPC, cp)
    dsa_moba_stage(S, od, dro, 2, cp)
    outproj_stage(S, xb, xa, dro['o_tok'], od_w_out, TPC, cp)
    ffn(xa, y, 'ffn2', 1, fin_g)
    return nc, S


def kernel(**inp):
    inp = {k: np.asarray(v) for k, v in inp.items()}
    nc, S = build_program()
    cpk = host_consts()
    c = np.ascontiguousarray
    shared = {'cpack': cpk}
    for L in range(2):
        for f in ('ffn1', 'ffn2'):
            shared[f'{f}_norm{L}'] = c(inp[f'{f}_norm'][L])
            shared[f'{f}_wg{L}'] = c(inp[f'{f}_w_gate'][L])
            shared[f'{f}_wu{L}'] = c(inp[f'{f}_w_up'][L])
            shared[f'{f}_wd{L}'] = c(inp[f'{f}_w_down'][L])
        shared[f'mix_norm{L}'] = c(inp['mix_norm'][L])
    shared.update({
        'ev_w_in': c(inp['ev_w_in'][0]), 'ev_sgu_norm': c(inp['ev_sgu_norm'][0]),
        'ev_sgu_wT': c(inp['ev_sgu_w'][0].transpose(0, 2, 1)), 'ev_sgu_bT': c(inp['ev_sgu_b'][0].T),
        'ev_w1k': c(inp['ev_cmp_w1_k'][0]), 'ev_w2k': c(inp['ev_cmp_w2_k'][0]), 'ev_pk': c(inp['ev_cmp_pos_k'][0].T),
        'ev_w1v': c(inp['ev_cmp_w1_v'][0]), 'ev_w2v': c(inp['ev_cmp_w2_v'][0]), 'ev_pv': c(inp['ev_cmp_pos_v'][0].T),
        'ev_w_out': c(inp['ev_w_out'][0]), 'od_w_in': c(inp['od_w_in'][0]), 'od_w_out': c(inp['od_w_out'][0]),
        'final_norm': c(inp['final_norm'])})
    in_maps = []
    for k in range(NCORES):
        m = dict(shared)
        m['x'] = c(inp['x'][2 * k:2 * k + 2].reshape(TPC, D))
        m['pos'] = c(inp['positions'][2 * k:2 * k + 2].reshape(TPC).astype(np.int32))
        in_maps.append(m)
    res = run_bass_kernel_spmd(nc, in_maps, core_ids=list(range(NCORES)))
    out = np.stack([np.asarray(r['y']).reshape(2, SEQ, D) for r in res.results], axis=0)
    return out.reshape(16, SEQ, D).astype(np.float32)
```

```python
from contextlib import ExitStack
import numpy as np
import concourse.bass as bass
import concourse.mybir as mybir
from concourse.bass_utils import run_bass_kernel_spmd

F32 = mybir.dt.float32
BF16 = mybir.dt.bfloat16
I32 = mybir.dt.int32
AF = mybir.ActivationFunctionType
ALU = mybir.AluOpType
AX = mybir.AxisListType

ENGS = ['pe', 'act', 'dve', 'pool', 'sp']
import os
BANKDEP = False
USE_NEW_ODD = False
NDSEM = 66
NSWSEM = 26


class T:
    _n = 0

    def __init__(self, t, name=None):
        self.t = t
        T._n += 1
        self.id = T._n
        self.name = name

    def __getitem__(self, idx):
        return self.t[idx]


class Op:
    __slots__ = ('eng', 'fn', 'deps', 'isdma', 'sem', 'cnt', 'signal', 'waits', 'vc')


class Sched:
    def __init__(self, nc):
        self.nc = nc
        self.esem = {e: nc.alloc_semaphore(name=f'es_{e}') for e in ENGS}
        self.ecnt = {e: 0 for e in ENGS}
        self.free_dsems = {False: [nc.alloc_semaphore(name=f'ds_{i}') for i in range(NDSEM)],
                           True: [nc.alloc_semaphore(name=f'dw_{i}') for i in range(NSWSEM)]}
        self.dcnt = {}
        self.n_inst = 0
        self.base = {}
        self._reset()

    def _reset(self):
        self.ops = []
        self.state = {}
        self.buf_dsem = {}
        self.stack = ExitStack()

    def sb(self, shape, dtype, name=None):
        t = self.stack.enter_context(self.nc.sbuf_tensor(f'{name or "sb"}_{T._n}', list(shape), dtype))
        return T(t, name)

    def ps(self, shape, dtype, name=None):
        t = self.stack.enter_context(self.nc.psum_tensor(f'{name or "ps"}_{T._n}', list(shape), dtype))
        return T(t, name)

    @staticmethod
    def _norm(item):
        if isinstance(item, T):
            return item.id, None
        return item[0].id, item[1]

    def _track(self, r, w, opi):
        deps = {}
        for item in r:
            tid, key = self._norm(item)
            st = self.state.setdefault(tid, {})
            for k, ent in st.items():
                if k == key or k is None or key is None:
                    if ent[0] is not None:
                        deps[ent[0]] = True
            st.setdefault(key, [None, []])[1].append(opi)
        for item in w:
            tid, key = self._norm(item)
            st = self.state.setdefault(tid, {})
            for k, ent in st.items():
                if k == key or k is None or key is None:
                    if ent[0] is not None:
                        deps[ent[0]] = True
                    for x in ent[1]:
                        deps.setdefault(x, False)
            if key is None:
                st.clear()
            st[key] = [opi, []]
        deps.pop(opi, None)
        return deps

    @staticmethod
    def _skip(p, o, raw):
        if p.isdma or o.isdma or p.eng != o.eng:
            return False
        return p.eng == 'pe' or not raw

    def op(self, eng, fn, r=(), w=()):
        o = Op()
        o.eng, o.fn, o.isdma, o.signal = eng, fn, False, False
        o.sem, o.cnt, o.waits, o.vc = None, 0, None, None
        o.deps = self._track(r, w, len(self.ops))
        self.ops.append(o)
        return o

    def dma(self, eng, out_ap, in_ap, r, w, **kw):
        o = self.op(eng, lambda e: e.dma_start(out=out_ap, in_=in_ap, **kw), r, w)
        o.isdma = True
        it = w[0] if isinstance(w[0], T) else w[0][0]
        if it.t is None and len(r) > 0:
            it = r[0] if isinstance(r[0], T) else r[0][0]
        tid = (it.id, eng == 'pool')
        if tid not in self.buf_dsem:
            self.buf_dsem[tid] = self.free_dsems[eng == 'pool'].pop()
            if not hasattr(self, 'sem_names'):
                self.sem_names = {}
            self.sem_names[self.buf_dsem[tid]] = it.name
        o.sem = self.buf_dsem[tid]
        self.dcnt[o.sem] = self.dcnt.get(o.sem, 0) + 16
        o.cnt = self.dcnt[o.sem]
        return o

    def pe(self, fn, r=(), w=()):
        return self.op('pe', fn, r, w)

    def act(self, fn, r=(), w=()):
        return self.op('act', fn, r, w)

    def dve(self, fn, r=(), w=()):
        return self.op('dve', fn, r, w)

    def pool(self, fn, r=(), w=()):
        return self.op('pool', fn, r, w)

    def flush(self):
        nc, ops = self.nc, self.ops
        for o in ops:
            for d, raw in o.deps.items():
                p = ops[d]
                if p.isdma or self._skip(p, o, raw):
                    continue
                p.signal = True
        for o in ops:
            if not o.isdma and o.signal:
                self.ecnt[o.eng] += 1
                o.cnt = self.ecnt[o.eng]
                o.sem = self.esem[o.eng]
        known = {e: dict(self.base) for e in ENGS}
        for o in ops:
            kn = known[o.eng]
            waits = {}
            for d in sorted(o.deps, reverse=True):
                p = ops[d]
                if self._skip(p, o, o.deps[d]):
                    continue
                if kn.get(p.sem, 0) >= p.cnt:
                    continue
                if waits.get(p.sem, 0) < p.cnt:
                    waits[p.sem] = p.cnt
                for s, c in p.vc.items():
                    if kn.get(s, 0) < c:
                        kn[s] = c
                kn[p.sem] = p.cnt
            o.waits = list(waits.items())
            if o.isdma and o.cnt > 16 and kn.get(o.sem, 0) < o.cnt - 16 and getattr(self, 'diag', False):
                print('DMA overlap on sem', self.sem_names.get(o.sem), 'cnt', o.cnt, 'known', kn.get(o.sem, 0))
            if o.isdma or o.signal:
                o.vc = dict(kn)
        by = {e: [o for o in ops if o.eng == e] for e in ENGS}
        final_d = [(s, self.dcnt[s]) for s in set(self.buf_dsem.values())]
        self.n_inst += len(ops)

        def emit(e, lst):
            for o in lst:
                for s, c in o.waits:
                    e.wait_ge(s, c)
                ins = o.fn(e)
                if o.isdma:
                    ins.then_inc(o.sem, 16)
                elif o.signal:
                    ins.then_inc(o.sem, 1)

        with nc.Block() as block:
            @block.tensor
            def _(e):
                emit(e, by['pe'])

            @block.scalar
            def _(e):
                emit(e, by['act'])

            @block.vector
            def _(e):
                emit(e, by['dve'])

            @block.gpsimd
            def _(e):
                emit(e, by['pool'])

            @block.sync
            def _(e):
                emit(e, by['sp'])
                for s, c in final_d:
                    e.wait_ge(s, c)
        for (tid_, sw), s in self.buf_dsem.items():
            self.free_dsems[sw].append(s)
        self.base = dict(self.dcnt)
        for e in ENGS:
            self.base[self.esem[e]] = self.ecnt[e]
        self.stack.close()
        self._reset()


D = 1024
DFF = 2816
NFC = DFF // 128
SEQ = 2048
EPS = 1e-6


def load_consts(S, cpack):
    c = {}
    idf = S.sb([128, 128], F32, 'idf')
    S.dma('sp', idf[:], cpack['ident'], r=[], w=[idf])
    idb = S.sb([128, 128], BF16, 'idb')
    S.dve(lambda e: e.tensor_copy(out=idb[:], in_=idf[:]), r=[idf], w=[idb])
    c['ident'] = idb
    return c


def rms_rstd(S, xt, junk, ss, rstd, width, key=None):
    S.act(lambda e: e.activation(out=junk[:], in_=xt[:], func=AF.Square, accum_out=ss[:]),
          r=[xt], w=[junk, ss])
    S.act(lambda e: e.activation(out=rstd[:], in_=ss[:], func=AF.Sqrt, bias=EPS, scale=1.0 / width),
          r=[ss], w=[rstd])
    S.dve(lambda e: e.reciprocal(out=rstd[:], in_=rstd[:]), r=[rstd], w=[rstd])


def ffn_stage(S, x_in, x_out, g_ap, wg, wu, wd, ntok, cpack, final_g=None, w16=None):
    CH = 1024
    NT = CH // 128
    consts = load_consts(S, cpack)
    ident = consts['ident']
    gb = S.sb([128, D], F32, 'gb')
    S.dma('sp', gb[:], g_ap.partition_broadcast(128), r=[], w=[gb])
    if final_g is not None:
        fgb = S.sb([128, D], F32, 'fgb')
        S.dma('sp', fgb[:], final_g.partition_broadcast(128), r=[], w=[fgb])
    hT = S.sb([128, 8, CH], BF16, 'hT')
    actT = S.sb([128, NFC, CH], BF16, 'actT')
    wdb = S.sb([128, NFC, D], BF16, 'wdb')
    xt = [S.sb([128, D], F32, f'xt{i}') for i in range(2)]
    hb = [S.sb([128, D], BF16, f'hb{i}') for i in range(2)]
    junk = S.sb([128, D], BF16, 'junk')
    ss = [S.sb([128, 1], F32, f'ss{i}') for i in range(2)]
    rstd = [S.sb([128, 1], F32, f'rstd{i}') for i in range(2)]
    FB = 256
    NB = DFF // FB
    wgs = [S.sb([128, 8, FB], F32, f'wgs{i}') for i in range(2)]
    wus = [S.sb([128, 8, FB], F32, f'wus{i}') for i in range(2)]
    wgb = [S.sb([128, 8, FB], BF16, f'wgb{i}') for i in range(2)]
    wub = [S.sb([128, 8, FB], BF16, f'wub{i}') for i in range(2)]
    wds = [S.sb([128, D], F32, f'wds{i}') for i in range(2)]
    sg = [S.sb([128, 512], F32, f'sg{i}') for i in range(2)]
    ot = [S.sb([128, D], F32, f'ot{i}') for i in range(2)]
    ptr = [S.ps([128, 8, 128], BF16, f'ptr{i}') for i in range(2)]
    pg = [S.ps([128, 512], F32, f'pg{i}') for i in range(2)]
    pu = [S.ps([128, 512], F32, f'pu{i}') for i in range(2)]
    py = [S.ps([128, 512], F32, f'py{i}') for i in range(2)]
    wg_v = wg.rearrange("(c p) f -> p c f", p=128)
    wu_v = wu.rearrange("(c p) f -> p c f", p=128)
    wd_v = wd.rearrange("(c p) m -> p c m", p=128)

    xt3 = [S.sb([128, D], F32, f'xt3{i}') for i in range(2)]

    def phase1_tile(ch, i):
        t0 = ch * CH
        b = i % 2
        x_t, h_b = xt[b], hb[b]
        S.dma('sp', x_t[:], x_in[t0 + i * 128:t0 + (i + 1) * 128, :], r=[], w=[x_t])
        rms_rstd(S, x_t, junk, ss[b], rstd[b], D)
        S.dve(lambda e: e.scalar_tensor_tensor(
            out=h_b[:], in0=x_t[:], scalar=rstd[b][:, 0:1], in1=gb[:], op0=ALU.mult, op1=ALU.mult),
            r=[x_t, rstd[b], gb], w=[h_b])
        p = ptr[b]
        for c in range(8):
            S.pe(lambda e, c=c: e.transpose(out=p[:, c, :], in_=h_b[:, c * 128:(c + 1) * 128], identity=ident[:]),
                 r=[h_b, ident], w=[(p, c)])
        S.act(lambda e: e.copy(out=hT[:, :, i * 128:(i + 1) * 128], in_=p[:]), r=[p], w=[(hT, i // 4)])

    wdT = T(None, 'wd16')

    def load_wd(ch, fc):
        if ch == 0 or w16 is None:
            s_ = wds[fc % 2]
            S.dma('sp', s_[:], wd_v[:, fc, :], r=[], w=[s_])
            S.act(lambda e: e.copy(out=wdb[:, fc, :], in_=s_[:]), r=[s_], w=[(wdb, fc)])
            if w16 is not None:
                S.dma('pool', w16['wd'][:, fc, :], wdb[:, fc, :], r=[(wdb, fc)], w=[(wdT, fc)])
        else:
            S.dma('sp', wdb[:, fc, :], w16['wd'][:, fc, :], r=[(wdT, fc)], w=[(wdb, fc)])

    wgT = T(None, 'wg16')

    def phase2(ch):
        for fb in range(NB):
            b = fb % 2
            if ch == 0 or w16 is None:
                S.dma('sp', wgs[b][:], wg_v[:, :, fb * FB:(fb + 1) * FB], r=[], w=[wgs[b]])
                S.dma('sp', wus[b][:], wu_v[:, :, fb * FB:(fb + 1) * FB], r=[], w=[wus[b]])
                S.dve(lambda e, b=b: e.tensor_copy(out=wgb[b][:], in_=wgs[b][:]), r=[wgs[b]], w=[wgb[b]])
                S.dve(lambda e, b=b: e.tensor_copy(out=wub[b][:], in_=wus[b][:]), r=[wus[b]], w=[wub[b]])
                if w16 is not None:
                    S.dma('pool', w16['wg'][:, fb, :, :], wgb[b][:], r=[wgb[b]], w=[(wgT, ('g', fb))])
                    S.dma('pool', w16['wu'][:, fb, :, :], wub[b][:], r=[wub[b]], w=[(wgT, ('u', fb))])
            else:
                S.dma('sp', wgb[b][:], w16['wg'][:, fb, :, :], r=[(wgT, ('g', fb))], w=[wgb[b]])
                S.dma('sp', wub[b][:], w16['wu'][:, fb, :, :], r=[(wgT, ('u', fb))], w=[wub[b]])
            load_wd(ch, 2 * fb)
            load_wd(ch, 2 * fb + 1)
            for fs in range(FB // 128):
                fc = fb * (FB // 128) + fs
                for tb in range(CH // 512):
                    q = (fc * 2 + tb) % 2
                    for c in range(8):
                        S.pe(lambda e, q=q, b=b, c=c, fs=fs, tb=tb: e.matmul(
                            pg[q][:], lhsT=wgb[b][:, c, fs * 128:(fs + 1) * 128],
                            rhs=hT[:, c, tb * 512:(tb + 1) * 512], start=(c == 0), stop=(c == 7)),
                            r=[wgb[b], (hT, tb)], w=[pg[q]])
                    for c in range(8):
                        S.pe(lambda e, q=q, b=b, c=c, fs=fs, tb=tb: e.matmul(
                            pu[q][:], lhsT=wub[b][:, c, fs * 128:(fs + 1) * 128],
                            rhs=hT[:, c, tb * 512:(tb + 1) * 512], start=(c == 0), stop=(c == 7)),
                            r=[wub[b], (hT, tb)], w=[pu[q]])
                    S.act(lambda e, q=q: e.activation(out=sg[q][:], in_=pg[q][:], func=AF.Silu),
                          r=[pg[q]], w=[sg[q]])
                    S.dve(lambda e, q=q, fc=fc, tb=tb: e.tensor_tensor(
                        out=actT[:, fc, tb * 512:(tb + 1) * 512], in0=pu[q][:], in1=sg[q][:], op=ALU.mult),
                        r=[pu[q], sg[q]], w=[(actT, (fc, tb))])

    def phase3_tile(ch, i):
        t0 = ch * CH
        b = i % 2
        x_t, o_t = xt3[b], ot[b]
        S.dma('sp', x_t[:], x_in[t0 + i * 128:t0 + (i + 1) * 128, :], r=[], w=[x_t])
        for mh in range(2):
            p = py[mh]
            for fc in range(NFC):
                S.pe(lambda e, p=p, fc=fc, mh=mh: e.matmul(
                    p[:], lhsT=actT[:, fc, i * 128:(i + 1) * 128], rhs=wdb[:, fc, mh * 512:(mh + 1) * 512],
                    start=(fc == 0), stop=(fc == NFC - 1)),
                    r=[(actT, (fc, i // 4)), (wdb, fc)], w=[p])
            S.dve(lambda e, p=p, mh=mh: e.scalar_tensor_tensor(
                out=o_t[:, mh * 512:(mh + 1) * 512], in0=p[:], scalar=0.5, in1=x_t[:, mh * 512:(mh + 1) * 512],
                op0=ALU.mult, op1=ALU.add), r=[p, x_t], w=[(o_t, mh)])
        if final_g is not None:
            rms_rstd(S, o_t, junk, ss3[b], rstd3[b], D)
            S.dve(lambda e: e.scalar_tensor_tensor(
                out=o_t[:], in0=o_t[:], scalar=rstd3[b][:, 0:1], in1=fgb[:], op0=ALU.mult, op1=ALU.mult),
                r=[o_t, rstd3[b], fgb], w=[o_t])
        S.dma('pool', x_out[t0 + i * 128:t0 + (i + 1) * 128, :], o_t[:], r=[o_t], w=[DW(S, x_out)])

    ss3 = [S.sb([128, 1], F32, f'ss3{i}') for i in range(2)]
    rstd3 = [S.sb([128, 1], F32, f'rstd3{i}') for i in range(2)]
    nch = ntok // CH
    for i in range(NT):
        phase1_tile(0, i)
    for ch in range(nch):
        phase2(ch)
        for i in range(NT):
            phase3_tile(ch, i)
            if ch + 1 < nch:
                phase1_tile(ch + 1, i)
    S.flush()


_dram_T = {}
_dkey = [0]


def x_out_T(S, ap):
    k = ap.name
    if k not in _dram_T:
        _dram_T[k] = T(None, k)
    return _dram_T[k]


def DW(S, ap):
    _dkey[0] += 1
    return (x_out_T(S, ap), _dkey[0])


THETA = 500000.0
NEG = -30000.0


def _cpack_layout():
    items = [('ident', 128), ('tri_ge', 128), ('band_lt', 128), ('tril_st', 128), ('ones', 128),
             ('invf64', 1), ('nsgn64', 1), ('P64', 128), ('invf32', 1), ('nsgn32', 1), ('P32', 128),
             ('cmpbias', 2048), ('overlap', 32), ('E32', 2048), ('E8', 2048),
             ('keep', 512), ('addm', 512), ('adm', 512), ('tri_qs', 128), ('tri01', 128), ('band01', 128), ('mbias', 128), ('mpast', 128), ('mown', 128)]
    off, o = {}, 0
    for k, n in items:
        off[k] = (o, n)
        o += n
    return off, o


CP_OFF, CP_N = _cpack_layout()


def host_consts():
    cp = np.zeros((128, CP_N), np.float32)

    def put(k, a):
        o, n = CP_OFF[k]
        a = np.asarray(a, np.float32)
        cp[:a.shape[0], o:o + a.shape[1]] = a
    p = np.arange(128)
    put('ident', np.eye(128))
    kk, qq = p[:, None], p[None, :]
    put('tri_ge', np.where(qq >= kk, 0.0, NEG))
    put('band_lt', np.where(qq < kk, 0.0, NEG))
    put('tri01', (qq >= kk).astype(np.float32))
    put('band01', (qq < kk).astype(np.float32))
    put('tril_st', (kk <= qq).astype(np.float32))
    put('tri_qs', np.where(qq <= kk, 0.0, -1e30))
    put('ones', np.ones((128, 128)))
    m64 = p % 64
    put('invf64', np.where(m64 < 16, THETA ** (-(2.0 * (m64 % 8)) / 16.0), 0.0)[:, None])
    put('nsgn64', np.where(m64 < 8, -1.0, np.where(m64 < 16, 1.0, 0.0))[:, None])
    P = np.zeros((128, 128))
    for m in range(128):
        if m % 64 < 8:
            P[m + 8, m] = 1
        elif m % 64 < 16:
            P[m - 8, m] = 1
    put('P64', P)
    m32 = p % 32
    put('invf32', np.where(m32 < 8, THETA ** (-(2.0 * (m32 % 4)) / 8.0), 0.0)[:, None])
    put('nsgn32', np.where(m32 < 4, -1.0, np.where(m32 < 8, 1.0, 0.0))[:, None])
    P = np.zeros((128, 128))
    for m in range(128):
        if m % 32 < 4:
            P[m + 4, m] = 1
        elif m % 32 < 8:
            P[m - 4, m] = 1
    put('P32', P)
    n = np.arange(127)
    t = np.arange(2048)
    put('cmpbias', np.where(16 * n[:, None] + 31 <= t[None, :], 0.0, NEG))
    c0 = n * 16
    s0 = np.arange(32) * 64
    put('overlap', ((c0[:, None] < s0[None, :] + 64) & (c0[:, None] + 32 > s0[None, :])).astype(np.float32))
    put('E32', (t[None, :] // 64 == np.arange(32)[:, None]).astype(np.float32))
    put('E8', (t[None, :] // 256 == np.arange(8)[:, None]).astype(np.float32))
    tt = (np.arange(16)[None, :, None] * 128 + p[:, None, None])
    j = np.arange(32)[None, None, :]
    adm = j * 64 <= tt
    forced = (j == 0) | (j == tt // 64)
    put('keep', (adm & ~forced).astype(np.float32).reshape(128, 512))
    put('addm', np.where(adm, np.where(forced, 1e4, 0.0), -1e30).reshape(128, 512))
    put('adm', adm.astype(np.float32).reshape(128, 512))
    own = (np.arange(16)[None, :, None] * 128 + p[:, None, None]) // 256
    j8 = np.arange(8)[None, None, :]
    put('mbias', np.where(j8 < own, 0.0, -1e30).reshape(128, 128))
    put('mpast', (j8 < own).astype(np.float32).reshape(128, 128))
    put('mown', (j8 == own).astype(np.float32).reshape(128, 128))
    return cp


class Consts:
    def __init__(self, S, cpack_ap):
        self.S, self.ap, self.cache = S, cpack_ap, {}

    def get(self, k, dtype=F32, rows=128):
        key = (k, dtype)
        if key in self.cache:
            return self.cache[key]
        S = self.S
        o, n = CP_OFF[k]
        if dtype == F32:
            f = S.sb([128, n], F32, 'c_' + k)
            S.dma('sp', f[:], self.ap[:, o:o + n], r=[], w=[f], allow_slow_non_contiguous=(n == 1))
            self.cache[key] = f
            return f
        if not hasattr(self, 'stg'):
            self.stg = S.sb([128, 2048], F32, 'c_stg')
        f = self.stg
        S.dma('sp', f[:, 0:n], self.ap[:, o:o + n], r=[], w=[f])
        b = S.sb([128, n], dtype, 'cb_' + k)
        S.dve(lambda e: e.tensor_copy(out=b[:], in_=f[:, 0:n]), r=[f], w=[b])
        self.cache[key] = b
        return b


def rope_tables(S, C, pos_ap, ntok, invk, sgnk, tmp):
    invf = C.get(invk)
    nsg = C.get(sgnk)
    if 'pi' not in tmp:
        tmp['pi'] = S.sb([128, 1024], I32, 'pos_i')
        tmp['ang'] = S.sb([128, 1024], F32, 'ang')
        tmp['kf'] = S.sb([128, 1024], F32, 'kf')
        tmp['ki'] = S.sb([128, 1024], I32, 'ki')
    pi_, ang, kf, ki = tmp['pi'], tmp['ang'], tmp['kf'], tmp['ki']
    ct = S.sb([128, ntok], F32, 'ropeC')
    st = S.sb([128, ntok], F32, 'ropeS')
    TWO_PI = 2.0 * np.pi
    for c0 in range(0, ntok, 1024):
        S.dma('sp', pi_[:], pos_ap[c0:c0 + 1024].partition_broadcast(128), r=[], w=[pi_])
        S.dve(lambda e: e.tensor_copy(out=ang[:], in_=pi_[:]), r=[pi_], w=[ang])
        S.dve(lambda e: e.tensor_scalar(out=ang[:], in0=ang[:], scalar1=invf[:, 0:1], scalar2=None, op0=ALU.mult),
              r=[ang, invf], w=[ang])

        def reduce_sin(dst, shift, post, c0=c0):
            S.dve(lambda e: e.tensor_scalar(out=kf[:], in0=ang[:], scalar1=shift, scalar2=1.0 / TWO_PI,
                                            op0=ALU.add, op1=ALU.mult), r=[ang], w=[kf])
            S.dve(lambda e: e.tensor_copy(out=ki[:], in_=kf[:]), r=[kf], w=[ki])
            S.dve(lambda e: e.tensor_copy(out=kf[:], in_=ki[:]), r=[ki], w=[kf])
            S.dve(lambda e: e.scalar_tensor_tensor(out=kf[:], in0=kf[:], scalar=-TWO_PI, in1=ang[:],
                                                   op0=ALU.mult, op1=ALU.add), r=[kf, ang], w=[kf])
            S.dve(lambda e: e.tensor_scalar(out=kf[:], in0=kf[:], scalar1=shift, scalar2=3.14159, op0=ALU.add,
                                            op1=ALU.min), r=[kf], w=[kf])
            S.dve(lambda e: e.tensor_scalar(out=kf[:], in0=kf[:], scalar1=-3.14159, scalar2=None, op0=ALU.max),
                  r=[kf], w=[kf])
            S.act(lambda e: e.activation(out=dst[:, c0:c0 + 1024], in_=kf[:], func=AF.Sin), r=[kf], w=[(dst, c0)])
            if post is not None:
                S.dve(lambda e: e.tensor_scalar(out=dst[:, c0:c0 + 1024], in0=dst[:, c0:c0 + 1024],
                                                scalar1=post[:, 0:1], scalar2=None, op0=ALU.mult),
                      r=[(dst, c0), post], w=[(dst, c0)])
        reduce_sin(ct, np.pi / 2.0, None)
        reduce_sin(st, 0.0, nsg)
    return ct, st


def proj_stage(S, x, w_in, nin, g_ap, pos, ntok, cpack, fm_specs, tm_groups, tm_post, use_idx=False):
    C = Consts(S, cpack)
    ident = C.get('ident', BF16)
    gb = S.sb([128, D], F32, 'gb')
    S.dma('sp', gb[:], g_ap.partition_broadcast(128), r=[], w=[gb])
    winb = S.sb([128, 8, nin], BF16, 'winb')
    wv = w_in.rearrange("(c p) f -> p c f", p=128)
    wst = [S.sb([128, 8, 256], F32, f'wst{i}') for i in range(2)]
    for bi, c0 in enumerate(range(0, nin, 256)):
        n = min(256, nin - c0)
        st = wst[bi % 2]
        S.dma('sp', st[:, :, 0:n], wv[:, :, c0:c0 + n], r=[], w=[st])
        S.dve(lambda e, st=st, c0=c0, n=n: e.tensor_copy(out=winb[:, :, c0:c0 + n], in_=st[:, :, 0:n]),
              r=[st], w=[(winb, bi)])
    ropes = {}
    rtmp = {}
    if any(sp[3] == 'r64' for sp in fm_specs):
        ct, sn = rope_tables(S, C, pos, ntok, 'invf64', 'nsgn64', rtmp)
        ropes['r64'] = (ct, sn, C.get('P64', BF16))
    if use_idx:
        ct, sn = rope_tables(S, C, pos, ntok, 'invf32', 'nsgn32', rtmp)
        ropes['r32'] = (ct, sn, C.get('P32', BF16))
    xt = [S.sb([128, D], F32, f'xt{i}') for i in range(2)]
    hb = [S.sb([128, D], BF16, f'hb{i}') for i in range(2)]
    junk = S.sb([128, D], F32, 'junk')
    ss = [S.sb([128, 1], F32, f'ss{i}') for i in range(2)]
    rstd = [S.sb([128, 1], F32, f'rstd{i}') for i in range(2)]
    hT = [S.sb([128, 8, 512], BF16, f'hT{i}') for i in range(2)]
    xh = [S.sb([128, 512], BF16, f'xh{i}') for i in range(2)]
    t1 = [S.sb([128, 512], F32, f't1{i}') for i in range(2)]
    t2 = [S.sb([128, 512], F32, f't2{i}') for i in range(2)]
    xr = [S.sb([128, 512], BF16, f'xr{i}') for i in range(2)]
    ptr = [S.ps([128, 8, 128], BF16, f'ptr{i}') for i in range(2)]
    pf = [S.ps([128, 512], F32, f'pf{i}') for i in range(2)]
    p2 = [S.ps([128, 512], F32, f'p2{i}') for i in range(2)]
    pt = [S.ps([128, 512], F32, f'pt{i}') for i in range(2)]
    nfm = 0
    ntm = 0
    for blk in range(ntok // 512):
        t0 = blk * 512
        hTb = hT[blk % 2]
        for i in range(4):
            b = i % 2
            x_t, h_b = xt[b], hb[b]
            S.dma('sp', x_t[:], x[t0 + i * 128:t0 + (i + 1) * 128, :], r=[], w=[x_t])
            rms_rstd(S, x_t, junk, ss[b], rstd[b], D)
            S.dve(lambda e, x_t=x_t, h_b=h_b, b=b: e.scalar_tensor_tensor(
                out=h_b[:], in0=x_t[:], scalar=rstd[b][:, 0:1], in1=gb[:], op0=ALU.mult, op1=ALU.mult),
                r=[x_t, rstd[b], gb], w=[h_b])
            p = ptr[b]
            for c in range(8):
                S.pe(lambda e, p=p, h_b=h_b, c=c: e.transpose(out=p[:, c, :], in_=h_b[:, c * 128:(c + 1) * 128],
                                                               identity=ident[:]), r=[h_b, ident], w=[(p, c)])
            S.act(lambda e, p=p, i=i, hTb=hTb: e.copy(out=hTb[:, :, i * 128:(i + 1) * 128], in_=p[:]),
                  r=[p], w=[(hTb, i)])
        for (col0, M, scale, rope, raw_dst, rot_dst) in fm_specs:
            q = nfm % 2
            nfm += 1
            pp = pf[q]
            for c in range(8):
                S.pe(lambda e, pp=pp, c=c, col0=col0, M=M, hTb=hTb: e.matmul(
                    pp[0:M, :], lhsT=winb[:, c, col0:col0 + M], rhs=hTb[:, c, :], start=(c == 0), stop=(c == 7)),
                    r=[winb, hTb], w=[pp])
            xq = xh[q]
            S.act(lambda e, xq=xq, pp=pp, M=M, scale=scale: e.mul(out=xq[0:M, :], in_=pp[0:M, :], mul=scale),
                  r=[pp], w=[xq])
            if raw_dst is not None:
                S.dma('pool', raw_dst[:, t0:t0 + 512], xq[0:M, :], r=[xq], w=[DW(S, raw_dst)])
            if rope is not None:
                ct, sn, Pm = ropes[rope]
                pq = p2[q]
                S.pe(lambda e, pq=pq, xq=xq, M=M, Pm=Pm: e.matmul(pq[0:M, :], lhsT=Pm[0:M, 0:M], rhs=xq[0:M, :],
                                                                  start=True, stop=True), r=[xq, Pm], w=[pq])
                S.dve(lambda e, q=q, xq=xq, M=M, ct=ct, t0=t0: e.tensor_tensor(
                    out=t1[q][0:M, :], in0=xq[0:M, :], in1=ct[0:M, t0:t0 + 512], op=ALU.mult),
                    r=[xq, ct], w=[t1[q]])
                S.dve(lambda e, q=q, pq=pq, M=M, sn=sn, t0=t0: e.tensor_tensor(
                    out=t2[q][0:M, :], in0=pq[0:M, :], in1=sn[0:M, t0:t0 + 512], op=ALU.mult),
                    r=[pq, sn], w=[t2[q]])
                S.pool(lambda e, q=q, M=M: e.tensor_tensor(out=xr[q][0:M, :], in0=t1[q][0:M, :], in1=t2[q][0:M, :],
                                                          op=ALU.add), r=[t1[q], t2[q]], w=[xr[q]])
                S.dma('pool', rot_dst[:, t0:t0 + 512], xr[q][0:M, :], r=[xr[q]], w=[DW(S, rot_dst)])
        for i in range(4):
            for gi, (col0, N) in enumerate(tm_groups):
                q = ntm % 2
                ntm += 1
                pp = pt[q]
                for c in range(8):
                    S.pe(lambda e, pp=pp, c=c, col0=col0, N=N, i=i, hTb=hTb: e.matmul(
                        pp[:, 0:N], lhsT=hTb[:, c, i * 128:(i + 1) * 128], rhs=winb[:, c, col0:col0 + N],
                        start=(c == 0), stop=(c == 7)), r=[winb, (hTb, i)], w=[pp])
                tm_post(gi, pp, t0 + i * 128, ntm)
    S.flush()


def even_proj(S, x, prm, dr, ntok, cpack):
    at = [S.sb([128, 512], BF16, f'at{i}') for i in range(2)]
    g1 = [S.sb([128, 512], F32, f'g1{i}') for i in range(2)]
    vb = [S.sb([128, 192], BF16, f'vb{i}') for i in range(2)]
    vb2 = [S.sb([128, 192], BF16, f'vb2{i}') for i in range(2)]
    gt = [S.sb([128, 36], F32, f'gt{i}') for i in range(2)]
    sgb = S.sb([128, 256], F32, 'sgb')
    S.dma('sp', sgb[:], prm['sgu_norm'].partition_broadcast(128), r=[], w=[sgb])
    junk = S.sb([128, 256], F32, 'junk2')
    ss = [S.sb([128, 1], F32, f'ssv{i}') for i in range(2)]
    rs = [S.sb([128, 1], F32, f'rsv{i}') for i in range(2)]
    cnt = [0]

    def tm_post(gi, pp, tok0, n):
        if gi == 0:
            b = cnt[0] % 2
            cnt[0] += 1
            g, a = g1[b], at[b]
            S.act(lambda e: e.activation(out=g[:], in_=pp[:], func=AF.Gelu_apprx_tanh), r=[pp], w=[g])
            S.pool(lambda e: e.tensor_copy(out=a[:, 0:256], in_=g[:, 0:256]), r=[g], w=[(a, 0)])
            S.act(lambda e: e.activation(out=junk[:], in_=g[:, 256:512], func=AF.Square, accum_out=ss[b][:]),
                  r=[g], w=[junk, ss[b]])
            S.act(lambda e: e.activation(out=rs[b][:], in_=ss[b][:], func=AF.Sqrt, bias=EPS, scale=1.0 / 256),
                  r=[ss[b]], w=[rs[b]])
            S.dve(lambda e: e.reciprocal(out=rs[b][:], in_=rs[b][:]), r=[rs[b]], w=[rs[b]])
            S.dve(lambda e: e.scalar_tensor_tensor(out=a[:, 256:512], in0=g[:, 256:512], scalar=rs[b][:, 0:1],
                                                   in1=sgb[:], op0=ALU.mult, op1=ALU.mult),
                  r=[g, rs[b], sgb], w=[(a, 1)])
            S.dma('pool', dr['a_tok'][tok0:tok0 + 128, :], a[:], r=[a], w=[DW(S, dr['a_tok'])])
        elif gi == 1:
            v = vb[(tok0 // 128) % 2]
            S.act(lambda e: e.copy(out=v[:], in_=pp[:, 0:192]), r=[pp], w=[v])
            S.dma('pool', dr['vsl'][tok0:tok0 + 128, :], v[:], r=[v], w=[DW(S, dr['vsl'])])
        else:
            v = vb2[(tok0 // 128) % 2]
            g = gt[(tok0 // 128) % 2]
            S.act(lambda e: e.copy(out=v[:], in_=pp[:, 0:192]), r=[pp], w=[v])
            S.act(lambda e: e.activation(out=g[:], in_=pp[:, 192:228], func=AF.Sigmoid), r=[pp], w=[g])
            S.dma('pool', dr['vw'][tok0:tok0 + 128, :], v[:], r=[v], w=[DW(S, dr['vw'])])
            S.dma('pool', dr['gate'][tok0:tok0 + 128, :], g[:], r=[g], w=[DW(S, dr['gate'])])
    fm = []
    for i in range(6):
        fm.append((512 + 128 * i, 128, 0.125, 'r64', dr['qraw'][128 * i:128 * (i + 1), :],
                   dr['qrot'][128 * i:128 * (i + 1), :]))
    for nm, c0, rope in (('kc', 1280, None), ('vc', 1472, None), ('ksl', 1664, 'r64'), ('kw', 2048, 'r64')):
        for (o, M) in ((0, 128), (128, 64)):
            dst = dr[nm][o:o + M, :]
            fm.append((c0 + o, M, 1.0, rope, dst if rope is None else None, dst if rope else None))
    proj_stage(S, x, prm['w_in'], 2468, prm['mix_norm'], prm['pos'], ntok, cpack, fm,
               [(0, 512), (1856, 192), (2240, 228)], tm_post)


def attn_chunk(S, c, tiles, qT, kT, extra, vext, STps, PT, Oacc, finalize):
    cover = {}
    for n, (kt, lo, hi, bt, blo) in enumerate(tiles):
        for j in range(lo // 128, hi // 128):
            cover.setdefault(j, []).append(n)
    NP = len(PT)

    def qk(n):
        kt, lo, hi, bt, blo = tiles[n]
        ps = STps[n % 2]
        S.pe(lambda e: e.matmul(ps[:, lo:hi], lhsT=kT[:, kt * 128:(kt + 1) * 128],
                                rhs=qT[:, c * 512 + lo:c * 512 + hi], start=True, stop=(extra is None)),
             r=[kT, qT], w=[ps])
        if extra:
            S.pe(lambda e: e.matmul(ps[:, lo:hi], lhsT=extra[0](kt), rhs=extra[1](kt, c, lo, hi),
                                    start=False, stop=True, skip_group_check=True), r=list(extra[2]), w=[ps])

    qk(0)
    for n, (kt, lo, hi, bt, blo) in enumerate(tiles):
        if n + 1 < len(tiles):
            qk(n + 1)
        ps, p = STps[n % 2], PT[n % NP]
        S.act(lambda e, ps=ps, p=p, lo=lo, hi=hi: e.activation(out=p[:, lo:hi], in_=ps[:, lo:hi], func=AF.Exp),
              r=[ps], w=[p])
        if bt is not None:
            S.dve(lambda e, p=p, bt=bt, blo=blo: e.tensor_tensor(out=p[:, blo:blo + 128], in0=p[:, blo:blo + 128],
                                                                 in1=bt[:], op=ALU.mult), r=[p, bt], w=[p])
        for j in range(lo // 128, hi // 128):
            S.pe(lambda e, p=p, j=j, kt=kt, n=n: e.matmul(
                Oacc[j][:, 0:65], lhsT=p[:, j * 128:(j + 1) * 128], rhs=vext[:, kt, :],
                start=(cover[j][0] == n), stop=(cover[j][-1] == n), skip_group_check=True),
                r=[p, vext], w=[Oacc[j]])
            if cover[j][-1] == n:
                finalize(c, j, Oacc[j])


def attn_run(S, jobs, STps, PT, Oacc, LA=2):
    flat = []
    for ji, jb in enumerate(jobs):
        cover = {}
        for n, (kt, lo, hi, bt, blo) in enumerate(jb['tiles']):
            for j in range(lo // 128, hi // 128):
                cover.setdefault(j, []).append(n)
        jb['cover'] = cover
        for n in range(len(jb['tiles'])):
            flat.append((ji, n))
    NS, NP = len(STps), len(PT)

    def qk(f):
        ji, n = flat[f]
        jb = jobs[ji]
        if n == 0 and jb.get('pre') is not None:
            jb['pre']()
        kt, lo, hi, bt, blo = jb['tiles'][n]
        c, qT, kT, extra = jb['c'], jb['qT'], jb['kT'], jb.get('extra')
        ps = STps[f % NS]
        S.pe(lambda e: e.matmul(ps[:, lo:hi], lhsT=kT[:, kt * 128:(kt + 1) * 128],
                                rhs=qT[:, c * 512 + lo:c * 512 + hi], start=True, stop=(extra is None)),
             r=[kT, qT], w=[ps])
        if extra:
            S.pe(lambda e: e.matmul(ps[:, lo:hi], lhsT=extra[0](kt), rhs=extra[1](kt, c, lo, hi),
                                    start=False, stop=True, skip_group_check=True), r=list(extra[2]), w=[ps])

    for f in range(min(LA, len(flat))):
        qk(f)
    for f, (ji, n) in enumerate(flat):
        if f + LA < len(flat):
            qk(f + LA)
        jb = jobs[ji]
        kt, lo, hi, bt, blo = jb['tiles'][n]
        cover, vext = jb['cover'], jb['vext']
        ps, p = STps[f % NS], PT[f % NP]
        S.act(lambda e, ps=ps, p=p, lo=lo, hi=hi: e.activation(out=p[:, lo:hi], in_=ps[:, lo:hi], func=AF.Exp),
              r=[ps], w=[p])
        if bt is not None:
            S.pool(lambda e, p=p, bt=bt, blo=blo: e.tensor_tensor(out=p[:, blo:blo + 128], in0=p[:, blo:blo + 128],
                                                                  in1=bt[:], op=ALU.mult), r=[p, bt], w=[p])
        for j in range(lo // 128, hi // 128):
            O = Oacc[j]
            S.pe(lambda e, p=p, j=j, kt=kt, O=O, vext=vext, first=(cover[j][0] == n), last=(cover[j][-1] == n):
                 e.matmul(O[:, 0:65], lhsT=p[:, j * 128:(j + 1) * 128], rhs=vext[:, kt, :], start=first, stop=last),
                 r=[p, vext], w=[O])
            if cover[j][-1] == n:
                jb['fin'](jb['c'], j, O, 0)


def causal_tiles(c, tri):
    out = []
    for kt in range(4 * c + 4):
        if kt < 4 * c:
            out.append((kt, 0, 512, None, 0))
        else:
            lo = (kt - 4 * c) * 128
            out.append((kt, lo, 512, tri, lo))
    return out


def window_tiles(c, tri, band):
    out = []
    if c >= 1:
        out.append((4 * c - 1, 0, 512, band, 384))
        for i in range(3):
            out.append((4 * c - 4 + i, 0, 128 * (i + 1), band, 128 * i))
    for i in range(4):
        out.append((4 * c + i, 128 * i, 512, tri, 128 * i))
    return out


def nsa_stage(S, prm, dr, nseq, cpack):
    C = Consts(S, cpack)
    ident = C.get('ident', BF16)
    tri = C.get('tri01', BF16)
    band = C.get('band01', BF16)
    ones = C.get('ones', BF16)
    cmpb = C.get('cmpbias', BF16)
    ovl = C.get('overlap', F32)
    E32 = C.get('E32', F32)
    keep, addm, adm = C.get('keep'), C.get('addm'), C.get('adm')
    tril = C.get('tril_st', F32)
    wTf = S.sb([128, 4, 128], F32, 'wTf')
    S.dma('sp', wTf[:], prm['sgu_wT'].rearrange("g s t -> s g t"), r=[], w=[wTf])
    wTb = S.sb([128, 4, 128], BF16, 'wTb')
    for g in range(4):
        S.dve(lambda e, g=g: e.tensor_tensor(out=wTb[:, g, :], in0=wTf[:, g, :], in1=tril[:], op=ALU.mult),
              r=[wTf, tril], w=[(wTb, g)])
    bT = S.sb([128, 4], F32, 'bT')
    S.dma('sp', bT[:], prm['sgu_bT'], r=[], w=[bT])
    cw = {}
    stg = [S.sb([64, 4, 256], F32, f'w1s{i}') for i in range(2)]
    w2s = S.sb([128, 2, 64], F32, 'w2s')
    pst = S.sb([64, 32], F32, 'pst')
    pm0 = S.ps([128, 512], F32, 'pm0')
    pmisc = [pm0, pm0]
    ptb = S.ps([128, 1024], BF16, 'ptb')
    k = 0
    for kv in ('k', 'v'):
        w1b = S.sb([64, 32, 256], BF16, 'w1b' + kv)
        w1v = prm['cmp_w1_' + kv].rearrange("(l e) c -> e l c", e=64)
        for q4 in range(8):
            st = stg[k % 2]
            k += 1
            S.dma('sp', st[:], w1v[:, q4 * 4:(q4 + 1) * 4, :], r=[], w=[st])
            S.dve(lambda e, st=st, w1b=w1b, q4=q4: e.tensor_copy(out=w1b[:, q4 * 4:(q4 + 1) * 4, :], in_=st[:]),
                  r=[st], w=[(w1b, q4)])
        w2b = S.sb([128, 2, 64], BF16, 'w2b' + kv)
        S.dma('sp', w2s[:], prm['cmp_w2_' + kv].rearrange("(c p) e -> p c e", p=128), r=[], w=[w2s])
        S.dve(lambda e, w2b=w2b: e.tensor_copy(out=w2b[:], in_=w2s[:]), r=[w2s], w=[w2b])
        posb = S.sb([64, 32], BF16, 'posb' + kv)
        S.dma('sp', pst[:], prm['cmp_posT_' + kv], r=[], w=[pst])
        S.dve(lambda e, posb=posb: e.tensor_copy(out=posb[:], in_=pst[:]), r=[pst], w=[posb])
        pbias = S.sb([128, 2], F32, 'pbias' + kv)
        for cc in range(2):
            pp = pmisc[cc]
            for l in range(32):
                S.pe(lambda e, pp=pp, w1b=w1b, posb=posb, l=l, cc=cc: e.matmul(
                    pp[:, 0:1], lhsT=w1b[:, l, cc * 128:(cc + 1) * 128], rhs=posb[:, l:l + 1],
                    start=(l == 0), stop=(l == 31)), r=[w1b, posb], w=[pp])
            S.dve(lambda e, pp=pp, pbias=pbias, cc=cc: e.tensor_copy(out=pbias[:, cc:cc + 1], in_=pp[:, 0:1]),
                  r=[pp], w=[(pbias, cc)])
        cw[kv] = (w1b, w2b, pbias)
    atok = S.sb([128, 16, 512], BF16, 'atok')
    gates = S.sb([128, 16, 36], F32, 'gates')
    otok = S.sb([128, 16, 1024], BF16, 'otok')
    accg = S.sb([128, 16, 256], F32, 'accg')
    kcT = S.sb([64, SEQ], BF16, 'kcT')
    vcT = S.sb([64, SEQ], BF16, 'vcT')
    kslT = S.sb([128, SEQ], BF16, 'kslT')
    kwT = S.sb([128, SEQ], BF16, 'kwT')
    S.dve(lambda e: e.memset(kslT[:], 0.0), r=[], w=[kslT])
    S.pool(lambda e: e.memset(kwT[:], 0.0), r=[], w=[kwT])
    S.dve(lambda e: e.tensor_copy(out=kslT[64:96, :], in_=E32[0:32, :]), r=[E32], w=[(kslT, 'e')])
    vsl = S.sb([128, 16, 65], BF16, 'vslx')
    vw = S.sb([128, 16, 65], BF16, 'vwx')
    S.pool(lambda e: e.memset(vsl[:], 1.0), r=[], w=[vsl])
    S.pool(lambda e: e.memset(vw[:], 1.0), r=[], w=[vw])
    qraw = S.sb([128, SEQ], BF16, 'qrawT')
    qrots = [S.sb([128, SEQ], BF16, f'qrotT{i}') for i in range(2)]
    S.pool(lambda e: e.memset(qraw[:], 0.0), r=[], w=[qraw])
    S.dve(lambda e: e.memset(qrots[0][:], 0.0), r=[], w=[qrots[0]])
    S.pool(lambda e: e.memset(qrots[1][:], 0.0), r=[], w=[qrots[1]])
    ghT = S.sb([128, 2, 128], BF16, 'ghT')
    kcmpT = S.sb([128, 128], BF16, 'kcmpT')
    S.dve(lambda e: e.memset(kcmpT[:], 0.0), r=[], w=[kcmpT])
    vcmp = S.sb([128, 64], BF16, 'vcmp')
    psumT = S.sb([128, SEQ], F32, 'psumT')
    pcT = [S.sb([128, 512], BF16, f'pcT{i}') for i in range(2)]
    rinv = [S.sb([128, 512], F32, f'rinv{i}') for i in range(2)]
    pnf = [S.sb([128, 512], F32, f'pnf{i}') for i in range(2)]
    pnb = [S.sb([128, 512], BF16, f'pnb{i}') for i in range(2)]
    PT = [S.sb([128, 512], BF16, f'PT{i}') for i in range(4)]
    gm = [S.sb([128, 256], F32, f'gm{i}') for i in range(2)]
    sel = [S.sb([128, 32], F32, f'sel{i}') for i in range(2)]
    m8 = [S.sb([128, 8], F32, f'm8{i}') for i in range(2)]
    nsel = [S.sb([128, 32], BF16, f'nsel{i}') for i in range(2)]
    fsc = [S.sb([128, 1], F32, f'fsc{i}') for i in range(4)]
    ftmp = [S.sb([128, 64], F32, f'ftmp{i}') for i in range(4)]
    STps = [S.ps([128, 512], F32, f'st{i}') for i in range(2)] + [pm0]
    Oacc = [S.ps([128, 512], F32, f'oa{i}') for i in range(4)]
    fcount = [0]

    def make_finalize(r, gidx, first):
        def fin(c, j, O, off):
            i = 4 * c + j
            f = fsc[fcount[0] % 4]
            fcount[0] += 1
            tm = ftmp[fcount[0] % 4]
            S.dve(lambda e: e.reciprocal(out=f[:], in_=O[:, off + 64:off + 65]), r=[O], w=[f])
            S.dve(lambda e: e.tensor_scalar(out=tm[:], in0=O[:, off:off + 64], scalar1=f[:, 0:1],
                                            scalar2=gates[:, i, gidx:gidx + 1], op0=ALU.mult, op1=ALU.mult),
                  r=[O, f, gates], w=[tm])
            S.pool(lambda e: e.tensor_tensor(out=accg[:, i, r * 64:(r + 1) * 64], in0=accg[:, i, r * 64:(r + 1) * 64],
                                             in1=tm[:], op=ALU.add), r=[tm, (accg, (i, r))], w=[(accg, (i, r))])
        return fin

    for sq in range(nseq):
        tb = sq * SEQ
        S.dma('sp', atok[:], dr['a_tok'][tb:tb + SEQ, :].rearrange("(n p) c -> p n c", p=128), r=[], w=[atok])
        S.dma('sp', gates[:], dr['gate'][tb:tb + SEQ, :].rearrange("(n p) c -> p n c", p=128), r=[], w=[gates])
        for i in range(16):
            pp = pmisc[i % 2]
            g_ = gm[i % 2]
            for g in range(4):
                S.pe(lambda e, pp=pp, g=g, i=i: e.matmul(pp[:, g * 64:(g + 1) * 64], lhsT=wTb[:, g, :],
                                                          rhs=atok[:, i, 256 + g * 64:256 + (g + 1) * 64],
                                                          start=True, stop=True), r=[wTb, atok], w=[pp])
                S.dve(lambda e, pp=pp, g_=g_, g=g: e.tensor_scalar(
                    out=g_[:, g * 64:(g + 1) * 64], in0=pp[:, g * 64:(g + 1) * 64], scalar1=bT[:, g:g + 1],
                    scalar2=None, op0=ALU.add), r=[pp, bT], w=[(g_, g)])
            S.dve(lambda e, g_=g_, i=i: e.tensor_tensor(out=otok[:, i, 0:256], in0=g_[:], in1=atok[:, i, 0:256],
                                                         op=ALU.mult), r=[g_, atok], w=[(otok, (i, 0))])
        for g in range(3):
            for nm, tl in (('kc', kcT), ('vc', vcT)):
                S.dma('sp', tl[:], dr[nm][g * 64:(g + 1) * 64, tb:tb + SEQ], r=[], w=[tl])
            for nm, tl in (('ksl', kslT), ('kw', kwT)):
                S.dma('sp', tl[0:64, :], dr[nm][g * 64:(g + 1) * 64, tb:tb + SEQ], r=[], w=[(tl, 'k')])
            for nm, tl in (('vsl', vsl), ('vw', vw)):
                S.dma('sp', tl[:, :, 0:64], dr[nm][tb:tb + SEQ, g * 64:(g + 1) * 64].rearrange(
                    "(n p) c -> p n c", p=128), r=[], w=[tl])
            for kv, src in (('k', kcT), ('v', vcT)):
                w1b, w2b, pbias = cw[kv]
                for cc in range(2):
                    pp = pmisc[cc]
                    for l in range(32):
                        S.pe(lambda e, pp=pp, w1b=w1b, src=src, l=l, cc=cc: e.matmul(
                            pp[:, 0:127], lhsT=w1b[:, l, cc * 128:(cc + 1) * 128], rhs=src[:, l:l + 2017:16],
                            start=(l == 0), stop=(l == 31)), r=[w1b, src], w=[pp])
                    S.act(lambda e, pp=pp, cc=cc, pbias=pbias: e.activation(
                        out=ghT[:, cc, 0:127], in_=pp[:, 0:127], func=AF.Gelu_apprx_tanh, bias=pbias[:, cc:cc + 1]),
                        r=[pp, pbias], w=[(ghT, cc)])
                pp = pmisc[0]
                if kv == 'k':
                    for cc in range(2):
                        S.pe(lambda e, pp=pp, cc=cc, w2b=w2b: e.matmul(pp[0:64, 0:127], lhsT=w2b[:, cc, :],
                                                                     rhs=ghT[:, cc, 0:127], start=(cc == 0),
                                                                     stop=(cc == 1)), r=[w2b, ghT], w=[pp])
                    S.act(lambda e, pp=pp: e.copy(out=kcmpT[0:64, 0:127], in_=pp[0:64, 0:127]), r=[pp], w=[(kcmpT, 'k')])
                else:
                    for cc in range(2):
                        S.pe(lambda e, pp=pp, cc=cc, w2b=w2b: e.matmul(pp[0:127, 0:64], lhsT=ghT[:, cc, 0:127],
                                                                     rhs=w2b[:, cc, :], start=(cc == 0),
                                                                     stop=(cc == 1)), r=[w2b, ghT], w=[pp])
                    S.act(lambda e, pp=pp: e.copy(out=vcmp[0:127, :], in_=pp[0:127, 0:64]), r=[pp], w=[vcmp])
            for r in range(4):
                h = 4 * g + r
                S.dma('sp', qraw[0:64, :], dr['qraw'][h * 64:(h + 1) * 64, tb:tb + SEQ], r=[], w=[(qraw, 'q')])
                for c in range(4):
                    b = c % 2
                    ps = STps[b]
                    S.pe(lambda e, ps=ps, c=c: e.matmul(ps[0:127, :], lhsT=kcmpT[:, 0:127],
                                                        rhs=qraw[:, c * 512:(c + 1) * 512], start=True, stop=False),
                         r=[kcmpT, qraw], w=[ps])
                    S.pe(lambda e, ps=ps, c=c: e.matmul(ps[0:127, :], lhsT=ident[0:127, 0:127],
                                                        rhs=cmpb[0:127, c * 512:(c + 1) * 512], start=False, stop=True),
                         r=[ident, cmpb], w=[ps])
                    S.act(lambda e, ps=ps, b=b: e.activation(out=pcT[b][0:127, :], in_=ps[0:127, :], func=AF.Exp),
                          r=[ps], w=[pcT[b]])
                    pd = pmisc[b]
                    S.pe(lambda e, pd=pd, b=b: e.matmul(pd[0:127, :], lhsT=ones[0:127, 0:127], rhs=pcT[b][0:127, :],
                                                        start=True, stop=True), r=[ones, pcT[b]], w=[pd])
                    S.dve(lambda e, pd=pd, b=b: e.tensor_scalar(out=rinv[b][0:127, :], in0=pd[0:127, :], scalar1=1e-30,
                                                                scalar2=None, op0=ALU.max), r=[pd], w=[rinv[b]])
                    S.dve(lambda e, b=b: e.reciprocal(out=rinv[b][0:127, :], in_=rinv[b][0:127, :]),
                          r=[rinv[b]], w=[rinv[b]])
                    S.dve(lambda e, b=b: e.tensor_tensor(out=pnf[b][0:127, :], in0=pcT[b][0:127, :],
                                                         in1=rinv[b][0:127, :], op=ALU.mult),
                          r=[pcT[b], rinv[b]], w=[pnf[b]])
                    S.pool(lambda e, b=b: e.tensor_copy(out=pnb[b][0:127, :], in_=pnf[b][0:127, :]),
                           r=[pnf[b]], w=[pnb[b]])
                    if r == 0:
                        S.pool(lambda e, b=b, c=c: e.tensor_copy(out=psumT[0:127, c * 512:(c + 1) * 512],
                                                                 in_=pnf[b][0:127, :]), r=[pnf[b]], w=[(psumT, c)])
                    else:
                        S.pool(lambda e, b=b, c=c: e.tensor_tensor(
                            out=psumT[0:127, c * 512:(c + 1) * 512], in0=psumT[0:127, c * 512:(c + 1) * 512],
                            in1=pnf[b][0:127, :], op=ALU.add), r=[pnf[b], (psumT, c)], w=[(psumT, c)])
                    for j in range(4):
                        i = 4 * c + j
                        O = Oacc[j]
                        off = 0
                        S.pe(lambda e, O=O, b=b, j=j, off=off: e.matmul(
                            O[:, off:off + 64], lhsT=pnb[b][0:127, j * 128:(j + 1) * 128], rhs=vcmp[0:127, :],
                            start=True, stop=True), r=[pnb[b], vcmp], w=[O])
                        S.dve(lambda e, O=O, i=i, r=r, h=h, off=off: e.tensor_scalar(
                            out=accg[:, i, r * 64:(r + 1) * 64], in0=O[:, off:off + 64],
                            scalar1=gates[:, i, 3 * h:3 * h + 1], scalar2=None, op0=ALU.mult),
                            r=[O, gates], w=[(accg, (i, r))])
            for i in range(16):
                b = i % 2
                pp = pmisc[b]
                S.pe(lambda e, pp=pp, i=i: e.matmul(pp[:, 0:32], lhsT=psumT[0:127, i * 128:(i + 1) * 128],
                                                    rhs=ovl[0:127, 0:32], start=True, stop=True),
                     r=[psumT, ovl], w=[pp])
                sl = sel[b]
                S.dve(lambda e, pp=pp, sl=sl, i=i: e.tensor_tensor(out=sl[:], in0=pp[:, 0:32],
                                                                  in1=keep[:, i * 32:(i + 1) * 32], op=ALU.mult),
                      r=[pp, keep], w=[sl])
                S.dve(lambda e, sl=sl, i=i: e.tensor_tensor(out=sl[:], in0=sl[:], in1=addm[:, i * 32:(i + 1) * 32],
                                                           op=ALU.add), r=[sl, addm], w=[sl])
                S.dve(lambda e, sl=sl, b=b: e.max(out=m8[b][:], in_=sl[:]), r=[sl], w=[m8[b]])
                S.dve(lambda e, sl=sl, b=b: e.tensor_scalar(out=sl[:], in0=sl[:], scalar1=m8[b][:, 7:8], scalar2=None,
                                                            op0=ALU.is_ge), r=[sl, m8[b]], w=[sl])
                S.dve(lambda e, sl=sl, i=i: e.tensor_tensor(out=sl[:], in0=sl[:], in1=adm[:, i * 32:(i + 1) * 32],
                                                           op=ALU.mult), r=[sl, adm], w=[sl])
                S.dve(lambda e, sl=sl, b=b: e.tensor_scalar(out=nsel[b][:], in0=sl[:], scalar1=-NEG, scalar2=NEG,
                                                            op0=ALU.mult, op1=ALU.add), r=[sl], w=[nsel[b]])
                S.pe(lambda e, b=b: e.transpose(out=ptb[0:32, b * 128:(b + 1) * 128], in_=nsel[b][:],
                                                identity=ident[:]), r=[nsel[b], ident], w=[(ptb, b)])
                for qb in qrots:
                    S.dve(lambda e, i=i, b=b, qb=qb: e.tensor_copy(out=qb[64:96, i * 128:(i + 1) * 128],
                                                                   in_=ptb[0:32, b * 128:(b + 1) * 128]),
                          r=[(ptb, b)], w=[(qb, ('n', i))])
            def load_q(r, g=g, tb=tb):
                h = 4 * g + r
                qb = qrots[r % 2]
                S.dma('sp', qb[0:64, :], dr['qrot'][h * 64:(h + 1) * 64, tb:tb + SEQ], r=[], w=[(qb, 'q')])
            load_q(0)
            jobs = []
            for r in range(4):
                h = 4 * g + r
                qb = qrots[r % 2]
                for c in range(4):
                    jobs.append(dict(c=c, tiles=causal_tiles(c, tri), qT=qb, kT=kslT, vext=vsl,
                                     fin=make_finalize(r, 3 * h + 1, False),
                                     pre=((lambda r=r: load_q(r + 1)) if (c == 0 and r < 3) else None)))
                for c in range(4):
                    jobs.append(dict(c=c, tiles=window_tiles(c, tri, band), qT=qb, kT=kwT, vext=vw,
                                     fin=make_finalize(r, 3 * h + 2, False)))
            attn_run(S, jobs, STps, PT, Oacc)
            S.act(lambda e, g=g: e.copy(out=otok[:, :, 256 + 256 * g:256 + 256 * (g + 1)], in_=accg[:]),
                  r=[accg], w=[(otok, ('g', g))])
        S.dma('sp', dr['o_tok'][tb:tb + SEQ, :].rearrange("(n p) c -> p n c", p=128), otok[:],
              r=[otok], w=[DW(S, dr['o_tok'])])
    S.flush()


def outproj_stage(S, x_in, x_out, o_tok, w_out, ntok, cpack):
    C = Consts(S, cpack)
    ident = C.get('ident', BF16)
    wob = S.sb([128, 8, D], BF16, 'wob')
    wst = [S.sb([128, D], F32, f'wos{i}') for i in range(2)]
    wv = w_out.rearrange("(c p) m -> p c m", p=128)
    for c in range(8):
        st = wst[c % 2]
        S.dma('sp', st[:], wv[:, c, :], r=[], w=[st])
        S.dve(lambda e, st=st, c=c: e.tensor_copy(out=wob[:, c, :], in_=st[:]), r=[st], w=[(wob, c)])
    ot = [S.sb([128, D], BF16, f'oo{i}') for i in range(2)]
    oT = [S.sb([128, 8, 128], BF16, f'oT{i}') for i in range(2)]
    xt = [S.sb([128, D], F32, f'xo{i}') for i in range(2)]
    xo = [S.sb([128, D], F32, f'xn{i}') for i in range(2)]
    ptr = [S.ps([128, 8, 128], BF16, f'ptr{i}') for i in range(2)]
    py = [S.ps([128, 512], F32, f'py{i}') for i in range(4)]
    for i in range(ntok // 128):
        b = i % 2
        S.dma('sp', ot[b][:], o_tok[i * 128:(i + 1) * 128, :], r=[], w=[ot[b]])
        S.dma('sp', xt[b][:], x_in[i * 128:(i + 1) * 128, :], r=[], w=[xt[b]])
        p = ptr[b]
        for c in range(8):
            S.pe(lambda e, p=p, b=b, c=c: e.transpose(out=p[:, c, :], in_=ot[b][:, c * 128:(c + 1) * 128],
                                                      identity=ident[:]), r=[ot[b], ident], w=[(p, c)])
        S.act(lambda e, p=p, b=b: e.copy(out=oT[b][:], in_=p[:]), r=[p], w=[oT[b]])
        for mh in range(2):
            pp = py[b * 2 + mh]
            for c in range(8):
                S.pe(lambda e, pp=pp, b=b, c=c, mh=mh: e.matmul(pp[:], lhsT=oT[b][:, c, :],
                                                                rhs=wob[:, c, mh * 512:(mh + 1) * 512],
                                                                start=(c == 0), stop=(c == 7)),
                     r=[oT[b], wob], w=[pp])
            S.dve(lambda e, pp=pp, b=b, mh=mh: e.tensor_tensor(out=xo[b][:, mh * 512:(mh + 1) * 512], in0=pp[:],
                                                               in1=xt[b][:, mh * 512:(mh + 1) * 512], op=ALU.add),
                  r=[pp, xt[b]], w=[(xo[b], mh)])
        S.dma('pool', x_out[i * 128:(i + 1) * 128, :], xo[b][:], r=[xo[b]], w=[DW(S, x_out)])
    S.flush()


def odd_proj(S, x, prm, dr, ntok, cpack):
    vb = [S.sb([128, 64], BF16, f'vb{i}') for i in range(2)]
    wb = [S.sb([128, 4], F32, f'wb{i}') for i in range(2)]
    vd = [S.sb([128, 512], BF16, f'vd{i}') for i in range(2)]

    def tm_post(gi, pp, tok0, n):
        b = (tok0 // 128) % 2
        if gi == 0:
            S.act(lambda e: e.copy(out=vb[b][:], in_=pp[:, 0:64]), r=[pp], w=[vb[b]])
            S.dma('pool', dr['vcd'][tok0:tok0 + 128, :], vb[b][:], r=[vb[b]], w=[DW(S, dr['vcd'])])
        elif gi == 1:
            S.act(lambda e: e.copy(out=wb[b][:], in_=pp[:, 0:4]), r=[pp], w=[wb[b]])
            S.dma('pool', dr['wi'][tok0:tok0 + 128, :], wb[b][:], r=[wb[b]], w=[DW(S, dr['wi'])])
        else:
            S.act(lambda e: e.copy(out=vd[b][:], in_=pp[:, 0:512]), r=[pp], w=[vd[b]])
            S.dma('pool', dr['vdd'][tok0:tok0 + 128, :], vd[b][:], r=[vd[b]], w=[DW(S, dr['vdd'])])
    fm = []
    for i in range(4):
        fm.append((128 * i, 128, 0.125, 'r64', None, dr['qc'][128 * i:128 * (i + 1), :]))
    fm.append((512, 64, 1.0, 'r64', None, dr['kcd'][:, :]))
    fm.append((640, 128, 1.0, 'r32', None, dr['qi'][:, :]))
    fm.append((768, 32, 1.0, 'r32', None, dr['ki'][:, :]))
    for i in range(4):
        fm.append((804 + 128 * i, 128, 0.125, 'r64', None, dr['qd'][128 * i:128 * (i + 1), :]))
    for i in range(4):
        fm.append((1316 + 128 * i, 128, 1.0, 'r64', None, dr['kd'][128 * i:128 * (i + 1), :]))
    proj_stage(S, x, prm['w_in'], 2340, prm['mix_norm'], prm['pos'], ntok, cpack, fm,
               [(576, 64), (800, 4), (1828, 512)], tm_post, use_idx=True)


NBIS = 14


def dsa_moba_stage(S, prm, dr, nseq, cpack):
    C = Consts(S, cpack)
    ident = C.get('ident', BF16)
    tri = C.get('tri01', BF16)
    trib = C.get('tri_ge', BF16)
    E8 = C.get('E8', F32)
    triqs = C.get('tri_qs', F32)
    mbias, mpast, mown = C.get('mbias'), C.get('mpast'), C.get('mown')
    zc = [0]

    def ztile(shape, name):
        t = S.sb(shape, BF16, name)
        zc[0] += 1
        if zc[0] % 2:
            S.dve(lambda e: e.memset(t[:], 0.0), r=[], w=[t])
        else:
            S.pool(lambda e: e.memset(t[:], 0.0), r=[], w=[t])
        return t
    qi = [ztile([128, SEQ], f'qi{h}') for h in range(4)]
    ki = ztile([128, SEQ], 'ki')
    wi = S.sb([128, 16, 4], F32, 'wi')
    absw = S.sb([128, 16, 4], F32, 'absw')
    sgnw = S.sb([128, 16, 4], F32, 'sgnw')
    scores = [S.sb([128, SEQ], F32, f'score{i}') for i in range(2)]
    rl = [S.sb([128, 512], F32, f'rl{i}') for i in range(2)]
    junkb = S.sb([128, SEQ], BF16, 'junkb')
    nmasks = [S.sb([128, SEQ], BF16, f'nmask{i}') for i in range(2)]
    nmTs = [S.sb([128, 16, 512], BF16, f'nmT{i}') for i in range(2)]
    kcT = ztile([128, SEQ], 'kcT')
    vcx = S.sb([128, 16, 65], BF16, 'vcx')
    vdxs = [S.sb([128, 16, 65], BF16, f'vdx{i}') for i in range(2)]
    S.pool(lambda e: e.memset(vcx[:], 1.0), r=[], w=[vcx])
    for v_ in vdxs:
        S.pool(lambda e, v_=v_: e.memset(v_[:], 1.0), r=[], w=[v_])
    qcall = [ztile([128, SEQ], f'qcall{h}') for h in range(8)]
    qds = [ztile([128, SEQ], f'qd{i}') for i in range(2)]
    kds = [ztile([128, SEQ], f'kd{i}') for i in range(2)]
    for kd_ in kds:
        S.dve(lambda e, kd_=kd_: e.tensor_copy(out=kd_[64:72, :], in_=E8[0:8, :]), r=[E8], w=[(kd_, 'e')])
    kmf = S.sb([64, 8], F32, 'kmf')
    kmbs = [ztile([128, 8], f'kmb{i}') for i in range(2)]
    gsb = S.sb([128, 128], F32, 'gsb')
    ns8all = S.sb([128, 16, 32], BF16, 'ns8all')
    otok = S.sb([128, 16, 1024], BF16, 'otok')
    PT = [S.sb([128, 512], BF16, f'PT{i}') for i in range(4)]
    st5 = [S.sb([128, 8], F32, f'st5{i}') for i in range(2)]
    gs = [S.sb([128, 8], F32, f'gs{i}') for i in range(2)]
    m8 = [S.sb([128, 8], F32, f'm8{i}') for i in range(2)]
    ns8 = [S.sb([128, 8], BF16, f'ns8{i}') for i in range(2)]
    fsc = [S.sb([128, 1], F32, f'fsc{i}') for i in range(4)]
    pl = S.ps([128, 512], F32, 'pl')
    STps = [S.ps([128, 512], F32, f'st{i}') for i in range(2)] + [pl]
    Oacc = [S.ps([128, 512], F32, f'oa{i}') for i in range(4)]
    ptb = S.ps([128, 8, 128], BF16, 'ptb')
    fcount = [0]

    def make_fin(col0):
        def fin(c, j, O, off):
            i = 4 * c + j
            f = fsc[fcount[0] % 4]
            fcount[0] += 1
            S.dve(lambda e: e.reciprocal(out=f[:], in_=O[:, off + 64:off + 65]), r=[O], w=[f])
            S.dve(lambda e: e.tensor_scalar(out=otok[:, i, col0:col0 + 64], in0=O[:, off:off + 64], scalar1=f[:, 0:1],
                                            scalar2=None, op0=ALU.mult), r=[O, f], w=[(otok, (i, col0))])
        return fin

    def index_steps(c):
        nmT = nmTs[c % 2]
        steps = []
        chains = {}
        for j in range(4):
            i = 4 * c + j
            W = 128 * (i + 1)
            score = scores[j % 2]
            nmask = nmasks[j % 2]
            steps = chains.setdefault(j, [])
            if i < 2:
                def trivial(i=i, j=j):
                    for st_ in range(i + 1):
                        if st_ == i:
                            S.pool(lambda e, st_=st_: e.tensor_copy(out=nmT[:, st_, j * 128:(j + 1) * 128], in_=trib[:]),
                                   r=[trib], w=[(nmT, (st_, j))])
                        else:
                            S.pool(lambda e, st_=st_: e.memset(nmT[:, st_, j * 128:(j + 1) * 128], 0.0),
                                   r=[], w=[(nmT, (st_, j))])
                steps.append(trivial)
                continue
            s5 = st5[i % 2]
            for h in range(4):
                def logits(h=h, i=i, W=W, score=score):
                    for sc in range((W + 511) // 512):
                        n = min(512, W - 512 * sc)
                        rb = rl[(h * 4 + sc) % 2]
                        S.pe(lambda e, sc=sc, n=n: e.matmul(pl[:, 0:n], lhsT=qi[h][:, i * 128:(i + 1) * 128],
                                                            rhs=ki[:, sc * 512:sc * 512 + n], start=True, stop=True),
                             r=[qi[h], ki], w=[pl])
                        S.act(lambda e, rb=rb, n=n: e.activation(out=rb[:, 0:n], in_=pl[:, 0:n], func=AF.Relu,
                                                                 scale=absw[:, i, h:h + 1]), r=[pl, absw], w=[rb])
                        if h == 0:
                            S.dve(lambda e, rb=rb, n=n, sc=sc: e.tensor_scalar(
                                out=score[:, sc * 512:sc * 512 + n], in0=rb[:, 0:n], scalar1=sgnw[:, i, h:h + 1],
                                scalar2=None, op0=ALU.mult), r=[rb, sgnw], w=[(score, sc)])
                        else:
                            S.dve(lambda e, rb=rb, n=n, sc=sc: e.scalar_tensor_tensor(
                                out=score[:, sc * 512:sc * 512 + n], in0=rb[:, 0:n], scalar=sgnw[:, i, h:h + 1],
                                in1=score[:, sc * 512:sc * 512 + n], op0=ALU.mult, op1=ALU.add),
                                r=[rb, sgnw, (score, sc)], w=[(score, sc)])
                steps.append(logits)

            def bounds(i=i, W=W, s5=s5, score=score):
                S.dve(lambda e: e.tensor_reduce(out=s5[:, 5:6], in_=score[:, 0:W], axis=AX.X, op=ALU.max),
                      r=[score], w=[(s5, 5)])
                S.dve(lambda e: e.tensor_reduce(out=s5[:, 0:1], in_=score[:, 0:W], axis=AX.X, op=ALU.min),
                      r=[score], w=[(s5, 0)])
                S.dve(lambda e: e.tensor_tensor(out=s5[:, 1:2], in0=s5[:, 5:6], in1=s5[:, 0:1], op=ALU.subtract),
                      r=[(s5, 5), (s5, 0)], w=[(s5, 1)])
                S.dve(lambda e: e.tensor_tensor(out=score[:, i * 128:(i + 1) * 128],
                                                in0=score[:, i * 128:(i + 1) * 128], in1=triqs[:], op=ALU.add),
                      r=[score, triqs], w=[score])
            steps.append(bounds)
            for it in range(NBIS):
                def bis(W=W, s5=s5, score=score):
                    S.dve(lambda e: e.tensor_scalar(out=s5[:, 1:2], in0=s5[:, 1:2], scalar1=0.5, scalar2=None,
                                                    op0=ALU.mult), r=[(s5, 1)], w=[(s5, 1)])
                    S.dve(lambda e: e.tensor_tensor(out=s5[:, 2:3], in0=s5[:, 0:1], in1=s5[:, 1:2], op=ALU.add),
                          r=[(s5, 0), (s5, 1)], w=[(s5, 2)])
                    S.dve(lambda e: e.tensor_scalar(out=junkb[:, 0:W], in0=score[:, 0:W], scalar1=s5[:, 2:3],
                                                    scalar2=0.0, op0=ALU.is_ge, op1=ALU.add, accum_out=s5[:, 3:4]),
                          r=[score, (s5, 2)], w=[junkb, (s5, 3)])
                    S.dve(lambda e: e.tensor_scalar(out=s5[:, 4:5], in0=s5[:, 3:4], scalar1=255.5, scalar2=None,
                                                    op0=ALU.is_ge), r=[(s5, 3)], w=[(s5, 4)])
                    S.dve(lambda e: e.scalar_tensor_tensor(out=s5[:, 0:1], in0=s5[:, 1:2], scalar=s5[:, 4:5],
                                                           in1=s5[:, 0:1], op0=ALU.mult, op1=ALU.add),
                          r=[(s5, 1), (s5, 4), (s5, 0)], w=[(s5, 0)])
                steps.append(bis)

            def fin_mask(i=i, j=j, W=W, s5=s5, score=score, nmask=nmask):
                S.dve(lambda e: e.tensor_scalar(out=nmask[:, 0:W], in0=score[:, 0:W], scalar1=s5[:, 0:1],
                                                scalar2=NEG, op0=ALU.is_lt, op1=ALU.mult),
                      r=[score, (s5, 0)], w=[nmask])
                for s0 in range(0, i + 1, 8):
                    n = min(8, i + 1 - s0)
                    for k in range(n):
                        S.pe(lambda e, s0=s0, k=k: e.transpose(out=ptb[:, k, :],
                                                               in_=nmask[:, (s0 + k) * 128:(s0 + k + 1) * 128],
                                                               identity=ident[:]), r=[nmask, ident], w=[(ptb, k)])
                    S.act(lambda e, s0=s0, n=n: e.copy(out=nmT[:, s0:s0 + n, j * 128:(j + 1) * 128],
                                                       in_=ptb[:, 0:n, :]), r=[ptb], w=[(nmT, ('b', s0, j))])
            steps.append(fin_mask)
        out = []
        for pair in ((0, 1), (2, 3)):
            la, lb = chains[pair[0]], chains[pair[1]]
            for k in range(max(len(la), len(lb))):
                if k < len(la):
                    out.append(la[k])
                if k < len(lb):
                    out.append(lb[k])
        return out

    for sq in range(nseq):
        tb = sq * SEQ
        for h in range(4):
            S.dma('sp', qi[h][0:32, :], dr['qi'][h * 32:(h + 1) * 32, tb:tb + SEQ], r=[], w=[(qi[h], 'q')])
        S.dma('sp', ki[0:32, :], dr['ki'][:, tb:tb + SEQ], r=[], w=[(ki, 'q')])
        S.dma('sp', wi[:], dr['wi'][tb:tb + SEQ, :].rearrange("(n p) c -> p n c", p=128), r=[], w=[wi])
        S.act(lambda e: e.activation(out=absw[:], in_=wi[:], func=AF.Abs), r=[wi], w=[absw])
        S.act(lambda e: e.activation(out=sgnw[:], in_=wi[:], func=AF.Sign), r=[wi], w=[sgnw])
        S.dma('sp', kcT[0:64, :], dr['kcd'][:, tb:tb + SEQ], r=[], w=[(kcT, 'q')])
        S.dma('sp', vcx[:, :, 0:64], dr['vcd'][tb:tb + SEQ, :].rearrange("(n p) c -> p n c", p=128), r=[], w=[vcx])
        for h in range(8):
            S.dma('sp', qcall[h][0:64, :], dr['qc'][h * 64:(h + 1) * 64, tb:tb + SEQ], r=[], w=[(qcall[h], 'q')])
        import os
        STOP = int(os.environ.get('STOP', '0'))
        if STOP == 1:
            break
        for st in index_steps(0):
            st()
        if STOP == 2:
            break
        jobs = []
        for c in range(4):
            nmT = nmTs[c % 2]
            nxt = index_steps(c + 1) if c < 3 else []
            per = (len(nxt) + 7) // 8
            for h in range(8):
                sl = nxt[h * per:(h + 1) * per]
                if os.environ.get('NOPRE'):
                    for st in sl:
                        st()
                    sl = []
                jobs.append(dict(c=c, tiles=causal_tiles(c, None), qT=qcall[h], kT=kcT, vext=vcx,
                                 extra=(lambda kt: ident[:], lambda kt, c, lo, hi, nmT=nmT: nmT[:, kt, lo:hi],
                                        [ident, nmT]),
                                 fin=make_fin(64 * h),
                                 pre=((lambda sl=sl: [st() for st in sl]) if sl else None)))
        import os
        if not os.environ.get('SKIP_DSA'):
            attn_run(S, jobs, STps, PT, Oacc)

        def moba_load(h, tb=tb):
            b = h % 2
            S.dma('sp', qds[b][0:64, :], dr['qd'][h * 64:(h + 1) * 64, tb:tb + SEQ], r=[], w=[(qds[b], 'q')])
            S.dma('sp', kds[b][0:64, :], dr['kd'][h * 64:(h + 1) * 64, tb:tb + SEQ], r=[], w=[(kds[b], 'q')])
            S.dma('sp', vdxs[b][:, :, 0:64], dr['vdd'][tb:tb + SEQ, h * 64:(h + 1) * 64].rearrange(
                "(n p) c -> p n c", p=128), r=[], w=[vdxs[b]])

        def gate_a(h):
            b = h % 2
            qd, kd, kmb = qds[b], kds[b], kmbs[b]
            S.dve(lambda e: e.tensor_reduce(out=kmf[:], in_=kd[0:64, :].rearrange("p (j k) -> p j k", k=256),
                                            axis=AX.X, op=ALU.add), r=[(kd, 'q')], w=[kmf])
            S.dve(lambda e: e.tensor_scalar(out=kmb[0:64, :], in0=kmf[:], scalar1=1.0 / 256, scalar2=None,
                                            op0=ALU.mult), r=[kmf], w=[(kmb, 'm')])
            for i in range(16):
                S.pe(lambda e, i=i: e.matmul(pl[:, i * 8:(i + 1) * 8], lhsT=qd[:, i * 128:(i + 1) * 128], rhs=kmb[:],
                                             start=True, stop=True), r=[qd, kmb], w=[(pl, i)])
            S.dve(lambda e: e.tensor_tensor(out=gsb[:], in0=pl[:, 0:128], in1=mbias[:], op=ALU.add),
                  r=[pl, mbias], w=[gsb])
            for i in range(16):
                m = m8[i % 2]
                S.dve(lambda e, i=i, m=m: e.max(out=m[:], in_=gsb[:, i * 8:(i + 1) * 8]), r=[(gsb, i)], w=[m])
                S.dve(lambda e, i=i, m=m: e.tensor_scalar(out=gsb[:, i * 8:(i + 1) * 8], in0=gsb[:, i * 8:(i + 1) * 8],
                                                          scalar1=m[:, 2:3], scalar2=None, op0=ALU.is_ge),
                      r=[(gsb, i), m], w=[(gsb, i)])
            S.dve(lambda e: e.tensor_tensor(out=gsb[:], in0=gsb[:], in1=mpast[:], op=ALU.mult), r=[gsb, mpast], w=[gsb])
            S.dve(lambda e: e.tensor_tensor(out=gsb[:], in0=gsb[:], in1=mown[:], op=ALU.add), r=[gsb, mown], w=[gsb])
            S.dve(lambda e: e.tensor_scalar(out=ns8all[:, :, 0:8], in0=gsb[:].rearrange("p (i k) -> p i k", k=8),
                                            scalar1=-NEG, scalar2=NEG, op0=ALU.mult, op1=ALU.add),
                  r=[gsb], w=[ns8all])

        def gate_b(h):
            qd = qds[h % 2]
            for half in range(2):
                for k in range(8):
                    i = half * 8 + k
                    S.pe(lambda e, k=k, i=i: e.transpose(out=ptb[0:8, k, :], in_=ns8all[:, i, 0:8],
                                                         identity=ident[:]), r=[ns8all, ident], w=[(ptb, k)])
                S.dve(lambda e, half=half: e.tensor_copy(
                    out=qd[64:72, half * 1024:(half + 1) * 1024].rearrange("p (k q) -> p k q", q=128),
                    in_=ptb[0:8, :, :]), r=[ptb], w=[(qd, ('n', half))])

        if STOP == 3:
            break
        moba_load(0)
        gate_a(0)
        if STOP == 4:
            break
        gate_b(0)
        if STOP == 5:
            break
        NH_ = int(os.environ.get('MOBA_H', '8'))
        for h in range(0 if not os.environ.get('SKIP_MOBA') else 8, NH_):
            b = h % 2
            if h + 1 < 8:
                moba_load(h + 1)
            jobs = []
            for c in range(4):
                jobs.append(dict(c=c, tiles=causal_tiles(c, tri), qT=qds[b], kT=kds[b], vext=vdxs[b],
                                 fin=make_fin(512 + 64 * h),
                                 pre=((lambda h=h: gate_a(h + 1)) if (c == 1 and h + 1 < 8 and not os.environ.get('NOGATE')) else None)))
            attn_run(S, jobs, STps, PT, Oacc)
            if h + 1 < 8:
                gate_b(h + 1)
        S.dma('sp', dr['o_tok'][tb:tb + SEQ, :].rearrange("(n p) c -> p n c", p=128), otok[:],
              r=[otok], w=[DW(S, dr['o_tok'])])
    S.flush()


def attn_chunk_v1(S, c, tiles, qT, kT, extra, vext, ident, STps, PT, Oacc, finalize, tag, qdep=None):
    cover = {}
    for n, (kt, lo, hi, bt, blo) in enumerate(tiles):
        for j in range(lo // 128, hi // 128):
            cover.setdefault(j, []).append(n)

    qd_ = qdep if qdep is not None else qT

    def qk(n):
        kt, lo, hi, bt, blo = tiles[n]
        ps = STps[n % 2]
        nterm = 1 + (1 if extra else 0) + (1 if bt is not None else 0)
        S.pe(lambda e: e.matmul(ps[:, lo:hi], lhsT=kT[:, kt * 128:(kt + 1) * 128],
                                rhs=qT[:, c * 512 + lo:c * 512 + hi], start=True, stop=(nterm == 1)),
             r=[kT, qd_], w=[ps])
        k = 1
        if extra:
            k += 1
            S.pe(lambda e: e.matmul(ps[:, lo:hi], lhsT=extra[0](kt), rhs=extra[1](kt, c, lo, hi),
                                    start=False, stop=(k == nterm), skip_group_check=True),
                 r=list(extra[2]), w=[ps])
        if bt is not None:
            S.pe(lambda e: e.matmul(ps[:, blo:blo + 128], lhsT=ident[:], rhs=bt[:], start=False, stop=True,
                                    skip_group_check=True), r=[ident, bt], w=[ps])

    qk(0)
    for n, (kt, lo, hi, bt, blo) in enumerate(tiles):
        if n + 1 < len(tiles):
            qk(n + 1)
        ps, p = STps[n % 2], PT[n % 2]
        S.act(lambda e, ps=ps, p=p, lo=lo, hi=hi: e.activation(out=p[:, lo:hi], in_=ps[:, lo:hi], func=AF.Exp),
              r=[ps], w=[p])
        for j in range(lo // 128, hi // 128):
            S.pe(lambda e, p=p, j=j, kt=kt, n=n: e.matmul(
                Oacc[j][:, 0:65], lhsT=p[:, j * 128:(j + 1) * 128], rhs=vext[:, kt, :],
                start=(cover[j][0] == n), stop=(cover[j][-1] == n), skip_group_check=True),
                r=[p, vext], w=[Oacc[j]])
            if cover[j][-1] == n:
                finalize(c, j, Oacc[j])


def causal_tiles_v1(c, tri):
    out = []
    for kt in range(4 * c + 4):
        if kt < 4 * c:
            out.append((kt, 0, 512, None, 0))
        else:
            lo = (kt - 4 * c) * 128
            out.append((kt, lo, 512, tri, lo))
    return out


def dsa_moba_stage_v1(S, prm, dr, nseq, cpack):
    C = Consts(S, cpack)
    ident = C.get('ident', BF16)
    tri = C.get('tri_ge', BF16)
    E8 = C.get('E8', BF16)
    triqs = C.get('tri_qs', F32)
    mbias, mpast, mown = C.get('mbias'), C.get('mpast'), C.get('mown')
    qi = [S.sb([32, SEQ], BF16, f'qi{h}') for h in range(4)]
    ki = S.sb([32, SEQ], BF16, 'ki')
    wi = S.sb([128, 16, 4], F32, 'wi')
    absw = S.sb([128, 16, 4], F32, 'absw')
    sgnw = S.sb([128, 16, 4], F32, 'sgnw')
    score = S.sb([128, SEQ], F32, 'score')
    rl = [S.sb([128, 512], F32, f'rl{i}') for i in range(2)]
    junkb = S.sb([128, SEQ], BF16, 'junkb')
    nmask = S.sb([128, SEQ], BF16, 'nmask')
    nmT = S.sb([128, 16, 512], BF16, 'nmT')
    kcT = S.sb([64, SEQ], BF16, 'kcT')
    vcx = S.sb([128, 16, 65], BF16, 'vcx')
    vdx = S.sb([128, 16, 65], BF16, 'vdx')
    S.pool(lambda e: e.memset(vcx[:], 1.0), r=[], w=[vcx])
    S.pool(lambda e: e.memset(vdx[:], 1.0), r=[], w=[vdx])
    qcall = [S.sb([64, SEQ], BF16, f'qcall{h}') for h in range(8)]
    qd = S.sb([64, SEQ], BF16, 'qd')
    kd = S.sb([64, SEQ], BF16, 'kd')
    kmf = S.sb([64, 8], F32, 'kmf')
    kmb = S.sb([64, 8], BF16, 'kmb')
    negsel8 = S.sb([8, SEQ], BF16, 'negsel8')
    otok = S.sb([128, 16, 1024], BF16, 'otok')
    PT = [S.sb([128, 512], BF16, f'PT{i}') for i in range(2)]
    st5 = [S.sb([128, 8], F32, f'st5{i}') for i in range(2)]
    gs = [S.sb([128, 8], F32, f'gs{i}') for i in range(2)]
    m8 = [S.sb([128, 8], F32, f'm8{i}') for i in range(2)]
    ns8 = [S.sb([128, 8], BF16, f'ns8{i}') for i in range(2)]
    fsc = [S.sb([128, 1], F32, f'fsc{i}') for i in range(4)]
    STps = [S.ps([128, 512], F32, f'st{i}') for i in range(2)]
    Oacc = [S.ps([128, 512], F32, f'oa{i}') for i in range(4)]
    ptb = S.ps([128, 8, 128], BF16, 'ptb')
    pl = S.ps([128, 512], F32, 'pl')
    fcount = [0]

    def make_fin(col0):
        def fin(c, j, O):
            i = 4 * c + j
            f = fsc[fcount[0] % 4]
            fcount[0] += 1
            S.dve(lambda e: e.tensor_scalar(out=f[:], in0=O[:, 64:65], scalar1=1e-30, scalar2=None, op0=ALU.max),
                  r=[O], w=[f])
            S.dve(lambda e: e.reciprocal(out=f[:], in_=f[:]), r=[f], w=[f])
            S.dve(lambda e: e.tensor_scalar(out=otok[:, i, col0:col0 + 64], in0=O[:, 0:64], scalar1=f[:, 0:1],
                                            scalar2=None, op0=ALU.mult), r=[O, f], w=[(otok, (i, col0))])
        return fin

    for sq in range(nseq):
        tb = sq * SEQ
        for h in range(4):
            S.dma('sp', qi[h][:], dr['qi'][h * 32:(h + 1) * 32, tb:tb + SEQ], r=[], w=[qi[h]])
        S.dma('sp', ki[:], dr['ki'][:, tb:tb + SEQ], r=[], w=[ki])
        S.dma('sp', wi[:], dr['wi'][tb:tb + SEQ, :].rearrange("(n p) c -> p n c", p=128), r=[], w=[wi])
        S.act(lambda e: e.activation(out=absw[:], in_=wi[:], func=AF.Abs), r=[wi], w=[absw])
        S.act(lambda e: e.activation(out=sgnw[:], in_=wi[:], func=AF.Sign), r=[wi], w=[sgnw])
        S.dma('sp', kcT[:], dr['kcd'][:, tb:tb + SEQ], r=[], w=[kcT])
        S.dma('sp', vcx[:, :, 0:64], dr['vcd'][tb:tb + SEQ, :].rearrange("(n p) c -> p n c", p=128), r=[], w=[vcx])
        for h in range(8):
            S.dma('sp', qcall[h][:], dr['qc'][h * 64:(h + 1) * 64, tb:tb + SEQ], r=[], w=[qcall[h]])
        for c in range(4):
            for j in range(4):
                i = 4 * c + j
                W = 128 * (i + 1)
                if i < 2:
                    for st_ in range(i + 1):
                        if st_ == i:
                            S.pool(lambda e, st_=st_, j=j: e.tensor_copy(out=nmT[:, st_, j * 128:(j + 1) * 128],
                                                                         in_=tri[:]), r=[tri], w=[(nmT, (st_, j))])
                        else:
                            S.pool(lambda e, st_=st_, j=j: e.memset(nmT[:, st_, j * 128:(j + 1) * 128], 0.0),
                                   r=[], w=[(nmT, (st_, j))])
                    continue
                for h in range(4):
                    for sc in range((W + 511) // 512):
                        n = min(512, W - 512 * sc)
                        rb = rl[(h * 4 + sc) % 2]
                        S.pe(lambda e, h=h, sc=sc, n=n, i=i: e.matmul(pl[:, 0:n], lhsT=qi[h][:, i * 128:(i + 1) * 128],
                                                                       rhs=ki[:, sc * 512:sc * 512 + n], start=True,
                                                                       stop=True), r=[qi[h], ki], w=[pl])
                        S.act(lambda e, rb=rb, n=n, i=i, h=h: e.activation(out=rb[:, 0:n], in_=pl[:, 0:n], func=AF.Relu,
                                                                           scale=absw[:, i, h:h + 1]),
                              r=[pl, absw], w=[rb])
                        if h == 0:
                            S.dve(lambda e, rb=rb, n=n, sc=sc, i=i, h=h: e.tensor_scalar(
                                out=score[:, sc * 512:sc * 512 + n], in0=rb[:, 0:n], scalar1=sgnw[:, i, h:h + 1],
                                scalar2=None, op0=ALU.mult), r=[rb, sgnw], w=[(score, sc)])
                        else:
                            S.dve(lambda e, rb=rb, n=n, sc=sc, i=i, h=h: e.scalar_tensor_tensor(
                                out=score[:, sc * 512:sc * 512 + n], in0=rb[:, 0:n], scalar=sgnw[:, i, h:h + 1],
                                in1=score[:, sc * 512:sc * 512 + n], op0=ALU.mult, op1=ALU.add),
                                r=[rb, sgnw, (score, sc)], w=[(score, sc)])
                s5 = st5[i % 2]
                S.dve(lambda e, s5=s5, W=W: e.tensor_reduce(out=s5[:, 5:6], in_=score[:, 0:W], axis=AX.X, op=ALU.max),
                      r=[score], w=[(s5, 5)])
                S.dve(lambda e, s5=s5, W=W: e.tensor_reduce(out=s5[:, 0:1], in_=score[:, 0:W], axis=AX.X, op=ALU.min),
                      r=[score], w=[(s5, 0)])
                S.dve(lambda e, s5=s5: e.tensor_tensor(out=s5[:, 1:2], in0=s5[:, 5:6], in1=s5[:, 0:1], op=ALU.subtract),
                      r=[(s5, 5), (s5, 0)], w=[(s5, 1)])
                S.dve(lambda e, i=i: e.tensor_tensor(out=score[:, i * 128:(i + 1) * 128],
                                                     in0=score[:, i * 128:(i + 1) * 128], in1=triqs[:], op=ALU.add),
                      r=[score, triqs], w=[score])
                for it in range(NBIS):
                    S.dve(lambda e, s5=s5: e.tensor_scalar(out=s5[:, 1:2], in0=s5[:, 1:2], scalar1=0.5, scalar2=None,
                                                           op0=ALU.mult), r=[(s5, 1)], w=[(s5, 1)])
                    S.dve(lambda e, s5=s5: e.tensor_tensor(out=s5[:, 2:3], in0=s5[:, 0:1], in1=s5[:, 1:2], op=ALU.add),
                          r=[(s5, 0), (s5, 1)], w=[(s5, 2)])
                    S.dve(lambda e, s5=s5, W=W: e.tensor_scalar(out=junkb[:, 0:W], in0=score[:, 0:W],
                                                                scalar1=s5[:, 2:3], scalar2=0.0, op0=ALU.is_ge,
                                                                op1=ALU.add, accum_out=s5[:, 3:4]),
                          r=[score, (s5, 2)], w=[junkb, (s5, 3)])
                    S.dve(lambda e, s5=s5: e.tensor_scalar(out=s5[:, 4:5], in0=s5[:, 3:4], scalar1=255.5, scalar2=None,
                                                           op0=ALU.is_ge), r=[(s5, 3)], w=[(s5, 4)])
                    S.dve(lambda e, s5=s5: e.scalar_tensor_tensor(out=s5[:, 0:1], in0=s5[:, 1:2], scalar=s5[:, 4:5],
                                                                  in1=s5[:, 0:1], op0=ALU.mult, op1=ALU.add),
                          r=[(s5, 1), (s5, 4), (s5, 0)], w=[(s5, 0)])
                S.dve(lambda e, s5=s5, W=W: e.tensor_scalar(out=nmask[:, 0:W], in0=score[:, 0:W], scalar1=s5[:, 0:1],
                                                            scalar2=NEG, op0=ALU.is_lt, op1=ALU.mult),
                      r=[score, (s5, 0)], w=[nmask])
                for s0 in range(0, i + 1, 8):
                    n = min(8, i + 1 - s0)
                    for k in range(n):
                        S.pe(lambda e, s0=s0, k=k: e.transpose(out=ptb[:, k, :],
                                                               in_=nmask[:, (s0 + k) * 128:(s0 + k + 1) * 128],
                                                               identity=ident[:]), r=[nmask, ident], w=[(ptb, k)])
                    S.act(lambda e, s0=s0, n=n, j=j: e.copy(out=nmT[:, s0:s0 + n, j * 128:(j + 1) * 128],
                                                            in_=ptb[:, 0:n, :]), r=[ptb], w=[(nmT, ('b', s0, j))])
            for h in range(8):
                attn_chunk_v1(S, c, causal_tiles_v1(c, None), qcall[h], kcT,
                           (lambda kt: ident[:], lambda kt, c, lo, hi: nmT[:, kt, lo:hi], [ident, nmT]),
                           vcx, ident, STps, PT, Oacc, make_fin(64 * h), 'dsa')
        for h in range(8):
            S.dma('sp', qd[:], dr['qd'][h * 64:(h + 1) * 64, tb:tb + SEQ], r=[], w=[qd])
            S.dma('sp', kd[:], dr['kd'][h * 64:(h + 1) * 64, tb:tb + SEQ], r=[], w=[kd])
            S.dma('sp', vdx[:, :, 0:64], dr['vdd'][tb:tb + SEQ, h * 64:(h + 1) * 64].rearrange(
                "(n p) c -> p n c", p=128), r=[], w=[vdx])
            S.dve(lambda e: e.tensor_reduce(out=kmf[:], in_=kd[:].rearrange("p (j k) -> p j k", k=256), axis=AX.X,
                                            op=ALU.add), r=[kd], w=[kmf])
            S.dve(lambda e: e.tensor_scalar(out=kmb[:], in0=kmf[:], scalar1=1.0 / 256, scalar2=None, op0=ALU.mult),
                  r=[kmf], w=[kmb])
            for i in range(16):
                b = i % 2
                S.pe(lambda e, i=i: e.matmul(pl[:, 0:8], lhsT=qd[:, i * 128:(i + 1) * 128], rhs=kmb[:], start=True,
                                             stop=True), r=[qd, kmb], w=[pl])
                g_ = gs[b]
                S.dve(lambda e, g_=g_, i=i: e.tensor_tensor(out=g_[:], in0=pl[:, 0:8], in1=mbias[:, i * 8:(i + 1) * 8],
                                                           op=ALU.add), r=[pl, mbias], w=[g_])
                S.dve(lambda e, g_=g_, b=b: e.max(out=m8[b][:], in_=g_[:]), r=[g_], w=[m8[b]])
                S.dve(lambda e, g_=g_, b=b: e.tensor_scalar(out=g_[:], in0=g_[:], scalar1=m8[b][:, 2:3], scalar2=None,
                                                            op0=ALU.is_ge), r=[g_, m8[b]], w=[g_])
                S.dve(lambda e, g_=g_, i=i: e.tensor_tensor(out=g_[:], in0=g_[:], in1=mpast[:, i * 8:(i + 1) * 8],
                                                           op=ALU.mult), r=[g_, mpast], w=[g_])
                S.dve(lambda e, g_=g_, i=i: e.tensor_tensor(out=g_[:], in0=g_[:], in1=mown[:, i * 8:(i + 1) * 8],
                                                           op=ALU.add), r=[g_, mown], w=[g_])
                S.dve(lambda e, g_=g_, b=b: e.tensor_scalar(out=ns8[b][:], in0=g_[:], scalar1=-NEG, scalar2=NEG,
                                                            op0=ALU.mult, op1=ALU.add), r=[g_], w=[ns8[b]])
                S.pe(lambda e, b=b: e.transpose(out=ptb[0:8, b, :], in_=ns8[b][:], identity=ident[:]),
                     r=[ns8[b], ident], w=[(ptb, b)])
                S.act(lambda e, b=b, i=i: e.copy(out=negsel8[:, i * 128:(i + 1) * 128], in_=ptb[0:8, b, :]),
                      r=[(ptb, b)], w=[(negsel8, i // 4)])
            for c in range(4):
                attn_chunk_v1(S, c, causal_tiles_v1(c, tri), qd, kd,
                           (lambda kt: E8[0:8, kt * 128:(kt + 1) * 128], lambda kt, c, lo, hi: negsel8[:, c * 512 + lo:c * 512 + hi],
                            [E8, negsel8]),
                           vdx, ident, STps, PT, Oacc, make_fin(512 + 64 * h), 'moba')
        S.dma('sp', dr['o_tok'][tb:tb + SEQ, :].rearrange("(n p) c -> p n c", p=128), otok[:],
              r=[otok], w=[DW(S, dr['o_tok'])])
    S.flush()


NCORES = 8
TPC = 2 * SEQ


def build_program(stages=None):
    nc = bass.Bass("TRN2", target_bir_lowering=False)
    ins = {}

    def din(name, shape, dt=F32):
        ins[name] = nc.dram_tensor(name, list(shape), dt, kind="ExternalInput").ap()
        return ins[name]

    def scr(name, shape, dt=BF16):
        return nc.dram_tensor(name, list(shape), dt, kind="Internal").ap()

    x = din('x', [TPC, D])
    pos = din('pos', [TPC], I32)
    cp = din('cpack', [128, CP_N])
    P = {}
    for L in range(2):
        for f in ('ffn1', 'ffn2'):
            P[f'{f}_norm{L}'] = din(f'{f}_norm{L}', [D])
            P[f'{f}_wg{L}'] = din(f'{f}_wg{L}', [D, DFF])
            P[f'{f}_wu{L}'] = din(f'{f}_wu{L}', [D, DFF])
            P[f'{f}_wd{L}'] = din(f'{f}_wd{L}', [DFF, D])
        P[f'mix_norm{L}'] = din(f'mix_norm{L}', [D])
    ev = {'w_in': din('ev_w_in', [D, 2468]), 'mix_norm': P['mix_norm0'], 'pos': pos,
          'sgu_norm': din('ev_sgu_norm', [256]), 'sgu_wT': din('ev_sgu_wT', [4, 128, 128]),
          'sgu_bT': din('ev_sgu_bT', [128, 4]),
          'cmp_w1_k': din('ev_w1k', [2048, 256]), 'cmp_w2_k': din('ev_w2k', [256, 64]),
          'cmp_posT_k': din('ev_pk', [64, 32]),
          'cmp_w1_v': din('ev_w1v', [2048, 256]), 'cmp_w2_v': din('ev_w2v', [256, 64]),
          'cmp_posT_v': din('ev_pv', [64, 32])}
    ev_w_out = din('ev_w_out', [D, D])
    od = {'w_in': din('od_w_in', [D, 2340]), 'mix_norm': P['mix_norm1'], 'pos': pos}
    od_w_out = din('od_w_out', [D, D])
    fin_g = din('final_norm', [D])
    y = nc.dram_tensor('y', [TPC, D], F32, kind="ExternalOutput").ap()
    xa = scr('xa', [TPC, D], F32)
    xb = scr('xb', [TPC, D], F32)
    T_ = TPC
    dre = {'a_tok': scr('a_tok', [T_, 512]), 'qraw': scr('qraw', [768, T_]), 'qrot': scr('qrot', [768, T_]),
           'kc': scr('kc', [192, T_]), 'vc': scr('vc', [192, T_]), 'ksl': scr('ksl', [192, T_]),
           'kw': scr('kw', [192, T_]), 'vsl': scr('vsl', [T_, 192]), 'vw': scr('vw', [T_, 192]),
           'gate': scr('gate', [T_, 36], F32), 'o_tok': scr('o_tok0', [T_, 1024])}
    dro = {'qc': scr('qc', [512, T_]), 'kcd': scr('kcd', [64, T_]), 'vcd': scr('vcd', [T_, 64]),
           'qi': scr('qi', [128, T_]), 'ki': scr('ki', [32, T_]), 'wi': scr('wi', [T_, 4], F32),
           'qd': scr('qd', [512, T_]), 'kd': scr('kd', [512, T_]), 'vdd': scr('vdd', [T_, 512]),
           'o_tok': scr('o_tok1', [T_, 1024])}
    S = Sched(nc)
    w16 = {'wg': scr('wg16', [128, DFF // 256, 8, 256]), 'wu': scr('wu16', [128, DFF // 256, 8, 256]),
           'wd': scr('wd16', [128, NFC, D])}

    def ffn(xi, xo, f, L, fg=None):
        ffn_stage(S, xi, xo, P[f'{f}_norm{L}'], P[f'{f}_wg{L}'], P[f'{f}_wu{L}'], P[f'{f}_wd{L}'], TPC, cp_d, fg, w16)
    cp_d = {'ident': cp[:, CP_OFF['ident'][0]:CP_OFF['ident'][0] + 128]}
    ffn(x, xa, 'ffn1', 0)
    even_proj(S, xa, ev, dre, TPC, cp)
    nsa_stage(S, ev, dre, 2, cp)
    outproj_stage(S, xa, xb, dre['o_tok'], ev_w_out, TPC, cp)
    ffn(xb, xa, 'ffn2', 0)
    ffn(xa, xb, 'ffn1', 1)
    odd_proj(S, xb, od, dro, TPC, cp)
    (dsa_moba_stage if USE_NEW_ODD else dsa_moba_stage_v1)(S, od, dro, 2, cp)
    outproj_stage(S, xb, xa, dro['o_tok'], od_w_out, TPC, cp)
    ffn(xa, y, 'ffn2', 1, fin_g)
    return nc, S


def kernel(**inp):
    inp = {k: np.asarray(v) for k, v in inp.items()}
    nc, S = build_program()
    cpk = host_consts()
    c = np.ascontiguousarray
    shared = {'cpack': cpk}
    for L in range(2):
        for f in ('ffn1', 'ffn2'):
            shared[f'{f}_norm{L}'] = c(inp[f'{f}_norm'][L])
            shared[f'{f}_wg{L}'] = c(inp[f'{f}_w_gate'][L])
            shared[f'{f}_wu{L}'] = c(inp[f'{f}_w_up'][L])
            shared[f'{f}_wd{L}'] = c(inp[f'{f}_w_down'][L])
        shared[f'mix_norm{L}'] = c(inp['mix_norm'][L])
    shared.update({
        'ev_w_in': c(inp['ev_w_in'][0]), 'ev_sgu_norm': c(inp['ev_sgu_norm'][0]),
        'ev_sgu_wT': c(inp['ev_sgu_w'][0].transpose(0, 2, 1)), 'ev_sgu_bT': c(inp['ev_sgu_b'][0].T),
        'ev_w1k': c(inp['ev_cmp_w1_k'][0]), 'ev_w2k': c(inp['ev_cmp_w2_k'][0]), 'ev_pk': c(inp['ev_cmp_pos_k'][0].T),
        'ev_w1v': c(inp['ev_cmp_w1_v'][0]), 'ev_w2v': c(inp['ev_cmp_w2_v'][0]), 'ev_pv': c(inp['ev_cmp_pos_v'][0].T),
        'ev_w_out': c(inp['ev_w_out'][0]), 'od_w_in': c(inp['od_w_in'][0]), 'od_w_out': c(inp['od_w_out'][0]),
        'final_norm': c(inp['final_norm'])})
    in_maps = []
    for k in range(NCORES):
        m = dict(shared)
        m['x'] = c(inp['x'][2 * k:2 * k + 2].reshape(TPC, D))
        m['pos'] = c(inp['positions'][2 * k:2 * k + 2].reshape(TPC).astype(np.int32))
        in_maps.append(m)
    res = run_bass_kernel_spmd(nc, in_maps, core_ids=list(range(NCORES)))
    out = np.stack([np.asarray(r['y']).reshape(2, SEQ, D) for r in res.results], axis=0)
    return out.reshape(16, SEQ, D).astype(np.float32)
```

```python
from contextlib import ExitStack
import numpy as np
import concourse.bass as bass
import concourse.mybir as mybir
from concourse.bass_utils import run_bass_kernel_spmd

F32 = mybir.dt.float32
BF16 = mybir.dt.bfloat16
I32 = mybir.dt.int32
AF = mybir.ActivationFunctionType
ALU = mybir.AluOpType
AX = mybir.AxisListType

ENGS = ['pe', 'act', 'dve', 'pool', 'sp']
import os
BANKDEP = False
USE_NEW_ODD = True
NDSEM = 66
NSWSEM = 26


class T:
    _n = 0

    def __init__(self, t, name=None):
        self.t = t
        T._n += 1
        self.id = T._n
        self.name = name

    def __getitem__(self, idx):
        return self.t[idx]


class Op:
    __slots__ = ('eng', 'fn', 'deps', 'isdma', 'sem', 'cnt', 'signal', 'waits', 'vc')


class Sched:
    def __init__(self, nc):
        self.nc = nc
        self.esem = {e: nc.alloc_semaphore(name=f'es_{e}') for e in ENGS}
        self.ecnt = {e: 0 for e in ENGS}
        self.free_dsems = {False: [nc.alloc_semaphore(name=f'ds_{i}') for i in range(NDSEM)],
                           True: [nc.alloc_semaphore(name=f'dw_{i}') for i in range(NSWSEM)]}
        self.dcnt = {}
        self.n_inst = 0
        self.base = {}
        self._reset()

    def _reset(self):
        self.ops = []
        self.state = {}
        self.buf_dsem = {}
        self.stack = ExitStack()

    def sb(self, shape, dtype, name=None):
        t = self.stack.enter_context(self.nc.sbuf_tensor(f'{name or "sb"}_{T._n}', list(shape), dtype))
        return T(t, name)

    def ps(self, shape, dtype, name=None):
        t = self.stack.enter_context(self.nc.psum_tensor(f'{name or "ps"}_{T._n}', list(shape), dtype))
        return T(t, name)

    @staticmethod
    def _norm(item):
        if isinstance(item, T):
            return item.id, None
        return item[0].id, item[1]

    def _track(self, r, w, opi):
        deps = {}
        for item in r:
            tid, key = self._norm(item)
            st = self.state.setdefault(tid, {})
            for k, ent in st.items():
                if k == key or k is None or key is None:
                    if ent[0] is not None:
                        deps[ent[0]] = True
            st.setdefault(key, [None, []])[1].append(opi)
        for item in w:
            tid, key = self._norm(item)
            st = self.state.setdefault(tid, {})
            for k, ent in st.items():
                if k == key or k is None or key is None:
                    if ent[0] is not None:
                        deps[ent[0]] = True
                    for x in ent[1]:
                        deps.setdefault(x, False)
            if key is None:
                st.clear()
            st[key] = [opi, []]
        deps.pop(opi, None)
        return deps

    @staticmethod
    def _skip(p, o, raw):
        if p.isdma or o.isdma or p.eng != o.eng:
            return False
        return p.eng == 'pe' or not raw

    def op(self, eng, fn, r=(), w=()):
        o = Op()
        o.eng, o.fn, o.isdma, o.signal = eng, fn, False, False
        o.sem, o.cnt, o.waits, o.vc = None, 0, None, None
        o.deps = self._track(r, w, len(self.ops))
        self.ops.append(o)
        return o

    def dma(self, eng, out_ap, in_ap, r, w, **kw):
        o = self.op(eng, lambda e: e.dma_start(out=out_ap, in_=in_ap, **kw), r, w)
        o.isdma = True
        it = w[0] if isinstance(w[0], T) else w[0][0]
        if it.t is None and len(r) > 0:
            it = r[0] if isinstance(r[0], T) else r[0][0]
        tid = (it.id, eng == 'pool')
        if tid not in self.buf_dsem:
            self.buf_dsem[tid] = self.free_dsems[eng == 'pool'].pop()
            if not hasattr(self, 'sem_names'):
                self.sem_names = {}
            self.sem_names[self.buf_dsem[tid]] = it.name
        o.sem = self.buf_dsem[tid]
        self.dcnt[o.sem] = self.dcnt.get(o.sem, 0) + 16
        o.cnt = self.dcnt[o.sem]
        return o

    def pe(self, fn, r=(), w=()):
        return self.op('pe', fn, r, w)

    def act(self, fn, r=(), w=()):
        return self.op('act', fn, r, w)

    def dve(self, fn, r=(), w=()):
        return self.op('dve', fn, r, w)

    def pool(self, fn, r=(), w=()):
        return self.op('pool', fn, r, w)

    def flush(self):
        nc, ops = self.nc, self.ops
        for o in ops:
            for d, raw in o.deps.items():
                p = ops[d]
                if p.isdma or self._skip(p, o, raw):
                    continue
                p.signal = True
        for o in ops:
            if not o.isdma and o.signal:
                self.ecnt[o.eng] += 1
                o.cnt = self.ecnt[o.eng]
                o.sem = self.esem[o.eng]
        known = {e: dict(self.base) for e in ENGS}
        for o in ops:
            kn = known[o.eng]
            waits = {}
            for d in sorted(o.deps, reverse=True):
                p = ops[d]
                if self._skip(p, o, o.deps[d]):
                    continue
                if kn.get(p.sem, 0) >= p.cnt:
                    continue
                if waits.get(p.sem, 0) < p.cnt:
                    waits[p.sem] = p.cnt
                for s, c in p.vc.items():
                    if kn.get(s, 0) < c:
                        kn[s] = c
                kn[p.sem] = p.cnt
            o.waits = list(waits.items())
            if o.isdma and o.cnt > 16 and kn.get(o.sem, 0) < o.cnt - 16 and getattr(self, 'diag', False):
                print('DMA overlap on sem', self.sem_names.get(o.sem), 'cnt', o.cnt, 'known', kn.get(o.sem, 0))
            if o.isdma or o.signal:
                o.vc = dict(kn)
        by = {e: [o for o in ops if o.eng == e] for e in ENGS}
        final_d = [(s, self.dcnt[s]) for s in set(self.buf_dsem.values())]
        self.n_inst += len(ops)

        def emit(e, lst):
            for o in lst:
                for s, c in o.waits:
                    e.wait_ge(s, c)
                ins = o.fn(e)
                if o.isdma:
                    ins.then_inc(o.sem, 16)
                elif o.signal:
                    ins.then_inc(o.sem, 1)

        with nc.Block() as block:
            @block.tensor
            def _(e):
                emit(e, by['pe'])

            @block.scalar
            def _(e):
                emit(e, by['act'])

            @block.vector
            def _(e):
                emit(e, by['dve'])

            @block.gpsimd
            def _(e):
                emit(e, by['pool'])

            @block.sync
            def _(e):
                emit(e, by['sp'])
                for s, c in final_d:
                    e.wait_ge(s, c)
        for (tid_, sw), s in self.buf_dsem.items():
            self.free_dsems[sw].append(s)
        self.base = dict(self.dcnt)
        for e in ENGS:
            self.base[self.esem[e]] = self.ecnt[e]
        self.stack.close()
        self._reset()


D = 1024
DFF = 2816
NFC = DFF // 128
SEQ = 2048
EPS = 1e-6


def load_consts(S, cpack):
    c = {}
    idf = S.sb([128, 128], F32, 'idf')
    S.dma('sp', idf[:], cpack['ident'], r=[], w=[idf])
    idb = S.sb([128, 128], BF16, 'idb')
    S.dve(lambda e: e.tensor_copy(out=idb[:], in_=idf[:]), r=[idf], w=[idb])
    c['ident'] = idb
    return c


def rms_rstd(S, xt, junk, ss, rstd, width, key=None):
    S.act(lambda e: e.activation(out=junk[:], in_=xt[:], func=AF.Square, accum_out=ss[:]),
          r=[xt], w=[junk, ss])
    S.act(lambda e: e.activation(out=rstd[:], in_=ss[:], func=AF.Sqrt, bias=EPS, scale=1.0 / width),
          r=[ss], w=[rstd])
    S.dve(lambda e: e.reciprocal(out=rstd[:], in_=rstd[:]), r=[rstd], w=[rstd])


def ffn_stage(S, x_in, x_out, g_ap, wg, wu, wd, ntok, cpack, final_g=None, w16=None):
    CH = 1024
    NT = CH // 128
    consts = load_consts(S, cpack)
    ident = consts['ident']
    gb = S.sb([128, D], F32, 'gb')
    S.dma('sp', gb[:], g_ap.partition_broadcast(128), r=[], w=[gb])
    if final_g is not None:
        fgb = S.sb([128, D], F32, 'fgb')
        S.dma('sp', fgb[:], final_g.partition_broadcast(128), r=[], w=[fgb])
    hT = S.sb([128, 8, CH], BF16, 'hT')
    actT = S.sb([128, NFC, CH], BF16, 'actT')
    wdb = S.sb([128, NFC, D], BF16, 'wdb')
    xt = [S.sb([128, D], F32, f'xt{i}') for i in range(2)]
    hb = [S.sb([128, D], BF16, f'hb{i}') for i in range(2)]
    junk = S.sb([128, D], BF16, 'junk')
    ss = [S.sb([128, 1], F32, f'ss{i}') for i in range(2)]
    rstd = [S.sb([128, 1], F32, f'rstd{i}') for i in range(2)]
    FB = 256
    NB = DFF // FB
    wgs = [S.sb([128, 8, FB], F32, f'wgs{i}') for i in range(2)]
    wus = [S.sb([128, 8, FB], F32, f'wus{i}') for i in range(2)]
    wgb = [S.sb([128, 8, FB], BF16, f'wgb{i}') for i in range(2)]
    wub = [S.sb([128, 8, FB], BF16, f'wub{i}') for i in range(2)]
    wds = [S.sb([128, D], F32, f'wds{i}') for i in range(2)]
    sg = [S.sb([128, 512], F32, f'sg{i}') for i in range(2)]
    ot = [S.sb([128, D], F32, f'ot{i}') for i in range(2)]
    ptr = [S.ps([128, 8, 128], BF16, f'ptr{i}') for i in range(2)]
    pg = [S.ps([128, 512], F32, f'pg{i}') for i in range(2)]
    pu = [S.ps([128, 512], F32, f'pu{i}') for i in range(2)]
    py = [S.ps([128, 512], F32, f'py{i}') for i in range(2)]
    wg_v = wg.rearrange("(c p) f -> p c f", p=128)
    wu_v = wu.rearrange("(c p) f -> p c f", p=128)
    wd_v = wd.rearrange("(c p) m -> p c m", p=128)

    xt3 = [S.sb([128, D], F32, f'xt3{i}') for i in range(2)]

    def phase1_tile(ch, i):
        t0 = ch * CH
        b = i % 2
        x_t, h_b = xt[b], hb[b]
        S.dma('sp', x_t[:], x_in[t0 + i * 128:t0 + (i + 1) * 128, :], r=[], w=[x_t])
        rms_rstd(S, x_t, junk, ss[b], rstd[b], D)
        S.dve(lambda e: e.scalar_tensor_tensor(
            out=h_b[:], in0=x_t[:], scalar=rstd[b][:, 0:1], in1=gb[:], op0=ALU.mult, op1=ALU.mult),
            r=[x_t, rstd[b], gb], w=[h_b])
        p = ptr[b]
        for c in range(8):
            S.pe(lambda e, c=c: e.transpose(out=p[:, c, :], in_=h_b[:, c * 128:(c + 1) * 128], identity=ident[:]),
                 r=[h_b, ident], w=[(p, c)])
        S.act(lambda e: e.copy(out=hT[:, :, i * 128:(i + 1) * 128], in_=p[:]), r=[p], w=[(hT, i // 4)])

    wdT = T(None, 'wd16')

    def load_wd(ch, fc):
        if ch == 0 or w16 is None:
            s_ = wds[fc % 2]
            S.dma('sp', s_[:], wd_v[:, fc, :], r=[], w=[s_])
            S.act(lambda e: e.copy(out=wdb[:, fc, :], in_=s_[:]), r=[s_], w=[(wdb, fc)])
            if w16 is not None:
                S.dma('pool', w16['wd'][:, fc, :], wdb[:, fc, :], r=[(wdb, fc)], w=[(wdT, fc)])
        else:
            S.dma('sp', wdb[:, fc, :], w16['wd'][:, fc, :], r=[(wdT, fc)], w=[(wdb, fc)])

    wgT = T(None, 'wg16')

    def phase2(ch):
        for fb in range(NB):
            b = fb % 2
            if ch == 0 or w16 is None:
                S.dma('sp', wgs[b][:], wg_v[:, :, fb * FB:(fb + 1) * FB], r=[], w=[wgs[b]])
                S.dma('sp', wus[b][:], wu_v[:, :, fb * FB:(fb + 1) * FB], r=[], w=[wus[b]])
                S.dve(lambda e, b=b: e.tensor_copy(out=wgb[b][:], in_=wgs[b][:]), r=[wgs[b]], w=[wgb[b]])
                S.dve(lambda e, b=b: e.tensor_copy(out=wub[b][:], in_=wus[b][:]), r=[wus[b]], w=[wub[b]])
                if w16 is not None:
                    S.dma('pool', w16['wg'][:, fb, :, :], wgb[b][:], r=[wgb[b]], w=[(wgT, ('g', fb))])
                    S.dma('pool', w16['wu'][:, fb, :, :], wub[b][:], r=[wub[b]], w=[(wgT, ('u', fb))])
            else:
                S.dma('sp', wgb[b][:], w16['wg'][:, fb, :, :], r=[(wgT, ('g', fb))], w=[wgb[b]])
                S.dma('sp', wub[b][:], w16['wu'][:, fb, :, :], r=[(wgT, ('u', fb))], w=[wub[b]])
            load_wd(ch, 2 * fb)
            load_wd(ch, 2 * fb + 1)
            for fs in range(FB // 128):
                fc = fb * (FB // 128) + fs
                for tb in range(CH // 512):
                    q = (fc * 2 + tb) % 2
                    for c in range(8):
                        S.pe(lambda e, q=q, b=b, c=c, fs=fs, tb=tb: e.matmul(
                            pg[q][:], lhsT=wgb[b][:, c, fs * 128:(fs + 1) * 128],
                            rhs=hT[:, c, tb * 512:(tb + 1) * 512], start=(c == 0), stop=(c == 7)),
                            r=[wgb[b], (hT, tb)], w=[pg[q]])
                    for c in range(8):
                        S.pe(lambda e, q=q, b=b, c=c, fs=fs, tb=tb: e.matmul(
                            pu[q][:], lhsT=wub[b][:, c, fs * 128:(fs + 1) * 128],
                            rhs=hT[:, c, tb * 512:(tb + 1) * 512], start=(c == 0), stop=(c == 7)),
                            r=[wub[b], (hT, tb)], w=[pu[q]])
                    S.act(lambda e, q=q: e.activation(out=sg[q][:], in_=pg[q][:], func=AF.Silu),
                          r=[pg[q]], w=[sg[q]])
                    S.dve(lambda e, q=q, fc=fc, tb=tb: e.tensor_tensor(
                        out=actT[:, fc, tb * 512:(tb + 1) * 512], in0=pu[q][:], in1=sg[q][:], op=ALU.mult),
                        r=[pu[q], sg[q]], w=[(actT, (fc, tb))])

    def phase3_tile(ch, i):
        t0 = ch * CH
        b = i % 2
        x_t, o_t = xt3[b], ot[b]
        S.dma('sp', x_t[:], x_in[t0 + i * 128:t0 + (i + 1) * 128, :], r=[], w=[x_t])
        for mh in range(2):
            p = py[mh]
            for fc in range(NFC):
                S.pe(lambda e, p=p, fc=fc, mh=mh: e.matmul(
                    p[:], lhsT=actT[:, fc, i * 128:(i + 1) * 128], rhs=wdb[:, fc, mh * 512:(mh + 1) * 512],
                    start=(fc == 0), stop=(fc == NFC - 1)),
                    r=[(actT, (fc, i // 4)), (wdb, fc)], w=[p])
            S.dve(lambda e, p=p, mh=mh: e.scalar_tensor_tensor(
                out=o_t[:, mh * 512:(mh + 1) * 512], in0=p[:], scalar=0.5, in1=x_t[:, mh * 512:(mh + 1) * 512],
                op0=ALU.mult, op1=ALU.add), r=[p, x_t], w=[(o_t, mh)])
        if final_g is not None:
            rms_rstd(S, o_t, junk, ss3[b], rstd3[b], D)
            S.dve(lambda e: e.scalar_tensor_tensor(
                out=o_t[:], in0=o_t[:], scalar=rstd3[b][:, 0:1], in1=fgb[:], op0=ALU.mult, op1=ALU.mult),
                r=[o_t, rstd3[b], fgb], w=[o_t])
        S.dma('pool', x_out[t0 + i * 128:t0 + (i + 1) * 128, :], o_t[:], r=[o_t], w=[DW(S, x_out)])

    ss3 = [S.sb([128, 1], F32, f'ss3{i}') for i in range(2)]
    rstd3 = [S.sb([128, 1], F32, f'rstd3{i}') for i in range(2)]
    nch = ntok // CH
    for i in range(NT):
        phase1_tile(0, i)
    for ch in range(nch):
        phase2(ch)
        for i in range(NT):
            phase3_tile(ch, i)
            if ch + 1 < nch:
                phase1_tile(ch + 1, i)
    S.flush()


_dram_T = {}
_dkey = [0]


def x_out_T(S, ap):
    k = ap.name
    if k not in _dram_T:
        _dram_T[k] = T(None, k)
    return _dram_T[k]


def DW(S, ap):
    _dkey[0] += 1
    return (x_out_T(S, ap), _dkey[0])


THETA = 500000.0
NEG = -30000.0


def _cpack_layout():
    items = [('ident', 128), ('tri_ge', 128), ('band_lt', 128), ('tril_st', 128), ('ones', 128),
             ('invf64', 1), ('nsgn64', 1), ('P64', 128), ('invf32', 1), ('nsgn32', 1), ('P32', 128),
             ('cmpbias', 2048), ('overlap', 32), ('E32', 2048), ('E8', 2048),
             ('keep', 512), ('addm', 512), ('adm', 512), ('tri_qs', 128), ('tri01', 128), ('band01', 128), ('mbias', 128), ('mpast', 128), ('mown', 128)]
    off, o = {}, 0
    for k, n in items:
        off[k] = (o, n)
        o += n
    return off, o


CP_OFF, CP_N = _cpack_layout()


def host_consts():
    cp = np.zeros((128, CP_N), np.float32)

    def put(k, a):
        o, n = CP_OFF[k]
        a = np.asarray(a, np.float32)
        cp[:a.shape[0], o:o + a.shape[1]] = a
    p = np.arange(128)
    put('ident', np.eye(128))
    kk, qq = p[:, None], p[None, :]
    put('tri_ge', np.where(qq >= kk, 0.0, NEG))
    put('band_lt', np.where(qq < kk, 0.0, NEG))
    put('tri01', (qq >= kk).astype(np.float32))
    put('band01', (qq < kk).astype(np.float32))
    put('tril_st', (kk <= qq).astype(np.float32))
    put('tri_qs', np.where(qq <= kk, 0.0, -1e30))
    put('ones', np.ones((128, 128)))
    m64 = p % 64
    put('invf64', np.where(m64 < 16, THETA ** (-(2.0 * (m64 % 8)) / 16.0), 0.0)[:, None])
    put('nsgn64', np.where(m64 < 8, -1.0, np.where(m64 < 16, 1.0, 0.0))[:, None])
    P = np.zeros((128, 128))
    for m in range(128):
        if m % 64 < 8:
            P[m + 8, m] = 1
        elif m % 64 < 16:
            P[m - 8, m] = 1
    put('P64', P)
    m32 = p % 32
    put('invf32', np.where(m32 < 8, THETA ** (-(2.0 * (m32 % 4)) / 8.0), 0.0)[:, None])
    put('nsgn32', np.where(m32 < 4, -1.0, np.where(m32 < 8, 1.0, 0.0))[:, None])
    P = np.zeros((128, 128))
    for m in range(128):
        if m % 32 < 4:
            P[m + 4, m] = 1
        elif m % 32 < 8:
            P[m - 4, m] = 1
    put('P32', P)
    n = np.arange(127)
    t = np.arange(2048)
    put('cmpbias', np.where(16 * n[:, None] + 31 <= t[None, :], 0.0, NEG))
    c0 = n * 16
    s0 = np.arange(32) * 64
    put('overlap', ((c0[:, None] < s0[None, :] + 64) & (c0[:, None] + 32 > s0[None, :])).astype(np.float32))
    put('E32', (t[None, :] // 64 == np.arange(32)[:, None]).astype(np.float32))
    put('E8', (t[None, :] // 256 == np.arange(8)[:, None]).astype(np.float32))
    tt = (np.arange(16)[None, :, None] * 128 + p[:, None, None])
    j = np.arange(32)[None, None, :]
    adm = j * 64 <= tt
    forced = (j == 0) | (j == tt // 64)
    put('keep', (adm & ~forced).astype(np.float32).reshape(128, 512))
    put('addm', np.where(adm, np.where(forced, 1e4, 0.0), -1e30).reshape(128, 512))
    put('adm', adm.astype(np.float32).reshape(128, 512))
    own = (np.arange(16)[None, :, None] * 128 + p[:, None, None]) // 256
    j8 = np.arange(8)[None, None, :]
    put('mbias', np.where(j8 < own, 0.0, -1e30).reshape(128, 128))
    put('mpast', (j8 < own).astype(np.float32).reshape(128, 128))
    put('mown', (j8 == own).astype(np.float32).reshape(128, 128))
    return cp


class Consts:
    def __init__(self, S, cpack_ap):
        self.S, self.ap, self.cache = S, cpack_ap, {}

    def get(self, k, dtype=F32, rows=128):
        key = (k, dtype)
        if key in self.cache:
            return self.cache[key]
        S = self.S
        o, n = CP_OFF[k]
        if dtype == F32:
            f = S.sb([128, n], F32, 'c_' + k)
            S.dma('sp', f[:], self.ap[:, o:o + n], r=[], w=[f], allow_slow_non_contiguous=(n == 1))
            self.cache[key] = f
            return f
        if not hasattr(self, 'stg'):
            self.stg = S.sb([128, 2048], F32, 'c_stg')
        f = self.stg
        S.dma('sp', f[:, 0:n], self.ap[:, o:o + n], r=[], w=[f])
        b = S.sb([128, n], dtype, 'cb_' + k)
        S.dve(lambda e: e.tensor_copy(out=b[:], in_=f[:, 0:n]), r=[f], w=[b])
        self.cache[key] = b
        return b


def rope_tables(S, C, pos_ap, ntok, invk, sgnk, tmp):
    invf = C.get(invk)
    nsg = C.get(sgnk)
    if 'pi' not in tmp:
        tmp['pi'] = S.sb([128, 1024], I32, 'pos_i')
        tmp['ang'] = S.sb([128, 1024], F32, 'ang')
        tmp['kf'] = S.sb([128, 1024], F32, 'kf')
        tmp['ki'] = S.sb([128, 1024], I32, 'ki')
    pi_, ang, kf, ki = tmp['pi'], tmp['ang'], tmp['kf'], tmp['ki']
    ct = S.sb([128, ntok], F32, 'ropeC')
    st = S.sb([128, ntok], F32, 'ropeS')
    TWO_PI = 2.0 * np.pi
    for c0 in range(0, ntok, 1024):
        S.dma('sp', pi_[:], pos_ap[c0:c0 + 1024].partition_broadcast(128), r=[], w=[pi_])
        S.dve(lambda e: e.tensor_copy(out=ang[:], in_=pi_[:]), r=[pi_], w=[ang])
        S.dve(lambda e: e.tensor_scalar(out=ang[:], in0=ang[:], scalar1=invf[:, 0:1], scalar2=None, op0=ALU.mult),
              r=[ang, invf], w=[ang])

        def reduce_sin(dst, shift, post, c0=c0):
            S.dve(lambda e: e.tensor_scalar(out=kf[:], in0=ang[:], scalar1=shift, scalar2=1.0 / TWO_PI,
                                            op0=ALU.add, op1=ALU.mult), r=[ang], w=[kf])
            S.dve(lambda e: e.tensor_copy(out=ki[:], in_=kf[:]), r=[kf], w=[ki])
            S.dve(lambda e: e.tensor_copy(out=kf[:], in_=ki[:]), r=[ki], w=[kf])
            S.dve(lambda e: e.scalar_tensor_tensor(out=kf[:], in0=kf[:], scalar=-TWO_PI, in1=ang[:],
                                                   op0=ALU.mult, op1=ALU.add), r=[kf, ang], w=[kf])
            S.dve(lambda e: e.tensor_scalar(out=kf[:], in0=kf[:], scalar1=shift, scalar2=3.14159, op0=ALU.add,
                                            op1=ALU.min), r=[kf], w=[kf])
            S.dve(lambda e: e.tensor_scalar(out=kf[:], in0=kf[:], scalar1=-3.14159, scalar2=None, op0=ALU.max),
                  r=[kf], w=[kf])
            S.act(lambda e: e.activation(out=dst[:, c0:c0 + 1024], in_=kf[:], func=AF.Sin), r=[kf], w=[(dst, c0)])
            if post is not None:
                S.dve(lambda e: e.tensor_scalar(out=dst[:, c0:c0 + 1024], in0=dst[:, c0:c0 + 1024],
                                                scalar1=post[:, 0:1], scalar2=None, op0=ALU.mult),
                      r=[(dst, c0), post], w=[(dst, c0)])
        reduce_sin(ct, np.pi / 2.0, None)
        reduce_sin(st, 0.0, nsg)
    return ct, st


def proj_stage(S, x, w_in, nin, g_ap, pos, ntok, cpack, fm_specs, tm_groups, tm_post, use_idx=False):
    C = Consts(S, cpack)
    ident = C.get('ident', BF16)
    gb = S.sb([128, D], F32, 'gb')
    S.dma('sp', gb[:], g_ap.partition_broadcast(128), r=[], w=[gb])
    winb = S.sb([128, 8, nin], BF16, 'winb')
    wv = w_in.rearrange("(c p) f -> p c f", p=128)
    wst = [S.sb([128, 8, 256], F32, f'wst{i}') for i in range(2)]
    for bi, c0 in enumerate(range(0, nin, 256)):
        n = min(256, nin - c0)
        st = wst[bi % 2]
        S.dma('sp', st[:, :, 0:n], wv[:, :, c0:c0 + n], r=[], w=[st])
        S.dve(lambda e, st=st, c0=c0, n=n: e.tensor_copy(out=winb[:, :, c0:c0 + n], in_=st[:, :, 0:n]),
              r=[st], w=[(winb, bi)])
    ropes = {}
    rtmp = {}
    if any(sp[3] == 'r64' for sp in fm_specs):
        ct, sn = rope_tables(S, C, pos, ntok, 'invf64', 'nsgn64', rtmp)
        ropes['r64'] = (ct, sn, C.get('P64', BF16))
    if use_idx:
        ct, sn = rope_tables(S, C, pos, ntok, 'invf32', 'nsgn32', rtmp)
        ropes['r32'] = (ct, sn, C.get('P32', BF16))
    xt = [S.sb([128, D], F32, f'xt{i}') for i in range(2)]
    hb = [S.sb([128, D], BF16, f'hb{i}') for i in range(2)]
    junk = S.sb([128, D], F32, 'junk')
    ss = [S.sb([128, 1], F32, f'ss{i}') for i in range(2)]
    rstd = [S.sb([128, 1], F32, f'rstd{i}') for i in range(2)]
    hT = [S.sb([128, 8, 512], BF16, f'hT{i}') for i in range(2)]
    xh = [S.sb([128, 512], BF16, f'xh{i}') for i in range(2)]
    t1 = [S.sb([128, 512], F32, f't1{i}') for i in range(2)]
    t2 = [S.sb([128, 512], F32, f't2{i}') for i in range(2)]
    xr = [S.sb([128, 512], BF16, f'xr{i}') for i in range(2)]
    ptr = [S.ps([128, 8, 128], BF16, f'ptr{i}') for i in range(2)]
    pf = [S.ps([128, 512], F32, f'pf{i}') for i in range(2)]
    p2 = [S.ps([128, 512], F32, f'p2{i}') for i in range(2)]
    pt = [S.ps([128, 512], F32, f'pt{i}') for i in range(2)]
    nfm = 0
    ntm = 0
    for blk in range(ntok // 512):
        t0 = blk * 512
        hTb = hT[blk % 2]
        for i in range(4):
            b = i % 2
            x_t, h_b = xt[b], hb[b]
            S.dma('sp', x_t[:], x[t0 + i * 128:t0 + (i + 1) * 128, :], r=[], w=[x_t])
            rms_rstd(S, x_t, junk, ss[b], rstd[b], D)
            S.dve(lambda e, x_t=x_t, h_b=h_b, b=b: e.scalar_tensor_tensor(
                out=h_b[:], in0=x_t[:], scalar=rstd[b][:, 0:1], in1=gb[:], op0=ALU.mult, op1=ALU.mult),
                r=[x_t, rstd[b], gb], w=[h_b])
            p = ptr[b]
            for c in range(8):
                S.pe(lambda e, p=p, h_b=h_b, c=c: e.transpose(out=p[:, c, :], in_=h_b[:, c * 128:(c + 1) * 128],
                                                               identity=ident[:]), r=[h_b, ident], w=[(p, c)])
            S.act(lambda e, p=p, i=i, hTb=hTb: e.copy(out=hTb[:, :, i * 128:(i + 1) * 128], in_=p[:]),
                  r=[p], w=[(hTb, i)])
        for (col0, M, scale, rope, raw_dst, rot_dst) in fm_specs:
            q = nfm % 2
            nfm += 1
            pp = pf[q]
            for c in range(8):
                S.pe(lambda e, pp=pp, c=c, col0=col0, M=M, hTb=hTb: e.matmul(
                    pp[0:M, :], lhsT=winb[:, c, col0:col0 + M], rhs=hTb[:, c, :], start=(c == 0), stop=(c == 7)),
                    r=[winb, hTb], w=[pp])
            xq = xh[q]
            S.act(lambda e, xq=xq, pp=pp, M=M, scale=scale: e.mul(out=xq[0:M, :], in_=pp[0:M, :], mul=scale),
                  r=[pp], w=[xq])
            if raw_dst is not None:
                S.dma('pool', raw_dst[:, t0:t0 + 512], xq[0:M, :], r=[xq], w=[DW(S, raw_dst)])
            if rope is not None:
                ct, sn, Pm = ropes[rope]
                pq = p2[q]
                S.pe(lambda e, pq=pq, xq=xq, M=M, Pm=Pm: e.matmul(pq[0:M, :], lhsT=Pm[0:M, 0:M], rhs=xq[0:M, :],
                                                                  start=True, stop=True), r=[xq, Pm], w=[pq])
                S.dve(lambda e, q=q, xq=xq, M=M, ct=ct, t0=t0: e.tensor_tensor(
                    out=t1[q][0:M, :], in0=xq[0:M, :], in1=ct[0:M, t0:t0 + 512], op=ALU.mult),
                    r=[xq, ct], w=[t1[q]])
                S.dve(lambda e, q=q, pq=pq, M=M, sn=sn, t0=t0: e.tensor_tensor(
                    out=t2[q][0:M, :], in0=pq[0:M, :], in1=sn[0:M, t0:t0 + 512], op=ALU.mult),
                    r=[pq, sn], w=[t2[q]])
                S.pool(lambda e, q=q, M=M: e.tensor_tensor(out=xr[q][0:M, :], in0=t1[q][0:M, :], in1=t2[q][0:M, :],
                                                          op=ALU.add), r=[t1[q], t2[q]], w=[xr[q]])
                S.dma('pool', rot_dst[:, t0:t0 + 512], xr[q][0:M, :], r=[xr[q]], w=[DW(S, rot_dst)])
        for i in range(4):
            for gi, (col0, N) in enumerate(tm_groups):
                q = ntm % 2
                ntm += 1
                pp = pt[q]
                for c in range(8):
                    S.pe(lambda e, pp=pp, c=c, col0=col0, N=N, i=i, hTb=hTb: e.matmul(
                        pp[:, 0:N], lhsT=hTb[:, c, i * 128:(i + 1) * 128], rhs=winb[:, c, col0:col0 + N],
                        start=(c == 0), stop=(c == 7)), r=[winb, (hTb, i)], w=[pp])
                tm_post(gi, pp, t0 + i * 128, ntm)
    S.flush()


def even_proj(S, x, prm, dr, ntok, cpack):
    at = [S.sb([128, 512], BF16, f'at{i}') for i in range(2)]
    g1 = [S.sb([128, 512], F32, f'g1{i}') for i in range(2)]
    vb = [S.sb([128, 192], BF16, f'vb{i}') for i in range(2)]
    vb2 = [S.sb([128, 192], BF16, f'vb2{i}') for i in range(2)]
    gt = [S.sb([128, 36], F32, f'gt{i}') for i in range(2)]
    sgb = S.sb([128, 256], F32, 'sgb')
    S.dma('sp', sgb[:], prm['sgu_norm'].partition_broadcast(128), r=[], w=[sgb])
    junk = S.sb([128, 256], F32, 'junk2')
    ss = [S.sb([128, 1], F32, f'ssv{i}') for i in range(2)]
    rs = [S.sb([128, 1], F32, f'rsv{i}') for i in range(2)]
    cnt = [0]

    def tm_post(gi, pp, tok0, n):
        if gi == 0:
            b = cnt[0] % 2
            cnt[0] += 1
            g, a = g1[b], at[b]
            S.act(lambda e: e.activation(out=g[:], in_=pp[:], func=AF.Gelu_apprx_tanh), r=[pp], w=[g])
            S.pool(lambda e: e.tensor_copy(out=a[:, 0:256], in_=g[:, 0:256]), r=[g], w=[(a, 0)])
            S.act(lambda e: e.activation(out=junk[:], in_=g[:, 256:512], func=AF.Square, accum_out=ss[b][:]),
                  r=[g], w=[junk, ss[b]])
            S.act(lambda e: e.activation(out=rs[b][:], in_=ss[b][:], func=AF.Sqrt, bias=EPS, scale=1.0 / 256),
                  r=[ss[b]], w=[rs[b]])
            S.dve(lambda e: e.reciprocal(out=rs[b][:], in_=rs[b][:]), r=[rs[b]], w=[rs[b]])
            S.dve(lambda e: e.scalar_tensor_tensor(out=a[:, 256:512], in0=g[:, 256:512], scalar=rs[b][:, 0:1],
                                                   in1=sgb[:], op0=ALU.mult, op1=ALU.mult),
                  r=[g, rs[b], sgb], w=[(a, 1)])
            S.dma('pool', dr['a_tok'][tok0:tok0 + 128, :], a[:], r=[a], w=[DW(S, dr['a_tok'])])
        elif gi == 1:
            v = vb[(tok0 // 128) % 2]
            S.act(lambda e: e.copy(out=v[:], in_=pp[:, 0:192]), r=[pp], w=[v])
            S.dma('pool', dr['vsl'][tok0:tok0 + 128, :], v[:], r=[v], w=[DW(S, dr['vsl'])])
        else:
            v = vb2[(tok0 // 128) % 2]
            g = gt[(tok0 // 128) % 2]
            S.act(lambda e: e.copy(out=v[:], in_=pp[:, 0:192]), r=[pp], w=[v])
            S.act(lambda e: e.activation(out=g[:], in_=pp[:, 192:228], func=AF.Sigmoid), r=[pp], w=[g])
            S.dma('pool', dr['vw'][tok0:tok0 + 128, :], v[:], r=[v], w=[DW(S, dr['vw'])])
            S.dma('pool', dr['gate'][tok0:tok0 + 128, :], g[:], r=[g], w=[DW(S, dr['gate'])])
    fm = []
    for i in range(6):
        fm.append((512 + 128 * i, 128, 0.125, 'r64', dr['qraw'][128 * i:128 * (i + 1), :],
                   dr['qrot'][128 * i:128 * (i + 1), :]))
    for nm, c0, rope in (('kc', 1280, None), ('vc', 1472, None), ('ksl', 1664, 'r64'), ('kw', 2048, 'r64')):
        for (o, M) in ((0, 128), (128, 64)):
            dst = dr[nm][o:o + M, :]
            fm.append((c0 + o, M, 1.0, rope, dst if rope is None else None, dst if rope else None))
    proj_stage(S, x, prm['w_in'], 2468, prm['mix_norm'], prm['pos'], ntok, cpack, fm,
               [(0, 512), (1856, 192), (2240, 228)], tm_post)


def attn_chunk(S, c, tiles, qT, kT, extra, vext, STps, PT, Oacc, finalize):
    cover = {}
    for n, (kt, lo, hi, bt, blo) in enumerate(tiles):
        for j in range(lo // 128, hi // 128):
            cover.setdefault(j, []).append(n)
    NP = len(PT)

    def qk(n):
        kt, lo, hi, bt, blo = tiles[n]
        ps = STps[n % 2]
        S.pe(lambda e: e.matmul(ps[:, lo:hi], lhsT=kT[:, kt * 128:(kt + 1) * 128],
                                rhs=qT[:, c * 512 + lo:c * 512 + hi], start=True, stop=(extra is None)),
             r=[kT, qT], w=[ps])
        if extra:
            S.pe(lambda e: e.matmul(ps[:, lo:hi], lhsT=extra[0](kt), rhs=extra[1](kt, c, lo, hi),
                                    start=False, stop=True, skip_group_check=True), r=list(extra[2]), w=[ps])

    qk(0)
    for n, (kt, lo, hi, bt, blo) in enumerate(tiles):
        if n + 1 < len(tiles):
            qk(n + 1)
        ps, p = STps[n % 2], PT[n % NP]
        S.act(lambda e, ps=ps, p=p, lo=lo, hi=hi: e.activation(out=p[:, lo:hi], in_=ps[:, lo:hi], func=AF.Exp),
              r=[ps], w=[p])
        if bt is not None:
            S.dve(lambda e, p=p, bt=bt, blo=blo: e.tensor_tensor(out=p[:, blo:blo + 128], in0=p[:, blo:blo + 128],
                                                                 in1=bt[:], op=ALU.mult), r=[p, bt], w=[p])
        for j in range(lo // 128, hi // 128):
            S.pe(lambda e, p=p, j=j, kt=kt, n=n: e.matmul(
                Oacc[j][:, 0:65], lhsT=p[:, j * 128:(j + 1) * 128], rhs=vext[:, kt, :],
                start=(cover[j][0] == n), stop=(cover[j][-1] == n), skip_group_check=True),
                r=[p, vext], w=[Oacc[j]])
            if cover[j][-1] == n:
                finalize(c, j, Oacc[j])


def attn_run(S, jobs, STps, PT, Oacc, LA=2):
    flat = []
    for ji, jb in enumerate(jobs):
        cover = {}
        for n, (kt, lo, hi, bt, blo) in enumerate(jb['tiles']):
            for j in range(lo // 128, hi // 128):
                cover.setdefault(j, []).append(n)
        jb['cover'] = cover
        for n in range(len(jb['tiles'])):
            flat.append((ji, n))
    NS, NP = len(STps), len(PT)

    def qk(f):
        ji, n = flat[f]
        jb = jobs[ji]
        if n == 0 and jb.get('pre') is not None:
            jb['pre']()
        kt, lo, hi, bt, blo = jb['tiles'][n]
        c, qT, kT, extra = jb['c'], jb['qT'], jb['kT'], jb.get('extra')
        ps = STps[f % NS]
        S.pe(lambda e: e.matmul(ps[:, lo:hi], lhsT=kT[:, kt * 128:(kt + 1) * 128],
                                rhs=qT[:, c * 512 + lo:c * 512 + hi], start=True, stop=(extra is None)),
             r=[kT, qT], w=[ps])
        if extra:
            S.pe(lambda e: e.matmul(ps[:, lo:hi], lhsT=extra[0](kt), rhs=extra[1](kt, c, lo, hi),
                                    start=False, stop=True), r=list(extra[2]), w=[ps])

    for f in range(min(LA, len(flat))):
        qk(f)
    for f, (ji, n) in enumerate(flat):
        if f + LA < len(flat):
            qk(f + LA)
        jb = jobs[ji]
        kt, lo, hi, bt, blo = jb['tiles'][n]
        cover, vext = jb['cover'], jb['vext']
        ps, p = STps[f % NS], PT[f % NP]
        S.act(lambda e, ps=ps, p=p, lo=lo, hi=hi: e.activation(out=p[:, lo:hi], in_=ps[:, lo:hi], func=AF.Exp),
              r=[ps], w=[p])
        if bt is not None:
            S.pool(lambda e, p=p, bt=bt, blo=blo: e.tensor_tensor(out=p[:, blo:blo + 128], in0=p[:, blo:blo + 128],
                                                                  in1=bt[:], op=ALU.mult), r=[p, bt], w=[p])
        for j in range(lo // 128, hi // 128):
            O = Oacc[j]
            S.pe(lambda e, p=p, j=j, kt=kt, O=O, vext=vext, first=(cover[j][0] == n), last=(cover[j][-1] == n):
                 e.matmul(O[:, 0:65], lhsT=p[:, j * 128:(j + 1) * 128], rhs=vext[:, kt, :], start=first, stop=last),
                 r=[p, vext], w=[O])
            if cover[j][-1] == n:
                jb['fin'](jb['c'], j, O, 0)


def causal_tiles(c, tri):
    out = []
    for kt in range(4 * c + 4):
        if kt < 4 * c:
            out.append((kt, 0, 512, None, 0))
        else:
            lo = (kt - 4 * c) * 128
            out.append((kt, lo, 512, tri, lo))
    return out


def window_tiles(c, tri, band):
    out = []
    if c >= 1:
        out.append((4 * c - 1, 0, 512, band, 384))
        for i in range(3):
            out.append((4 * c - 4 + i, 0, 128 * (i + 1), band, 128 * i))
    for i in range(4):
        out.append((4 * c + i, 128 * i, 512, tri, 128 * i))
    return out


def nsa_stage(S, prm, dr, nseq, cpack):
    C = Consts(S, cpack)
    ident = C.get('ident', BF16)
    tri = C.get('tri01', BF16)
    band = C.get('band01', BF16)
    ones = C.get('ones', BF16)
    cmpb = C.get('cmpbias', BF16)
    ovl = C.get('overlap', F32)
    E32 = C.get('E32', F32)
    keep, addm, adm = C.get('keep'), C.get('addm'), C.get('adm')
    tril = C.get('tril_st', F32)
    wTf = S.sb([128, 4, 128], F32, 'wTf')
    S.dma('sp', wTf[:], prm['sgu_wT'].rearrange("g s t -> s g t"), r=[], w=[wTf])
    wTb = S.sb([128, 4, 128], BF16, 'wTb')
    for g in range(4):
        S.dve(lambda e, g=g: e.tensor_tensor(out=wTb[:, g, :], in0=wTf[:, g, :], in1=tril[:], op=ALU.mult),
              r=[wTf, tril], w=[(wTb, g)])
    bT = S.sb([128, 4], F32, 'bT')
    S.dma('sp', bT[:], prm['sgu_bT'], r=[], w=[bT])
    cw = {}
    stg = [S.sb([64, 4, 256], F32, f'w1s{i}') for i in range(2)]
    w2s = S.sb([128, 2, 64], F32, 'w2s')
    pst = S.sb([64, 32], F32, 'pst')
    pm0 = S.ps([128, 512], F32, 'pm0')
    pmisc = [pm0, pm0]
    ptb = S.ps([128, 1024], BF16, 'ptb')
    k = 0
    for kv in ('k', 'v'):
        w1b = S.sb([64, 32, 256], BF16, 'w1b' + kv)
        w1v = prm['cmp_w1_' + kv].rearrange("(l e) c -> e l c", e=64)
        for q4 in range(8):
            st = stg[k % 2]
            k += 1
            S.dma('sp', st[:], w1v[:, q4 * 4:(q4 + 1) * 4, :], r=[], w=[st])
            S.dve(lambda e, st=st, w1b=w1b, q4=q4: e.tensor_copy(out=w1b[:, q4 * 4:(q4 + 1) * 4, :], in_=st[:]),
                  r=[st], w=[(w1b, q4)])
        w2b = S.sb([128, 2, 64], BF16, 'w2b' + kv)
        S.dma('sp', w2s[:], prm['cmp_w2_' + kv].rearrange("(c p) e -> p c e", p=128), r=[], w=[w2s])
        S.dve(lambda e, w2b=w2b: e.tensor_copy(out=w2b[:], in_=w2s[:]), r=[w2s], w=[w2b])
        posb = S.sb([64, 32], BF16, 'posb' + kv)
        S.dma('sp', pst[:], prm['cmp_posT_' + kv], r=[], w=[pst])
        S.dve(lambda e, posb=posb: e.tensor_copy(out=posb[:], in_=pst[:]), r=[pst], w=[posb])
        pbias = S.sb([128, 2], F32, 'pbias' + kv)
        for cc in range(2):
            pp = pmisc[cc]
            for l in range(32):
                S.pe(lambda e, pp=pp, w1b=w1b, posb=posb, l=l, cc=cc: e.matmul(
                    pp[:, 0:1], lhsT=w1b[:, l, cc * 128:(cc + 1) * 128], rhs=posb[:, l:l + 1],
                    start=(l == 0), stop=(l == 31)), r=[w1b, posb], w=[pp])
            S.dve(lambda e, pp=pp, pbias=pbias, cc=cc: e.tensor_copy(out=pbias[:, cc:cc + 1], in_=pp[:, 0:1]),
                  r=[pp], w=[(pbias, cc)])
        cw[kv] = (w1b, w2b, pbias)
    atok = S.sb([128, 16, 512], BF16, 'atok')
    gates = S.sb([128, 16, 36], F32, 'gates')
    otok = S.sb([128, 16, 1024], BF16, 'otok')
    accg = S.sb([128, 16, 256], F32, 'accg')
    kcT = S.sb([64, SEQ], BF16, 'kcT')
    vcT = S.sb([64, SEQ], BF16, 'vcT')
    kslT = S.sb([128, SEQ], BF16, 'kslT')
    kwT = S.sb([128, SEQ], BF16, 'kwT')
    S.dve(lambda e: e.memset(kslT[:], 0.0), r=[], w=[kslT])
    S.pool(lambda e: e.memset(kwT[:], 0.0), r=[], w=[kwT])
    S.dve(lambda e: e.tensor_copy(out=kslT[64:96, :], in_=E32[0:32, :]), r=[E32], w=[(kslT, 'e')])
    vsl = S.sb([128, 16, 65], BF16, 'vslx')
    vw = S.sb([128, 16, 65], BF16, 'vwx')
    S.pool(lambda e: e.memset(vsl[:], 1.0), r=[], w=[vsl])
    S.pool(lambda e: e.memset(vw[:], 1.0), r=[], w=[vw])
    qraw = S.sb([128, SEQ], BF16, 'qrawT')
    qrots = [S.sb([128, SEQ], BF16, f'qrotT{i}') for i in range(2)]
    S.pool(lambda e: e.memset(qraw[:], 0.0), r=[], w=[qraw])
    S.dve(lambda e: e.memset(qrots[0][:], 0.0), r=[], w=[qrots[0]])
    S.pool(lambda e: e.memset(qrots[1][:], 0.0), r=[], w=[qrots[1]])
    ghT = S.sb([128, 2, 128], BF16, 'ghT')
    kcmpT = S.sb([128, 128], BF16, 'kcmpT')
    S.dve(lambda e: e.memset(kcmpT[:], 0.0), r=[], w=[kcmpT])
    vcmp = S.sb([128, 64], BF16, 'vcmp')
    psumT = S.sb([128, SEQ], F32, 'psumT')
    pcT = [S.sb([128, 512], BF16, f'pcT{i}') for i in range(2)]
    rinv = [S.sb([128, 512], F32, f'rinv{i}') for i in range(2)]
    pnf = [S.sb([128, 512], F32, f'pnf{i}') for i in range(2)]
    pnb = [S.sb([128, 512], BF16, f'pnb{i}') for i in range(2)]
    PT = [S.sb([128, 512], BF16, f'PT{i}') for i in range(4)]
    gm = [S.sb([128, 256], F32, f'gm{i}') for i in range(2)]
    sel = [S.sb([128, 32], F32, f'sel{i}') for i in range(2)]
    m8 = [S.sb([128, 8], F32, f'm8{i}') for i in range(2)]
    nsel = [S.sb([128, 32], BF16, f'nsel{i}') for i in range(2)]
    fsc = [S.sb([128, 1], F32, f'fsc{i}') for i in range(4)]
    ftmp = [S.sb([128, 64], F32, f'ftmp{i}') for i in range(4)]
    STps = [S.ps([128, 512], F32, f'st{i}') for i in range(2)] + [pm0]
    Oacc = [S.ps([128, 512], F32, f'oa{i}') for i in range(4)]
    fcount = [0]

    def make_finalize(r, gidx, first):
        def fin(c, j, O, off):
            i = 4 * c + j
            f = fsc[fcount[0] % 4]
            fcount[0] += 1
            tm = ftmp[fcount[0] % 4]
            S.dve(lambda e: e.reciprocal(out=f[:], in_=O[:, off + 64:off + 65]), r=[O], w=[f])
            S.dve(lambda e: e.tensor_scalar(out=tm[:], in0=O[:, off:off + 64], scalar1=f[:, 0:1],
                                            scalar2=gates[:, i, gidx:gidx + 1], op0=ALU.mult, op1=ALU.mult),
                  r=[O, f, gates], w=[tm])
            S.pool(lambda e: e.tensor_tensor(out=accg[:, i, r * 64:(r + 1) * 64], in0=accg[:, i, r * 64:(r + 1) * 64],
                                             in1=tm[:], op=ALU.add), r=[tm, (accg, (i, r))], w=[(accg, (i, r))])
        return fin

    for sq in range(nseq):
        tb = sq * SEQ
        S.dma('sp', atok[:], dr['a_tok'][tb:tb + SEQ, :].rearrange("(n p) c -> p n c", p=128), r=[], w=[atok])
        S.dma('sp', gates[:], dr['gate'][tb:tb + SEQ, :].rearrange("(n p) c -> p n c", p=128), r=[], w=[gates])
        for i in range(16):
            pp = pmisc[i % 2]
            g_ = gm[i % 2]
            for g in range(4):
                S.pe(lambda e, pp=pp, g=g, i=i: e.matmul(pp[:, g * 64:(g + 1) * 64], lhsT=wTb[:, g, :],
                                                          rhs=atok[:, i, 256 + g * 64:256 + (g + 1) * 64],
                                                          start=True, stop=True), r=[wTb, atok], w=[pp])
                S.dve(lambda e, pp=pp, g_=g_, g=g: e.tensor_scalar(
                    out=g_[:, g * 64:(g + 1) * 64], in0=pp[:, g * 64:(g + 1) * 64], scalar1=bT[:, g:g + 1],
                    scalar2=None, op0=ALU.add), r=[pp, bT], w=[(g_, g)])
            S.dve(lambda e, g_=g_, i=i: e.tensor_tensor(out=otok[:, i, 0:256], in0=g_[:], in1=atok[:, i, 0:256],
                                                         op=ALU.mult), r=[g_, atok], w=[(otok, (i, 0))])
        for g in range(3):
            for nm, tl in (('kc', kcT), ('vc', vcT)):
                S.dma('sp', tl[:], dr[nm][g * 64:(g + 1) * 64, tb:tb + SEQ], r=[], w=[tl])
            for nm, tl in (('ksl', kslT), ('kw', kwT)):
                S.dma('sp', tl[0:64, :], dr[nm][g * 64:(g + 1) * 64, tb:tb + SEQ], r=[], w=[(tl, 'k')])
            for nm, tl in (('vsl', vsl), ('vw', vw)):
                S.dma('sp', tl[:, :, 0:64], dr[nm][tb:tb + SEQ, g * 64:(g + 1) * 64].rearrange(
                    "(n p) c -> p n c", p=128), r=[], w=[tl])
            for kv, src in (('k', kcT), ('v', vcT)):
                w1b, w2b, pbias = cw[kv]
                for cc in range(2):
                    pp = pmisc[cc]
                    for l in range(32):
                        S.pe(lambda e, pp=pp, w1b=w1b, src=src, l=l, cc=cc: e.matmul(
                            pp[:, 0:127], lhsT=w1b[:, l, cc * 128:(cc + 1) * 128], rhs=src[:, l:l + 2017:16],
                            start=(l == 0), stop=(l == 31)), r=[w1b, src], w=[pp])
                    S.act(lambda e, pp=pp, cc=cc, pbias=pbias: e.activation(
                        out=ghT[:, cc, 0:127], in_=pp[:, 0:127], func=AF.Gelu_apprx_tanh, bias=pbias[:, cc:cc + 1]),
                        r=[pp, pbias], w=[(ghT, cc)])
                pp = pmisc[0]
                if kv == 'k':
                    for cc in range(2):
                        S.pe(lambda e, pp=pp, cc=cc, w2b=w2b: e.matmul(pp[0:64, 0:127], lhsT=w2b[:, cc, :],
                                                                     rhs=ghT[:, cc, 0:127], start=(cc == 0),
                                                                     stop=(cc == 1)), r=[w2b, ghT], w=[pp])
                    S.act(lambda e, pp=pp: e.copy(out=kcmpT[0:64, 0:127], in_=pp[0:64, 0:127]), r=[pp], w=[(kcmpT, 'k')])
                else:
                    for cc in range(2):
                        S.pe(lambda e, pp=pp, cc=cc, w2b=w2b: e.matmul(pp[0:127, 0:64], lhsT=ghT[:, cc, 0:127],
                                                                     rhs=w2b[:, cc, :], start=(cc == 0),
                                                                     stop=(cc == 1)), r=[w2b, ghT], w=[pp])
                    S.act(lambda e, pp=pp: e.copy(out=vcmp[0:127, :], in_=pp[0:127, 0:64]), r=[pp], w=[vcmp])
            for r in range(4):
                h = 4 * g + r
                S.dma('sp', qraw[0:64, :], dr['qraw'][h * 64:(h + 1) * 64, tb:tb + SEQ], r=[], w=[(qraw, 'q')])
                for c in range(4):
                    b = c % 2
                    ps = STps[b]
                    S.pe(lambda e, ps=ps, c=c: e.matmul(ps[0:127, :], lhsT=kcmpT[:, 0:127],
                                                        rhs=qraw[:, c * 512:(c + 1) * 512], start=True, stop=False),
                         r=[kcmpT, qraw], w=[ps])
                    S.pe(lambda e, ps=ps, c=c: e.matmul(ps[0:127, :], lhsT=ident[0:127, 0:127],
                                                        rhs=cmpb[0:127, c * 512:(c + 1) * 512], start=False, stop=True),
                         r=[ident, cmpb], w=[ps])
                    S.act(lambda e, ps=ps, b=b: e.activation(out=pcT[b][0:127, :], in_=ps[0:127, :], func=AF.Exp),
                          r=[ps], w=[pcT[b]])
                    pd = pmisc[b]
                    S.pe(lambda e, pd=pd, b=b: e.matmul(pd[0:127, :], lhsT=ones[0:127, 0:127], rhs=pcT[b][0:127, :],
                                                        start=True, stop=True), r=[ones, pcT[b]], w=[pd])
                    S.dve(lambda e, pd=pd, b=b: e.tensor_scalar(out=rinv[b][0:127, :], in0=pd[0:127, :], scalar1=1e-30,
                                                                scalar2=None, op0=ALU.max), r=[pd], w=[rinv[b]])
                    S.dve(lambda e, b=b: e.reciprocal(out=rinv[b][0:127, :], in_=rinv[b][0:127, :]),
                          r=[rinv[b]], w=[rinv[b]])
                    S.dve(lambda e, b=b: e.tensor_tensor(out=pnf[b][0:127, :], in0=pcT[b][0:127, :],
                                                         in1=rinv[b][0:127, :], op=ALU.mult),
                          r=[pcT[b], rinv[b]], w=[pnf[b]])
                    S.pool(lambda e, b=b: e.tensor_copy(out=pnb[b][0:127, :], in_=pnf[b][0:127, :]),
                           r=[pnf[b]], w=[pnb[b]])
                    if r == 0:
                        S.pool(lambda e, b=b, c=c: e.tensor_copy(out=psumT[0:127, c * 512:(c + 1) * 512],
                                                                 in_=pnf[b][0:127, :]), r=[pnf[b]], w=[(psumT, c)])
                    else:
                        S.pool(lambda e, b=b, c=c: e.tensor_tensor(
                            out=psumT[0:127, c * 512:(c + 1) * 512], in0=psumT[0:127, c * 512:(c + 1) * 512],
                            in1=pnf[b][0:127, :], op=ALU.add), r=[pnf[b], (psumT, c)], w=[(psumT, c)])
                    for j in range(4):
                        i = 4 * c + j
                        O = Oacc[j]
                        off = 0
                        S.pe(lambda e, O=O, b=b, j=j, off=off: e.matmul(
                            O[:, off:off + 64], lhsT=pnb[b][0:127, j * 128:(j + 1) * 128], rhs=vcmp[0:127, :],
                            start=True, stop=True), r=[pnb[b], vcmp], w=[O])
                        S.dve(lambda e, O=O, i=i, r=r, h=h, off=off: e.tensor_scalar(
                            out=accg[:, i, r * 64:(r + 1) * 64], in0=O[:, off:off + 64],
                            scalar1=gates[:, i, 3 * h:3 * h + 1], scalar2=None, op0=ALU.mult),
                            r=[O, gates], w=[(accg, (i, r))])
            for i in range(16):
                b = i % 2
                pp = pmisc[b]
                S.pe(lambda e, pp=pp, i=i: e.matmul(pp[:, 0:32], lhsT=psumT[0:127, i * 128:(i + 1) * 128],
                                                    rhs=ovl[0:127, 0:32], start=True, stop=True),
                     r=[psumT, ovl], w=[pp])
                sl = sel[b]
                S.dve(lambda e, pp=pp, sl=sl, i=i: e.tensor_tensor(out=sl[:], in0=pp[:, 0:32],
                                                                  in1=keep[:, i * 32:(i + 1) * 32], op=ALU.mult),
                      r=[pp, keep], w=[sl])
                S.dve(lambda e, sl=sl, i=i: e.tensor_tensor(out=sl[:], in0=sl[:], in1=addm[:, i * 32:(i + 1) * 32],
                                                           op=ALU.add), r=[sl, addm], w=[sl])
                S.dve(lambda e, sl=sl, b=b: e.max(out=m8[b][:], in_=sl[:]), r=[sl], w=[m8[b]])
                S.dve(lambda e, sl=sl, b=b: e.tensor_scalar(out=sl[:], in0=sl[:], scalar1=m8[b][:, 7:8], scalar2=None,
                                                            op0=ALU.is_ge), r=[sl, m8[b]], w=[sl])
                S.dve(lambda e, sl=sl, i=i: e.tensor_tensor(out=sl[:], in0=sl[:], in1=adm[:, i * 32:(i + 1) * 32],
                                                           op=ALU.mult), r=[sl, adm], w=[sl])
                S.dve(lambda e, sl=sl, b=b: e.tensor_scalar(out=nsel[b][:], in0=sl[:], scalar1=-NEG, scalar2=NEG,
                                                            op0=ALU.mult, op1=ALU.add), r=[sl], w=[nsel[b]])
                S.pe(lambda e, b=b: e.transpose(out=ptb[0:32, b * 128:(b + 1) * 128], in_=nsel[b][:],
                                                identity=ident[:]), r=[nsel[b], ident], w=[(ptb, b)])
                for qb in qrots:
                    S.dve(lambda e, i=i, b=b, qb=qb: e.tensor_copy(out=qb[64:96, i * 128:(i + 1) * 128],
                                                                   in_=ptb[0:32, b * 128:(b + 1) * 128]),
                          r=[(ptb, b)], w=[(qb, ('n', i))])
            def load_q(r, g=g, tb=tb):
                h = 4 * g + r
                qb = qrots[r % 2]
                S.dma('sp', qb[0:64, :], dr['qrot'][h * 64:(h + 1) * 64, tb:tb + SEQ], r=[], w=[(qb, 'q')])
            load_q(0)
            jobs = []
            for r in range(4):
                h = 4 * g + r
                qb = qrots[r % 2]
                for c in range(4):
                    jobs.append(dict(c=c, tiles=causal_tiles(c, tri), qT=qb, kT=kslT, vext=vsl,
                                     fin=make_finalize(r, 3 * h + 1, False),
                                     pre=((lambda r=r: load_q(r + 1)) if (c == 0 and r < 3) else None)))
                for c in range(4):
                    jobs.append(dict(c=c, tiles=window_tiles(c, tri, band), qT=qb, kT=kwT, vext=vw,
                                     fin=make_finalize(r, 3 * h + 2, False)))
            attn_run(S, jobs, STps, PT, Oacc)
            S.act(lambda e, g=g: e.copy(out=otok[:, :, 256 + 256 * g:256 + 256 * (g + 1)], in_=accg[:]),
                  r=[accg], w=[(otok, ('g', g))])
        S.dma('sp', dr['o_tok'][tb:tb + SEQ, :].rearrange("(n p) c -> p n c", p=128), otok[:],
              r=[otok], w=[DW(S, dr['o_tok'])])
    S.flush()


def outproj_stage(S, x_in, x_out, o_tok, w_out, ntok, cpack):
    C = Consts(S, cpack)
    ident = C.get('ident', BF16)
    wob = S.sb([128, 8, D], BF16, 'wob')
    wst = [S.sb([128, D], F32, f'wos{i}') for i in range(2)]
    wv = w_out.rearrange("(c p) m -> p c m", p=128)
    for c in range(8):
        st = wst[c % 2]
        S.dma('sp', st[:], wv[:, c, :], r=[], w=[st])
        S.dve(lambda e, st=st, c=c: e.tensor_copy(out=wob[:, c, :], in_=st[:]), r=[st], w=[(wob, c)])
    ot = [S.sb([128, D], BF16, f'oo{i}') for i in range(2)]
    oT = [S.sb([128, 8, 128], BF16, f'oT{i}') for i in range(2)]
    xt = [S.sb([128, D], F32, f'xo{i}') for i in range(2)]
    xo = [S.sb([128, D], F32, f'xn{i}') for i in range(2)]
    ptr = [S.ps([128, 8, 128], BF16, f'ptr{i}') for i in range(2)]
    py = [S.ps([128, 512], F32, f'py{i}') for i in range(4)]
    for i in range(ntok // 128):
        b = i % 2
        S.dma('sp', ot[b][:], o_tok[i * 128:(i + 1) * 128, :], r=[], w=[ot[b]])
        S.dma('sp', xt[b][:], x_in[i * 128:(i + 1) * 128, :], r=[], w=[xt[b]])
        p = ptr[b]
        for c in range(8):
            S.pe(lambda e, p=p, b=b, c=c: e.transpose(out=p[:, c, :], in_=ot[b][:, c * 128:(c + 1) * 128],
                                                      identity=ident[:]), r=[ot[b], ident], w=[(p, c)])
        S.act(lambda e, p=p, b=b: e.copy(out=oT[b][:], in_=p[:]), r=[p], w=[oT[b]])
        for mh in range(2):
            pp = py[b * 2 + mh]
            for c in range(8):
                S.pe(lambda e, pp=pp, b=b, c=c, mh=mh: e.matmul(pp[:], lhsT=oT[b][:, c, :],
                                                                rhs=wob[:, c, mh * 512:(mh + 1) * 512],
                                                                start=(c == 0), stop=(c == 7)),
                     r=[oT[b], wob], w=[pp])
            S.dve(lambda e, pp=pp, b=b, mh=mh: e.tensor_tensor(out=xo[b][:, mh * 512:(mh + 1) * 512], in0=pp[:],
                                                               in1=xt[b][:, mh * 512:(mh + 1) * 512], op=ALU.add),
                  r=[pp, xt[b]], w=[(xo[b], mh)])
        S.dma('pool', x_out[i * 128:(i + 1) * 128, :], xo[b][:], r=[xo[b]], w=[DW(S, x_out)])
    S.flush()


def odd_proj(S, x, prm, dr, ntok, cpack):
    vb = [S.sb([128, 64], BF16, f'vb{i}') for i in range(2)]
    wb = [S.sb([128, 4], F32, f'wb{i}') for i in range(2)]
    vd = [S.sb([128, 512], BF16, f'vd{i}') for i in range(2)]

    def tm_post(gi, pp, tok0, n):
        b = (tok0 // 128) % 2
        if gi == 0:
            S.act(lambda e: e.copy(out=vb[b][:], in_=pp[:, 0:64]), r=[pp], w=[vb[b]])
            S.dma('pool', dr['vcd'][tok0:tok0 + 128, :], vb[b][:], r=[vb[b]], w=[DW(S, dr['vcd'])])
        elif gi == 1:
            S.act(lambda e: e.copy(out=wb[b][:], in_=pp[:, 0:4]), r=[pp], w=[wb[b]])
            S.dma('pool', dr['wi'][tok0:tok0 + 128, :], wb[b][:], r=[wb[b]], w=[DW(S, dr['wi'])])
        else:
            S.act(lambda e: e.copy(out=vd[b][:], in_=pp[:, 0:512]), r=[pp], w=[vd[b]])
            S.dma('pool', dr['vdd'][tok0:tok0 + 128, :], vd[b][:], r=[vd[b]], w=[DW(S, dr['vdd'])])
    fm = []
    for i in range(4):
        fm.append((128 * i, 128, 0.125, 'r64', None, dr['qc'][128 * i:128 * (i + 1), :]))
    fm.append((512, 64, 1.0, 'r64', None, dr['kcd'][:, :]))
    fm.append((640, 128, 1.0, 'r32', None, dr['qi'][:, :]))
    fm.append((768, 32, 1.0, 'r32', None, dr['ki'][:, :]))
    for i in range(4):
        fm.append((804 + 128 * i, 128, 0.125, 'r64', None, dr['qd'][128 * i:128 * (i + 1), :]))
    for i in range(4):
        fm.append((1316 + 128 * i, 128, 1.0, 'r64', None, dr['kd'][128 * i:128 * (i + 1), :]))
    proj_stage(S, x, prm['w_in'], 2340, prm['mix_norm'], prm['pos'], ntok, cpack, fm,
               [(576, 64), (800, 4), (1828, 512)], tm_post, use_idx=True)


NBIS = 14


def dsa_moba_stage(S, prm, dr, nseq, cpack):
    C = Consts(S, cpack)
    ident = C.get('ident', BF16)
    tri = C.get('tri01', BF16)
    trib = C.get('tri_ge', BF16)
    E8 = C.get('E8', F32)
    triqs = C.get('tri_qs', F32)
    mbias, mpast, mown = C.get('mbias'), C.get('mpast'), C.get('mown')
    zc = [0]

    def ztile(shape, name):
        t = S.sb(shape, BF16, name)
        zc[0] += 1
        if zc[0] % 2:
            S.dve(lambda e: e.memset(t[:], 0.0), r=[], w=[t])
        else:
            S.pool(lambda e: e.memset(t[:], 0.0), r=[], w=[t])
        return t
    qi = [ztile([128, SEQ], f'qi{h}') for h in range(4)]
    ki = ztile([128, SEQ], 'ki')
    wi = S.sb([128, 16, 4], F32, 'wi')
    absw = S.sb([128, 16, 4], F32, 'absw')
    sgnw = S.sb([128, 16, 4], F32, 'sgnw')
    scores = [S.sb([128, SEQ], F32, f'score{i}') for i in range(2)]
    rl = [S.sb([128, 512], F32, f'rl{i}') for i in range(2)]
    junkb = S.sb([128, SEQ], BF16, 'junkb')
    nmasks = [S.sb([128, SEQ], BF16, f'nmask{i}') for i in range(2)]
    nmTs = [S.sb([128, 16, 512], BF16, f'nmT{i}') for i in range(2)]
    kcT = ztile([128, SEQ], 'kcT')
    vcx = S.sb([128, 16, 65], BF16, 'vcx')
    vdxs = [S.sb([128, 16, 65], BF16, f'vdx{i}') for i in range(2)]
    S.pool(lambda e: e.memset(vcx[:], 1.0), r=[], w=[vcx])
    for v_ in vdxs:
        S.pool(lambda e, v_=v_: e.memset(v_[:], 1.0), r=[], w=[v_])
    qcall = [ztile([128, SEQ], f'qcall{h}') for h in range(8)]
    qds = [ztile([128, SEQ], f'qd{i}') for i in range(2)]
    kds = [ztile([128, SEQ], f'kd{i}') for i in range(2)]
    for kd_ in kds:
        S.dve(lambda e, kd_=kd_: e.tensor_copy(out=kd_[64:72, :], in_=E8[0:8, :]), r=[E8], w=[(kd_, 'e')])
    kmf = S.sb([64, 8], F32, 'kmf')
    kmbs = [ztile([128, 8], f'kmb{i}') for i in range(2)]
    gsb = S.sb([128, 128], F32, 'gsb')
    ns8all = S.sb([128, 16, 32], BF16, 'ns8all')
    otok = S.sb([128, 16, 1024], BF16, 'otok')
    PT = [S.sb([128, 512], BF16, f'PT{i}') for i in range(4)]
    st5 = [S.sb([128, 8], F32, f'st5{i}') for i in range(2)]
    gs = [S.sb([128, 8], F32, f'gs{i}') for i in range(2)]
    m8 = [S.sb([128, 8], F32, f'm8{i}') for i in range(2)]
    ns8 = [S.sb([128, 8], BF16, f'ns8{i}') for i in range(2)]
    fsc = [S.sb([128, 1], F32, f'fsc{i}') for i in range(4)]
    pl = S.ps([128, 512], F32, 'pl')
    STps = [S.ps([128, 512], F32, f'st{i}') for i in range(2)]
    Oacc = [S.ps([128, 512], F32, f'oa{i}') for i in range(4)]
    ptb = S.ps([128, 8, 128], BF16, 'ptb')
    fcount = [0]

    def make_fin(col0):
        def fin(c, j, O, off):
            i = 4 * c + j
            f = fsc[fcount[0] % 4]
            fcount[0] += 1
            S.dve(lambda e: e.reciprocal(out=f[:], in_=O[:, off + 64:off + 65]), r=[O], w=[f])
            S.dve(lambda e: e.tensor_scalar(out=otok[:, i, col0:col0 + 64], in0=O[:, off:off + 64], scalar1=f[:, 0:1],
                                            scalar2=None, op0=ALU.mult), r=[O, f], w=[(otok, (i, col0))])
        return fin

    def index_steps(c):
        nmT = nmTs[c % 2]
        steps = []
        chains = {}
        for j in range(4):
            i = 4 * c + j
            W = 128 * (i + 1)
            score = scores[j % 2]
            nmask = nmasks[j % 2]
            steps = chains.setdefault(j, [])
            if i < 2:
                def trivial(i=i, j=j):
                    for st_ in range(i + 1):
                        if st_ == i:
                            S.pool(lambda e, st_=st_: e.tensor_copy(out=nmT[:, st_, j * 128:(j + 1) * 128], in_=trib[:]),
                                   r=[trib], w=[(nmT, (st_, j))])
                        else:
                            S.pool(lambda e, st_=st_: e.memset(nmT[:, st_, j * 128:(j + 1) * 128], 0.0),
                                   r=[], w=[(nmT, (st_, j))])
                steps.append(trivial)
                continue
            s5 = st5[i % 2]
            for h in range(4):
                def logits(h=h, i=i, W=W, score=score):
                    for sc in range((W + 511) // 512):
                        n = min(512, W - 512 * sc)
                        rb = rl[(h * 4 + sc) % 2]
                        S.pe(lambda e, sc=sc, n=n: e.matmul(pl[:, 0:n], lhsT=qi[h][:, i * 128:(i + 1) * 128],
                                                            rhs=ki[:, sc * 512:sc * 512 + n], start=True, stop=True),
                             r=[qi[h], ki], w=[pl])
                        S.act(lambda e, rb=rb, n=n: e.activation(out=rb[:, 0:n], in_=pl[:, 0:n], func=AF.Relu,
                                                                 scale=absw[:, i, h:h + 1]), r=[pl, absw], w=[rb])
                        if h == 0:
                            S.dve(lambda e, rb=rb, n=n, sc=sc: e.tensor_scalar(
                                out=score[:, sc * 512:sc * 512 + n], in0=rb[:, 0:n], scalar1=sgnw[:, i, h:h + 1],
                                scalar2=None, op0=ALU.mult), r=[rb, sgnw], w=[(score, sc)])
                        else:
                            S.dve(lambda e, rb=rb, n=n, sc=sc: e.scalar_tensor_tensor(
                                out=score[:, sc * 512:sc * 512 + n], in0=rb[:, 0:n], scalar=sgnw[:, i, h:h + 1],
                                in1=score[:, sc * 512:sc * 512 + n], op0=ALU.mult, op1=ALU.add),
                                r=[rb, sgnw, (score, sc)], w=[(score, sc)])
                steps.append(logits)

            def bounds(i=i, W=W, s5=s5, score=score):
                S.dve(lambda e: e.tensor_reduce(out=s5[:, 5:6], in_=score[:, 0:W], axis=AX.X, op=ALU.max),
                      r=[score], w=[(s5, 5)])
                S.dve(lambda e: e.tensor_reduce(out=s5[:, 0:1], in_=score[:, 0:W], axis=AX.X, op=ALU.min),
                      r=[score], w=[(s5, 0)])
                S.dve(lambda e: e.tensor_tensor(out=s5[:, 1:2], in0=s5[:, 5:6], in1=s5[:, 0:1], op=ALU.subtract),
                      r=[(s5, 5), (s5, 0)], w=[(s5, 1)])
                S.dve(lambda e: e.tensor_tensor(out=score[:, i * 128:(i + 1) * 128],
                                                in0=score[:, i * 128:(i + 1) * 128], in1=triqs[:], op=ALU.add),
                      r=[score, triqs], w=[score])
            steps.append(bounds)
            for it in range(NBIS):
                def bis(W=W, s5=s5, score=score):
                    S.dve(lambda e: e.tensor_scalar(out=s5[:, 1:2], in0=s5[:, 1:2], scalar1=0.5, scalar2=None,
                                                    op0=ALU.mult), r=[(s5, 1)], w=[(s5, 1)])
                    S.dve(lambda e: e.tensor_tensor(out=s5[:, 2:3], in0=s5[:, 0:1], in1=s5[:, 1:2], op=ALU.add),
                          r=[(s5, 0), (s5, 1)], w=[(s5, 2)])
                    S.dve(lambda e: e.tensor_scalar(out=junkb[:, 0:W], in0=score[:, 0:W], scalar1=s5[:, 2:3],
                                                    scalar2=0.0, op0=ALU.is_ge, op1=ALU.add, accum_out=s5[:, 3:4]),
                          r=[score, (s5, 2)], w=[junkb, (s5, 3)])
                    S.dve(lambda e: e.tensor_scalar(out=s5[:, 4:5], in0=s5[:, 3:4], scalar1=255.5, scalar2=None,
                                                    op0=ALU.is_ge), r=[(s5, 3)], w=[(s5, 4)])
                    S.dve(lambda e: e.scalar_tensor_tensor(out=s5[:, 0:1], in0=s5[:, 1:2], scalar=s5[:, 4:5],
                                                           in1=s5[:, 0:1], op0=ALU.mult, op1=ALU.add),
                          r=[(s5, 1), (s5, 4), (s5, 0)], w=[(s5, 0)])
                steps.append(bis)

            def fin_mask(i=i, j=j, W=W, s5=s5, score=score, nmask=nmask):
                S.dve(lambda e: e.tensor_scalar(out=nmask[:, 0:W], in0=score[:, 0:W], scalar1=s5[:, 0:1],
                                                scalar2=NEG, op0=ALU.is_lt, op1=ALU.mult),
                      r=[score, (s5, 0)], w=[nmask])
                for s0 in range(0, i + 1, 8):
                    n = min(8, i + 1 - s0)
                    for k in range(n):
                        S.pe(lambda e, s0=s0, k=k: e.transpose(out=ptb[:, k, :],
                                                               in_=nmask[:, (s0 + k) * 128:(s0 + k + 1) * 128],
                                                               identity=ident[:]), r=[nmask, ident], w=[(ptb, k)])
                    S.act(lambda e, s0=s0, n=n: e.copy(out=nmT[:, s0:s0 + n, j * 128:(j + 1) * 128],
                                                       in_=ptb[:, 0:n, :]), r=[ptb], w=[(nmT, ('b', s0, j))])
            steps.append(fin_mask)
        out = []
        for pair in ((0, 1), (2, 3)):
            la, lb = chains[pair[0]], chains[pair[1]]
            for k in range(max(len(la), len(lb))):
                if k < len(la):
                    out.append(la[k])
                if k < len(lb):
                    out.append(lb[k])
        return out

    for sq in range(nseq):
        tb = sq * SEQ
        for h in range(4):
            S.dma('sp', qi[h][0:32, :], dr['qi'][h * 32:(h + 1) * 32, tb:tb + SEQ], r=[], w=[(qi[h], 'q')])
        S.dma('sp', ki[0:32, :], dr['ki'][:, tb:tb + SEQ], r=[], w=[(ki, 'q')])
        S.dma('sp', wi[:], dr['wi'][tb:tb + SEQ, :].rearrange("(n p) c -> p n c", p=128), r=[], w=[wi])
        S.act(lambda e: e.activation(out=absw[:], in_=wi[:], func=AF.Abs), r=[wi], w=[absw])
        S.act(lambda e: e.activation(out=sgnw[:], in_=wi[:], func=AF.Sign), r=[wi], w=[sgnw])
        S.dma('sp', kcT[0:64, :], dr['kcd'][:, tb:tb + SEQ], r=[], w=[(kcT, 'q')])
        S.dma('sp', vcx[:, :, 0:64], dr['vcd'][tb:tb + SEQ, :].rearrange("(n p) c -> p n c", p=128), r=[], w=[vcx])
        for h in range(8):
            S.dma('sp', qcall[h][0:64, :], dr['qc'][h * 64:(h + 1) * 64, tb:tb + SEQ], r=[], w=[(qcall[h], 'q')])
        import os
        STOP = int(os.environ.get('STOP', '0'))
        if STOP == 1:
            break
        for st in index_steps(0):
            st()
        if STOP == 2:
            break
        jobs = []
        for c in range(4):
            nmT = nmTs[c % 2]
            nxt = index_steps(c + 1) if c < 3 else []
            per = (len(nxt) + 7) // 8
            for h in range(8):
                sl = nxt[h * per:(h + 1) * per]
                if os.environ.get('NOPRE'):
                    for st in sl:
                        st()
                    sl = []
                jobs.append(dict(c=c, tiles=causal_tiles(c, None), qT=qcall[h], kT=kcT, vext=vcx,
                                 extra=(lambda kt: ident[:], lambda kt, c, lo, hi, nmT=nmT: nmT[:, kt, lo:hi],
                                        [ident, nmT]),
                                 fin=make_fin(64 * h),
                                 pre=((lambda sl=sl: [st() for st in sl]) if sl else None)))
        import os
        if not os.environ.get('SKIP_DSA'):
            attn_run(S, jobs, STps, PT, Oacc, LA=1)

        def moba_load(h, tb=tb):
            b = h % 2
            S.dma('sp', qds[b][0:64, :], dr['qd'][h * 64:(h + 1) * 64, tb:tb + SEQ], r=[], w=[(qds[b], 'q')])
            S.dma('sp', kds[b][0:64, :], dr['kd'][h * 64:(h + 1) * 64, tb:tb + SEQ], r=[], w=[(kds[b], 'q')])
            S.dma('sp', vdxs[b][:, :, 0:64], dr['vdd'][tb:tb + SEQ, h * 64:(h + 1) * 64].rearrange(
                "(n p) c -> p n c", p=128), r=[], w=[vdxs[b]])

        def gate_a(h):
            b = h % 2
            qd, kd, kmb = qds[b], kds[b], kmbs[b]
            S.dve(lambda e: e.tensor_reduce(out=kmf[:], in_=kd[0:64, :].rearrange("p (j k) -> p j k", k=256),
                                            axis=AX.X, op=ALU.add), r=[(kd, 'q')], w=[kmf])
            S.dve(lambda e: e.tensor_scalar(out=kmb[0:64, :], in0=kmf[:], scalar1=1.0 / 256, scalar2=None,
                                            op0=ALU.mult), r=[kmf], w=[(kmb, 'm')])
            for i in range(16):
                S.pe(lambda e, i=i: e.matmul(pl[:, i * 8:(i + 1) * 8], lhsT=qd[:, i * 128:(i + 1) * 128], rhs=kmb[:],
                                             start=True, stop=True), r=[qd, kmb], w=[(pl, i)])
            S.dve(lambda e: e.tensor_tensor(out=gsb[:], in0=pl[:, 0:128], in1=mbias[:], op=ALU.add),
                  r=[pl, mbias], w=[gsb])
            for i in range(16):
                m = m8[i % 2]
                S.dve(lambda e, i=i, m=m: e.max(out=m[:], in_=gsb[:, i * 8:(i + 1) * 8]), r=[(gsb, i)], w=[m])
                S.dve(lambda e, i=i, m=m: e.tensor_scalar(out=gsb[:, i * 8:(i + 1) * 8], in0=gsb[:, i * 8:(i + 1) * 8],
                                                          scalar1=m[:, 2:3], scalar2=None, op0=ALU.is_ge),
                      r=[(gsb, i), m], w=[(gsb, i)])
            S.dve(lambda e: e.tensor_tensor(out=gsb[:], in0=gsb[:], in1=mpast[:], op=ALU.mult), r=[gsb, mpast], w=[gsb])
            S.dve(lambda e: e.tensor_tensor(out=gsb[:], in0=gsb[:], in1=mown[:], op=ALU.add), r=[gsb, mown], w=[gsb])
            S.dve(lambda e: e.tensor_scalar(out=ns8all[:, :, 0:8], in0=gsb[:].rearrange("p (i k) -> p i k", k=8),
                                            scalar1=-NEG, scalar2=NEG, op0=ALU.mult, op1=ALU.add),
                  r=[gsb], w=[ns8all])

        def gate_b(h):
            qd = qds[h % 2]
            for half in range(2):
                for k in range(8):
                    i = half * 8 + k
                    S.pe(lambda e, k=k, i=i: e.transpose(out=ptb[0:8, k, :], in_=ns8all[:, i, 0:8],
                                                         identity=ident[:]), r=[ns8all, ident], w=[(ptb, k)])
                S.dve(lambda e, half=half: e.tensor_copy(
                    out=qd[64:72, half * 1024:(half + 1) * 1024].rearrange("p (k q) -> p k q", q=128),
                    in_=ptb[0:8, :, :]), r=[ptb], w=[(qd, ('n', half))])

        if STOP == 3:
            break
        moba_load(0)
        gate_a(0)
        if STOP == 4:
            break
        gate_b(0)
        if STOP == 5:
            break
        NH_ = int(os.environ.get('MOBA_H', '8'))
        for h in range(0 if not os.environ.get('SKIP_MOBA') else 8, NH_):
            b = h % 2
            if h + 1 < 8:
                moba_load(h + 1)
            jobs = []
            for c in range(4):
                jobs.append(dict(c=c, tiles=causal_tiles(c, tri), qT=qds[b], kT=kds[b], vext=vdxs[b],
                                 fin=make_fin(512 + 64 * h),
                                 pre=((lambda h=h: gate_a(h + 1)) if (c == 1 and h + 1 < 8 and not os.environ.get('NOGATE')) else None)))
            attn_run(S, jobs, STps, PT, Oacc, LA=1)
            if h + 1 < 8:
                gate_b(h + 1)
        S.dma('sp', dr['o_tok'][tb:tb + SEQ, :].rearrange("(n p) c -> p n c", p=128), otok[:],
              r=[otok], w=[DW(S, dr['o_tok'])])
    S.flush()


def attn_chunk_v1(S, c, tiles, qT, kT, extra, vext, ident, STps, PT, Oacc, finalize, tag, qdep=None):
    cover = {}
    for n, (kt, lo, hi, bt, blo) in enumerate(tiles):
        for j in range(lo // 128, hi // 128):
            cover.setdefault(j, []).append(n)

    qd_ = qdep if qdep is not None else qT

    def qk(n):
        kt, lo, hi, bt, blo = tiles[n]
        ps = STps[n % 2]
        nterm = 1 + (1 if extra else 0) + (1 if bt is not None else 0)
        S.pe(lambda e: e.matmul(ps[:, lo:hi], lhsT=kT[:, kt * 128:(kt + 1) * 128],
                                rhs=qT[:, c * 512 + lo:c * 512 + hi], start=True, stop=(nterm == 1)),
             r=[kT, qd_], w=[ps])
        k = 1
        if extra:
            k += 1
            S.pe(lambda e: e.matmul(ps[:, lo:hi], lhsT=extra[0](kt), rhs=extra[1](kt, c, lo, hi),
                                    start=False, stop=(k == nterm), skip_group_check=True),
                 r=list(extra[2]), w=[ps])
        if bt is not None:
            S.pe(lambda e: e.matmul(ps[:, blo:blo + 128], lhsT=ident[:], rhs=bt[:], start=False, stop=True,
                                    skip_group_check=True), r=[ident, bt], w=[ps])

    qk(0)
    for n, (kt, lo, hi, bt, blo) in enumerate(tiles):
        if n + 1 < len(tiles):
            qk(n + 1)
        ps, p = STps[n % 2], PT[n % 2]
        S.act(lambda e, ps=ps, p=p, lo=lo, hi=hi: e.activation(out=p[:, lo:hi], in_=ps[:, lo:hi], func=AF.Exp),
              r=[ps], w=[p])
        for j in range(lo // 128, hi // 128):
            S.pe(lambda e, p=p, j=j, kt=kt, n=n: e.matmul(
                Oacc[j][:, 0:65], lhsT=p[:, j * 128:(j + 1) * 128], rhs=vext[:, kt, :],
                start=(cover[j][0] == n), stop=(cover[j][-1] == n), skip_group_check=True),
                r=[p, vext], w=[Oacc[j]])
            if cover[j][-1] == n:
                finalize(c, j, Oacc[j])


def causal_tiles_v1(c, tri):
    out = []
    for kt in range(4 * c + 4):
        if kt < 4 * c:
            out.append((kt, 0, 512, None, 0))
        else:
            lo = (kt - 4 * c) * 128
            out.append((kt, lo, 512, tri, lo))
    return out


def dsa_moba_stage_v1(S, prm, dr, nseq, cpack):
    C = Consts(S, cpack)
    ident = C.get('ident', BF16)
    tri = C.get('tri_ge', BF16)
    E8 = C.get('E8', BF16)
    triqs = C.get('tri_qs', F32)
    mbias, mpast, mown = C.get('mbias'), C.get('mpast'), C.get('mown')
    qi = [S.sb([32, SEQ], BF16, f'qi{h}') for h in range(4)]
    ki = S.sb([32, SEQ], BF16, 'ki')
    wi = S.sb([128, 16, 4], F32, 'wi')
    absw = S.sb([128, 16, 4], F32, 'absw')
    sgnw = S.sb([128, 16, 4], F32, 'sgnw')
    score = S.sb([128, SEQ], F32, 'score')
    rl = [S.sb([128, 512], F32, f'rl{i}') for i in range(2)]
    junkb = S.sb([128, SEQ], BF16, 'junkb')
    nmask = S.sb([128, SEQ], BF16, 'nmask')
    nmT = S.sb([128, 16, 512], BF16, 'nmT')
    kcT = S.sb([64, SEQ], BF16, 'kcT')
    vcx = S.sb([128, 16, 65], BF16, 'vcx')
    vdx = S.sb([128, 16, 65], BF16, 'vdx')
    S.pool(lambda e: e.memset(vcx[:], 1.0), r=[], w=[vcx])
    S.pool(lambda e: e.memset(vdx[:], 1.0), r=[], w=[vdx])
    qcall = [S.sb([64, SEQ], BF16, f'qcall{h}') for h in range(8)]
    qd = S.sb([64, SEQ], BF16, 'qd')
    kd = S.sb([64, SEQ], BF16, 'kd')
    kmf = S.sb([64, 8], F32, 'kmf')
    kmb = S.sb([64, 8], BF16, 'kmb')
    negsel8 = S.sb([8, SEQ], BF16, 'negsel8')
    otok = S.sb([128, 16, 1024], BF16, 'otok')
    PT = [S.sb([128, 512], BF16, f'PT{i}') for i in range(2)]
    st5 = [S.sb([128, 8], F32, f'st5{i}') for i in range(2)]
    gs = [S.sb([128, 8], F32, f'gs{i}') for i in range(2)]
    m8 = [S.sb([128, 8], F32, f'm8{i}') for i in range(2)]
    ns8 = [S.sb([128, 8], BF16, f'ns8{i}') for i in range(2)]
    fsc = [S.sb([128, 1], F32, f'fsc{i}') for i in range(4)]
    STps = [S.ps([128, 512], F32, f'st{i}') for i in range(2)]
    Oacc = [S.ps([128, 512], F32, f'oa{i}') for i in range(4)]
    ptb = S.ps([128, 8, 128], BF16, 'ptb')
    pl = S.ps([128, 512], F32, 'pl')
    fcount = [0]

    def make_fin(col0):
        def fin(c, j, O):
            i = 4 * c + j
            f = fsc[fcount[0] % 4]
            fcount[0] += 1
            S.dve(lambda e: e.tensor_scalar(out=f[:], in0=O[:, 64:65], scalar1=1e-30, scalar2=None, op0=ALU.max),
                  r=[O], w=[f])
            S.dve(lambda e: e.reciprocal(out=f[:], in_=f[:]), r=[f], w=[f])
            S.dve(lambda e: e.tensor_scalar(out=otok[:, i, col0:col0 + 64], in0=O[:, 0:64], scalar1=f[:, 0:1],
                                            scalar2=None, op0=ALU.mult), r=[O, f], w=[(otok, (i, col0))])
        return fin

    for sq in range(nseq):
        tb = sq * SEQ
        for h in range(4):
            S.dma('sp', qi[h][:], dr['qi'][h * 32:(h + 1) * 32, tb:tb + SEQ], r=[], w=[qi[h]])
        S.dma('sp', ki[:], dr['ki'][:, tb:tb + SEQ], r=[], w=[ki])
        S.dma('sp', wi[:], dr['wi'][tb:tb + SEQ, :].rearrange("(n p) c -> p n c", p=128), r=[], w=[wi])
        S.act(lambda e: e.activation(out=absw[:], in_=wi[:], func=AF.Abs), r=[wi], w=[absw])
        S.act(lambda e: e.activation(out=sgnw[:], in_=wi[:], func=AF.Sign), r=[wi], w=[sgnw])
        S.dma('sp', kcT[:], dr['kcd'][:, tb:tb + SEQ], r=[], w=[kcT])
        S.dma('sp', vcx[:, :, 0:64], dr['vcd'][tb:tb + SEQ, :].rearrange("(n p) c -> p n c", p=128), r=[], w=[vcx])
        for h in range(8):
            S.dma('sp', qcall[h][:], dr['qc'][h * 64:(h + 1) * 64, tb:tb + SEQ], r=[], w=[qcall[h]])
        for c in range(4):
            for j in range(4):
                i = 4 * c + j
                W = 128 * (i + 1)
                if i < 2:
                    for st_ in range(i + 1):
                        if st_ == i:
                            S.pool(lambda e, st_=st_, j=j: e.tensor_copy(out=nmT[:, st_, j * 128:(j + 1) * 128],
                                                                         in_=tri[:]), r=[tri], w=[(nmT, (st_, j))])
                        else:
                            S.pool(lambda e, st_=st_, j=j: e.memset(nmT[:, st_, j * 128:(j + 1) * 128], 0.0),
                                   r=[], w=[(nmT, (st_, j))])
                    continue
                for h in range(4):
                    for sc in range((W + 511) // 512):
                        n = min(512, W - 512 * sc)
                        rb = rl[(h * 4 + sc) % 2]
                        S.pe(lambda e, h=h, sc=sc, n=n, i=i: e.matmul(pl[:, 0:n], lhsT=qi[h][:, i * 128:(i + 1) * 128],
                                                                       rhs=ki[:, sc * 512:sc * 512 + n], start=True,
                                                                       stop=True), r=[qi[h], ki], w=[pl])
                        S.act(lambda e, rb=rb, n=n, i=i, h=h: e.activation(out=rb[:, 0:n], in_=pl[:, 0:n], func=AF.Relu,
                                                                           scale=absw[:, i, h:h + 1]),
                              r=[pl, absw], w=[rb])
                        if h == 0:
                            S.dve(lambda e, rb=rb, n=n, sc=sc, i=i, h=h: e.tensor_scalar(
                                out=score[:, sc * 512:sc * 512 + n], in0=rb[:, 0:n], scalar1=sgnw[:, i, h:h + 1],
                                scalar2=None, op0=ALU.mult), r=[rb, sgnw], w=[(score, sc)])
                        else:
                            S.dve(lambda e, rb=rb, n=n, sc=sc, i=i, h=h: e.scalar_tensor_tensor(
                                out=score[:, sc * 512:sc * 512 + n], in0=rb[:, 0:n], scalar=sgnw[:, i, h:h + 1],
                                in1=score[:, sc * 512:sc * 512 + n], op0=ALU.mult, op1=ALU.add),
                                r=[rb, sgnw, (score, sc)], w=[(score, sc)])
                s5 = st5[i % 2]
                S.dve(lambda e, s5=s5, W=W: e.tensor_reduce(out=s5[:, 5:6], in_=score[:, 0:W], axis=AX.X, op=ALU.max),
                      r=[score], w=[(s5, 5)])
                S.dve(lambda e, s5=s5, W=W: e.tensor_reduce(out=s5[:, 0:1], in_=score[:, 0:W], axis=AX.X, op=ALU.min),
                      r=[score], w=[(s5, 0)])
                S.dve(lambda e, s5=s5: e.tensor_tensor(out=s5[:, 1:2], in0=s5[:, 5:6], in1=s5[:, 0:1], op=ALU.subtract),
                      r=[(s5, 5), (s5, 0)], w=[(s5, 1)])
                S.dve(lambda e, i=i: e.tensor_tensor(out=score[:, i * 128:(i + 1) * 128],
                                                     in0=score[:, i * 128:(i + 1) * 128], in1=triqs[:], op=ALU.add),
                      r=[score, triqs], w=[score])
                for it in range(NBIS):
                    S.dve(lambda e, s5=s5: e.tensor_scalar(out=s5[:, 1:2], in0=s5[:, 1:2], scalar1=0.5, scalar2=None,
                                                           op0=ALU.mult), r=[(s5, 1)], w=[(s5, 1)])
                    S.dve(lambda e, s5=s5: e.tensor_tensor(out=s5[:, 2:3], in0=s5[:, 0:1], in1=s5[:, 1:2], op=ALU.add),
                          r=[(s5, 0), (s5, 1)], w=[(s5, 2)])
                    S.dve(lambda e, s5=s5, W=W: e.tensor_scalar(out=junkb[:, 0:W], in0=score[:, 0:W],
                                                                scalar1=s5[:, 2:3], scalar2=0.0, op0=ALU.is_ge,
                                                                op1=ALU.add, accum_out=s5[:, 3:4]),
                          r=[score, (s5, 2)], w=[junkb, (s5, 3)])
                    S.dve(lambda e, s5=s5: e.tensor_scalar(out=s5[:, 4:5], in0=s5[:, 3:4], scalar1=255.5, scalar2=None,
                                                           op0=ALU.is_ge), r=[(s5, 3)], w=[(s5, 4)])
                    S.dve(lambda e, s5=s5: e.scalar_tensor_tensor(out=s5[:, 0:1], in0=s5[:, 1:2], scalar=s5[:, 4:5],
                                                                  in1=s5[:, 0:1], op0=ALU.mult, op1=ALU.add),
                          r=[(s5, 1), (s5, 4), (s5, 0)], w=[(s5, 0)])
                S.dve(lambda e, s5=s5, W=W: e.tensor_scalar(out=nmask[:, 0:W], in0=score[:, 0:W], scalar1=s5[:, 0:1],
                                                            scalar2=NEG, op0=ALU.is_lt, op1=ALU.mult),
                      r=[score, (s5, 0)], w=[nmask])
                for s0 in range(0, i + 1, 8):
                    n = min(8, i + 1 - s0)
                    for k in range(n):
                        S.pe(lambda e, s0=s0, k=k: e.transpose(out=ptb[:, k, :],
                                                               in_=nmask[:, (s0 + k) * 128:(s0 + k + 1) * 128],
                                                               identity=ident[:]), r=[nmask, ident], w=[(ptb, k)])
                    S.act(lambda e, s0=s0, n=n, j=j: e.copy(out=nmT[:, s0:s0 + n, j * 128:(j + 1) * 128],
                                                            in_=ptb[:, 0:n, :]), r=[ptb], w=[(nmT, ('b', s0, j))])
            for h in range(8):
                attn_chunk_v1(S, c, causal_tiles_v1(c, None), qcall[h], kcT,
                           (lambda kt: ident[:], lambda kt, c, lo, hi: nmT[:, kt, lo:hi], [ident, nmT]),
                           vcx, ident, STps, PT, Oacc, make_fin(64 * h), 'dsa')
        for h in range(8):
            S.dma('sp', qd[:], dr['qd'][h * 64:(h + 1) * 64, tb:tb + SEQ], r=[], w=[qd])
            S.dma('sp', kd[:], dr['kd'][h * 64:(h + 1) * 64, tb:tb + SEQ], r=[], w=[kd])
            S.dma('sp', vdx[:, :, 0:64], dr['vdd'][tb:tb + SEQ, h * 64:(h + 1) * 64].rearrange(
                "(n p) c -> p n c", p=128), r=[], w=[vdx])
            S.dve(lambda e: e.tensor_reduce(out=kmf[:], in_=kd[:].rearrange("p (j k) -> p j k", k=256), axis=AX.X,
                                            op=ALU.add), r=[kd], w=[kmf])
            S.dve(lambda e: e.tensor_scalar(out=kmb[:], in0=kmf[:], scalar1=1.0 / 256, scalar2=None, op0=ALU.mult),
                  r=[kmf], w=[kmb])
            for i in range(16):
                b = i % 2
                S.pe(lambda e, i=i: e.matmul(pl[:, 0:8], lhsT=qd[:, i * 128:(i + 1) * 128], rhs=kmb[:], start=True,
                                             stop=True), r=[qd, kmb], w=[pl])
                g_ = gs[b]
                S.dve(lambda e, g_=g_, i=i: e.tensor_tensor(out=g_[:], in0=pl[:, 0:8], in1=mbias[:, i * 8:(i + 1) * 8],
                                                           op=ALU.add), r=[pl, mbias], w=[g_])
                S.dve(lambda e, g_=g_, b=b: e.max(out=m8[b][:], in_=g_[:]), r=[g_], w=[m8[b]])
                S.dve(lambda e, g_=g_, b=b: e.tensor_scalar(out=g_[:], in0=g_[:], scalar1=m8[b][:, 2:3], scalar2=None,
                                                            op0=ALU.is_ge), r=[g_, m8[b]], w=[g_])
                S.dve(lambda e, g_=g_, i=i: e.tensor_tensor(out=g_[:], in0=g_[:], in1=mpast[:, i * 8:(i + 1) * 8],
                                                           op=ALU.mult), r=[g_, mpast], w=[g_])
                S.dve(lambda e, g_=g_, i=i: e.tensor_tensor(out=g_[:], in0=g_[:], in1=mown[:, i * 8:(i + 1) * 8],
                                                           op=ALU.add), r=[g_, mown], w=[g_])
                S.dve(lambda e, g_=g_, b=b: e.tensor_scalar(out=ns8[b][:], in0=g_[:], scalar1=-NEG, scalar2=NEG,
                                                            op0=ALU.mult, op1=ALU.add), r=[g_], w=[ns8[b]])
                S.pe(lambda e, b=b: e.transpose(out=ptb[0:8, b, :], in_=ns8[b][:], identity=ident[:]),
                     r=[ns8[b], ident], w=[(ptb, b)])
                S.act(lambda e, b=b, i=i: e.copy(out=negsel8[:, i * 128:(i + 1) * 128], in_=ptb[0:8, b, :]),
                      r=[(ptb, b)], w=[(negsel8, i // 4)])
            for c in range(4):
                attn_chunk_v1(S, c, causal_tiles_v1(c, tri), qd, kd,
                           (lambda kt: E8[0:8, kt * 128:(kt + 1) * 128], lambda kt, c, lo, hi: negsel8[:, c * 512 + lo:c * 512 + hi],
                            [E8, negsel8]),
                           vdx, ident, STps, PT, Oacc, make_fin(512 + 64 * h), 'moba')
        S.dma('sp', dr['o_tok'][tb:tb + SEQ, :].rearrange("(n p) c -> p n c", p=128), otok[:],
              r=[otok], w=[DW(S, dr['o_tok'])])
    S.flush()


NCORES = 8
TPC = 2 * SEQ


def build_program(stages=None):
    nc = bass.Bass("TRN2", target_bir_lowering=False)
    ins = {}

    def din(name, shape, dt=F32):
        ins[name] = nc.dram_tensor(name, list(shape), dt, kind="ExternalInput").ap()
        return ins[name]

    def scr(name, shape, dt=BF16):
        return nc.dram_tensor(name, list(shape), dt, kind="Internal").ap()

    x = din('x', [TPC, D])
    pos = din('pos', [TPC], I32)
    cp = din('cpack', [128, CP_N])
    P = {}
    for L in range(2):
        for f in ('ffn1', 'ffn2'):
            P[f'{f}_norm{L}'] = din(f'{f}_norm{L}', [D])
            P[f'{f}_wg{L}'] = din(f'{f}_wg{L}', [D, DFF])
            P[f'{f}_wu{L}'] = din(f'{f}_wu{L}', [D, DFF])
            P[f'{f}_wd{L}'] = din(f'{f}_wd{L}', [DFF, D])
        P[f'mix_norm{L}'] = din(f'mix_norm{L}', [D])
    ev = {'w_in': din('ev_w_in', [D, 2468]), 'mix_norm': P['mix_norm0'], 'pos': pos,
          'sgu_norm': din('ev_sgu_norm', [256]), 'sgu_wT': din('ev_sgu_wT', [4, 128, 128]),
          'sgu_bT': din('ev_sgu_bT', [128, 4]),
          'cmp_w1_k': din('ev_w1k', [2048, 256]), 'cmp_w2_k': din('ev_w2k', [256, 64]),
          'cmp_posT_k': din('ev_pk', [64, 32]),
          'cmp_w1_v': din('ev_w1v', [2048, 256]), 'cmp_w2_v': din('ev_w2v', [256, 64]),
          'cmp_posT_v': din('ev_pv', [64, 32])}
    ev_w_out = din('ev_w_out', [D, D])
    od = {'w_in': din('od_w_in', [D, 2340]), 'mix_norm': P['mix_norm1'], 'pos': pos}
    od_w_out = din('od_w_out', [D, D])
    fin_g = din('final_norm', [D])
    y = nc.dram_tensor('y', [TPC, D], F32, kind="ExternalOutput").ap()
    xa = scr('xa', [TPC, D], F32)
    xb = scr('xb', [TPC, D], F32)
    T_ = TPC
    dre = {'a_tok': scr('a_tok', [T_, 512]), 'qraw': scr('qraw', [768, T_]), 'qrot': scr('qrot', [768, T_]),
           'kc': scr('kc', [192, T_]), 'vc': scr('vc', [192, T_]), 'ksl': scr('ksl', [192, T_]),
           'kw': scr('kw', [192, T_]), 'vsl': scr('vsl', [T_, 192]), 'vw': scr('vw', [T_, 192]),
           'gate': scr('gate', [T_, 36], F32), 'o_tok': scr('o_tok0', [T_, 1024])}
    dro = {'qc': scr('qc', [512, T_]), 'kcd': scr('kcd', [64, T_]), 'vcd': scr('vcd', [T_, 64]),
           'qi': scr('qi', [128, T_]), 'ki': scr('ki', [32, T_]), 'wi': scr('wi', [T_, 4], F32),
           'qd': scr('qd', [512, T_]), 'kd': scr('kd', [512, T_]), 'vdd': scr('vdd', [T_, 512]),
           'o_tok': scr('o_tok1', [T_, 1024])}
    S = Sched(nc)
    w16 = {'wg': scr('wg16', [128, DFF // 256, 8, 256]), 'wu': scr('wu16', [128, DFF // 256, 8, 256]),
           'wd': scr('wd16', [128, NFC, D])}

    def ffn(xi, xo, f, L, fg=None):
        ffn_stage(S, xi, xo, P[f'{f}_norm{L}'], P[f'{f}_wg{L}'], P[f'{f}_wu{L}'], P[f'{f}_wd{L}'], TPC, cp_d, fg, w16)
    cp_d = {'ident': cp[:, CP_OFF['ident'][0]:CP_OFF['ident'][0] + 128]}
    ffn(x, xa, 'ffn1', 0)
    even_proj(S, xa, ev, dre, TPC, cp)
    nsa_stage(S, ev, dre, 2, cp)
    outproj_stage(S, xa, xb, dre['o_tok'], ev_w_out, TPC, cp)
    ffn(xb, xa, 'ffn2', 0)
    ffn(xa, xb, 'ffn1', 1)
    odd_proj(S, xb, od, dro, TPC, cp)
    (dsa_moba_stage if USE_NEW_ODD else dsa_moba_stage_v1)(S, od, dro, 2, cp)
    outproj_stage(S, xb, xa, dro['o_tok'], od_w_out, TPC, cp)
    ffn(xa, y, 'ffn2', 1, fin_g)
    return nc, S


def kernel(**inp):
    inp = {k: np.asarray(v) for k, v in inp.items()}
    nc, S = build_program()
    cpk = host_consts()
    c = np.ascontiguousarray
    shared = {'cpack': cpk}
    for L in range(2):
        for f in ('ffn1', 'ffn2'):
            shared[f'{f}_norm{L}'] = c(inp[f'{f}_norm'][L])
            shared[f'{f}_wg{L}'] = c(inp[f'{f}_w_gate'][L])
            shared[f'{f}_wu{L}'] = c(inp[f'{f}_w_up'][L])
            shared[f'{f}_wd{L}'] = c(inp[f'{f}_w_down'][L])
        shared[f'mix_norm{L}'] = c(inp['mix_norm'][L])
    shared.update({
        'ev_w_in': c(inp['ev_w_in'][0]), 'ev_sgu_norm': c(inp['ev_sgu_norm'][0]),
        'ev_sgu_wT': c(inp['ev_sgu_w'][0].transpose(0, 2, 1)), 'ev_sgu_bT': c(inp['ev_sgu_b'][0].T),
        'ev_w1k': c(inp['ev_cmp_w1_k'][0]), 'ev_w2k': c(inp['ev_cmp_w2_k'][0]), 'ev_pk': c(inp['ev_cmp_pos_k'][0].T),
        'ev_w1v': c(inp['ev_cmp_w1_v'][0]), 'ev_w2v': c(inp['ev_cmp_w2_v'][0]), 'ev_pv': c(inp['ev_cmp_pos_v'][0].T),
        'ev_w_out': c(inp['ev_w_out'][0]), 'od_w_in': c(inp['od_w_in'][0]), 'od_w_out': c(inp['od_w_out'][0]),
        'final_norm': c(inp['final_norm'])})
    in_maps = []
    for k in range(NCORES):
        m = dict(shared)
        m['x'] = c(inp['x'][2 * k:2 * k + 2].reshape(TPC, D))
        m['pos'] = c(inp['positions'][2 * k:2 * k + 2].reshape(TPC).astype(np.int32))
        in_maps.append(m)
    res = run_bass_kernel_spmd(nc, in_maps, core_ids=list(range(NCORES)))
    out = np.stack([np.asarray(r['y']).reshape(2, SEQ, D) for r in res.results], axis=0)
    return out.reshape(16, SEQ, D).astype(np.float32)
```

```python
from contextlib import ExitStack
import numpy as np
import concourse.bass as bass
import concourse.mybir as mybir
from concourse.bass_utils import run_bass_kernel_spmd

F32 = mybir.dt.float32
BF16 = mybir.dt.bfloat16
I32 = mybir.dt.int32
AF = mybir.ActivationFunctionType
ALU = mybir.AluOpType
AX = mybir.AxisListType

ENGS = ['pe', 'act', 'dve', 'pool', 'sp']
import os
BANKDEP = False
USE_NEW_ODD = True
NDSEM = 66
NSWSEM = 26


class T:
    _n = 0

    def __init__(self, t, name=None):
        self.t = t
        T._n += 1
        self.id = T._n
        self.name = name

    def __getitem__(self, idx):
        return self.t[idx]


class Op:
    __slots__ = ('eng', 'fn', 'deps', 'isdma', 'sem', 'cnt', 'signal', 'waits', 'vc')


class Sched:
    def __init__(self, nc):
        self.nc = nc
        self.esem = {e: nc.alloc_semaphore(name=f'es_{e}') for e in ENGS}
        self.ecnt = {e: 0 for e in ENGS}
        self.free_dsems = {False: [nc.alloc_semaphore(name=f'ds_{i}') for i in range(NDSEM)],
                           True: [nc.alloc_semaphore(name=f'dw_{i}') for i in range(NSWSEM)]}
        self.dcnt = {}
        self.n_inst = 0
        self.base = {}
        self._reset()

    def _reset(self):
        self.ops = []
        self.state = {}
        self.buf_dsem = {}
        self.stack = ExitStack()

    def sb(self, shape, dtype, name=None):
        t = self.stack.enter_context(self.nc.sbuf_tensor(f'{name or "sb"}_{T._n}', list(shape), dtype))
        return T(t, name)

    def ps(self, shape, dtype, name=None):
        t = self.stack.enter_context(self.nc.psum_tensor(f'{name or "ps"}_{T._n}', list(shape), dtype))
        return T(t, name)

    @staticmethod
    def _norm(item):
        if isinstance(item, T):
            return item.id, None
        return item[0].id, item[1]

    def _track(self, r, w, opi):
        deps = {}
        for item in r:
            tid, key = self._norm(item)
            st = self.state.setdefault(tid, {})
            for k, ent in st.items():
                if k == key or k is None or key is None:
                    if ent[0] is not None:
                        deps[ent[0]] = True
            st.setdefault(key, [None, []])[1].append(opi)
        for item in w:
            tid, key = self._norm(item)
            st = self.state.setdefault(tid, {})
            for k, ent in st.items():
                if k == key or k is None or key is None:
                    if ent[0] is not None:
                        deps[ent[0]] = True
                    for x in ent[1]:
                        deps.setdefault(x, False)
            if key is None:
                st.clear()
            st[key] = [opi, []]
        deps.pop(opi, None)
        return deps

    @staticmethod
    def _skip(p, o, raw):
        if p.isdma or o.isdma or p.eng != o.eng:
            return False
        return p.eng == 'pe' or not raw

    def op(self, eng, fn, r=(), w=()):
        o = Op()
        o.eng, o.fn, o.isdma, o.signal = eng, fn, False, False
        o.sem, o.cnt, o.waits, o.vc = None, 0, None, None
        o.deps = self._track(r, w, len(self.ops))
        self.ops.append(o)
        return o

    def dma(self, eng, out_ap, in_ap, r, w, **kw):
        o = self.op(eng, lambda e: e.dma_start(out=out_ap, in_=in_ap, **kw), r, w)
        o.isdma = True
        it = w[0] if isinstance(w[0], T) else w[0][0]
        if it.t is None and len(r) > 0:
            it = r[0] if isinstance(r[0], T) else r[0][0]
        tid = (it.id, eng == 'pool')
        if tid not in self.buf_dsem:
            self.buf_dsem[tid] = self.free_dsems[eng == 'pool'].pop()
            if not hasattr(self, 'sem_names'):
                self.sem_names = {}
            self.sem_names[self.buf_dsem[tid]] = it.name
        o.sem = self.buf_dsem[tid]
        self.dcnt[o.sem] = self.dcnt.get(o.sem, 0) + 16
        o.cnt = self.dcnt[o.sem]
        return o

    def pe(self, fn, r=(), w=()):
        return self.op('pe', fn, r, w)

    def act(self, fn, r=(), w=()):
        return self.op('act', fn, r, w)

    def dve(self, fn, r=(), w=()):
        return self.op('dve', fn, r, w)

    def pool(self, fn, r=(), w=()):
        return self.op('pool', fn, r, w)

    def flush(self):
        nc, ops = self.nc, self.ops
        for o in ops:
            for d, raw in o.deps.items():
                p = ops[d]
                if p.isdma or self._skip(p, o, raw):
                    continue
                p.signal = True
        for o in ops:
            if not o.isdma and o.signal:
                self.ecnt[o.eng] += 1
                o.cnt = self.ecnt[o.eng]
                o.sem = self.esem[o.eng]
        known = {e: dict(self.base) for e in ENGS}
        for o in ops:
            kn = known[o.eng]
            waits = {}
            for d in sorted(o.deps, reverse=True):
                p = ops[d]
                if self._skip(p, o, o.deps[d]):
                    continue
                if kn.get(p.sem, 0) >= p.cnt:
                    continue
                if waits.get(p.sem, 0) < p.cnt:
                    waits[p.sem] = p.cnt
                for s, c in p.vc.items():
                    if kn.get(s, 0) < c:
                        kn[s] = c
                kn[p.sem] = p.cnt
            o.waits = list(waits.items())
            if o.isdma and o.cnt > 16 and kn.get(o.sem, 0) < o.cnt - 16 and getattr(self, 'diag', False):
                print('DMA overlap on sem', self.sem_names.get(o.sem), 'cnt', o.cnt, 'known', kn.get(o.sem, 0))
            if o.isdma or o.signal:
                o.vc = dict(kn)
        by = {e: [o for o in ops if o.eng == e] for e in ENGS}
        final_d = [(s, self.dcnt[s]) for s in set(self.buf_dsem.values())]
        self.n_inst += len(ops)

        def emit(e, lst):
            for o in lst:
                for s, c in o.waits:
                    e.wait_ge(s, c)
                ins = o.fn(e)
                if o.isdma:
                    ins.then_inc(o.sem, 16)
                elif o.signal:
                    ins.then_inc(o.sem, 1)

        with nc.Block() as block:
            @block.tensor
            def _(e):
                emit(e, by['pe'])

            @block.scalar
            def _(e):
                emit(e, by['act'])

            @block.vector
            def _(e):
                emit(e, by['dve'])

            @block.gpsimd
            def _(e):
                emit(e, by['pool'])

            @block.sync
            def _(e):
                emit(e, by['sp'])
                for s, c in final_d:
                    e.wait_ge(s, c)
        for (tid_, sw), s in self.buf_dsem.items():
            self.free_dsems[sw].append(s)
        self.base = dict(self.dcnt)
        for e in ENGS:
            self.base[self.esem[e]] = self.ecnt[e]
        self.stack.close()
        self._reset()


D = 1024
DFF = 2816
NFC = DFF // 128
SEQ = 2048
EPS = 1e-6


def load_consts(S, cpack):
    c = {}
    idf = S.sb([128, 128], F32, 'idf')
    S.dma('sp', idf[:], cpack['ident'], r=[], w=[idf])
    idb = S.sb([128, 128], BF16, 'idb')
    S.dve(lambda e: e.tensor_copy(out=idb[:], in_=idf[:]), r=[idf], w=[idb])
    c['ident'] = idb
    return c


def rms_rstd(S, xt, junk, ss, rstd, width, key=None):
    S.act(lambda e: e.activation(out=junk[:], in_=xt[:], func=AF.Square, accum_out=ss[:]),
          r=[xt], w=[junk, ss])
    S.act(lambda e: e.activation(out=rstd[:], in_=ss[:], func=AF.Sqrt, bias=EPS, scale=1.0 / width),
          r=[ss], w=[rstd])
    S.dve(lambda e: e.reciprocal(out=rstd[:], in_=rstd[:]), r=[rstd], w=[rstd])


def ffn_stage(S, x_in, x_out, g_ap, wg, wu, wd, ntok, cpack, final_g=None, w16=None):
    CH = 1024
    NT = CH // 128
    consts = load_consts(S, cpack)
    ident = consts['ident']
    gb = S.sb([128, D], F32, 'gb')
    S.dma('sp', gb[:], g_ap.partition_broadcast(128), r=[], w=[gb])
    if final_g is not None:
        fgb = S.sb([128, D], F32, 'fgb')
        S.dma('sp', fgb[:], final_g.partition_broadcast(128), r=[], w=[fgb])
    hT = S.sb([128, 8, CH], BF16, 'hT')
    actT = S.sb([128, NFC, CH], BF16, 'actT')
    wdb = S.sb([128, NFC, D], BF16, 'wdb')
    xt = [S.sb([128, D], F32, f'xt{i}') for i in range(2)]
    hb = [S.sb([128, D], BF16, f'hb{i}') for i in range(2)]
    junk = S.sb([128, D], BF16, 'junk')
    ss = [S.sb([128, 1], F32, f'ss{i}') for i in range(2)]
    rstd = [S.sb([128, 1], F32, f'rstd{i}') for i in range(2)]
    FB = 256
    NB = DFF // FB
    wgs = [S.sb([128, 8, FB], F32, f'wgs{i}') for i in range(2)]
    wus = [S.sb([128, 8, FB], F32, f'wus{i}') for i in range(2)]
    wgb = [S.sb([128, 8, FB], BF16, f'wgb{i}') for i in range(2)]
    wub = [S.sb([128, 8, FB], BF16, f'wub{i}') for i in range(2)]
    wds = [S.sb([128, D], F32, f'wds{i}') for i in range(2)]
    sg = [S.sb([128, 512], F32, f'sg{i}') for i in range(2)]
    ot = [S.sb([128, D], F32, f'ot{i}') for i in range(2)]
    ptr = [S.ps([128, 8, 128], BF16, f'ptr{i}') for i in range(2)]
    pg = [S.ps([128, 512], F32, f'pg{i}') for i in range(2)]
    pu = [S.ps([128, 512], F32, f'pu{i}') for i in range(2)]
    py = [S.ps([128, 512], F32, f'py{i}') for i in range(2)]
    wg_v = wg.rearrange("(c p) f -> p c f", p=128)
    wu_v = wu.rearrange("(c p) f -> p c f", p=128)
    wd_v = wd.rearrange("(c p) m -> p c m", p=128)

    xt3 = [S.sb([128, D], F32, f'xt3{i}') for i in range(2)]

    def phase1_tile(ch, i):
        t0 = ch * CH
        b = i % 2
        x_t, h_b = xt[b], hb[b]
        S.dma('sp', x_t[:], x_in[t0 + i * 128:t0 + (i + 1) * 128, :], r=[], w=[x_t])
        rms_rstd(S, x_t, junk, ss[b], rstd[b], D)
        S.dve(lambda e: e.scalar_tensor_tensor(
            out=h_b[:], in0=x_t[:], scalar=rstd[b][:, 0:1], in1=gb[:], op0=ALU.mult, op1=ALU.mult),
            r=[x_t, rstd[b], gb], w=[h_b])
        p = ptr[b]
        for c in range(8):
            S.pe(lambda e, c=c: e.transpose(out=p[:, c, :], in_=h_b[:, c * 128:(c + 1) * 128], identity=ident[:]),
                 r=[h_b, ident], w=[(p, c)])
        S.act(lambda e: e.copy(out=hT[:, :, i * 128:(i + 1) * 128], in_=p[:]), r=[p], w=[(hT, i // 4)])

    wdT = T(None, 'wd16')

    def load_wd(ch, fc):
        if ch == 0 or w16 is None:
            s_ = wds[fc % 2]
            S.dma('sp', s_[:], wd_v[:, fc, :], r=[], w=[s_])
            S.act(lambda e: e.copy(out=wdb[:, fc, :], in_=s_[:]), r=[s_], w=[(wdb, fc)])
            if w16 is not None:
                S.dma('pool', w16['wd'][:, fc, :], wdb[:, fc, :], r=[(wdb, fc)], w=[(wdT, fc)])
        else:
            S.dma('sp', wdb[:, fc, :], w16['wd'][:, fc, :], r=[(wdT, fc)], w=[(wdb, fc)])

    wgT = T(None, 'wg16')

    def phase2(ch):
        for fb in range(NB):
            b = fb % 2
            if ch == 0 or w16 is None:
                S.dma('sp', wgs[b][:], wg_v[:, :, fb * FB:(fb + 1) * FB], r=[], w=[wgs[b]])
                S.dma('sp', wus[b][:], wu_v[:, :, fb * FB:(fb + 1) * FB], r=[], w=[wus[b]])
                S.dve(lambda e, b=b: e.tensor_copy(out=wgb[b][:], in_=wgs[b][:]), r=[wgs[b]], w=[wgb[b]])
                S.dve(lambda e, b=b: e.tensor_copy(out=wub[b][:], in_=wus[b][:]), r=[wus[b]], w=[wub[b]])
                if w16 is not None:
                    S.dma('pool', w16['wg'][:, fb, :, :], wgb[b][:], r=[wgb[b]], w=[(wgT, ('g', fb))])
                    S.dma('pool', w16['wu'][:, fb, :, :], wub[b][:], r=[wub[b]], w=[(wgT, ('u', fb))])
            else:
                S.dma('sp', wgb[b][:], w16['wg'][:, fb, :, :], r=[(wgT, ('g', fb))], w=[wgb[b]])
                S.dma('sp', wub[b][:], w16['wu'][:, fb, :, :], r=[(wgT, ('u', fb))], w=[wub[b]])
            load_wd(ch, 2 * fb)
            load_wd(ch, 2 * fb + 1)
            for fs in range(FB // 128):
                fc = fb * (FB // 128) + fs
                for tb in range(CH // 512):
                    q = (fc * 2 + tb) % 2
                    for c in range(8):
                        S.pe(lambda e, q=q, b=b, c=c, fs=fs, tb=tb: e.matmul(
                            pg[q][:], lhsT=wgb[b][:, c, fs * 128:(fs + 1) * 128],
                            rhs=hT[:, c, tb * 512:(tb + 1) * 512], start=(c == 0), stop=(c == 7)),
                            r=[wgb[b], (hT, tb)], w=[pg[q]])
                    for c in range(8):
                        S.pe(lambda e, q=q, b=b, c=c, fs=fs, tb=tb: e.matmul(
                            pu[q][:], lhsT=wub[b][:, c, fs * 128:(fs + 1) * 128],
                            rhs=hT[:, c, tb * 512:(tb + 1) * 512], start=(c == 0), stop=(c == 7)),
                            r=[wub[b], (hT, tb)], w=[pu[q]])
                    S.act(lambda e, q=q: e.activation(out=sg[q][:], in_=pg[q][:], func=AF.Silu),
                          r=[pg[q]], w=[sg[q]])
                    S.dve(lambda e, q=q, fc=fc, tb=tb: e.tensor_tensor(
                        out=actT[:, fc, tb * 512:(tb + 1) * 512], in0=pu[q][:], in1=sg[q][:], op=ALU.mult),
                        r=[pu[q], sg[q]], w=[(actT, (fc, tb))])

    def phase3_tile(ch, i):
        t0 = ch * CH
        b = i % 2
        x_t, o_t = xt3[b], ot[b]
        S.dma('sp', x_t[:], x_in[t0 + i * 128:t0 + (i + 1) * 128, :], r=[], w=[x_t])
        for mh in range(2):
            p = py[mh]
            for fc in range(NFC):
                S.pe(lambda e, p=p, fc=fc, mh=mh: e.matmul(
                    p[:], lhsT=actT[:, fc, i * 128:(i + 1) * 128], rhs=wdb[:, fc, mh * 512:(mh + 1) * 512],
                    start=(fc == 0), stop=(fc == NFC - 1)),
                    r=[(actT, (fc, i // 4)), (wdb, fc)], w=[p])
            S.dve(lambda e, p=p, mh=mh: e.scalar_tensor_tensor(
                out=o_t[:, mh * 512:(mh + 1) * 512], in0=p[:], scalar=0.5, in1=x_t[:, mh * 512:(mh + 1) * 512],
                op0=ALU.mult, op1=ALU.add), r=[p, x_t], w=[(o_t, mh)])
        if final_g is not None:
            rms_rstd(S, o_t, junk, ss3[b], rstd3[b], D)
            S.dve(lambda e: e.scalar_tensor_tensor(
                out=o_t[:], in0=o_t[:], scalar=rstd3[b][:, 0:1], in1=fgb[:], op0=ALU.mult, op1=ALU.mult),
                r=[o_t, rstd3[b], fgb], w=[o_t])
        S.dma('pool', x_out[t0 + i * 128:t0 + (i + 1) * 128, :], o_t[:], r=[o_t], w=[DW(S, x_out)])

    ss3 = [S.sb([128, 1], F32, f'ss3{i}') for i in range(2)]
    rstd3 = [S.sb([128, 1], F32, f'rstd3{i}') for i in range(2)]
    nch = ntok // CH
    for i in range(NT):
        phase1_tile(0, i)
    for ch in range(nch):
        phase2(ch)
        for i in range(NT):
            phase3_tile(ch, i)
            if ch + 1 < nch:
                phase1_tile(ch + 1, i)
    S.flush()


_dram_T = {}
_dkey = [0]


def x_out_T(S, ap):
    k = ap.name
    if k not in _dram_T:
        _dram_T[k] = T(None, k)
    return _dram_T[k]


def DW(S, ap):
    _dkey[0] += 1
    return (x_out_T(S, ap), _dkey[0])


THETA = 500000.0
NEG = -30000.0


def _cpack_layout():
    items = [('ident', 128), ('tri_ge', 128), ('band_lt', 128), ('tril_st', 128), ('ones', 128),
             ('invf64', 1), ('nsgn64', 1), ('P64', 128), ('invf32', 1), ('nsgn32', 1), ('P32', 128),
             ('cmpbias', 2048), ('overlap', 32), ('E32', 2048), ('E8', 2048),
             ('keep', 512), ('addm', 512), ('adm', 512), ('tri_qs', 128), ('tri01', 128), ('band01', 128), ('mbias', 128), ('mpast', 128), ('mown', 128)]
    off, o = {}, 0
    for k, n in items:
        off[k] = (o, n)
        o += n
    return off, o


CP_OFF, CP_N = _cpack_layout()


def host_consts():
    cp = np.zeros((128, CP_N), np.float32)

    def put(k, a):
        o, n = CP_OFF[k]
        a = np.asarray(a, np.float32)
        cp[:a.shape[0], o:o + a.shape[1]] = a
    p = np.arange(128)
    put('ident', np.eye(128))
    kk, qq = p[:, None], p[None, :]
    put('tri_ge', np.where(qq >= kk, 0.0, NEG))
    put('band_lt', np.where(qq < kk, 0.0, NEG))
    put('tri01', (qq >= kk).astype(np.float32))
    put('band01', (qq < kk).astype(np.float32))
    put('tril_st', (kk <= qq).astype(np.float32))
    put('tri_qs', np.where(qq <= kk, 0.0, -1e30))
    put('ones', np.ones((128, 128)))
    m64 = p % 64
    put('invf64', np.where(m64 < 16, THETA ** (-(2.0 * (m64 % 8)) / 16.0), 0.0)[:, None])
    put('nsgn64', np.where(m64 < 8, -1.0, np.where(m64 < 16, 1.0, 0.0))[:, None])
    P = np.zeros((128, 128))
    for m in range(128):
        if m % 64 < 8:
            P[m + 8, m] = 1
        elif m % 64 < 16:
            P[m - 8, m] = 1
    put('P64', P)
    m32 = p % 32
    put('invf32', np.where(m32 < 8, THETA ** (-(2.0 * (m32 % 4)) / 8.0), 0.0)[:, None])
    put('nsgn32', np.where(m32 < 4, -1.0, np.where(m32 < 8, 1.0, 0.0))[:, None])
    P = np.zeros((128, 128))
    for m in range(128):
        if m % 32 < 4:
            P[m + 4, m] = 1
        elif m % 32 < 8:
            P[m - 4, m] = 1
    put('P32', P)
    n = np.arange(127)
    t = np.arange(2048)
    put('cmpbias', np.where(16 * n[:, None] + 31 <= t[None, :], 0.0, NEG))
    c0 = n * 16
    s0 = np.arange(32) * 64
    put('overlap', ((c0[:, None] < s0[None, :] + 64) & (c0[:, None] + 32 > s0[None, :])).astype(np.float32))
    put('E32', (t[None, :] // 64 == np.arange(32)[:, None]).astype(np.float32))
    put('E8', (t[None, :] // 256 == np.arange(8)[:, None]).astype(np.float32))
    tt = (np.arange(16)[None, :, None] * 128 + p[:, None, None])
    j = np.arange(32)[None, None, :]
    adm = j * 64 <= tt
    forced = (j == 0) | (j == tt // 64)
    put('keep', (adm & ~forced).astype(np.float32).reshape(128, 512))
    put('addm', np.where(adm, np.where(forced, 1e4, 0.0), -1e30).reshape(128, 512))
    put('adm', adm.astype(np.float32).reshape(128, 512))
    own = (np.arange(16)[None, :, None] * 128 + p[:, None, None]) // 256
    j8 = np.arange(8)[None, None, :]
    put('mbias', np.where(j8 < own, 0.0, -1e30).reshape(128, 128))
    put('mpast', (j8 < own).astype(np.float32).reshape(128, 128))
    put('mown', (j8 == own).astype(np.float32).reshape(128, 128))
    return cp


class Consts:
    def __init__(self, S, cpack_ap):
        self.S, self.ap, self.cache = S, cpack_ap, {}

    def get(self, k, dtype=F32, rows=128):
        key = (k, dtype)
        if key in self.cache:
            return self.cache[key]
        S = self.S
        o, n = CP_OFF[k]
        if dtype == F32:
            f = S.sb([128, n], F32, 'c_' + k)
            S.dma('sp', f[:], self.ap[:, o:o + n], r=[], w=[f], allow_slow_non_contiguous=(n == 1))
            self.cache[key] = f
            return f
        if not hasattr(self, 'stg'):
            self.stg = S.sb([128, 2048], F32, 'c_stg')
        f = self.stg
        S.dma('sp', f[:, 0:n], self.ap[:, o:o + n], r=[], w=[f])
        b = S.sb([128, n], dtype, 'cb_' + k)
        S.dve(lambda e: e.tensor_copy(out=b[:], in_=f[:, 0:n]), r=[f], w=[b])
        self.cache[key] = b
        return b


def rope_tables(S, C, pos_ap, ntok, invk, sgnk, tmp):
    invf = C.get(invk)
    nsg = C.get(sgnk)
    if 'pi' not in tmp:
        tmp['pi'] = S.sb([128, 1024], I32, 'pos_i')
        tmp['ang'] = S.sb([128, 1024], F32, 'ang')
        tmp['kf'] = S.sb([128, 1024], F32, 'kf')
        tmp['ki'] = S.sb([128, 1024], I32, 'ki')
    pi_, ang, kf, ki = tmp['pi'], tmp['ang'], tmp['kf'], tmp['ki']
    ct = S.sb([128, ntok], F32, 'ropeC')
    st = S.sb([128, ntok], F32, 'ropeS')
    TWO_PI = 2.0 * np.pi
    for c0 in range(0, ntok, 1024):
        S.dma('sp', pi_[:], pos_ap[c0:c0 + 1024].partition_broadcast(128), r=[], w=[pi_])
        S.dve(lambda e: e.tensor_copy(out=ang[:], in_=pi_[:]), r=[pi_], w=[ang])
        S.dve(lambda e: e.tensor_scalar(out=ang[:], in0=ang[:], scalar1=invf[:, 0:1], scalar2=None, op0=ALU.mult),
              r=[ang, invf], w=[ang])

        def reduce_sin(dst, shift, post, c0=c0):
            S.dve(lambda e: e.tensor_scalar(out=kf[:], in0=ang[:], scalar1=shift, scalar2=1.0 / TWO_PI,
                                            op0=ALU.add, op1=ALU.mult), r=[ang], w=[kf])
            S.dve(lambda e: e.tensor_copy(out=ki[:], in_=kf[:]), r=[kf], w=[ki])
            S.dve(lambda e: e.tensor_copy(out=kf[:], in_=ki[:]), r=[ki], w=[kf])
            S.dve(lambda e: e.scalar_tensor_tensor(out=kf[:], in0=kf[:], scalar=-TWO_PI, in1=ang[:],
                                                   op0=ALU.mult, op1=ALU.add), r=[kf, ang], w=[kf])
            S.dve(lambda e: e.tensor_scalar(out=kf[:], in0=kf[:], scalar1=shift, scalar2=3.14159, op0=ALU.add,
                                            op1=ALU.min), r=[kf], w=[kf])
            S.dve(lambda e: e.tensor_scalar(out=kf[:], in0=kf[:], scalar1=-3.14159, scalar2=None, op0=ALU.max),
                  r=[kf], w=[kf])
            S.act(lambda e: e.activation(out=dst[:, c0:c0 + 1024], in_=kf[:], func=AF.Sin), r=[kf], w=[(dst, c0)])
            if post is not None:
                S.dve(lambda e: e.tensor_scalar(out=dst[:, c0:c0 + 1024], in0=dst[:, c0:c0 + 1024],
                                                scalar1=post[:, 0:1], scalar2=None, op0=ALU.mult),
                      r=[(dst, c0), post], w=[(dst, c0)])
        reduce_sin(ct, np.pi / 2.0, None)
        reduce_sin(st, 0.0, nsg)
    return ct, st


def proj_stage(S, x, w_in, nin, g_ap, pos, ntok, cpack, fm_specs, tm_groups, tm_post, use_idx=False):
    C = Consts(S, cpack)
    ident = C.get('ident', BF16)
    gb = S.sb([128, D], F32, 'gb')
    S.dma('sp', gb[:], g_ap.partition_broadcast(128), r=[], w=[gb])
    winb = S.sb([128, 8, nin], BF16, 'winb')
    wv = w_in.rearrange("(c p) f -> p c f", p=128)
    wst = [S.sb([128, 8, 256], F32, f'wst{i}') for i in range(2)]
    for bi, c0 in enumerate(range(0, nin, 256)):
        n = min(256, nin - c0)
        st = wst[bi % 2]
        S.dma('sp', st[:, :, 0:n], wv[:, :, c0:c0 + n], r=[], w=[st])
        S.dve(lambda e, st=st, c0=c0, n=n: e.tensor_copy(out=winb[:, :, c0:c0 + n], in_=st[:, :, 0:n]),
              r=[st], w=[(winb, bi)])
    ropes = {}
    rtmp = {}
    if any(sp[3] == 'r64' for sp in fm_specs):
        ct, sn = rope_tables(S, C, pos, ntok, 'invf64', 'nsgn64', rtmp)
        ropes['r64'] = (ct, sn, C.get('P64', BF16))
    if use_idx:
        ct, sn = rope_tables(S, C, pos, ntok, 'invf32', 'nsgn32', rtmp)
        ropes['r32'] = (ct, sn, C.get('P32', BF16))
    xt = [S.sb([128, D], F32, f'xt{i}') for i in range(2)]
    hb = [S.sb([128, D], BF16, f'hb{i}') for i in range(2)]
    junk = S.sb([128, D], F32, 'junk')
    ss = [S.sb([128, 1], F32, f'ss{i}') for i in range(2)]
    rstd = [S.sb([128, 1], F32, f'rstd{i}') for i in range(2)]
    hT = [S.sb([128, 8, 512], BF16, f'hT{i}') for i in range(2)]
    xh = [S.sb([128, 512], BF16, f'xh{i}') for i in range(2)]
    t1 = [S.sb([128, 512], F32, f't1{i}') for i in range(2)]
    t2 = [S.sb([128, 512], F32, f't2{i}') for i in range(2)]
    xr = [S.sb([128, 512], BF16, f'xr{i}') for i in range(2)]
    ptr = [S.ps([128, 8, 128], BF16, f'ptr{i}') for i in range(2)]
    pf = [S.ps([128, 512], F32, f'pf{i}') for i in range(2)]
    p2 = [S.ps([128, 512], F32, f'p2{i}') for i in range(2)]
    pt = [S.ps([128, 512], F32, f'pt{i}') for i in range(2)]
    nfm = 0
    ntm = 0
    for blk in range(ntok // 512):
        t0 = blk * 512
        hTb = hT[blk % 2]
        for i in range(4):
            b = i % 2
            x_t, h_b = xt[b], hb[b]
            S.dma('sp', x_t[:], x[t0 + i * 128:t0 + (i + 1) * 128, :], r=[], w=[x_t])
            rms_rstd(S, x_t, junk, ss[b], rstd[b], D)
            S.dve(lambda e, x_t=x_t, h_b=h_b, b=b: e.scalar_tensor_tensor(
                out=h_b[:], in0=x_t[:], scalar=rstd[b][:, 0:1], in1=gb[:], op0=ALU.mult, op1=ALU.mult),
                r=[x_t, rstd[b], gb], w=[h_b])
            p = ptr[b]
            for c in range(8):
                S.pe(lambda e, p=p, h_b=h_b, c=c: e.transpose(out=p[:, c, :], in_=h_b[:, c * 128:(c + 1) * 128],
                                                               identity=ident[:]), r=[h_b, ident], w=[(p, c)])
            S.act(lambda e, p=p, i=i, hTb=hTb: e.copy(out=hTb[:, :, i * 128:(i + 1) * 128], in_=p[:]),
                  r=[p], w=[(hTb, i)])
        for (col0, M, scale, rope, raw_dst, rot_dst) in fm_specs:
            q = nfm % 2
            nfm += 1
            pp = pf[q]
            for c in range(8):
                S.pe(lambda e, pp=pp, c=c, col0=col0, M=M, hTb=hTb: e.matmul(
                    pp[0:M, :], lhsT=winb[:, c, col0:col0 + M], rhs=hTb[:, c, :], start=(c == 0), stop=(c == 7)),
                    r=[winb, hTb], w=[pp])
            xq = xh[q]
            S.act(lambda e, xq=xq, pp=pp, M=M, scale=scale: e.mul(out=xq[0:M, :], in_=pp[0:M, :], mul=scale),
                  r=[pp], w=[xq])
            if raw_dst is not None:
                S.dma('pool', raw_dst[:, t0:t0 + 512], xq[0:M, :], r=[xq], w=[DW(S, raw_dst)])
            if rope is not None:
                ct, sn, Pm = ropes[rope]
                pq = p2[q]
                S.pe(lambda e, pq=pq, xq=xq, M=M, Pm=Pm: e.matmul(pq[0:M, :], lhsT=Pm[0:M, 0:M], rhs=xq[0:M, :],
                                                                  start=True, stop=True), r=[xq, Pm], w=[pq])
                S.dve(lambda e, q=q, xq=xq, M=M, ct=ct, t0=t0: e.tensor_tensor(
                    out=t1[q][0:M, :], in0=xq[0:M, :], in1=ct[0:M, t0:t0 + 512], op=ALU.mult),
                    r=[xq, ct], w=[t1[q]])
                S.dve(lambda e, q=q, pq=pq, M=M, sn=sn, t0=t0: e.tensor_tensor(
                    out=t2[q][0:M, :], in0=pq[0:M, :], in1=sn[0:M, t0:t0 + 512], op=ALU.mult),
                    r=[pq, sn], w=[t2[q]])
                S.dve(lambda e, q=q, M=M: e.tensor_tensor(out=xr[q][0:M, :], in0=t1[q][0:M, :], in1=t2[q][0:M, :],
                                                         op=ALU.add), r=[t1[q], t2[q]], w=[xr[q]])
                S.dma('pool', rot_dst[:, t0:t0 + 512], xr[q][0:M, :], r=[xr[q]], w=[DW(S, rot_dst)])
        for i in range(4):
            for gi, (col0, N) in enumerate(tm_groups):
                q = ntm % 2
                ntm += 1
                pp = pt[q]
                for c in range(8):
                    S.pe(lambda e, pp=pp, c=c, col0=col0, N=N, i=i, hTb=hTb: e.matmul(
                        pp[:, 0:N], lhsT=hTb[:, c, i * 128:(i + 1) * 128], rhs=winb[:, c, col0:col0 + N],
                        start=(c == 0), stop=(c == 7)), r=[winb, (hTb, i)], w=[pp])
                tm_post(gi, pp, t0 + i * 128, ntm)
    S.flush()


def even_proj(S, x, prm, dr, ntok, cpack):
    at = [S.sb([128, 512], BF16, f'at{i}') for i in range(2)]
    g1 = [S.sb([128, 512], F32, f'g1{i}') for i in range(2)]
    vb = [S.sb([128, 192], BF16, f'vb{i}') for i in range(2)]
    vb2 = [S.sb([128, 192], BF16, f'vb2{i}') for i in range(2)]
    gt = [S.sb([128, 36], F32, f'gt{i}') for i in range(2)]
    sgb = S.sb([128, 256], F32, 'sgb')
    S.dma('sp', sgb[:], prm['sgu_norm'].partition_broadcast(128), r=[], w=[sgb])
    junk = S.sb([128, 256], F32, 'junk2')
    ss = [S.sb([128, 1], F32, f'ssv{i}') for i in range(2)]
    rs = [S.sb([128, 1], F32, f'rsv{i}') for i in range(2)]
    cnt = [0]

    def tm_post(gi, pp, tok0, n):
        if gi == 0:
            b = cnt[0] % 2
            cnt[0] += 1
            g, a = g1[b], at[b]
            S.act(lambda e: e.activation(out=g[:], in_=pp[:], func=AF.Gelu_apprx_tanh), r=[pp], w=[g])
            S.pool(lambda e: e.tensor_copy(out=a[:, 0:256], in_=g[:, 0:256]), r=[g], w=[(a, 0)])
            S.act(lambda e: e.activation(out=junk[:], in_=g[:, 256:512], func=AF.Square, accum_out=ss[b][:]),
                  r=[g], w=[junk, ss[b]])
            S.act(lambda e: e.activation(out=rs[b][:], in_=ss[b][:], func=AF.Sqrt, bias=EPS, scale=1.0 / 256),
                  r=[ss[b]], w=[rs[b]])
            S.dve(lambda e: e.reciprocal(out=rs[b][:], in_=rs[b][:]), r=[rs[b]], w=[rs[b]])
            S.dve(lambda e: e.scalar_tensor_tensor(out=a[:, 256:512], in0=g[:, 256:512], scalar=rs[b][:, 0:1],
                                                   in1=sgb[:], op0=ALU.mult, op1=ALU.mult),
                  r=[g, rs[b], sgb], w=[(a, 1)])
            S.dma('pool', dr['a_tok'][tok0:tok0 + 128, :], a[:], r=[a], w=[DW(S, dr['a_tok'])])
        elif gi == 1:
            v = vb[(tok0 // 128) % 2]
            S.act(lambda e: e.copy(out=v[:], in_=pp[:, 0:192]), r=[pp], w=[v])
            S.dma('pool', dr['vsl'][tok0:tok0 + 128, :], v[:], r=[v], w=[DW(S, dr['vsl'])])
        else:
            v = vb2[(tok0 // 128) % 2]
            g = gt[(tok0 // 128) % 2]
            S.act(lambda e: e.copy(out=v[:], in_=pp[:, 0:192]), r=[pp], w=[v])
            S.act(lambda e: e.activation(out=g[:], in_=pp[:, 192:228], func=AF.Sigmoid), r=[pp], w=[g])
            S.dma('pool', dr['vw'][tok0:tok0 + 128, :], v[:], r=[v], w=[DW(S, dr['vw'])])
            S.dma('pool', dr['gate'][tok0:tok0 + 128, :], g[:], r=[g], w=[DW(S, dr['gate'])])
    fm = []
    for i in range(6):
        fm.append((512 + 128 * i, 128, 0.125, 'r64', dr['qraw'][128 * i:128 * (i + 1), :],
                   dr['qrot'][128 * i:128 * (i + 1), :]))
    for nm, c0, rope in (('kc', 1280, None), ('vc', 1472, None), ('ksl', 1664, 'r64'), ('kw', 2048, 'r64')):
        for (o, M) in ((0, 128), (128, 64)):
            dst = dr[nm][o:o + M, :]
            fm.append((c0 + o, M, 1.0, rope, dst if rope is None else None, dst if rope else None))
    proj_stage(S, x, prm['w_in'], 2468, prm['mix_norm'], prm['pos'], ntok, cpack, fm,
               [(0, 512), (1856, 192), (2240, 228)], tm_post)


def attn_chunk(S, c, tiles, qT, kT, extra, vext, STps, PT, Oacc, finalize):
    cover = {}
    for n, (kt, lo, hi, bt, blo) in enumerate(tiles):
        for j in range(lo // 128, hi // 128):
            cover.setdefault(j, []).append(n)
    NP = len(PT)

    def qk(n):
        kt, lo, hi, bt, blo = tiles[n]
        ps = STps[n % 2]
        S.pe(lambda e: e.matmul(ps[:, lo:hi], lhsT=kT[:, kt * 128:(kt + 1) * 128],
                                rhs=qT[:, c * 512 + lo:c * 512 + hi], start=True, stop=(extra is None)),
             r=[kT, qT], w=[ps])
        if extra:
            S.pe(lambda e: e.matmul(ps[:, lo:hi], lhsT=extra[0](kt), rhs=extra[1](kt, c, lo, hi),
                                    start=False, stop=True, skip_group_check=True), r=list(extra[2]), w=[ps])

    qk(0)
    for n, (kt, lo, hi, bt, blo) in enumerate(tiles):
        if n + 1 < len(tiles):
            qk(n + 1)
        ps, p = STps[n % 2], PT[n % NP]
        S.act(lambda e, ps=ps, p=p, lo=lo, hi=hi: e.activation(out=p[:, lo:hi], in_=ps[:, lo:hi], func=AF.Exp),
              r=[ps], w=[p])
        if bt is not None:
            S.dve(lambda e, p=p, bt=bt, blo=blo: e.tensor_tensor(out=p[:, blo:blo + 128], in0=p[:, blo:blo + 128],
                                                                 in1=bt[:], op=ALU.mult), r=[p, bt], w=[p])
        for j in range(lo // 128, hi // 128):
            S.pe(lambda e, p=p, j=j, kt=kt, n=n: e.matmul(
                Oacc[j][:, 0:65], lhsT=p[:, j * 128:(j + 1) * 128], rhs=vext[:, kt, :],
                start=(cover[j][0] == n), stop=(cover[j][-1] == n), skip_group_check=True),
                r=[p, vext], w=[Oacc[j]])
            if cover[j][-1] == n:
                finalize(c, j, Oacc[j])


def attn_run(S, jobs, STps, PT, Oacc, LA=2):
    flat = []
    for ji, jb in enumerate(jobs):
        cover = {}
        for n, (kt, lo, hi, bt, blo) in enumerate(jb['tiles']):
            for j in range(lo // 128, hi // 128):
                cover.setdefault(j, []).append(n)
        jb['cover'] = cover
        for n in range(len(jb['tiles'])):
            flat.append((ji, n))
    NS, NP = len(STps), len(PT)

    def qk(f):
        ji, n = flat[f]
        jb = jobs[ji]
        if n == 0 and jb.get('pre') is not None:
            jb['pre']()
        kt, lo, hi, bt, blo = jb['tiles'][n]
        c, qT, kT, extra = jb['c'], jb['qT'], jb['kT'], jb.get('extra')
        ps = STps[f % NS]
        S.pe(lambda e: e.matmul(ps[:, lo:hi], lhsT=kT[:, kt * 128:(kt + 1) * 128],
                                rhs=qT[:, c * 512 + lo:c * 512 + hi], start=True, stop=(extra is None)),
             r=[kT, qT], w=[ps])
        if extra:
            S.pe(lambda e: e.matmul(ps[:, lo:hi], lhsT=extra[0](kt), rhs=extra[1](kt, c, lo, hi),
                                    start=False, stop=True), r=list(extra[2]), w=[ps])

    for f in range(min(LA, len(flat))):
        qk(f)
    for f, (ji, n) in enumerate(flat):
        if f + LA < len(flat):
            qk(f + LA)
        jb = jobs[ji]
        kt, lo, hi, bt, blo = jb['tiles'][n]
        cover, vext = jb['cover'], jb['vext']
        ps, p = STps[f % NS], PT[f % NP]
        S.act(lambda e, ps=ps, p=p, lo=lo, hi=hi: e.activation(out=p[:, lo:hi], in_=ps[:, lo:hi], func=AF.Exp),
              r=[ps], w=[p])
        if bt is not None:
            S.pool(lambda e, p=p, bt=bt, blo=blo: e.tensor_tensor(out=p[:, blo:blo + 128], in0=p[:, blo:blo + 128],
                                                                  in1=bt[:], op=ALU.mult), r=[p, bt], w=[p])
        for j in range(lo // 128, hi // 128):
            O = Oacc[j]
            S.pe(lambda e, p=p, j=j, kt=kt, O=O, vext=vext, first=(cover[j][0] == n), last=(cover[j][-1] == n):
                 e.matmul(O[:, 0:65], lhsT=p[:, j * 128:(j + 1) * 128], rhs=vext[:, kt, :], start=first, stop=last),
                 r=[p, vext], w=[O])
            if cover[j][-1] == n:
                jb['fin'](jb['c'], j, O, 0)


def causal_tiles(c, tri):
    out = []
    for kt in range(4 * c + 4):
        if kt < 4 * c:
            out.append((kt, 0, 512, None, 0))
        else:
            lo = (kt - 4 * c) * 128
            out.append((kt, lo, 512, tri, lo))
    return out


def window_tiles(c, tri, band):
    out = []
    if c >= 1:
        out.append((4 * c - 1, 0, 512, band, 384))
        for i in range(3):
            out.append((4 * c - 4 + i, 0, 128 * (i + 1), band, 128 * i))
    for i in range(4):
        out.append((4 * c + i, 128 * i, 512, tri, 128 * i))
    return out


def nsa_stage(S, prm, dr, nseq, cpack):
    C = Consts(S, cpack)
    ident = C.get('ident', BF16)
    tri = C.get('tri01', BF16)
    band = C.get('band01', BF16)
    ones = C.get('ones', BF16)
    cmpb = C.get('cmpbias', BF16)
    ovl = C.get('overlap', F32)
    E32 = C.get('E32', F32)
    keep, addm, adm = C.get('keep'), C.get('addm'), C.get('adm')
    tril = C.get('tril_st', F32)
    wTf = S.sb([128, 4, 128], F32, 'wTf')
    S.dma('sp', wTf[:], prm['sgu_wT'].rearrange("g s t -> s g t"), r=[], w=[wTf])
    wTb = S.sb([128, 4, 128], BF16, 'wTb')
    for g in range(4):
        S.dve(lambda e, g=g: e.tensor_tensor(out=wTb[:, g, :], in0=wTf[:, g, :], in1=tril[:], op=ALU.mult),
              r=[wTf, tril], w=[(wTb, g)])
    bT = S.sb([128, 4], F32, 'bT')
    S.dma('sp', bT[:], prm['sgu_bT'], r=[], w=[bT])
    cw = {}
    stg = [S.sb([64, 4, 256], F32, f'w1s{i}') for i in range(2)]
    w2s = S.sb([128, 2, 64], F32, 'w2s')
    pst = S.sb([64, 32], F32, 'pst')
    pm0 = S.ps([128, 512], F32, 'pm0')
    pmisc = [pm0, pm0]
    ptb = S.ps([128, 1024], BF16, 'ptb')
    k = 0
    for kv in ('k', 'v'):
        w1b = S.sb([64, 32, 256], BF16, 'w1b' + kv)
        w1v = prm['cmp_w1_' + kv].rearrange("(l e) c -> e l c", e=64)
        for q4 in range(8):
            st = stg[k % 2]
            k += 1
            S.dma('sp', st[:], w1v[:, q4 * 4:(q4 + 1) * 4, :], r=[], w=[st])
            S.dve(lambda e, st=st, w1b=w1b, q4=q4: e.tensor_copy(out=w1b[:, q4 * 4:(q4 + 1) * 4, :], in_=st[:]),
                  r=[st], w=[(w1b, q4)])
        w2b = S.sb([128, 2, 64], BF16, 'w2b' + kv)
        S.dma('sp', w2s[:], prm['cmp_w2_' + kv].rearrange("(c p) e -> p c e", p=128), r=[], w=[w2s])
        S.dve(lambda e, w2b=w2b: e.tensor_copy(out=w2b[:], in_=w2s[:]), r=[w2s], w=[w2b])
        posb = S.sb([64, 32], BF16, 'posb' + kv)
        S.dma('sp', pst[:], prm['cmp_posT_' + kv], r=[], w=[pst])
        S.dve(lambda e, posb=posb: e.tensor_copy(out=posb[:], in_=pst[:]), r=[pst], w=[posb])
        pbias = S.sb([128, 2], F32, 'pbias' + kv)
        for cc in range(2):
            pp = pmisc[cc]
            for l in range(32):
                S.pe(lambda e, pp=pp, w1b=w1b, posb=posb, l=l, cc=cc: e.matmul(
                    pp[:, 0:1], lhsT=w1b[:, l, cc * 128:(cc + 1) * 128], rhs=posb[:, l:l + 1],
                    start=(l == 0), stop=(l == 31)), r=[w1b, posb], w=[pp])
            S.dve(lambda e, pp=pp, pbias=pbias, cc=cc: e.tensor_copy(out=pbias[:, cc:cc + 1], in_=pp[:, 0:1]),
                  r=[pp], w=[(pbias, cc)])
        cw[kv] = (w1b, w2b, pbias)
    atok = S.sb([128, 16, 512], BF16, 'atok')
    gates = S.sb([128, 16, 36], F32, 'gates')
    otok = S.sb([128, 16, 1024], BF16, 'otok')
    accg = S.sb([128, 16, 256], F32, 'accg')
    kcT = S.sb([64, SEQ], BF16, 'kcT')
    vcT = S.sb([64, SEQ], BF16, 'vcT')
    kslT = S.sb([128, SEQ], BF16, 'kslT')
    kwT = S.sb([128, SEQ], BF16, 'kwT')
    S.dve(lambda e: e.memset(kslT[:], 0.0), r=[], w=[kslT])
    S.pool(lambda e: e.memset(kwT[:], 0.0), r=[], w=[kwT])
    S.dve(lambda e: e.tensor_copy(out=kslT[64:96, :], in_=E32[0:32, :]), r=[E32], w=[(kslT, 'e')])
    vsl = S.sb([128, 16, 65], BF16, 'vslx')
    vw = S.sb([128, 16, 65], BF16, 'vwx')
    S.pool(lambda e: e.memset(vsl[:], 1.0), r=[], w=[vsl])
    S.pool(lambda e: e.memset(vw[:], 1.0), r=[], w=[vw])
    qraw = S.sb([128, SEQ], BF16, 'qrawT')
    qrots = [S.sb([128, SEQ], BF16, f'qrotT{i}') for i in range(2)]
    S.pool(lambda e: e.memset(qraw[:], 0.0), r=[], w=[qraw])
    S.dve(lambda e: e.memset(qrots[0][:], 0.0), r=[], w=[qrots[0]])
    S.pool(lambda e: e.memset(qrots[1][:], 0.0), r=[], w=[qrots[1]])
    ghT = S.sb([128, 2, 128], BF16, 'ghT')
    kcmpT = S.sb([128, 128], BF16, 'kcmpT')
    S.dve(lambda e: e.memset(kcmpT[:], 0.0), r=[], w=[kcmpT])
    vcmp = S.sb([128, 64], BF16, 'vcmp')
    psumT = S.sb([128, SEQ], F32, 'psumT')
    pcT = [S.sb([128, 512], BF16, f'pcT{i}') for i in range(2)]
    rinv = [S.sb([128, 512], F32, f'rinv{i}') for i in range(2)]
    pnf = [S.sb([128, 512], F32, f'pnf{i}') for i in range(2)]
    pnb = [S.sb([128, 512], BF16, f'pnb{i}') for i in range(2)]
    PT = [S.sb([128, 512], BF16, f'PT{i}') for i in range(4)]
    gm = [S.sb([128, 256], F32, f'gm{i}') for i in range(2)]
    sel = [S.sb([128, 32], F32, f'sel{i}') for i in range(2)]
    m8 = [S.sb([128, 8], F32, f'm8{i}') for i in range(2)]
    nsel = [S.sb([128, 32], BF16, f'nsel{i}') for i in range(2)]
    fsc = [S.sb([128, 1], F32, f'fsc{i}') for i in range(4)]
    ftmp = [S.sb([128, 64], F32, f'ftmp{i}') for i in range(4)]
    STps = [S.ps([128, 512], F32, f'st{i}') for i in range(2)] + [pm0]
    Oacc = [S.ps([128, 512], F32, f'oa{i}') for i in range(4)]
    fcount = [0]

    def make_finalize(r, gidx, first):
        def fin(c, j, O, off):
            i = 4 * c + j
            f = fsc[fcount[0] % 4]
            fcount[0] += 1
            tm = ftmp[fcount[0] % 4]
            S.dve(lambda e: e.reciprocal(out=f[:], in_=O[:, off + 64:off + 65]), r=[O], w=[f])
            S.dve(lambda e: e.tensor_scalar(out=tm[:], in0=O[:, off:off + 64], scalar1=f[:, 0:1],
                                            scalar2=gates[:, i, gidx:gidx + 1], op0=ALU.mult, op1=ALU.mult),
                  r=[O, f, gates], w=[tm])
            S.pool(lambda e: e.tensor_tensor(out=accg[:, i, r * 64:(r + 1) * 64], in0=accg[:, i, r * 64:(r + 1) * 64],
                                             in1=tm[:], op=ALU.add), r=[tm, (accg, (i, r))], w=[(accg, (i, r))])
        return fin

    for sq in range(nseq):
        tb = sq * SEQ
        S.dma('sp', atok[:], dr['a_tok'][tb:tb + SEQ, :].rearrange("(n p) c -> p n c", p=128), r=[], w=[atok])
        S.dma('sp', gates[:], dr['gate'][tb:tb + SEQ, :].rearrange("(n p) c -> p n c", p=128), r=[], w=[gates])
        for i in range(16):
            pp = pmisc[i % 2]
            g_ = gm[i % 2]
            for g in range(4):
                S.pe(lambda e, pp=pp, g=g, i=i: e.matmul(pp[:, g * 64:(g + 1) * 64], lhsT=wTb[:, g, :],
                                                          rhs=atok[:, i, 256 + g * 64:256 + (g + 1) * 64],
                                                          start=True, stop=True), r=[wTb, atok], w=[pp])
                S.dve(lambda e, pp=pp, g_=g_, g=g: e.tensor_scalar(
                    out=g_[:, g * 64:(g + 1) * 64], in0=pp[:, g * 64:(g + 1) * 64], scalar1=bT[:, g:g + 1],
                    scalar2=None, op0=ALU.add), r=[pp, bT], w=[(g_, g)])
            S.dve(lambda e, g_=g_, i=i: e.tensor_tensor(out=otok[:, i, 0:256], in0=g_[:], in1=atok[:, i, 0:256],
                                                         op=ALU.mult), r=[g_, atok], w=[(otok, (i, 0))])
        for g in range(3):
            for nm, tl in (('kc', kcT), ('vc', vcT)):
                S.dma('sp', tl[:], dr[nm][g * 64:(g + 1) * 64, tb:tb + SEQ], r=[], w=[tl])
            for nm, tl in (('ksl', kslT), ('kw', kwT)):
                S.dma('sp', tl[0:64, :], dr[nm][g * 64:(g + 1) * 64, tb:tb + SEQ], r=[], w=[(tl, 'k')])
            for nm, tl in (('vsl', vsl), ('vw', vw)):
                S.dma('sp', tl[:, :, 0:64], dr[nm][tb:tb + SEQ, g * 64:(g + 1) * 64].rearrange(
                    "(n p) c -> p n c", p=128), r=[], w=[tl])
            for kv, src in (('k', kcT), ('v', vcT)):
                w1b, w2b, pbias = cw[kv]
                for cc in range(2):
                    pp = pmisc[cc]
                    for l in range(32):
                        S.pe(lambda e, pp=pp, w1b=w1b, src=src, l=l, cc=cc: e.matmul(
                            pp[:, 0:127], lhsT=w1b[:, l, cc * 128:(cc + 1) * 128], rhs=src[:, l:l + 2017:16],
                            start=(l == 0), stop=(l == 31)), r=[w1b, src], w=[pp])
                    S.act(lambda e, pp=pp, cc=cc, pbias=pbias: e.activation(
                        out=ghT[:, cc, 0:127], in_=pp[:, 0:127], func=AF.Gelu_apprx_tanh, bias=pbias[:, cc:cc + 1]),
                        r=[pp, pbias], w=[(ghT, cc)])
                pp = pmisc[0]
                if kv == 'k':
                    for cc in range(2):
                        S.pe(lambda e, pp=pp, cc=cc, w2b=w2b: e.matmul(pp[0:64, 0:127], lhsT=w2b[:, cc, :],
                                                                     rhs=ghT[:, cc, 0:127], start=(cc == 0),
                                                                     stop=(cc == 1)), r=[w2b, ghT], w=[pp])
                    S.act(lambda e, pp=pp: e.copy(out=kcmpT[0:64, 0:127], in_=pp[0:64, 0:127]), r=[pp], w=[(kcmpT, 'k')])
                else:
                    for cc in range(2):
                        S.pe(lambda e, pp=pp, cc=cc, w2b=w2b: e.matmul(pp[0:127, 0:64], lhsT=ghT[:, cc, 0:127],
                                                                     rhs=w2b[:, cc, :], start=(cc == 0),
                                                                     stop=(cc == 1)), r=[w2b, ghT], w=[pp])
                    S.act(lambda e, pp=pp: e.copy(out=vcmp[0:127, :], in_=pp[0:127, 0:64]), r=[pp], w=[vcmp])
            for r in range(4):
                h = 4 * g + r
                S.dma('sp', qraw[0:64, :], dr['qraw'][h * 64:(h + 1) * 64, tb:tb + SEQ], r=[], w=[(qraw, 'q')])
                for c in range(4):
                    b = c % 2
                    ps = STps[b]
                    S.pe(lambda e, ps=ps, c=c: e.matmul(ps[0:127, :], lhsT=kcmpT[:, 0:127],
                                                        rhs=qraw[:, c * 512:(c + 1) * 512], start=True, stop=False),
                         r=[kcmpT, qraw], w=[ps])
                    S.pe(lambda e, ps=ps, c=c: e.matmul(ps[0:127, :], lhsT=ident[0:127, 0:127],
                                                        rhs=cmpb[0:127, c * 512:(c + 1) * 512], start=False, stop=True),
                         r=[ident, cmpb], w=[ps])
                    S.act(lambda e, ps=ps, b=b: e.activation(out=pcT[b][0:127, :], in_=ps[0:127, :], func=AF.Exp),
                          r=[ps], w=[pcT[b]])
                    pd = pmisc[b]
                    S.pe(lambda e, pd=pd, b=b: e.matmul(pd[0:127, :], lhsT=ones[0:127, 0:127], rhs=pcT[b][0:127, :],
                                                        start=True, stop=True), r=[ones, pcT[b]], w=[pd])
                    S.dve(lambda e, pd=pd, b=b: e.tensor_scalar(out=rinv[b][0:127, :], in0=pd[0:127, :], scalar1=1e-30,
                                                                scalar2=None, op0=ALU.max), r=[pd], w=[rinv[b]])
                    S.dve(lambda e, b=b: e.reciprocal(out=rinv[b][0:127, :], in_=rinv[b][0:127, :]),
                          r=[rinv[b]], w=[rinv[b]])
                    S.dve(lambda e, b=b: e.tensor_tensor(out=pnf[b][0:127, :], in0=pcT[b][0:127, :],
                                                         in1=rinv[b][0:127, :], op=ALU.mult),
                          r=[pcT[b], rinv[b]], w=[pnf[b]])
                    S.pool(lambda e, b=b: e.tensor_copy(out=pnb[b][0:127, :], in_=pnf[b][0:127, :]),
                           r=[pnf[b]], w=[pnb[b]])
                    if r == 0:
                        S.pool(lambda e, b=b, c=c: e.tensor_copy(out=psumT[0:127, c * 512:(c + 1) * 512],
                                                                 in_=pnf[b][0:127, :]), r=[pnf[b]], w=[(psumT, c)])
                    else:
                        S.pool(lambda e, b=b, c=c: e.tensor_tensor(
                            out=psumT[0:127, c * 512:(c + 1) * 512], in0=psumT[0:127, c * 512:(c + 1) * 512],
                            in1=pnf[b][0:127, :], op=ALU.add), r=[pnf[b], (psumT, c)], w=[(psumT, c)])
                    for j in range(4):
                        i = 4 * c + j
                        O = Oacc[j]
                        off = 0
                        S.pe(lambda e, O=O, b=b, j=j, off=off: e.matmul(
                            O[:, off:off + 64], lhsT=pnb[b][0:127, j * 128:(j + 1) * 128], rhs=vcmp[0:127, :],
                            start=True, stop=True), r=[pnb[b], vcmp], w=[O])
                        S.dve(lambda e, O=O, i=i, r=r, h=h, off=off: e.tensor_scalar(
                            out=accg[:, i, r * 64:(r + 1) * 64], in0=O[:, off:off + 64],
                            scalar1=gates[:, i, 3 * h:3 * h + 1], scalar2=None, op0=ALU.mult),
                            r=[O, gates], w=[(accg, (i, r))])
            for i in range(16):
                b = i % 2
                pp = pmisc[b]
                S.pe(lambda e, pp=pp, i=i: e.matmul(pp[:, 0:32], lhsT=psumT[0:127, i * 128:(i + 1) * 128],
                                                    rhs=ovl[0:127, 0:32], start=True, stop=True),
                     r=[psumT, ovl], w=[pp])
                sl = sel[b]
                S.dve(lambda e, pp=pp, sl=sl, i=i: e.tensor_tensor(out=sl[:], in0=pp[:, 0:32],
                                                                  in1=keep[:, i * 32:(i + 1) * 32], op=ALU.mult),
                      r=[pp, keep], w=[sl])
                S.dve(lambda e, sl=sl, i=i: e.tensor_tensor(out=sl[:], in0=sl[:], in1=addm[:, i * 32:(i + 1) * 32],
                                                           op=ALU.add), r=[sl, addm], w=[sl])
                S.dve(lambda e, sl=sl, b=b: e.max(out=m8[b][:], in_=sl[:]), r=[sl], w=[m8[b]])
                S.dve(lambda e, sl=sl, b=b: e.tensor_scalar(out=sl[:], in0=sl[:], scalar1=m8[b][:, 7:8], scalar2=None,
                                                            op0=ALU.is_ge), r=[sl, m8[b]], w=[sl])
                S.dve(lambda e, sl=sl, i=i: e.tensor_tensor(out=sl[:], in0=sl[:], in1=adm[:, i * 32:(i + 1) * 32],
                                                           op=ALU.mult), r=[sl, adm], w=[sl])
                S.dve(lambda e, sl=sl, b=b: e.tensor_scalar(out=nsel[b][:], in0=sl[:], scalar1=-NEG, scalar2=NEG,
                                                            op0=ALU.mult, op1=ALU.add), r=[sl], w=[nsel[b]])
                S.pe(lambda e, b=b: e.transpose(out=ptb[0:32, b * 128:(b + 1) * 128], in_=nsel[b][:],
                                                identity=ident[:]), r=[nsel[b], ident], w=[(ptb, b)])
                for qb in qrots:
                    S.dve(lambda e, i=i, b=b, qb=qb: e.tensor_copy(out=qb[64:96, i * 128:(i + 1) * 128],
                                                                   in_=ptb[0:32, b * 128:(b + 1) * 128]),
                          r=[(ptb, b)], w=[(qb, ('n', i))])
            def load_q(r, g=g, tb=tb):
                h = 4 * g + r
                qb = qrots[r % 2]
                S.dma('sp', qb[0:64, :], dr['qrot'][h * 64:(h + 1) * 64, tb:tb + SEQ], r=[], w=[(qb, 'q')])
            load_q(0)
            jobs = []
            for r in range(4):
                h = 4 * g + r
                qb = qrots[r % 2]
                for c in range(4):
                    jobs.append(dict(c=c, tiles=causal_tiles(c, tri), qT=qb, kT=kslT, vext=vsl,
                                     fin=make_finalize(r, 3 * h + 1, False),
                                     pre=((lambda r=r: load_q(r + 1)) if (c == 0 and r < 3) else None)))
                for c in range(4):
                    jobs.append(dict(c=c, tiles=window_tiles(c, tri, band), qT=qb, kT=kwT, vext=vw,
                                     fin=make_finalize(r, 3 * h + 2, False)))
            attn_run(S, jobs, STps, PT, Oacc)
            S.act(lambda e, g=g: e.copy(out=otok[:, :, 256 + 256 * g:256 + 256 * (g + 1)], in_=accg[:]),
                  r=[accg], w=[(otok, ('g', g))])
        S.dma('sp', dr['o_tok'][tb:tb + SEQ, :].rearrange("(n p) c -> p n c", p=128), otok[:],
              r=[otok], w=[DW(S, dr['o_tok'])])
    S.flush()


def outproj_stage(S, x_in, x_out, o_tok, w_out, ntok, cpack):
    C = Consts(S, cpack)
    ident = C.get('ident', BF16)
    wob = S.sb([128, 8, D], BF16, 'wob')
    wst = [S.sb([128, D], F32, f'wos{i}') for i in range(2)]
    wv = w_out.rearrange("(c p) m -> p c m", p=128)
    for c in range(8):
        st = wst[c % 2]
        S.dma('sp', st[:], wv[:, c, :], r=[], w=[st])
        S.dve(lambda e, st=st, c=c: e.tensor_copy(out=wob[:, c, :], in_=st[:]), r=[st], w=[(wob, c)])
    ot = [S.sb([128, D], BF16, f'oo{i}') for i in range(2)]
    oT = [S.sb([128, 8, 128], BF16, f'oT{i}') for i in range(2)]
    xt = [S.sb([128, D], F32, f'xo{i}') for i in range(2)]
    xo = [S.sb([128, D], F32, f'xn{i}') for i in range(2)]
    ptr = [S.ps([128, 8, 128], BF16, f'ptr{i}') for i in range(2)]
    py = [S.ps([128, 512], F32, f'py{i}') for i in range(4)]
    for i in range(ntok // 128):
        b = i % 2
        S.dma('sp', ot[b][:], o_tok[i * 128:(i + 1) * 128, :], r=[], w=[ot[b]])
        S.dma('sp', xt[b][:], x_in[i * 128:(i + 1) * 128, :], r=[], w=[xt[b]])
        p = ptr[b]
        for c in range(8):
            S.pe(lambda e, p=p, b=b, c=c: e.transpose(out=p[:, c, :], in_=ot[b][:, c * 128:(c + 1) * 128],
                                                      identity=ident[:]), r=[ot[b], ident], w=[(p, c)])
        S.act(lambda e, p=p, b=b: e.copy(out=oT[b][:], in_=p[:]), r=[p], w=[oT[b]])
        for mh in range(2):
            pp = py[b * 2 + mh]
            for c in range(8):
                S.pe(lambda e, pp=pp, b=b, c=c, mh=mh: e.matmul(pp[:], lhsT=oT[b][:, c, :],
                                                                rhs=wob[:, c, mh * 512:(mh + 1) * 512],
                                                                start=(c == 0), stop=(c == 7)),
                     r=[oT[b], wob], w=[pp])
            S.dve(lambda e, pp=pp, b=b, mh=mh: e.tensor_tensor(out=xo[b][:, mh * 512:(mh + 1) * 512], in0=pp[:],
                                                               in1=xt[b][:, mh * 512:(mh + 1) * 512], op=ALU.add),
                  r=[pp, xt[b]], w=[(xo[b], mh)])
        S.dma('pool', x_out[i * 128:(i + 1) * 128, :], xo[b][:], r=[xo[b]], w=[DW(S, x_out)])
    S.flush()


def odd_proj(S, x, prm, dr, ntok, cpack):
    vb = [S.sb([128, 64], BF16, f'vb{i}') for i in range(2)]
    wb = [S.sb([128, 4], F32, f'wb{i}') for i in range(2)]
    vd = [S.sb([128, 512], BF16, f'vd{i}') for i in range(2)]

    def tm_post(gi, pp, tok0, n):
        b = (tok0 // 128) % 2
        if gi == 0:
            S.act(lambda e: e.copy(out=vb[b][:], in_=pp[:, 0:64]), r=[pp], w=[vb[b]])
            S.dma('pool', dr['vcd'][tok0:tok0 + 128, :], vb[b][:], r=[vb[b]], w=[DW(S, dr['vcd'])])
        elif gi == 1:
            S.act(lambda e: e.copy(out=wb[b][:], in_=pp[:, 0:4]), r=[pp], w=[wb[b]])
            S.dma('pool', dr['wi'][tok0:tok0 + 128, :], wb[b][:], r=[wb[b]], w=[DW(S, dr['wi'])])
        else:
            S.act(lambda e: e.copy(out=vd[b][:], in_=pp[:, 0:512]), r=[pp], w=[vd[b]])
            S.dma('pool', dr['vdd'][tok0:tok0 + 128, :], vd[b][:], r=[vd[b]], w=[DW(S, dr['vdd'])])
    fm = []
    for i in range(4):
        fm.append((128 * i, 128, 0.125, 'r64', None, dr['qc'][128 * i:128 * (i + 1), :]))
    fm.append((512, 64, 1.0, 'r64', None, dr['kcd'][:, :]))
    fm.append((640, 128, 1.0, 'r32', None, dr['qi'][:, :]))
    fm.append((768, 32, 1.0, 'r32', None, dr['ki'][:, :]))
    for i in range(4):
        fm.append((804 + 128 * i, 128, 0.125, 'r64', None, dr['qd'][128 * i:128 * (i + 1), :]))
    for i in range(4):
        fm.append((1316 + 128 * i, 128, 1.0, 'r64', None, dr['kd'][128 * i:128 * (i + 1), :]))
    proj_stage(S, x, prm['w_in'], 2340, prm['mix_norm'], prm['pos'], ntok, cpack, fm,
               [(576, 64), (800, 4), (1828, 512)], tm_post, use_idx=True)


NBIS = 14


def dsa_moba_stage(S, prm, dr, nseq, cpack):
    C = Consts(S, cpack)
    ident = C.get('ident', BF16)
    tri = C.get('tri01', BF16)
    trib = C.get('tri_ge', BF16)
    E8 = C.get('E8', F32)
    triqs = C.get('tri_qs', F32)
    mbias, mpast, mown = C.get('mbias'), C.get('mpast'), C.get('mown')
    zc = [0]

    def ztile(shape, name):
        t = S.sb(shape, BF16, name)
        zc[0] += 1
        if zc[0] % 2:
            S.dve(lambda e: e.memset(t[:], 0.0), r=[], w=[t])
        else:
            S.pool(lambda e: e.memset(t[:], 0.0), r=[], w=[t])
        return t
    qi = [ztile([128, SEQ], f'qi{h}') for h in range(4)]
    ki = ztile([128, SEQ], 'ki')
    wi = S.sb([128, 16, 4], F32, 'wi')
    absw = S.sb([128, 16, 4], F32, 'absw')
    sgnw = S.sb([128, 16, 4], F32, 'sgnw')
    scores = [S.sb([128, SEQ], F32, f'score{i}') for i in range(2)]
    rl = [S.sb([128, 512], F32, f'rl{i}') for i in range(2)]
    junkb = S.sb([128, SEQ], BF16, 'junkb')
    nmasks = [S.sb([128, SEQ], BF16, f'nmask{i}') for i in range(2)]
    nmTs = [S.sb([128, 16, 512], BF16, f'nmT{i}') for i in range(2)]
    kcT = ztile([128, SEQ], 'kcT')
    vcx = S.sb([128, 16, 65], BF16, 'vcx')
    vdxs = [S.sb([128, 16, 65], BF16, f'vdx{i}') for i in range(2)]
    S.pool(lambda e: e.memset(vcx[:], 1.0), r=[], w=[vcx])
    for v_ in vdxs:
        S.pool(lambda e, v_=v_: e.memset(v_[:], 1.0), r=[], w=[v_])
    qcall = [ztile([128, SEQ], f'qcall{h}') for h in range(8)]
    qds = [ztile([128, SEQ], f'qd{i}') for i in range(2)]
    kds = [ztile([128, SEQ], f'kd{i}') for i in range(2)]
    for kd_ in kds:
        S.dve(lambda e, kd_=kd_: e.tensor_copy(out=kd_[64:72, :], in_=E8[0:8, :]), r=[E8], w=[(kd_, 'e')])
    kmf = S.sb([64, 8], F32, 'kmf')
    kmbs = [ztile([128, 8], f'kmb{i}') for i in range(2)]
    gsb = S.sb([128, 128], F32, 'gsb')
    ns8all = S.sb([128, 16, 32], BF16, 'ns8all')
    otok = S.sb([128, 16, 1024], BF16, 'otok')
    PT = [S.sb([128, 512], BF16, f'PT{i}') for i in range(4)]
    st5 = [S.sb([128, 8], F32, f'st5{i}') for i in range(2)]
    gs = [S.sb([128, 8], F32, f'gs{i}') for i in range(2)]
    m8 = [S.sb([128, 8], F32, f'm8{i}') for i in range(2)]
    ns8 = [S.sb([128, 8], BF16, f'ns8{i}') for i in range(2)]
    fsc = [S.sb([128, 1], F32, f'fsc{i}') for i in range(4)]
    pl = S.ps([128, 512], F32, 'pl')
    STps = [S.ps([128, 512], F32, f'st{i}') for i in range(2)]
    Oacc = [S.ps([128, 512], F32, f'oa{i}') for i in range(4)]
    ptb = S.ps([128, 8, 128], BF16, 'ptb')
    fcount = [0]

    def make_fin(col0):
        def fin(c, j, O, off):
            i = 4 * c + j
            f = fsc[fcount[0] % 4]
            fcount[0] += 1
            S.dve(lambda e: e.reciprocal(out=f[:], in_=O[:, off + 64:off + 65]), r=[O], w=[f])
            S.dve(lambda e: e.tensor_scalar(out=otok[:, i, col0:col0 + 64], in0=O[:, off:off + 64], scalar1=f[:, 0:1],
                                            scalar2=None, op0=ALU.mult), r=[O, f], w=[(otok, (i, col0))])
        return fin

    def index_steps(c):
        nmT = nmTs[c % 2]
        steps = []
        chains = {}
        for j in range(4):
            i = 4 * c + j
            W = 128 * (i + 1)
            score = scores[j % 2]
            nmask = nmasks[j % 2]
            steps = chains.setdefault(j, [])
            if i < 2:
                def trivial(i=i, j=j):
                    for st_ in range(i + 1):
                        if st_ == i:
                            S.pool(lambda e, st_=st_: e.tensor_copy(out=nmT[:, st_, j * 128:(j + 1) * 128], in_=trib[:]),
                                   r=[trib], w=[(nmT, (st_, j))])
                        else:
                            S.pool(lambda e, st_=st_: e.memset(nmT[:, st_, j * 128:(j + 1) * 128], 0.0),
                                   r=[], w=[(nmT, (st_, j))])
                steps.append(trivial)
                continue
            s5 = st5[i % 2]
            for h in range(4):
                def logits(h=h, i=i, W=W, score=score):
                    for sc in range((W + 511) // 512):
                        n = min(512, W - 512 * sc)
                        rb = rl[(h * 4 + sc) % 2]
                        S.pe(lambda e, sc=sc, n=n: e.matmul(pl[:, 0:n], lhsT=qi[h][:, i * 128:(i + 1) * 128],
                                                            rhs=ki[:, sc * 512:sc * 512 + n], start=True, stop=True),
                             r=[qi[h], ki], w=[pl])
                        S.act(lambda e, rb=rb, n=n: e.activation(out=rb[:, 0:n], in_=pl[:, 0:n], func=AF.Relu,
                                                                 scale=absw[:, i, h:h + 1]), r=[pl, absw], w=[rb])
                        if h == 0:
                            S.dve(lambda e, rb=rb, n=n, sc=sc: e.tensor_scalar(
                                out=score[:, sc * 512:sc * 512 + n], in0=rb[:, 0:n], scalar1=sgnw[:, i, h:h + 1],
                                scalar2=None, op0=ALU.mult), r=[rb, sgnw], w=[(score, sc)])
                        else:
                            S.dve(lambda e, rb=rb, n=n, sc=sc: e.scalar_tensor_tensor(
                                out=score[:, sc * 512:sc * 512 + n], in0=rb[:, 0:n], scalar=sgnw[:, i, h:h + 1],
                                in1=score[:, sc * 512:sc * 512 + n], op0=ALU.mult, op1=ALU.add),
                                r=[rb, sgnw, (score, sc)], w=[(score, sc)])
                steps.append(logits)

            def bounds(i=i, W=W, s5=s5, score=score):
                S.dve(lambda e: e.tensor_reduce(out=s5[:, 5:6], in_=score[:, 0:W], axis=AX.X, op=ALU.max),
                      r=[score], w=[(s5, 5)])
                S.dve(lambda e: e.tensor_reduce(out=s5[:, 0:1], in_=score[:, 0:W], axis=AX.X, op=ALU.min),
                      r=[score], w=[(s5, 0)])
                S.dve(lambda e: e.tensor_tensor(out=s5[:, 1:2], in0=s5[:, 5:6], in1=s5[:, 0:1], op=ALU.subtract),
                      r=[(s5, 5), (s5, 0)], w=[(s5, 1)])
                S.dve(lambda e: e.tensor_tensor(out=score[:, i * 128:(i + 1) * 128],
                                                in0=score[:, i * 128:(i + 1) * 128], in1=triqs[:], op=ALU.add),
                      r=[score, triqs], w=[score])
            steps.append(bounds)
            for it in range(NBIS):
                def bis(W=W, s5=s5, score=score):
                    S.dve(lambda e: e.tensor_scalar(out=s5[:, 1:2], in0=s5[:, 1:2], scalar1=0.5, scalar2=None,
                                                    op0=ALU.mult), r=[(s5, 1)], w=[(s5, 1)])
                    S.dve(lambda e: e.tensor_tensor(out=s5[:, 2:3], in0=s5[:, 0:1], in1=s5[:, 1:2], op=ALU.add),
                          r=[(s5, 0), (s5, 1)], w=[(s5, 2)])
                    S.dve(lambda e: e.tensor_scalar(out=junkb[:, 0:W], in0=score[:, 0:W], scalar1=s5[:, 2:3],
                                                    scalar2=0.0, op0=ALU.is_ge, op1=ALU.add, accum_out=s5[:, 3:4]),
                          r=[score, (s5, 2)], w=[junkb, (s5, 3)])
                    S.dve(lambda e: e.tensor_scalar(out=s5[:, 4:5], in0=s5[:, 3:4], scalar1=255.5, scalar2=None,
                                                    op0=ALU.is_ge), r=[(s5, 3)], w=[(s5, 4)])
                    S.dve(lambda e: e.scalar_tensor_tensor(out=s5[:, 0:1], in0=s5[:, 1:2], scalar=s5[:, 4:5],
                                                           in1=s5[:, 0:1], op0=ALU.mult, op1=ALU.add),
                          r=[(s5, 1), (s5, 4), (s5, 0)], w=[(s5, 0)])
                steps.append(bis)

            def fin_mask(i=i, j=j, W=W, s5=s5, score=score, nmask=nmask):
                S.dve(lambda e: e.tensor_scalar(out=nmask[:, 0:W], in0=score[:, 0:W], scalar1=s5[:, 0:1],
                                                scalar2=NEG, op0=ALU.is_lt, op1=ALU.mult),
                      r=[score, (s5, 0)], w=[nmask])
                for s0 in range(0, i + 1, 8):
                    n = min(8, i + 1 - s0)
                    for k in range(n):
                        S.pe(lambda e, s0=s0, k=k: e.transpose(out=ptb[:, k, :],
                                                               in_=nmask[:, (s0 + k) * 128:(s0 + k + 1) * 128],
                                                               identity=ident[:]), r=[nmask, ident], w=[(ptb, k)])
                    S.act(lambda e, s0=s0, n=n: e.copy(out=nmT[:, s0:s0 + n, j * 128:(j + 1) * 128],
                                                       in_=ptb[:, 0:n, :]), r=[ptb], w=[(nmT, ('b', s0, j))])
            steps.append(fin_mask)
        out = []
        for pair in ((0, 1), (2, 3)):
            la, lb = chains[pair[0]], chains[pair[1]]
            for k in range(max(len(la), len(lb))):
                if k < len(la):
                    out.append(la[k])
                if k < len(lb):
                    out.append(lb[k])
        return out

    for sq in range(nseq):
        tb = sq * SEQ
        for h in range(4):
            S.dma('sp', qi[h][0:32, :], dr['qi'][h * 32:(h + 1) * 32, tb:tb + SEQ], r=[], w=[(qi[h], 'q')])
        S.dma('sp', ki[0:32, :], dr['ki'][:, tb:tb + SEQ], r=[], w=[(ki, 'q')])
        S.dma('sp', wi[:], dr['wi'][tb:tb + SEQ, :].rearrange("(n p) c -> p n c", p=128), r=[], w=[wi])
        S.act(lambda e: e.activation(out=absw[:], in_=wi[:], func=AF.Abs), r=[wi], w=[absw])
        S.act(lambda e: e.activation(out=sgnw[:], in_=wi[:], func=AF.Sign), r=[wi], w=[sgnw])
        S.dma('sp', kcT[0:64, :], dr['kcd'][:, tb:tb + SEQ], r=[], w=[(kcT, 'q')])
        S.dma('sp', vcx[:, :, 0:64], dr['vcd'][tb:tb + SEQ, :].rearrange("(n p) c -> p n c", p=128), r=[], w=[vcx])
        for h in range(8):
            S.dma('sp', qcall[h][0:64, :], dr['qc'][h * 64:(h + 1) * 64, tb:tb + SEQ], r=[], w=[(qcall[h], 'q')])
        import os
        STOP = int(os.environ.get('STOP', '0'))
        if STOP == 1:
            break
        for st in index_steps(0):
            st()
        if STOP == 2:
            break
        jobs = []
        for c in range(4):
            nmT = nmTs[c % 2]
            nxt = index_steps(c + 1) if c < 3 else []
            per = (len(nxt) + 7) // 8
            for h in range(8):
                sl = nxt[h * per:(h + 1) * per]
                if os.environ.get('NOPRE'):
                    for st in sl:
                        st()
                    sl = []
                jobs.append(dict(c=c, tiles=causal_tiles(c, None), qT=qcall[h], kT=kcT, vext=vcx,
                                 extra=(lambda kt: ident[:], lambda kt, c, lo, hi, nmT=nmT: nmT[:, kt, lo:hi],
                                        [ident, nmT]),
                                 fin=make_fin(64 * h),
                                 pre=((lambda sl=sl: [st() for st in sl]) if sl else None)))
        import os
        if not os.environ.get('SKIP_DSA'):
            attn_run(S, jobs, STps, PT, Oacc, LA=1)

        def moba_load(h, tb=tb):
            b = h % 2
            S.dma('sp', qds[b][0:64, :], dr['qd'][h * 64:(h + 1) * 64, tb:tb + SEQ], r=[], w=[(qds[b], 'q')])
            S.dma('sp', kds[b][0:64, :], dr['kd'][h * 64:(h + 1) * 64, tb:tb + SEQ], r=[], w=[(kds[b], 'q')])
            S.dma('sp', vdxs[b][:, :, 0:64], dr['vdd'][tb:tb + SEQ, h * 64:(h + 1) * 64].rearrange(
                "(n p) c -> p n c", p=128), r=[], w=[vdxs[b]])

        def gate_a(h):
            b = h % 2
            qd, kd, kmb = qds[b], kds[b], kmbs[b]
            S.dve(lambda e: e.tensor_reduce(out=kmf[:], in_=kd[0:64, :].rearrange("p (j k) -> p j k", k=256),
                                            axis=AX.X, op=ALU.add), r=[(kd, 'q')], w=[kmf])
            S.dve(lambda e: e.tensor_scalar(out=kmb[0:64, :], in0=kmf[:], scalar1=1.0 / 256, scalar2=None,
                                            op0=ALU.mult), r=[kmf], w=[(kmb, 'm')])
            for i in range(16):
                S.pe(lambda e, i=i: e.matmul(pl[:, i * 8:(i + 1) * 8], lhsT=qd[:, i * 128:(i + 1) * 128], rhs=kmb[:],
                                             start=True, stop=True), r=[qd, kmb], w=[(pl, i)])
            S.dve(lambda e: e.tensor_tensor(out=gsb[:], in0=pl[:, 0:128], in1=mbias[:], op=ALU.add),
                  r=[pl, mbias], w=[gsb])
            for i in range(16):
                m = m8[i % 2]
                S.dve(lambda e, i=i, m=m: e.max(out=m[:], in_=gsb[:, i * 8:(i + 1) * 8]), r=[(gsb, i)], w=[m])
                S.dve(lambda e, i=i, m=m: e.tensor_scalar(out=gsb[:, i * 8:(i + 1) * 8], in0=gsb[:, i * 8:(i + 1) * 8],
                                                          scalar1=m[:, 2:3], scalar2=None, op0=ALU.is_ge),
                      r=[(gsb, i), m], w=[(gsb, i)])
            S.dve(lambda e: e.tensor_tensor(out=gsb[:], in0=gsb[:], in1=mpast[:], op=ALU.mult), r=[gsb, mpast], w=[gsb])
            S.dve(lambda e: e.tensor_tensor(out=gsb[:], in0=gsb[:], in1=mown[:], op=ALU.add), r=[gsb, mown], w=[gsb])
            S.dve(lambda e: e.tensor_scalar(out=ns8all[:, :, 0:8], in0=gsb[:].rearrange("p (i k) -> p i k", k=8),
                                            scalar1=-NEG, scalar2=NEG, op0=ALU.mult, op1=ALU.add),
                  r=[gsb], w=[ns8all])

        def gate_b(h):
            qd = qds[h % 2]
            for half in range(2):
                for k in range(8):
                    i = half * 8 + k
                    S.pe(lambda e, k=k, i=i: e.transpose(out=ptb[0:8, k, :], in_=ns8all[:, i, 0:8],
                                                         identity=ident[:]), r=[ns8all, ident], w=[(ptb, k)])
                S.dve(lambda e, half=half: e.tensor_copy(
                    out=qd[64:72, half * 1024:(half + 1) * 1024].rearrange("p (k q) -> p k q", q=128),
                    in_=ptb[0:8, :, :]), r=[ptb], w=[(qd, ('n', half))])

        if STOP == 3:
            break
        moba_load(0)
        gate_a(0)
        if STOP == 4:
            break
        gate_b(0)
        if STOP == 5:
            break
        NH_ = int(os.environ.get('MOBA_H', '8'))
        for h in range(0 if not os.environ.get('SKIP_MOBA') else 8, NH_):
            b = h % 2
            if h + 1 < 8:
                moba_load(h + 1)
            jobs = []
            for c in range(4):
                jobs.append(dict(c=c, tiles=causal_tiles(c, tri), qT=qds[b], kT=kds[b], vext=vdxs[b],
                                 fin=make_fin(512 + 64 * h),
                                 pre=((lambda h=h: gate_a(h + 1)) if (c == 1 and h + 1 < 8 and not os.environ.get('NOGATE')) else None)))
            attn_run(S, jobs, STps, PT, Oacc, LA=1)
            if h + 1 < 8:
                gate_b(h + 1)
        S.dma('sp', dr['o_tok'][tb:tb + SEQ, :].rearrange("(n p) c -> p n c", p=128), otok[:],
              r=[otok], w=[DW(S, dr['o_tok'])])
    S.flush()


def attn_chunk_v1(S, c, tiles, qT, kT, extra, vext, ident, STps, PT, Oacc, finalize, tag, qdep=None):
    cover = {}
    for n, (kt, lo, hi, bt, blo) in enumerate(tiles):
        for j in range(lo // 128, hi // 128):
            cover.setdefault(j, []).append(n)

    qd_ = qdep if qdep is not None else qT

    def qk(n):
        kt, lo, hi, bt, blo = tiles[n]
        ps = STps[n % 2]
        nterm = 1 + (1 if extra else 0) + (1 if bt is not None else 0)
        S.pe(lambda e: e.matmul(ps[:, lo:hi], lhsT=kT[:, kt * 128:(kt + 1) * 128],
                                rhs=qT[:, c * 512 + lo:c * 512 + hi], start=True, stop=(nterm == 1)),
             r=[kT, qd_], w=[ps])
        k = 1
        if extra:
            k += 1
            S.pe(lambda e: e.matmul(ps[:, lo:hi], lhsT=extra[0](kt), rhs=extra[1](kt, c, lo, hi),
                                    start=False, stop=(k == nterm), skip_group_check=True),
                 r=list(extra[2]), w=[ps])
        if bt is not None:
            S.pe(lambda e: e.matmul(ps[:, blo:blo + 128], lhsT=ident[:], rhs=bt[:], start=False, stop=True,
                                    skip_group_check=True), r=[ident, bt], w=[ps])

    qk(0)
    for n, (kt, lo, hi, bt, blo) in enumerate(tiles):
        if n + 1 < len(tiles):
            qk(n + 1)
        ps, p = STps[n % 2], PT[n % 2]
        S.act(lambda e, ps=ps, p=p, lo=lo, hi=hi: e.activation(out=p[:, lo:hi], in_=ps[:, lo:hi], func=AF.Exp),
              r=[ps], w=[p])
        for j in range(lo // 128, hi // 128):
            S.pe(lambda e, p=p, j=j, kt=kt, n=n: e.matmul(
                Oacc[j][:, 0:65], lhsT=p[:, j * 128:(j + 1) * 128], rhs=vext[:, kt, :],
                start=(cover[j][0] == n), stop=(cover[j][-1] == n), skip_group_check=True),
                r=[p, vext], w=[Oacc[j]])
            if cover[j][-1] == n:
                finalize(c, j, Oacc[j])


def causal_tiles_v1(c, tri):
    out = []
    for kt in range(4 * c + 4):
        if kt < 4 * c:
            out.append((kt, 0, 512, None, 0))
        else:
            lo = (kt - 4 * c) * 128
            out.append((kt, lo, 512, tri, lo))
    return out


def dsa_moba_stage_v1(S, prm, dr, nseq, cpack):
    C = Consts(S, cpack)
    ident = C.get('ident', BF16)
    tri = C.get('tri_ge', BF16)
    E8 = C.get('E8', BF16)
    triqs = C.get('tri_qs', F32)
    mbias, mpast, mown = C.get('mbias'), C.get('mpast'), C.get('mown')
    qi = [S.sb([32, SEQ], BF16, f'qi{h}') for h in range(4)]
    ki = S.sb([32, SEQ], BF16, 'ki')
    wi = S.sb([128, 16, 4], F32, 'wi')
    absw = S.sb([128, 16, 4], F32, 'absw')
    sgnw = S.sb([128, 16, 4], F32, 'sgnw')
    score = S.sb([128, SEQ], F32, 'score')
    rl = [S.sb([128, 512], F32, f'rl{i}') for i in range(2)]
    junkb = S.sb([128, SEQ], BF16, 'junkb')
    nmask = S.sb([128, SEQ], BF16, 'nmask')
    nmT = S.sb([128, 16, 512], BF16, 'nmT')
    kcT = S.sb([64, SEQ], BF16, 'kcT')
    vcx = S.sb([128, 16, 65], BF16, 'vcx')
    vdx = S.sb([128, 16, 65], BF16, 'vdx')
    S.pool(lambda e: e.memset(vcx[:], 1.0), r=[], w=[vcx])
    S.pool(lambda e: e.memset(vdx[:], 1.0), r=[], w=[vdx])
    qcall = [S.sb([64, SEQ], BF16, f'qcall{h}') for h in range(8)]
    qd = S.sb([64, SEQ], BF16, 'qd')
    kd = S.sb([64, SEQ], BF16, 'kd')
    kmf = S.sb([64, 8], F32, 'kmf')
    kmb = S.sb([64, 8], BF16, 'kmb')
    negsel8 = S.sb([8, SEQ], BF16, 'negsel8')
    otok = S.sb([128, 16, 1024], BF16, 'otok')
    PT = [S.sb([128, 512], BF16, f'PT{i}') for i in range(2)]
    st5 = [S.sb([128, 8], F32, f'st5{i}') for i in range(2)]
    gs = [S.sb([128, 8], F32, f'gs{i}') for i in range(2)]
    m8 = [S.sb([128, 8], F32, f'm8{i}') for i in range(2)]
    ns8 = [S.sb([128, 8], BF16, f'ns8{i}') for i in range(2)]
    fsc = [S.sb([128, 1], F32, f'fsc{i}') for i in range(4)]
    STps = [S.ps([128, 512], F32, f'st{i}') for i in range(2)]
    Oacc = [S.ps([128, 512], F32, f'oa{i}') for i in range(4)]
    ptb = S.ps([128, 8, 128], BF16, 'ptb')
    pl = S.ps([128, 512], F32, 'pl')
    fcount = [0]

    def make_fin(col0):
        def fin(c, j, O):
            i = 4 * c + j
            f = fsc[fcount[0] % 4]
            fcount[0] += 1
            S.dve(lambda e: e.tensor_scalar(out=f[:], in0=O[:, 64:65], scalar1=1e-30, scalar2=None, op0=ALU.max),
                  r=[O], w=[f])
            S.dve(lambda e: e.reciprocal(out=f[:], in_=f[:]), r=[f], w=[f])
            S.dve(lambda e: e.tensor_scalar(out=otok[:, i, col0:col0 + 64], in0=O[:, 0:64], scalar1=f[:, 0:1],
                                            scalar2=None, op0=ALU.mult), r=[O, f], w=[(otok, (i, col0))])
        return fin

    for sq in range(nseq):
        tb = sq * SEQ
        for h in range(4):
            S.dma('sp', qi[h][:], dr['qi'][h * 32:(h + 1) * 32, tb:tb + SEQ], r=[], w=[qi[h]])
        S.dma('sp', ki[:], dr['ki'][:, tb:tb + SEQ], r=[], w=[ki])
        S.dma('sp', wi[:], dr['wi'][tb:tb + SEQ, :].rearrange("(n p) c -> p n c", p=128), r=[], w=[wi])
        S.act(lambda e: e.activation(out=absw[:], in_=wi[:], func=AF.Abs), r=[wi], w=[absw])
        S.act(lambda e: e.activation(out=sgnw[:], in_=wi[:], func=AF.Sign), r=[wi], w=[sgnw])
        S.dma('sp', kcT[:], dr['kcd'][:, tb:tb + SEQ], r=[], w=[kcT])
        S.dma('sp', vcx[:, :, 0:64], dr['vcd'][tb:tb + SEQ, :].rearrange("(n p) c -> p n c", p=128), r=[], w=[vcx])
        for h in range(8):
            S.dma('sp', qcall[h][:], dr['qc'][h * 64:(h + 1) * 64, tb:tb + SEQ], r=[], w=[qcall[h]])
        for c in range(4):
            for j in range(4):
                i = 4 * c + j
                W = 128 * (i + 1)
                if i < 2:
                    for st_ in range(i + 1):
                        if st_ == i:
                            S.pool(lambda e, st_=st_, j=j: e.tensor_copy(out=nmT[:, st_, j * 128:(j + 1) * 128],
                                                                         in_=tri[:]), r=[tri], w=[(nmT, (st_, j))])
                        else:
                            S.pool(lambda e, st_=st_, j=j: e.memset(nmT[:, st_, j * 128:(j + 1) * 128], 0.0),
                                   r=[], w=[(nmT, (st_, j))])
                    continue
                for h in range(4):
                    for sc in range((W + 511) // 512):
                        n = min(512, W - 512 * sc)
                        rb = rl[(h * 4 + sc) % 2]
                        S.pe(lambda e, h=h, sc=sc, n=n, i=i: e.matmul(pl[:, 0:n], lhsT=qi[h][:, i * 128:(i + 1) * 128],
                                                                       rhs=ki[:, sc * 512:sc * 512 + n], start=True,
                                                                       stop=True), r=[qi[h], ki], w=[pl])
                        S.act(lambda e, rb=rb, n=n, i=i, h=h: e.activation(out=rb[:, 0:n], in_=pl[:, 0:n], func=AF.Relu,
                                                                           scale=absw[:, i, h:h + 1]),
                              r=[pl, absw], w=[rb])
                        if h == 0:
                            S.dve(lambda e, rb=rb, n=n, sc=sc, i=i, h=h: e.tensor_scalar(
                                out=score[:, sc * 512:sc * 512 + n], in0=rb[:, 0:n], scalar1=sgnw[:, i, h:h + 1],
                                scalar2=None, op0=ALU.mult), r=[rb, sgnw], w=[(score, sc)])
                        else:
                            S.dve(lambda e, rb=rb, n=n, sc=sc, i=i, h=h: e.scalar_tensor_tensor(
                                out=score[:, sc * 512:sc * 512 + n], in0=rb[:, 0:n], scalar=sgnw[:, i, h:h + 1],
                                in1=score[:, sc * 512:sc * 512 + n], op0=ALU.mult, op1=ALU.add),
                                r=[rb, sgnw, (score, sc)], w=[(score, sc)])
                s5 = st5[i % 2]
                S.dve(lambda e, s5=s5, W=W: e.tensor_reduce(out=s5[:, 5:6], in_=score[:, 0:W], axis=AX.X, op=ALU.max),
                      r=[score], w=[(s5, 5)])
                S.dve(lambda e, s5=s5, W=W: e.tensor_reduce(out=s5[:, 0:1], in_=score[:, 0:W], axis=AX.X, op=ALU.min),
                      r=[score], w=[(s5, 0)])
                S.dve(lambda e, s5=s5: e.tensor_tensor(out=s5[:, 1:2], in0=s5[:, 5:6], in1=s5[:, 0:1], op=ALU.subtract),
                      r=[(s5, 5), (s5, 0)], w=[(s5, 1)])
                S.dve(lambda e, i=i: e.tensor_tensor(out=score[:, i * 128:(i + 1) * 128],
                                                     in0=score[:, i * 128:(i + 1) * 128], in1=triqs[:], op=ALU.add),
                      r=[score, triqs], w=[score])
                for it in range(NBIS):
                    S.dve(lambda e, s5=s5: e.tensor_scalar(out=s5[:, 1:2], in0=s5[:, 1:2], scalar1=0.5, scalar2=None,
                                                           op0=ALU.mult), r=[(s5, 1)], w=[(s5, 1)])
                    S.dve(lambda e, s5=s5: e.tensor_tensor(out=s5[:, 2:3], in0=s5[:, 0:1], in1=s5[:, 1:2], op=ALU.add),
                          r=[(s5, 0), (s5, 1)], w=[(s5, 2)])
                    S.dve(lambda e, s5=s5, W=W: e.tensor_scalar(out=junkb[:, 0:W], in0=score[:, 0:W],
                                                                scalar1=s5[:, 2:3], scalar2=0.0, op0=ALU.is_ge,
                                                                op1=ALU.add, accum_out=s5[:, 3:4]),
                          r=[score, (s5, 2)], w=[junkb, (s5, 3)])
                    S.dve(lambda e, s5=s5: e.tensor_scalar(out=s5[:, 4:5], in0=s5[:, 3:4], scalar1=255.5, scalar2=None,
                                                           op0=ALU.is_ge), r=[(s5, 3)], w=[(s5, 4)])
                    S.dve(lambda e, s5=s5: e.scalar_tensor_tensor(out=s5[:, 0:1], in0=s5[:, 1:2], scalar=s5[:, 4:5],
                                                                  in1=s5[:, 0:1], op0=ALU.mult, op1=ALU.add),
                          r=[(s5, 1), (s5, 4), (s5, 0)], w=[(s5, 0)])
                S.dve(lambda e, s5=s5, W=W: e.tensor_scalar(out=nmask[:, 0:W], in0=score[:, 0:W], scalar1=s5[:, 0:1],
                                                            scalar2=NEG, op0=ALU.is_lt, op1=ALU.mult),
                      r=[score, (s5, 0)], w=[nmask])
                for s0 in range(0, i + 1, 8):
                    n = min(8, i + 1 - s0)
                    for k in range(n):
                        S.pe(lambda e, s0=s0, k=k: e.transpose(out=ptb[:, k, :],
                                                               in_=nmask[:, (s0 + k) * 128:(s0 + k + 1) * 128],
                                                               identity=ident[:]), r=[nmask, ident], w=[(ptb, k)])
                    S.act(lambda e, s0=s0, n=n, j=j: e.copy(out=nmT[:, s0:s0 + n, j * 128:(j + 1) * 128],
                                                            in_=ptb[:, 0:n, :]), r=[ptb], w=[(nmT, ('b', s0, j))])
            for h in range(8):
                attn_chunk_v1(S, c, causal_tiles_v1(c, None), qcall[h], kcT,
                           (lambda kt: ident[:], lambda kt, c, lo, hi: nmT[:, kt, lo:hi], [ident, nmT]),
                           vcx, ident, STps, PT, Oacc, make_fin(64 * h), 'dsa')
        for h in range(8):
            S.dma('sp', qd[:], dr['qd'][h * 64:(h + 1) * 64, tb:tb + SEQ], r=[], w=[qd])
            S.dma('sp', kd[:], dr['kd'][h * 64:(h + 1) * 64, tb:tb + SEQ], r=[], w=[kd])
            S.dma('sp', vdx[:, :, 0:64], dr['vdd'][tb:tb + SEQ, h * 64:(h + 1) * 64].rearrange(
                "(n p) c -> p n c", p=128), r=[], w=[vdx])
            S.dve(lambda e: e.tensor_reduce(out=kmf[:], in_=kd[:].rearrange("p (j k) -> p j k", k=256), axis=AX.X,
                                            op=ALU.add), r=[kd], w=[kmf])
            S.dve(lambda e: e.tensor_scalar(out=kmb[:], in0=kmf[:], scalar1=1.0 / 256, scalar2=None, op0=ALU.mult),
                  r=[kmf], w=[kmb])
            for i in range(16):
                b = i % 2
                S.pe(lambda e, i=i: e.matmul(pl[:, 0:8], lhsT=qd[:, i * 128:(i + 1) * 128], rhs=kmb[:], start=True,
                                             stop=True), r=[qd, kmb], w=[pl])
                g_ = gs[b]
                S.dve(lambda e, g_=g_, i=i: e.tensor_tensor(out=g_[:], in0=pl[:, 0:8], in1=mbias[:, i * 8:(i + 1) * 8],
                                                           op=ALU.add), r=[pl, mbias], w=[g_])
                S.dve(lambda e, g_=g_, b=b: e.max(out=m8[b][:], in_=g_[:]), r=[g_], w=[m8[b]])
                S.dve(lambda e, g_=g_, b=b: e.tensor_scalar(out=g_[:], in0=g_[:], scalar1=m8[b][:, 2:3], scalar2=None,
                                                            op0=ALU.is_ge), r=[g_, m8[b]], w=[g_])
                S.dve(lambda e, g_=g_, i=i: e.tensor_tensor(out=g_[:], in0=g_[:], in1=mpast[:, i * 8:(i + 1) * 8],
                                                           op=ALU.mult), r=[g_, mpast], w=[g_])
                S.dve(lambda e, g_=g_, i=i: e.tensor_tensor(out=g_[:], in0=g_[:], in1=mown[:, i * 8:(i + 1) * 8],
                                                           op=ALU.add), r=[g_, mown], w=[g_])
                S.dve(lambda e, g_=g_, b=b: e.tensor_scalar(out=ns8[b][:], in0=g_[:], scalar1=-NEG, scalar2=NEG,
                                                            op0=ALU.mult, op1=ALU.add), r=[g_], w=[ns8[b]])
                S.pe(lambda e, b=b: e.transpose(out=ptb[0:8, b, :], in_=ns8[b][:], identity=ident[:]),
                     r=[ns8[b], ident], w=[(ptb, b)])
                S.act(lambda e, b=b, i=i: e.copy(out=negsel8[:, i * 128:(i + 1) * 128], in_=ptb[0:8, b, :]),
                      r=[(ptb, b)], w=[(negsel8, i // 4)])
            for c in range(4):
                attn_chunk_v1(S, c, causal_tiles_v1(c, tri), qd, kd,
                           (lambda kt: E8[0:8, kt * 128:(kt + 1) * 128], lambda kt, c, lo, hi: negsel8[:, c * 512 + lo:c * 512 + hi],
                            [E8, negsel8]),
                           vdx, ident, STps, PT, Oacc, make_fin(512 + 64 * h), 'moba')
        S.dma('sp', dr['o_tok'][tb:tb + SEQ, :].rearrange("(n p) c -> p n c", p=128), otok[:],
              r=[otok], w=[DW(S, dr['o_tok'])])
    S.flush()


NCORES = 8
TPC = 2 * SEQ


def build_program(stages=None):
    nc = bass.Bass("TRN2", target_bir_lowering=False)
    ins = {}

    def din(name, shape, dt=F32):
        ins[name] = nc.dram_tensor(name, list(shape), dt, kind="ExternalInput").ap()
        return ins[name]

    def scr(name, shape, dt=BF16):
        return nc.dram_tensor(name, list(shape), dt, kind="Internal").ap()

    x = din('x', [TPC, D])
    pos = din('pos', [TPC], I32)
    cp = din('cpack', [128, CP_N])
    P = {}
    for L in range(2):
        for f in ('ffn1', 'ffn2'):
            P[f'{f}_norm{L}'] = din(f'{f}_norm{L}', [D])
            P[f'{f}_wg{L}'] = din(f'{f}_wg{L}', [D, DFF])
            P[f'{f}_wu{L}'] = din(f'{f}_wu{L}', [D, DFF])
            P[f'{f}_wd{L}'] = din(f'{f}_wd{L}', [DFF, D])
        P[f'mix_norm{L}'] = din(f'mix_norm{L}', [D])
    ev = {'w_in': din('ev_w_in', [D, 2468]), 'mix_norm': P['mix_norm0'], 'pos': pos,
          'sgu_norm': din('ev_sgu_norm', [256]), 'sgu_wT': din('ev_sgu_wT', [4, 128, 128]),
          'sgu_bT': din('ev_sgu_bT', [128, 4]),
          'cmp_w1_k': din('ev_w1k', [2048, 256]), 'cmp_w2_k': din('ev_w2k', [256, 64]),
          'cmp_posT_k': din('ev_pk', [64, 32]),
          'cmp_w1_v': din('ev_w1v', [2048, 256]), 'cmp_w2_v': din('ev_w2v', [256, 64]),
          'cmp_posT_v': din('ev_pv', [64, 32])}
    ev_w_out = din('ev_w_out', [D, D])
    od = {'w_in': din('od_w_in', [D, 2340]), 'mix_norm': P['mix_norm1'], 'pos': pos}
    od_w_out = din('od_w_out', [D, D])
    fin_g = din('final_norm', [D])
    y = nc.dram_tensor('y', [TPC, D], F32, kind="ExternalOutput").ap()
    xa = scr('xa', [TPC, D], F32)
    xb = scr('xb', [TPC, D], F32)
    T_ = TPC
    dre = {'a_tok': scr('a_tok', [T_, 512]), 'qraw': scr('qraw', [768, T_]), 'qrot': scr('qrot', [768, T_]),
           'kc': scr('kc', [192, T_]), 'vc': scr('vc', [192, T_]), 'ksl': scr('ksl', [192, T_]),
           'kw': scr('kw', [192, T_]), 'vsl': scr('vsl', [T_, 192]), 'vw': scr('vw', [T_, 192]),
           'gate': scr('gate', [T_, 36], F32), 'o_tok': scr('o_tok0', [T_, 1024])}
    dro = {'qc': scr('qc', [512, T_]), 'kcd': scr('kcd', [64, T_]), 'vcd': scr('vcd', [T_, 64]),
           'qi': scr('qi', [128, T_]), 'ki': scr('ki', [32, T_]), 'wi': scr('wi', [T_, 4], F32),
           'qd': scr('qd', [512, T_]), 'kd': scr('kd', [512, T_]), 'vdd': scr('vdd', [T_, 512]),
           'o_tok': scr('o_tok1', [T_, 1024])}
    S = Sched(nc)
    w16 = {'wg': scr('wg16', [128, DFF // 256, 8, 256]), 'wu': scr('wu16', [128, DFF // 256, 8, 256]),
           'wd': scr('wd16', [128, NFC, D])}

    def ffn(xi, xo, f, L, fg=None):
        ffn_stage(S, xi, xo, P[f'{f}_norm{L}'], P[f'{f}_wg{L}'], P[f'{f}_wu{L}'], P[f'{f}_wd{L}'], TPC, cp_d, fg, w16)
    cp_d = {'ident': cp[:, CP_OFF['ident'][0]:CP_OFF['ident'][0] + 128]}
    ffn(x, xa, 'ffn1', 0)
    even_proj(S, xa, ev, dre, TPC, cp)
    nsa_stage(S, ev, dre, 2, cp)
    outproj_stage(S, xa, xb, dre['o_tok'], ev_w_out, TPC, cp)
    ffn(xb, xa, 'ffn2', 0)
    ffn(xa, xb, 'ffn1', 1)
    odd_proj(S, xb, od, dro, TPC, cp)
    (dsa_moba_stage if USE_NEW_ODD else dsa_moba_stage_v1)(S, od, dro, 2, cp)
    outproj_stage(S, xb, xa, dro['o_tok'], od_w_out, TPC, cp)
    ffn(xa, y, 'ffn2', 1, fin_g)
    return nc, S


def kernel(**inp):
    inp = {k: np.asarray(v) for k, v in inp.items()}
    nc, S = build_program()
    cpk = host_consts()
    c = np.ascontiguousarray
    shared = {'cpack': cpk}
    for L in range(2):
        for f in ('ffn1', 'ffn2'):
            shared[f'{f}_norm{L}'] = c(inp[f'{f}_norm'][L])
            shared[f'{f}_wg{L}'] = c(inp[f'{f}_w_gate'][L])
            shared[f'{f}_wu{L}'] = c(inp[f'{f}_w_up'][L])
            shared[f'{f}_wd{L}'] = c(inp[f'{f}_w_down'][L])
        shared[f'mix_norm{L}'] = c(inp['mix_norm'][L])
    shared.update({
        'ev_w_in': c(inp['ev_w_in'][0]), 'ev_sgu_norm': c(inp['ev_sgu_norm'][0]),
        'ev_sgu_wT': c(inp['ev_sgu_w'][0].transpose(0, 2, 1)), 'ev_sgu_bT': c(inp['ev_sgu_b'][0].T),
        'ev_w1k': c(inp['ev_cmp_w1_k'][0]), 'ev_w2k': c(inp['ev_cmp_w2_k'][0]), 'ev_pk': c(inp['ev_cmp_pos_k'][0].T),
        'ev_w1v': c(inp['ev_cmp_w1_v'][0]), 'ev_w2v': c(inp['ev_cmp_w2_v'][0]), 'ev_pv': c(inp['ev_cmp_pos_v'][0].T),
        'ev_w_out': c(inp['ev_w_out'][0]), 'od_w_in': c(inp['od_w_in'][0]), 'od_w_out': c(inp['od_w_out'][0]),
        'final_norm': c(inp['final_norm'])})
    in_maps = []
    for k in range(NCORES):
        m = dict(shared)
        m['x'] = c(inp['x'][2 * k:2 * k + 2].reshape(TPC, D))
        m['pos'] = c(inp['positions'][2 * k:2 * k + 2].reshape(TPC).astype(np.int32))
        in_maps.append(m)
    res = run_bass_kernel_spmd(nc, in_maps, core_ids=list(range(NCORES)))
    out = np.stack([np.asarray(r['y']).reshape(2, SEQ, D) for r in res.results], axis=0)
    return out.reshape(16, SEQ, D).astype(np.float32)
```

```python
from contextlib import ExitStack
import numpy as np
import concourse.bass as bass
import concourse.mybir as mybir
from concourse.bass_utils import run_bass_kernel_spmd

F32 = mybir.dt.float32
BF16 = mybir.dt.bfloat16
I32 = mybir.dt.int32
AF = mybir.ActivationFunctionType
ALU = mybir.AluOpType
AX = mybir.AxisListType

ENGS = ['pe', 'act', 'dve', 'pool', 'sp']
import os
BANKDEP = False
USE_NEW_ODD = True
NDSEM = 66
NSWSEM = 26


class T:
    _n = 0

    def __init__(self, t, name=None):
        self.t = t
        T._n += 1
        self.id = T._n
        self.name = name

    def __getitem__(self, idx):
        return self.t[idx]


class Op:
    __slots__ = ('eng', 'fn', 'deps', 'isdma', 'sem', 'cnt', 'signal', 'waits', 'vc')


class Sched:
    def __init__(self, nc):
        self.nc = nc
        self.esem = {e: nc.alloc_semaphore(name=f'es_{e}') for e in ENGS}
        self.ecnt = {e: 0 for e in ENGS}
        self.free_dsems = {False: [nc.alloc_semaphore(name=f'ds_{i}') for i in range(NDSEM)],
                           True: [nc.alloc_semaphore(name=f'dw_{i}') for i in range(NSWSEM)]}
        self.dcnt = {}
        self.n_inst = 0
        self.base = {}
        self._reset()

    def _reset(self):
        self.ops = []
        self.state = {}
        self.buf_dsem = {}
        self.stack = ExitStack()

    def sb(self, shape, dtype, name=None):
        t = self.stack.enter_context(self.nc.sbuf_tensor(f'{name or "sb"}_{T._n}', list(shape), dtype))
        return T(t, name)

    def ps(self, shape, dtype, name=None):
        t = self.stack.enter_context(self.nc.psum_tensor(f'{name or "ps"}_{T._n}', list(shape), dtype))
        return T(t, name)

    @staticmethod
    def _norm(item):
        if isinstance(item, T):
            return item.id, None
        return item[0].id, item[1]

    def _track(self, r, w, opi):
        deps = {}
        for item in r:
            tid, key = self._norm(item)
            st = self.state.setdefault(tid, {})
            for k, ent in st.items():
                if k == key or k is None or key is None:
                    if ent[0] is not None:
                        deps[ent[0]] = True
            st.setdefault(key, [None, []])[1].append(opi)
        for item in w:
            tid, key = self._norm(item)
            st = self.state.setdefault(tid, {})
            for k, ent in st.items():
                if k == key or k is None or key is None:
                    if ent[0] is not None:
                        deps[ent[0]] = True
                    for x in ent[1]:
                        deps.setdefault(x, False)
            if key is None:
                st.clear()
            st[key] = [opi, []]
        deps.pop(opi, None)
        return deps

    @staticmethod
    def _skip(p, o, raw):
        if p.isdma or o.isdma or p.eng != o.eng:
            return False
        return p.eng == 'pe' or not raw

    def op(self, eng, fn, r=(), w=()):
        o = Op()
        o.eng, o.fn, o.isdma, o.signal = eng, fn, False, False
        o.sem, o.cnt, o.waits, o.vc = None, 0, None, None
        o.deps = self._track(r, w, len(self.ops))
        self.ops.append(o)
        return o

    def dma(self, eng, out_ap, in_ap, r, w, **kw):
        o = self.op(eng, lambda e: e.dma_start(out=out_ap, in_=in_ap, **kw), r, w)
        o.isdma = True
        it = w[0] if isinstance(w[0], T) else w[0][0]
        if it.t is None and len(r) > 0:
            it = r[0] if isinstance(r[0], T) else r[0][0]
        tid = (it.id, eng == 'pool')
        if tid not in self.buf_dsem:
            self.buf_dsem[tid] = self.free_dsems[eng == 'pool'].pop()
            if not hasattr(self, 'sem_names'):
                self.sem_names = {}
            self.sem_names[self.buf_dsem[tid]] = it.name
        o.sem = self.buf_dsem[tid]
        self.dcnt[o.sem] = self.dcnt.get(o.sem, 0) + 16
        o.cnt = self.dcnt[o.sem]
        return o

    def pe(self, fn, r=(), w=()):
        return self.op('pe', fn, r, w)

    def act(self, fn, r=(), w=()):
        return self.op('act', fn, r, w)

    def dve(self, fn, r=(), w=()):
        return self.op('dve', fn, r, w)

    def pool(self, fn, r=(), w=()):
        return self.op('pool', fn, r, w)

    def flush(self):
        nc, ops = self.nc, self.ops
        for o in ops:
            for d, raw in o.deps.items():
                p = ops[d]
                if p.isdma or self._skip(p, o, raw):
                    continue
                p.signal = True
        for o in ops:
            if not o.isdma and o.signal:
                self.ecnt[o.eng] += 1
                o.cnt = self.ecnt[o.eng]
                o.sem = self.esem[o.eng]
        known = {e: dict(self.base) for e in ENGS}
        for o in ops:
            kn = known[o.eng]
            waits = {}
            for d in sorted(o.deps, reverse=True):
                p = ops[d]
                if self._skip(p, o, o.deps[d]):
                    continue
                if kn.get(p.sem, 0) >= p.cnt:
                    continue
                if waits.get(p.sem, 0) < p.cnt:
                    waits[p.sem] = p.cnt
                for s, c in p.vc.items():
                    if kn.get(s, 0) < c:
                        kn[s] = c
                kn[p.sem] = p.cnt
            o.waits = list(waits.items())
            if o.isdma and o.cnt > 16 and kn.get(o.sem, 0) < o.cnt - 16 and getattr(self, 'diag', False):
                print('DMA overlap on sem', self.sem_names.get(o.sem), 'cnt', o.cnt, 'known', kn.get(o.sem, 0))
            if o.isdma or o.signal:
                o.vc = dict(kn)
        by = {e: [o for o in ops if o.eng == e] for e in ENGS}
        final_d = [(s, self.dcnt[s]) for s in set(self.buf_dsem.values())]
        self.n_inst += len(ops)

        def emit(e, lst):
            for o in lst:
                for s, c in o.waits:
                    e.wait_ge(s, c)
                ins = o.fn(e)
                if o.isdma:
                    ins.then_inc(o.sem, 16)
                elif o.signal:
                    ins.then_inc(o.sem, 1)

        with nc.Block() as block:
            @block.tensor
            def _(e):
                emit(e, by['pe'])

            @block.scalar
            def _(e):
                emit(e, by['act'])

            @block.vector
            def _(e):
                emit(e, by['dve'])

            @block.gpsimd
            def _(e):
                emit(e, by['pool'])

            @block.sync
            def _(e):
                emit(e, by['sp'])
                for s, c in final_d:
                    e.wait_ge(s, c)
        for (tid_, sw), s in self.buf_dsem.items():
            self.free_dsems[sw].append(s)
        self.base = dict(self.dcnt)
        for e in ENGS:
            self.base[self.esem[e]] = self.ecnt[e]
        self.stack.close()
        self._reset()


D = 1024
DFF = 2816
NFC = DFF // 128
SEQ = 2048
EPS = 1e-6


def load_consts(S, cpack):
    c = {}
    idf = S.sb([128, 128], F32, 'idf')
    S.dma('sp', idf[:], cpack['ident'], r=[], w=[idf])
    idb = S.sb([128, 128], BF16, 'idb')
    S.dve(lambda e: e.tensor_copy(out=idb[:], in_=idf[:]), r=[idf], w=[idb])
    c['ident'] = idb
    return c


def rms_rstd(S, xt, junk, ss, rstd, width, key=None):
    S.act(lambda e: e.activation(out=junk[:], in_=xt[:], func=AF.Square, accum_out=ss[:]),
          r=[xt], w=[junk, ss])
    S.act(lambda e: e.activation(out=rstd[:], in_=ss[:], func=AF.Sqrt, bias=EPS, scale=1.0 / width),
          r=[ss], w=[rstd])
    S.dve(lambda e: e.reciprocal(out=rstd[:], in_=rstd[:]), r=[rstd], w=[rstd])


def ffn_stage(S, x_in, x_out, g_ap, wg, wu, wd, ntok, cpack, final_g=None, w16=None):
    CH = 1024
    NT = CH // 128
    consts = load_consts(S, cpack)
    ident = consts['ident']
    gb = S.sb([128, D], F32, 'gb')
    S.dma('sp', gb[:], g_ap.partition_broadcast(128), r=[], w=[gb])
    if final_g is not None:
        fgb = S.sb([128, D], F32, 'fgb')
        S.dma('sp', fgb[:], final_g.partition_broadcast(128), r=[], w=[fgb])
    hT = S.sb([128, 8, CH], BF16, 'hT')
    actT = S.sb([128, NFC, CH], BF16, 'actT')
    wdb = S.sb([128, NFC, D], BF16, 'wdb')
    xt = [S.sb([128, D], F32, f'xt{i}') for i in range(2)]
    hb = [S.sb([128, D], BF16, f'hb{i}') for i in range(2)]
    junk = S.sb([128, D], BF16, 'junk')
    ss = [S.sb([128, 1], F32, f'ss{i}') for i in range(2)]
    rstd = [S.sb([128, 1], F32, f'rstd{i}') for i in range(2)]
    FB = 256
    NB = DFF // FB
    wgs = [S.sb([128, 8, FB], F32, f'wgs{i}') for i in range(2)]
    wus = [S.sb([128, 8, FB], F32, f'wus{i}') for i in range(2)]
    wgb = [S.sb([128, 8, FB], BF16, f'wgb{i}') for i in range(2)]
    wub = [S.sb([128, 8, FB], BF16, f'wub{i}') for i in range(2)]
    wds = [S.sb([128, D], F32, f'wds{i}') for i in range(2)]
    sg = [S.sb([128, 512], F32, f'sg{i}') for i in range(2)]
    ot = [S.sb([128, D], F32, f'ot{i}') for i in range(2)]
    ptr = [S.ps([128, 8, 128], BF16, f'ptr{i}') for i in range(2)]
    pg = [S.ps([128, 512], F32, f'pg{i}') for i in range(2)]
    pu = [S.ps([128, 512], F32, f'pu{i}') for i in range(2)]
    py = [S.ps([128, 512], F32, f'py{i}') for i in range(2)]
    wg_v = wg.rearrange("(c p) f -> p c f", p=128)
    wu_v = wu.rearrange("(c p) f -> p c f", p=128)
    wd_v = wd.rearrange("(c p) m -> p c m", p=128)

    xt3 = [S.sb([128, D], F32, f'xt3{i}') for i in range(2)]

    def phase1_tile(ch, i):
        t0 = ch * CH
        b = i % 2
        x_t, h_b = xt[b], hb[b]
        S.dma('sp', x_t[:], x_in[t0 + i * 128:t0 + (i + 1) * 128, :], r=[], w=[x_t])
        rms_rstd(S, x_t, junk, ss[b], rstd[b], D)
        S.dve(lambda e: e.scalar_tensor_tensor(
            out=h_b[:], in0=x_t[:], scalar=rstd[b][:, 0:1], in1=gb[:], op0=ALU.mult, op1=ALU.mult),
            r=[x_t, rstd[b], gb], w=[h_b])
        p = ptr[b]
        for c in range(8):
            S.pe(lambda e, c=c: e.transpose(out=p[:, c, :], in_=h_b[:, c * 128:(c + 1) * 128], identity=ident[:]),
                 r=[h_b, ident], w=[(p, c)])
        S.act(lambda e: e.copy(out=hT[:, :, i * 128:(i + 1) * 128], in_=p[:]), r=[p], w=[(hT, i // 4)])

    wdT = T(None, 'wd16')

    def load_wd(ch, fc):
        if ch == 0 or w16 is None:
            s_ = wds[fc % 2]
            S.dma('sp', s_[:], wd_v[:, fc, :], r=[], w=[s_])
            S.act(lambda e: e.copy(out=wdb[:, fc, :], in_=s_[:]), r=[s_], w=[(wdb, fc)])
            if w16 is not None:
                S.dma('pool', w16['wd'][:, fc, :], wdb[:, fc, :], r=[(wdb, fc)], w=[(wdT, fc)])
        else:
            S.dma('sp', wdb[:, fc, :], w16['wd'][:, fc, :], r=[wdT], w=[(wdb, fc)])

    wgT = T(None, 'wg16')

    def phase2(ch):
        for fb in range(NB):
            b = fb % 2
            if ch == 0 or w16 is None:
                S.dma('sp', wgs[b][:], wg_v[:, :, fb * FB:(fb + 1) * FB], r=[], w=[wgs[b]])
                S.dma('sp', wus[b][:], wu_v[:, :, fb * FB:(fb + 1) * FB], r=[], w=[wus[b]])
                S.dve(lambda e, b=b: e.tensor_copy(out=wgb[b][:], in_=wgs[b][:]), r=[wgs[b]], w=[wgb[b]])
                S.dve(lambda e, b=b: e.tensor_copy(out=wub[b][:], in_=wus[b][:]), r=[wus[b]], w=[wub[b]])
                if w16 is not None:
                    S.dma('pool', w16['wg'][:, fb, :, :], wgb[b][:], r=[wgb[b]], w=[(wgT, ('g', fb))])
                    S.dma('pool', w16['wu'][:, fb, :, :], wub[b][:], r=[wub[b]], w=[(wgT, ('u', fb))])
            else:
                S.dma('sp', wgb[b][:], w16['wg'][:, fb, :, :], r=[wgT], w=[wgb[b]])
                S.dma('sp', wub[b][:], w16['wu'][:, fb, :, :], r=[wgT], w=[wub[b]])
            load_wd(ch, 2 * fb)
            load_wd(ch, 2 * fb + 1)
            for fs in range(FB // 128):
                fc = fb * (FB // 128) + fs
                for tb in range(CH // 512):
                    q = (fc * 2 + tb) % 2
                    for c in range(8):
                        S.pe(lambda e, q=q, b=b, c=c, fs=fs, tb=tb: e.matmul(
                            pg[q][:], lhsT=wgb[b][:, c, fs * 128:(fs + 1) * 128],
                            rhs=hT[:, c, tb * 512:(tb + 1) * 512], start=(c == 0), stop=(c == 7)),
                            r=[wgb[b], (hT, tb)], w=[pg[q]])
                    for c in range(8):
                        S.pe(lambda e, q=q, b=b, c=c, fs=fs, tb=tb: e.matmul(
                            pu[q][:], lhsT=wub[b][:, c, fs * 128:(fs + 1) * 128],
                            rhs=hT[:, c, tb * 512:(tb + 1) * 512], start=(c == 0), stop=(c == 7)),
                            r=[wub[b], (hT, tb)], w=[pu[q]])
                    S.act(lambda e, q=q: e.activation(out=sg[q][:], in_=pg[q][:], func=AF.Silu),
                          r=[pg[q]], w=[sg[q]])
                    S.dve(lambda e, q=q, fc=fc, tb=tb: e.tensor_tensor(
                        out=actT[:, fc, tb * 512:(tb + 1) * 512], in0=pu[q][:], in1=sg[q][:], op=ALU.mult),
                        r=[pu[q], sg[q]], w=[(actT, (fc, tb))])

    def phase3_tile(ch, i):
        t0 = ch * CH
        b = i % 2
        x_t, o_t = xt3[b], ot[b]
        S.dma('sp', x_t[:], x_in[t0 + i * 128:t0 + (i + 1) * 128, :], r=[], w=[x_t])
        for mh in range(2):
            p = py[mh]
            for fc in range(NFC):
                S.pe(lambda e, p=p, fc=fc, mh=mh: e.matmul(
                    p[:], lhsT=actT[:, fc, i * 128:(i + 1) * 128], rhs=wdb[:, fc, mh * 512:(mh + 1) * 512],
                    start=(fc == 0), stop=(fc == NFC - 1)),
                    r=[(actT, (fc, i // 4)), (wdb, fc)], w=[p])
            S.dve(lambda e, p=p, mh=mh: e.scalar_tensor_tensor(
                out=o_t[:, mh * 512:(mh + 1) * 512], in0=p[:], scalar=0.5, in1=x_t[:, mh * 512:(mh + 1) * 512],
                op0=ALU.mult, op1=ALU.add), r=[p, x_t], w=[(o_t, mh)])
        if final_g is not None:
            rms_rstd(S, o_t, junk, ss3[b], rstd3[b], D)
            S.dve(lambda e: e.scalar_tensor_tensor(
                out=o_t[:], in0=o_t[:], scalar=rstd3[b][:, 0:1], in1=fgb[:], op0=ALU.mult, op1=ALU.mult),
                r=[o_t, rstd3[b], fgb], w=[o_t])
        S.dma('pool', x_out[t0 + i * 128:t0 + (i + 1) * 128, :], o_t[:], r=[o_t], w=[DW(S, x_out)])

    ss3 = [S.sb([128, 1], F32, f'ss3{i}') for i in range(2)]
    rstd3 = [S.sb([128, 1], F32, f'rstd3{i}') for i in range(2)]
    nch = ntok // CH
    for i in range(NT):
        phase1_tile(0, i)
    for ch in range(nch):
        phase2(ch)
        for i in range(NT):
            phase3_tile(ch, i)
            if ch + 1 < nch:
                phase1_tile(ch + 1, i)
    S.flush()


_dram_T = {}
_dkey = [0]


def x_out_T(S, ap):
    k = ap.name
    if k not in _dram_T:
        _dram_T[k] = T(None, k)
    return _dram_T[k]


def DW(S, ap):
    _dkey[0] += 1
    return (x_out_T(S, ap), _dkey[0])


THETA = 500000.0
NEG = -30000.0


def _cpack_layout():
    items = [('ident', 128), ('tri_ge', 128), ('band_lt', 128), ('tril_st', 128), ('ones', 128),
             ('invf64', 1), ('nsgn64', 1), ('P64', 128), ('invf32', 1), ('nsgn32', 1), ('P32', 128),
             ('cmpbias', 2048), ('overlap', 32), ('E32', 2048), ('E8', 2048),
             ('keep', 512), ('addm', 512), ('adm', 512), ('tri_qs', 128), ('tri01', 128), ('band01', 128), ('mbias', 128), ('mpast', 128), ('mown', 128)]
    off, o = {}, 0
    for k, n in items:
        off[k] = (o, n)
        o += n
    return off, o


CP_OFF, CP_N = _cpack_layout()


def host_consts():
    cp = np.zeros((128, CP_N), np.float32)

    def put(k, a):
        o, n = CP_OFF[k]
        a = np.asarray(a, np.float32)
        cp[:a.shape[0], o:o + a.shape[1]] = a
    p = np.arange(128)
    put('ident', np.eye(128))
    kk, qq = p[:, None], p[None, :]
    put('tri_ge', np.where(qq >= kk, 0.0, NEG))
    put('band_lt', np.where(qq < kk, 0.0, NEG))
    put('tri01', (qq >= kk).astype(np.float32))
    put('band01', (qq < kk).astype(np.float32))
    put('tril_st', (kk <= qq).astype(np.float32))
    put('tri_qs', np.where(qq <= kk, 0.0, -1e30))
    put('ones', np.ones((128, 128)))
    m64 = p % 64
    put('invf64', np.where(m64 < 16, THETA ** (-(2.0 * (m64 % 8)) / 16.0), 0.0)[:, None])
    put('nsgn64', np.where(m64 < 8, -1.0, np.where(m64 < 16, 1.0, 0.0))[:, None])
    P = np.zeros((128, 128))
    for m in range(128):
        if m % 64 < 8:
            P[m + 8, m] = 1
        elif m % 64 < 16:
            P[m - 8, m] = 1
    put('P64', P)
    m32 = p % 32
    put('invf32', np.where(m32 < 8, THETA ** (-(2.0 * (m32 % 4)) / 8.0), 0.0)[:, None])
    put('nsgn32', np.where(m32 < 4, -1.0, np.where(m32 < 8, 1.0, 0.0))[:, None])
    P = np.zeros((128, 128))
    for m in range(128):
        if m % 32 < 4:
            P[m + 4, m] = 1
        elif m % 32 < 8:
            P[m - 4, m] = 1
    put('P32', P)
    n = np.arange(127)
    t = np.arange(2048)
    put('cmpbias', np.where(16 * n[:, None] + 31 <= t[None, :], 0.0, NEG))
    c0 = n * 16
    s0 = np.arange(32) * 64
    put('overlap', ((c0[:, None] < s0[None, :] + 64) & (c0[:, None] + 32 > s0[None, :])).astype(np.float32))
    put('E32', (t[None, :] // 64 == np.arange(32)[:, None]).astype(np.float32))
    put('E8', (t[None, :] // 256 == np.arange(8)[:, None]).astype(np.float32))
    tt = (np.arange(16)[None, :, None] * 128 + p[:, None, None])
    j = np.arange(32)[None, None, :]
    adm = j * 64 <= tt
    forced = (j == 0) | (j == tt // 64)
    put('keep', (adm & ~forced).astype(np.float32).reshape(128, 512))
    put('addm', np.where(adm, np.where(forced, 1e4, 0.0), -1e30).reshape(128, 512))
    put('adm', adm.astype(np.float32).reshape(128, 512))
    own = (np.arange(16)[None, :, None] * 128 + p[:, None, None]) // 256
    j8 = np.arange(8)[None, None, :]
    put('mbias', np.where(j8 < own, 0.0, -1e30).reshape(128, 128))
    put('mpast', (j8 < own).astype(np.float32).reshape(128, 128))
    put('mown', (j8 == own).astype(np.float32).reshape(128, 128))
    return cp


class Consts:
    def __init__(self, S, cpack_ap):
        self.S, self.ap, self.cache = S, cpack_ap, {}

    def get(self, k, dtype=F32, rows=128):
        key = (k, dtype)
        if key in self.cache:
            return self.cache[key]
        S = self.S
        o, n = CP_OFF[k]
        if dtype == F32:
            f = S.sb([128, n], F32, 'c_' + k)
            S.dma('sp', f[:], self.ap[:, o:o + n], r=[], w=[f], allow_slow_non_contiguous=(n == 1))
            self.cache[key] = f
            return f
        if not hasattr(self, 'stg'):
            self.stg = S.sb([128, 2048], F32, 'c_stg')
        f = self.stg
        S.dma('sp', f[:, 0:n], self.ap[:, o:o + n], r=[], w=[f])
        b = S.sb([128, n], dtype, 'cb_' + k)
        S.dve(lambda e: e.tensor_copy(out=b[:], in_=f[:, 0:n]), r=[f], w=[b])
        self.cache[key] = b
        return b


def rope_tables(S, C, pos_ap, ntok, invk, sgnk, tmp):
    invf = C.get(invk)
    nsg = C.get(sgnk)
    if 'pi' not in tmp:
        tmp['pi'] = S.sb([128, 1024], I32, 'pos_i')
        tmp['ang'] = S.sb([128, 1024], F32, 'ang')
        tmp['kf'] = S.sb([128, 1024], F32, 'kf')
        tmp['ki'] = S.sb([128, 1024], I32, 'ki')
    pi_, ang, kf, ki = tmp['pi'], tmp['ang'], tmp['kf'], tmp['ki']
    ct = S.sb([128, ntok], F32, 'ropeC')
    st = S.sb([128, ntok], F32, 'ropeS')
    TWO_PI = 2.0 * np.pi
    for c0 in range(0, ntok, 1024):
        S.dma('sp', pi_[:], pos_ap[c0:c0 + 1024].partition_broadcast(128), r=[], w=[pi_])
        S.dve(lambda e: e.tensor_copy(out=ang[:], in_=pi_[:]), r=[pi_], w=[ang])
        S.dve(lambda e: e.tensor_scalar(out=ang[:], in0=ang[:], scalar1=invf[:, 0:1], scalar2=None, op0=ALU.mult),
              r=[ang, invf], w=[ang])

        def reduce_sin(dst, shift, post, c0=c0):
            S.dve(lambda e: e.tensor_scalar(out=kf[:], in0=ang[:], scalar1=shift, scalar2=1.0 / TWO_PI,
                                            op0=ALU.add, op1=ALU.mult), r=[ang], w=[kf])
            S.dve(lambda e: e.tensor_copy(out=ki[:], in_=kf[:]), r=[kf], w=[ki])
            S.dve(lambda e: e.tensor_copy(out=kf[:], in_=ki[:]), r=[ki], w=[kf])
            S.dve(lambda e: e.scalar_tensor_tensor(out=kf[:], in0=kf[:], scalar=-TWO_PI, in1=ang[:],
                                                   op0=ALU.mult, op1=ALU.add), r=[kf, ang], w=[kf])
            S.dve(lambda e: e.tensor_scalar(out=kf[:], in0=kf[:], scalar1=shift, scalar2=3.14159, op0=ALU.add,
                                            op1=ALU.min), r=[kf], w=[kf])
            S.dve(lambda e: e.tensor_scalar(out=kf[:], in0=kf[:], scalar1=-3.14159, scalar2=None, op0=ALU.max),
                  r=[kf], w=[kf])
            S.act(lambda e: e.activation(out=dst[:, c0:c0 + 1024], in_=kf[:], func=AF.Sin), r=[kf], w=[(dst, c0)])
            if post is not None:
                S.dve(lambda e: e.tensor_scalar(out=dst[:, c0:c0 + 1024], in0=dst[:, c0:c0 + 1024],
                                                scalar1=post[:, 0:1], scalar2=None, op0=ALU.mult),
                      r=[(dst, c0), post], w=[(dst, c0)])
        reduce_sin(ct, np.pi / 2.0, None)
        reduce_sin(st, 0.0, nsg)
    return ct, st


def proj_stage(S, x, w_in, nin, g_ap, pos, ntok, cpack, fm_specs, tm_groups, tm_post, use_idx=False):
    C = Consts(S, cpack)
    ident = C.get('ident', BF16)
    gb = S.sb([128, D], F32, 'gb')
    S.dma('sp', gb[:], g_ap.partition_broadcast(128), r=[], w=[gb])
    winb = S.sb([128, 8, nin], BF16, 'winb')
    wv = w_in.rearrange("(c p) f -> p c f", p=128)
    wst = [S.sb([128, 8, 256], F32, f'wst{i}') for i in range(2)]
    for bi, c0 in enumerate(range(0, nin, 256)):
        n = min(256, nin - c0)
        st = wst[bi % 2]
        S.dma('sp', st[:, :, 0:n], wv[:, :, c0:c0 + n], r=[], w=[st])
        S.dve(lambda e, st=st, c0=c0, n=n: e.tensor_copy(out=winb[:, :, c0:c0 + n], in_=st[:, :, 0:n]),
              r=[st], w=[(winb, bi)])
    ropes = {}
    rtmp = {}
    if any(sp[3] == 'r64' for sp in fm_specs):
        ct, sn = rope_tables(S, C, pos, ntok, 'invf64', 'nsgn64', rtmp)
        ropes['r64'] = (ct, sn, C.get('P64', BF16))
    if use_idx:
        ct, sn = rope_tables(S, C, pos, ntok, 'invf32', 'nsgn32', rtmp)
        ropes['r32'] = (ct, sn, C.get('P32', BF16))
    xt = [S.sb([128, D], F32, f'xt{i}') for i in range(2)]
    hb = [S.sb([128, D], BF16, f'hb{i}') for i in range(2)]
    junk = S.sb([128, D], F32, 'junk')
    ss = [S.sb([128, 1], F32, f'ss{i}') for i in range(2)]
    rstd = [S.sb([128, 1], F32, f'rstd{i}') for i in range(2)]
    hT = [S.sb([128, 8, 512], BF16, f'hT{i}') for i in range(2)]
    xh = [S.sb([128, 512], BF16, f'xh{i}') for i in range(2)]
    t1 = [S.sb([128, 512], F32, f't1{i}') for i in range(2)]
    t2 = [S.sb([128, 512], F32, f't2{i}') for i in range(2)]
    xr = [S.sb([128, 512], BF16, f'xr{i}') for i in range(2)]
    ptr = [S.ps([128, 8, 128], BF16, f'ptr{i}') for i in range(2)]
    pf = [S.ps([128, 512], F32, f'pf{i}') for i in range(2)]
    p2 = [S.ps([128, 512], F32, f'p2{i}') for i in range(2)]
    pt = [S.ps([128, 512], F32, f'pt{i}') for i in range(2)]
    nfm = 0
    ntm = 0
    for blk in range(ntok // 512):
        t0 = blk * 512
        hTb = hT[blk % 2]
        for i in range(4):
            b = i % 2
            x_t, h_b = xt[b], hb[b]
            S.dma('sp', x_t[:], x[t0 + i * 128:t0 + (i + 1) * 128, :], r=[], w=[x_t])
            rms_rstd(S, x_t, junk, ss[b], rstd[b], D)
            S.dve(lambda e, x_t=x_t, h_b=h_b, b=b: e.scalar_tensor_tensor(
                out=h_b[:], in0=x_t[:], scalar=rstd[b][:, 0:1], in1=gb[:], op0=ALU.mult, op1=ALU.mult),
                r=[x_t, rstd[b], gb], w=[h_b])
            p = ptr[b]
            for c in range(8):
                S.pe(lambda e, p=p, h_b=h_b, c=c: e.transpose(out=p[:, c, :], in_=h_b[:, c * 128:(c + 1) * 128],
                                                               identity=ident[:]), r=[h_b, ident], w=[(p, c)])
            S.act(lambda e, p=p, i=i, hTb=hTb: e.copy(out=hTb[:, :, i * 128:(i + 1) * 128], in_=p[:]),
                  r=[p], w=[(hTb, i)])
        for (col0, M, scale, rope, raw_dst, rot_dst) in fm_specs:
            q = nfm % 2
            nfm += 1
            pp = pf[q]
            for c in range(8):
                S.pe(lambda e, pp=pp, c=c, col0=col0, M=M, hTb=hTb: e.matmul(
                    pp[0:M, :], lhsT=winb[:, c, col0:col0 + M], rhs=hTb[:, c, :], start=(c == 0), stop=(c == 7)),
                    r=[winb, hTb], w=[pp])
            xq = xh[q]
            S.act(lambda e, xq=xq, pp=pp, M=M, scale=scale: e.mul(out=xq[0:M, :], in_=pp[0:M, :], mul=scale),
                  r=[pp], w=[xq])
            if raw_dst is not None:
                S.dma('pool', raw_dst[:, t0:t0 + 512], xq[0:M, :], r=[xq], w=[DW(S, raw_dst)])
            if rope is not None:
                ct, sn, Pm = ropes[rope]
                pq = p2[q]
                S.pe(lambda e, pq=pq, xq=xq, M=M, Pm=Pm: e.matmul(pq[0:M, :], lhsT=Pm[0:M, 0:M], rhs=xq[0:M, :],
                                                                  start=True, stop=True), r=[xq, Pm], w=[pq])
                S.dve(lambda e, q=q, xq=xq, M=M, ct=ct, t0=t0: e.tensor_tensor(
                    out=t1[q][0:M, :], in0=xq[0:M, :], in1=ct[0:M, t0:t0 + 512], op=ALU.mult),
                    r=[xq, ct], w=[t1[q]])
                S.dve(lambda e, q=q, pq=pq, M=M, sn=sn, t0=t0: e.tensor_tensor(
                    out=t2[q][0:M, :], in0=pq[0:M, :], in1=sn[0:M, t0:t0 + 512], op=ALU.mult),
                    r=[pq, sn], w=[t2[q]])
                S.dve(lambda e, q=q, M=M: e.tensor_tensor(out=xr[q][0:M, :], in0=t1[q][0:M, :], in1=t2[q][0:M, :],
                                                         op=ALU.add), r=[t1[q], t2[q]], w=[xr[q]])
                S.dma('pool', rot_dst[:, t0:t0 + 512], xr[q][0:M, :], r=[xr[q]], w=[DW(S, rot_dst)])
        for i in range(4):
            for gi, (col0, N) in enumerate(tm_groups):
                q = ntm % 2
                ntm += 1
                pp = pt[q]
                for c in range(8):
                    S.pe(lambda e, pp=pp, c=c, col0=col0, N=N, i=i, hTb=hTb: e.matmul(
                        pp[:, 0:N], lhsT=hTb[:, c, i * 128:(i + 1) * 128], rhs=winb[:, c, col0:col0 + N],
                        start=(c == 0), stop=(c == 7)), r=[winb, (hTb, i)], w=[pp])
                tm_post(gi, pp, t0 + i * 128, ntm)
    S.flush()


def even_proj(S, x, prm, dr, ntok, cpack):
    at = [S.sb([128, 512], BF16, f'at{i}') for i in range(2)]
    g1 = [S.sb([128, 512], F32, f'g1{i}') for i in range(2)]
    vb = [S.sb([128, 192], BF16, f'vb{i}') for i in range(2)]
    vb2 = [S.sb([128, 192], BF16, f'vb2{i}') for i in range(2)]
    gt = [S.sb([128, 36], F32, f'gt{i}') for i in range(2)]
    sgb = S.sb([128, 256], F32, 'sgb')
    S.dma('sp', sgb[:], prm['sgu_norm'].partition_broadcast(128), r=[], w=[sgb])
    junk = S.sb([128, 256], F32, 'junk2')
    ss = [S.sb([128, 1], F32, f'ssv{i}') for i in range(2)]
    rs = [S.sb([128, 1], F32, f'rsv{i}') for i in range(2)]
    cnt = [0]

    def tm_post(gi, pp, tok0, n):
        if gi == 0:
            b = cnt[0] % 2
            cnt[0] += 1
            g, a = g1[b], at[b]
            S.act(lambda e: e.activation(out=g[:], in_=pp[:], func=AF.Gelu_apprx_tanh), r=[pp], w=[g])
            S.pool(lambda e: e.tensor_copy(out=a[:, 0:256], in_=g[:, 0:256]), r=[g], w=[(a, 0)])
            S.act(lambda e: e.activation(out=junk[:], in_=g[:, 256:512], func=AF.Square, accum_out=ss[b][:]),
                  r=[g], w=[junk, ss[b]])
            S.act(lambda e: e.activation(out=rs[b][:], in_=ss[b][:], func=AF.Sqrt, bias=EPS, scale=1.0 / 256),
                  r=[ss[b]], w=[rs[b]])
            S.dve(lambda e: e.reciprocal(out=rs[b][:], in_=rs[b][:]), r=[rs[b]], w=[rs[b]])
            S.dve(lambda e: e.scalar_tensor_tensor(out=a[:, 256:512], in0=g[:, 256:512], scalar=rs[b][:, 0:1],
                                                   in1=sgb[:], op0=ALU.mult, op1=ALU.mult),
                  r=[g, rs[b], sgb], w=[(a, 1)])
            S.dma('pool', dr['a_tok'][tok0:tok0 + 128, :], a[:], r=[a], w=[DW(S, dr['a_tok'])])
        elif gi == 1:
            v = vb[(tok0 // 128) % 2]
            S.act(lambda e: e.copy(out=v[:], in_=pp[:, 0:192]), r=[pp], w=[v])
            S.dma('pool', dr['vsl'][tok0:tok0 + 128, :], v[:], r=[v], w=[DW(S, dr['vsl'])])
        else:
            v = vb2[(tok0 // 128) % 2]
            g = gt[(tok0 // 128) % 2]
            S.act(lambda e: e.copy(out=v[:], in_=pp[:, 0:192]), r=[pp], w=[v])
            S.act(lambda e: e.activation(out=g[:], in_=pp[:, 192:228], func=AF.Sigmoid), r=[pp], w=[g])
            S.dma('pool', dr['vw'][tok0:tok0 + 128, :], v[:], r=[v], w=[DW(S, dr['vw'])])
            S.dma('pool', dr['gate'][tok0:tok0 + 128, :], g[:], r=[g], w=[DW(S, dr['gate'])])
    fm = []
    for i in range(6):
        fm.append((512 + 128 * i, 128, 0.125, 'r64', dr['qraw'][128 * i:128 * (i + 1), :],
                   dr['qrot'][128 * i:128 * (i + 1), :]))
    for nm, c0, rope in (('kc', 1280, None), ('vc', 1472, None), ('ksl', 1664, 'r64'), ('kw', 2048, 'r64')):
        for (o, M) in ((0, 128), (128, 64)):
            dst = dr[nm][o:o + M, :]
            fm.append((c0 + o, M, 1.0, rope, dst if rope is None else None, dst if rope else None))
    proj_stage(S, x, prm['w_in'], 2468, prm['mix_norm'], prm['pos'], ntok, cpack, fm,
               [(0, 512), (1856, 192), (2240, 228)], tm_post)


def attn_chunk(S, c, tiles, qT, kT, extra, vext, STps, PT, Oacc, finalize):
    cover = {}
    for n, (kt, lo, hi, bt, blo) in enumerate(tiles):
        for j in range(lo // 128, hi // 128):
            cover.setdefault(j, []).append(n)
    NP = len(PT)

    def qk(n):
        kt, lo, hi, bt, blo = tiles[n]
        ps = STps[n % 2]
        S.pe(lambda e: e.matmul(ps[:, lo:hi], lhsT=kT[:, kt * 128:(kt + 1) * 128],
                                rhs=qT[:, c * 512 + lo:c * 512 + hi], start=True, stop=(extra is None)),
             r=[kT, qT], w=[ps])
        if extra:
            S.pe(lambda e: e.matmul(ps[:, lo:hi], lhsT=extra[0](kt), rhs=extra[1](kt, c, lo, hi),
                                    start=False, stop=True, skip_group_check=True), r=list(extra[2]), w=[ps])

    qk(0)
    for n, (kt, lo, hi, bt, blo) in enumerate(tiles):
        if n + 1 < len(tiles):
            qk(n + 1)
        ps, p = STps[n % 2], PT[n % NP]
        S.act(lambda e, ps=ps, p=p, lo=lo, hi=hi: e.activation(out=p[:, lo:hi], in_=ps[:, lo:hi], func=AF.Exp),
              r=[ps], w=[p])
        if bt is not None:
            S.dve(lambda e, p=p, bt=bt, blo=blo: e.tensor_tensor(out=p[:, blo:blo + 128], in0=p[:, blo:blo + 128],
                                                                 in1=bt[:], op=ALU.mult), r=[p, bt], w=[p])
        for j in range(lo // 128, hi // 128):
            S.pe(lambda e, p=p, j=j, kt=kt, n=n: e.matmul(
                Oacc[j][:, 0:65], lhsT=p[:, j * 128:(j + 1) * 128], rhs=vext[:, kt, :],
                start=(cover[j][0] == n), stop=(cover[j][-1] == n), skip_group_check=True),
                r=[p, vext], w=[Oacc[j]])
            if cover[j][-1] == n:
                finalize(c, j, Oacc[j])


def attn_run(S, jobs, STps, PT, Oacc, LA=2):
    flat = []
    for ji, jb in enumerate(jobs):
        cover = {}
        for n, (kt, lo, hi, bt, blo) in enumerate(jb['tiles']):
            for j in range(lo // 128, hi // 128):
                cover.setdefault(j, []).append(n)
        jb['cover'] = cover
        for n in range(len(jb['tiles'])):
            flat.append((ji, n))
    NS, NP = len(STps), len(PT)

    def qk(f):
        ji, n = flat[f]
        jb = jobs[ji]
        if n == 0 and jb.get('pre') is not None:
            jb['pre']()
        kt, lo, hi, bt, blo = jb['tiles'][n]
        c, qT, kT, extra = jb['c'], jb['qT'], jb['kT'], jb.get('extra')
        ps = STps[f % NS]
        S.pe(lambda e: e.matmul(ps[:, lo:hi], lhsT=kT[:, kt * 128:(kt + 1) * 128],
                                rhs=qT[:, c * 512 + lo:c * 512 + hi], start=True, stop=(extra is None)),
             r=[kT, qT], w=[ps])
        if extra:
            S.pe(lambda e: e.matmul(ps[:, lo:hi], lhsT=extra[0](kt), rhs=extra[1](kt, c, lo, hi),
                                    start=False, stop=True), r=list(extra[2]), w=[ps])

    for f in range(min(LA, len(flat))):
        qk(f)
    for f, (ji, n) in enumerate(flat):
        if f + LA < len(flat):
            qk(f + LA)
        jb = jobs[ji]
        kt, lo, hi, bt, blo = jb['tiles'][n]
        cover, vext = jb['cover'], jb['vext']
        ps, p = STps[f % NS], PT[f % NP]
        S.act(lambda e, ps=ps, p=p, lo=lo, hi=hi: e.activation(out=p[:, lo:hi], in_=ps[:, lo:hi], func=AF.Exp),
              r=[ps], w=[p])
        if bt is not None:
            S.pool(lambda e, p=p, bt=bt, blo=blo: e.tensor_tensor(out=p[:, blo:blo + 128], in0=p[:, blo:blo + 128],
                                                                  in1=bt[:], op=ALU.mult), r=[p, bt], w=[p])
        for j in range(lo // 128, hi // 128):
            O = Oacc[j]
            S.pe(lambda e, p=p, j=j, kt=kt, O=O, vext=vext, first=(cover[j][0] == n), last=(cover[j][-1] == n):
                 e.matmul(O[:, 0:65], lhsT=p[:, j * 128:(j + 1) * 128], rhs=vext[:, kt, :], start=first, stop=last),
                 r=[p, vext], w=[O])
            if cover[j][-1] == n:
                jb['fin'](jb['c'], j, O, 0)


def causal_tiles(c, tri):
    out = []
    for kt in range(4 * c + 4):
        if kt < 4 * c:
            out.append((kt, 0, 512, None, 0))
        else:
            lo = (kt - 4 * c) * 128
            out.append((kt, lo, 512, tri, lo))
    return out


def window_tiles(c, tri, band):
    out = []
    if c >= 1:
        out.append((4 * c - 1, 0, 512, band, 384))
        for i in range(3):
            out.append((4 * c - 4 + i, 0, 128 * (i + 1), band, 128 * i))
    for i in range(4):
        out.append((4 * c + i, 128 * i, 512, tri, 128 * i))
    return out


def nsa_stage(S, prm, dr, nseq, cpack):
    C = Consts(S, cpack)
    ident = C.get('ident', BF16)
    tri = C.get('tri01', BF16)
    band = C.get('band01', BF16)
    ones = C.get('ones', BF16)
    cmpb = C.get('cmpbias', BF16)
    ovl = C.get('overlap', F32)
    E32 = C.get('E32', F32)
    keep, addm, adm = C.get('keep'), C.get('addm'), C.get('adm')
    tril = C.get('tril_st', F32)
    wTf = S.sb([128, 4, 128], F32, 'wTf')
    S.dma('sp', wTf[:], prm['sgu_wT'].rearrange("g s t -> s g t"), r=[], w=[wTf])
    wTb = S.sb([128, 4, 128], BF16, 'wTb')
    for g in range(4):
        S.dve(lambda e, g=g: e.tensor_tensor(out=wTb[:, g, :], in0=wTf[:, g, :], in1=tril[:], op=ALU.mult),
              r=[wTf, tril], w=[(wTb, g)])
    bT = S.sb([128, 4], F32, 'bT')
    S.dma('sp', bT[:], prm['sgu_bT'], r=[], w=[bT])
    cw = {}
    stg = [S.sb([64, 4, 256], F32, f'w1s{i}') for i in range(2)]
    w2s = S.sb([128, 2, 64], F32, 'w2s')
    pst = S.sb([64, 32], F32, 'pst')
    pm0 = S.ps([128, 512], F32, 'pm0')
    pmisc = [pm0, pm0]
    ptb = S.ps([128, 1024], BF16, 'ptb')
    k = 0
    for kv in ('k', 'v'):
        w1b = S.sb([64, 32, 256], BF16, 'w1b' + kv)
        w1v = prm['cmp_w1_' + kv].rearrange("(l e) c -> e l c", e=64)
        for q4 in range(8):
            st = stg[k % 2]
            k += 1
            S.dma('sp', st[:], w1v[:, q4 * 4:(q4 + 1) * 4, :], r=[], w=[st])
            S.dve(lambda e, st=st, w1b=w1b, q4=q4: e.tensor_copy(out=w1b[:, q4 * 4:(q4 + 1) * 4, :], in_=st[:]),
                  r=[st], w=[(w1b, q4)])
        w2b = S.sb([128, 2, 64], BF16, 'w2b' + kv)
        S.dma('sp', w2s[:], prm['cmp_w2_' + kv].rearrange("(c p) e -> p c e", p=128), r=[], w=[w2s])
        S.dve(lambda e, w2b=w2b: e.tensor_copy(out=w2b[:], in_=w2s[:]), r=[w2s], w=[w2b])
        posb = S.sb([64, 32], BF16, 'posb' + kv)
        S.dma('sp', pst[:], prm['cmp_posT_' + kv], r=[], w=[pst])
        S.dve(lambda e, posb=posb: e.tensor_copy(out=posb[:], in_=pst[:]), r=[pst], w=[posb])
        pbias = S.sb([128, 2], F32, 'pbias' + kv)
        for cc in range(2):
            pp = pmisc[cc]
            for l in range(32):
                S.pe(lambda e, pp=pp, w1b=w1b, posb=posb, l=l, cc=cc: e.matmul(
                    pp[:, 0:1], lhsT=w1b[:, l, cc * 128:(cc + 1) * 128], rhs=posb[:, l:l + 1],
                    start=(l == 0), stop=(l == 31)), r=[w1b, posb], w=[pp])
            S.dve(lambda e, pp=pp, pbias=pbias, cc=cc: e.tensor_copy(out=pbias[:, cc:cc + 1], in_=pp[:, 0:1]),
                  r=[pp], w=[(pbias, cc)])
        cw[kv] = (w1b, w2b, pbias)
    atok = S.sb([128, 16, 512], BF16, 'atok')
    gates = S.sb([128, 16, 36], F32, 'gates')
    otok = S.sb([128, 16, 1024], BF16, 'otok')
    accg = S.sb([128, 16, 256], F32, 'accg')
    kcT = S.sb([64, SEQ], BF16, 'kcT')
    vcT = S.sb([64, SEQ], BF16, 'vcT')
    kslT = S.sb([128, SEQ], BF16, 'kslT')
    kwT = S.sb([128, SEQ], BF16, 'kwT')
    S.dve(lambda e: e.memset(kslT[:], 0.0), r=[], w=[kslT])
    S.pool(lambda e: e.memset(kwT[:], 0.0), r=[], w=[kwT])
    S.dve(lambda e: e.tensor_copy(out=kslT[64:96, :], in_=E32[0:32, :]), r=[E32], w=[(kslT, 'e')])
    vsl = S.sb([128, 16, 65], BF16, 'vslx')
    vw = S.sb([128, 16, 65], BF16, 'vwx')
    S.pool(lambda e: e.memset(vsl[:], 1.0), r=[], w=[vsl])
    S.pool(lambda e: e.memset(vw[:], 1.0), r=[], w=[vw])
    qraw = S.sb([128, SEQ], BF16, 'qrawT')
    qrots = [S.sb([128, SEQ], BF16, f'qrotT{i}') for i in range(2)]
    S.pool(lambda e: e.memset(qraw[:], 0.0), r=[], w=[qraw])
    S.dve(lambda e: e.memset(qrots[0][:], 0.0), r=[], w=[qrots[0]])
    S.pool(lambda e: e.memset(qrots[1][:], 0.0), r=[], w=[qrots[1]])
    ghT = S.sb([128, 2, 128], BF16, 'ghT')
    kcmpT = S.sb([128, 128], BF16, 'kcmpT')
    S.dve(lambda e: e.memset(kcmpT[:], 0.0), r=[], w=[kcmpT])
    vcmpx = S.sb([128, 65], BF16, 'vcmpx')
    S.dve(lambda e: e.memset(vcmpx[:], 1.0), r=[], w=[vcmpx])
    impacc = S.sb([128, 16, 32], F32, 'impacc')
    pcf = [S.sb([128, 512], BF16, f'pcf{i}') for i in range(2)]
    ovlb = C.get('overlap', BF16)
    PT = [S.sb([128, 512], BF16, f'PT{i}') for i in range(4)]
    gm = [S.sb([128, 256], F32, f'gm{i}') for i in range(2)]
    sel = [S.sb([128, 32], F32, f'sel{i}') for i in range(2)]
    m8 = [S.sb([128, 8], F32, f'm8{i}') for i in range(2)]
    nsel = [S.sb([128, 32], BF16, f'nsel{i}') for i in range(2)]
    fsc = [S.sb([128, 1], F32, f'fsc{i}') for i in range(4)]
    ftmp = [S.sb([128, 64], F32, f'ftmp{i}') for i in range(4)]
    STps = [S.ps([128, 512], F32, f'st{i}') for i in range(2)] + [pm0]
    Oacc = [S.ps([128, 512], F32, f'oa{i}') for i in range(4)]
    fcount = [0]

    def make_finalize(r, gidx, first):
        def fin(c, j, O, off):
            i = 4 * c + j
            f = fsc[fcount[0] % 4]
            fcount[0] += 1
            tm = ftmp[fcount[0] % 4]
            S.dve(lambda e: e.reciprocal(out=f[:], in_=O[:, off + 64:off + 65]), r=[O], w=[f])
            S.dve(lambda e: e.tensor_scalar(out=tm[:], in0=O[:, off:off + 64], scalar1=f[:, 0:1],
                                            scalar2=gates[:, i, gidx:gidx + 1], op0=ALU.mult, op1=ALU.mult),
                  r=[O, f, gates], w=[tm])
            S.pool(lambda e: e.tensor_tensor(out=accg[:, i, r * 64:(r + 1) * 64], in0=accg[:, i, r * 64:(r + 1) * 64],
                                             in1=tm[:], op=ALU.add), r=[tm, (accg, (i, r))], w=[(accg, (i, r))])
        return fin

    for sq in range(nseq):
        tb = sq * SEQ
        S.dma('sp', atok[:], dr['a_tok'][tb:tb + SEQ, :].rearrange("(n p) c -> p n c", p=128), r=[], w=[atok])
        S.dma('sp', gates[:], dr['gate'][tb:tb + SEQ, :].rearrange("(n p) c -> p n c", p=128), r=[], w=[gates])
        for i in range(16):
            pp = pmisc[i % 2]
            g_ = gm[i % 2]
            for g in range(4):
                S.pe(lambda e, pp=pp, g=g, i=i: e.matmul(pp[:, g * 64:(g + 1) * 64], lhsT=wTb[:, g, :],
                                                          rhs=atok[:, i, 256 + g * 64:256 + (g + 1) * 64],
                                                          start=True, stop=True), r=[wTb, atok], w=[pp])
                S.dve(lambda e, pp=pp, g_=g_, g=g: e.tensor_scalar(
                    out=g_[:, g * 64:(g + 1) * 64], in0=pp[:, g * 64:(g + 1) * 64], scalar1=bT[:, g:g + 1],
                    scalar2=None, op0=ALU.add), r=[pp, bT], w=[(g_, g)])
            S.dve(lambda e, g_=g_, i=i: e.tensor_tensor(out=otok[:, i, 0:256], in0=g_[:], in1=atok[:, i, 0:256],
                                                         op=ALU.mult), r=[g_, atok], w=[(otok, (i, 0))])
        for g in range(3):
            for nm, tl in (('kc', kcT), ('vc', vcT)):
                S.dma('sp', tl[:], dr[nm][g * 64:(g + 1) * 64, tb:tb + SEQ], r=[], w=[tl])
            for nm, tl in (('ksl', kslT), ('kw', kwT)):
                S.dma('sp', tl[0:64, :], dr[nm][g * 64:(g + 1) * 64, tb:tb + SEQ], r=[], w=[(tl, 'k')])
            for nm, tl in (('vsl', vsl), ('vw', vw)):
                S.dma('sp', tl[:, :, 0:64], dr[nm][tb:tb + SEQ, g * 64:(g + 1) * 64].rearrange(
                    "(n p) c -> p n c", p=128), r=[], w=[tl])
            for kv, src in (('k', kcT), ('v', vcT)):
                w1b, w2b, pbias = cw[kv]
                for cc in range(2):
                    pp = pmisc[cc]
                    for l in range(32):
                        S.pe(lambda e, pp=pp, w1b=w1b, src=src, l=l, cc=cc: e.matmul(
                            pp[:, 0:127], lhsT=w1b[:, l, cc * 128:(cc + 1) * 128], rhs=src[:, l:l + 2017:16],
                            start=(l == 0), stop=(l == 31)), r=[w1b, src], w=[pp])
                    S.act(lambda e, pp=pp, cc=cc, pbias=pbias: e.activation(
                        out=ghT[:, cc, 0:127], in_=pp[:, 0:127], func=AF.Gelu_apprx_tanh, bias=pbias[:, cc:cc + 1]),
                        r=[pp, pbias], w=[(ghT, cc)])
                pp = pmisc[0]
                if kv == 'k':
                    for cc in range(2):
                        S.pe(lambda e, pp=pp, cc=cc, w2b=w2b: e.matmul(pp[0:64, 0:127], lhsT=w2b[:, cc, :],
                                                                     rhs=ghT[:, cc, 0:127], start=(cc == 0),
                                                                     stop=(cc == 1)), r=[w2b, ghT], w=[pp])
                    S.act(lambda e, pp=pp: e.copy(out=kcmpT[0:64, 0:127], in_=pp[0:64, 0:127]), r=[pp], w=[(kcmpT, 'k')])
                else:
                    for cc in range(2):
                        S.pe(lambda e, pp=pp, cc=cc, w2b=w2b: e.matmul(pp[0:127, 0:64], lhsT=ghT[:, cc, 0:127],
                                                                     rhs=w2b[:, cc, :], start=(cc == 0),
                                                                     stop=(cc == 1)), r=[w2b, ghT], w=[pp])
                    S.act(lambda e, pp=pp: e.copy(out=vcmpx[0:127, 0:64], in_=pp[0:127, 0:64]), r=[pp], w=[(vcmpx, 'v')])
            for r in range(4):
                h = 4 * g + r
                S.dma('sp', qraw[0:64, :], dr['qraw'][h * 64:(h + 1) * 64, tb:tb + SEQ], r=[], w=[(qraw, 'q')])
                for c in range(4):
                    b = c % 2
                    ps = STps[b]
                    S.pe(lambda e, ps=ps, c=c: e.matmul(ps[0:127, :], lhsT=kcmpT[:, 0:127],
                                                        rhs=qraw[:, c * 512:(c + 1) * 512], start=True, stop=False),
                         r=[kcmpT, qraw], w=[ps])
                    S.pe(lambda e, ps=ps, c=c: e.matmul(ps[0:127, :], lhsT=ident[0:127, 0:127],
                                                        rhs=cmpb[0:127, c * 512:(c + 1) * 512], start=False, stop=True),
                         r=[ident, cmpb], w=[ps])
                    S.act(lambda e, ps=ps, b=b: e.activation(out=pcf[b][0:127, :], in_=ps[0:127, :], func=AF.Exp),
                          r=[ps], w=[pcf[b]])
                    for j in range(4):
                        i = 4 * c + j
                        O = Oacc[j]
                        S.pe(lambda e, O=O, b=b, j=j: e.matmul(O[:, 0:65], lhsT=pcf[b][0:127, j * 128:(j + 1) * 128],
                                                               rhs=vcmpx[0:127, :], start=True, stop=True),
                             r=[pcf[b], vcmpx], w=[O])
                        S.pe(lambda e, O=O, b=b, j=j: e.matmul(O[:, 128:160], lhsT=pcf[b][0:127, j * 128:(j + 1) * 128],
                                                               rhs=ovlb[0:127, 0:32], start=True, stop=True),
                             r=[pcf[b], ovlb], w=[O])
                        f = fsc[fcount[0] % 4]
                        fcount[0] += 1
                        S.dve(lambda e, O=O, f=f: e.tensor_scalar(out=f[:], in0=O[:, 64:65], scalar1=1e-30, scalar2=None,
                                                                  op0=ALU.max), r=[O], w=[f])
                        S.dve(lambda e, f=f: e.reciprocal(out=f[:], in_=f[:]), r=[f], w=[f])
                        S.dve(lambda e, O=O, i=i, r=r, h=h, f=f: e.tensor_scalar(
                            out=accg[:, i, r * 64:(r + 1) * 64], in0=O[:, 0:64], scalar1=f[:, 0:1],
                            scalar2=gates[:, i, 3 * h:3 * h + 1], op0=ALU.mult, op1=ALU.mult),
                            r=[O, f, gates], w=[(accg, (i, r))])
                        if r == 0:
                            S.dve(lambda e, O=O, i=i, f=f: e.tensor_scalar(out=impacc[:, i, :], in0=O[:, 128:160],
                                                                           scalar1=f[:, 0:1], scalar2=None, op0=ALU.mult),
                                  r=[O, f], w=[(impacc, i)])
                        else:
                            S.dve(lambda e, O=O, i=i, f=f: e.scalar_tensor_tensor(
                                out=impacc[:, i, :], in0=O[:, 128:160], scalar=f[:, 0:1], in1=impacc[:, i, :],
                                op0=ALU.mult, op1=ALU.add), r=[O, f, (impacc, i)], w=[(impacc, i)])
            for i in range(16):
                b = i % 2
                sl = sel[b]
                S.dve(lambda e, sl=sl, i=i: e.tensor_tensor(out=sl[:], in0=impacc[:, i, :],
                                                           in1=keep[:, i * 32:(i + 1) * 32], op=ALU.mult),
                      r=[(impacc, i), keep], w=[sl])
                S.dve(lambda e, sl=sl, i=i: e.tensor_tensor(out=sl[:], in0=sl[:], in1=addm[:, i * 32:(i + 1) * 32],
                                                           op=ALU.add), r=[sl, addm], w=[sl])
                S.dve(lambda e, sl=sl, b=b: e.max(out=m8[b][:], in_=sl[:]), r=[sl], w=[m8[b]])
                S.dve(lambda e, sl=sl, b=b: e.tensor_scalar(out=sl[:], in0=sl[:], scalar1=m8[b][:, 7:8], scalar2=None,
                                                            op0=ALU.is_ge), r=[sl, m8[b]], w=[sl])
                S.dve(lambda e, sl=sl, i=i: e.tensor_tensor(out=sl[:], in0=sl[:], in1=adm[:, i * 32:(i + 1) * 32],
                                                           op=ALU.mult), r=[sl, adm], w=[sl])
                S.dve(lambda e, sl=sl, b=b: e.tensor_scalar(out=nsel[b][:], in0=sl[:], scalar1=-NEG, scalar2=NEG,
                                                            op0=ALU.mult, op1=ALU.add), r=[sl], w=[nsel[b]])
                S.pe(lambda e, b=b: e.transpose(out=ptb[0:32, b * 128:(b + 1) * 128], in_=nsel[b][:],
                                                identity=ident[:]), r=[nsel[b], ident], w=[(ptb, b)])
                for qb in qrots:
                    S.dve(lambda e, i=i, b=b, qb=qb: e.tensor_copy(out=qb[64:96, i * 128:(i + 1) * 128],
                                                                   in_=ptb[0:32, b * 128:(b + 1) * 128]),
                          r=[(ptb, b)], w=[(qb, ('n', i))])
            def load_q(r, g=g, tb=tb):
                h = 4 * g + r
                qb = qrots[r % 2]
                S.dma('sp', qb[0:64, :], dr['qrot'][h * 64:(h + 1) * 64, tb:tb + SEQ], r=[], w=[(qb, 'q')])
            load_q(0)
            jobs = []
            for r in range(4):
                h = 4 * g + r
                qb = qrots[r % 2]
                for c in range(4):
                    jobs.append(dict(c=c, tiles=causal_tiles(c, tri), qT=qb, kT=kslT, vext=vsl,
                                     fin=make_finalize(r, 3 * h + 1, False),
                                     pre=((lambda r=r: load_q(r + 1)) if (c == 0 and r < 3) else None)))
                for c in range(4):
                    jobs.append(dict(c=c, tiles=window_tiles(c, tri, band), qT=qb, kT=kwT, vext=vw,
                                     fin=make_finalize(r, 3 * h + 2, False)))
            attn_run(S, jobs, STps, PT, Oacc)
            S.act(lambda e, g=g: e.copy(out=otok[:, :, 256 + 256 * g:256 + 256 * (g + 1)], in_=accg[:]),
                  r=[accg], w=[(otok, ('g', g))])
        S.dma('sp', dr['o_tok'][tb:tb + SEQ, :].rearrange("(n p) c -> p n c", p=128), otok[:],
              r=[otok], w=[DW(S, dr['o_tok'])])
    S.flush()


def outproj_stage(S, x_in, x_out, o_tok, w_out, ntok, cpack):
    C = Consts(S, cpack)
    ident = C.get('ident', BF16)
    wob = S.sb([128, 8, D], BF16, 'wob')
    wst = [S.sb([128, D], F32, f'wos{i}') for i in range(2)]
    wv = w_out.rearrange("(c p) m -> p c m", p=128)
    for c in range(8):
        st = wst[c % 2]
        S.dma('sp', st[:], wv[:, c, :], r=[], w=[st])
        S.dve(lambda e, st=st, c=c: e.tensor_copy(out=wob[:, c, :], in_=st[:]), r=[st], w=[(wob, c)])
    ot = [S.sb([128, D], BF16, f'oo{i}') for i in range(2)]
    oT = [S.sb([128, 8, 128], BF16, f'oT{i}') for i in range(2)]
    xt = [S.sb([128, D], F32, f'xo{i}') for i in range(2)]
    xo = [S.sb([128, D], F32, f'xn{i}') for i in range(2)]
    ptr = [S.ps([128, 8, 128], BF16, f'ptr{i}') for i in range(2)]
    py = [S.ps([128, 512], F32, f'py{i}') for i in range(4)]
    for i in range(ntok // 128):
        b = i % 2
        S.dma('sp', ot[b][:], o_tok[i * 128:(i + 1) * 128, :], r=[], w=[ot[b]])
        S.dma('sp', xt[b][:], x_in[i * 128:(i + 1) * 128, :], r=[], w=[xt[b]])
        p = ptr[b]
        for c in range(8):
            S.pe(lambda e, p=p, b=b, c=c: e.transpose(out=p[:, c, :], in_=ot[b][:, c * 128:(c + 1) * 128],
                                                      identity=ident[:]), r=[ot[b], ident], w=[(p, c)])
        S.act(lambda e, p=p, b=b: e.copy(out=oT[b][:], in_=p[:]), r=[p], w=[oT[b]])
        for mh in range(2):
            pp = py[b * 2 + mh]
            for c in range(8):
                S.pe(lambda e, pp=pp, b=b, c=c, mh=mh: e.matmul(pp[:], lhsT=oT[b][:, c, :],
                                                                rhs=wob[:, c, mh * 512:(mh + 1) * 512],
                                                                start=(c == 0), stop=(c == 7)),
                     r=[oT[b], wob], w=[pp])
            S.dve(lambda e, pp=pp, b=b, mh=mh: e.tensor_tensor(out=xo[b][:, mh * 512:(mh + 1) * 512], in0=pp[:],
                                                               in1=xt[b][:, mh * 512:(mh + 1) * 512], op=ALU.add),
                  r=[pp, xt[b]], w=[(xo[b], mh)])
        S.dma('pool', x_out[i * 128:(i + 1) * 128, :], xo[b][:], r=[xo[b]], w=[DW(S, x_out)])
    S.flush()


def odd_proj(S, x, prm, dr, ntok, cpack):
    vb = [S.sb([128, 64], BF16, f'vb{i}') for i in range(2)]
    wb = [S.sb([128, 4], F32, f'wb{i}') for i in range(2)]
    vd = [S.sb([128, 512], BF16, f'vd{i}') for i in range(2)]

    def tm_post(gi, pp, tok0, n):
        b = (tok0 // 128) % 2
        if gi == 0:
            S.act(lambda e: e.copy(out=vb[b][:], in_=pp[:, 0:64]), r=[pp], w=[vb[b]])
            S.dma('pool', dr['vcd'][tok0:tok0 + 128, :], vb[b][:], r=[vb[b]], w=[DW(S, dr['vcd'])])
        elif gi == 1:
            S.act(lambda e: e.copy(out=wb[b][:], in_=pp[:, 0:4]), r=[pp], w=[wb[b]])
            S.dma('pool', dr['wi'][tok0:tok0 + 128, :], wb[b][:], r=[wb[b]], w=[DW(S, dr['wi'])])
        else:
            S.act(lambda e: e.copy(out=vd[b][:], in_=pp[:, 0:512]), r=[pp], w=[vd[b]])
            S.dma('pool', dr['vdd'][tok0:tok0 + 128, :], vd[b][:], r=[vd[b]], w=[DW(S, dr['vdd'])])
    fm = []
    for i in range(4):
        fm.append((128 * i, 128, 0.125, 'r64', None, dr['qc'][128 * i:128 * (i + 1), :]))
    fm.append((512, 64, 1.0, 'r64', None, dr['kcd'][:, :]))
    fm.append((640, 128, 1.0, 'r32', None, dr['qi'][:, :]))
    fm.append((768, 32, 1.0, 'r32', None, dr['ki'][:, :]))
    for i in range(4):
        fm.append((804 + 128 * i, 128, 0.125, 'r64', None, dr['qd'][128 * i:128 * (i + 1), :]))
    for i in range(4):
        fm.append((1316 + 128 * i, 128, 1.0, 'r64', None, dr['kd'][128 * i:128 * (i + 1), :]))
    proj_stage(S, x, prm['w_in'], 2340, prm['mix_norm'], prm['pos'], ntok, cpack, fm,
               [(576, 64), (800, 4), (1828, 512)], tm_post, use_idx=True)


NBIS = 14


def dsa_moba_stage(S, prm, dr, nseq, cpack):
    C = Consts(S, cpack)
    ident = C.get('ident', BF16)
    tri = C.get('tri01', BF16)
    trib = C.get('tri_ge', BF16)
    E8 = C.get('E8', F32)
    triqs = C.get('tri_qs', F32)
    mbias, mpast, mown = C.get('mbias'), C.get('mpast'), C.get('mown')
    zc = [0]

    def ztile(shape, name):
        t = S.sb(shape, BF16, name)
        zc[0] += 1
        if zc[0] % 2:
            S.dve(lambda e: e.memset(t[:], 0.0), r=[], w=[t])
        else:
            S.pool(lambda e: e.memset(t[:], 0.0), r=[], w=[t])
        return t
    qi = [ztile([128, SEQ], f'qi{h}') for h in range(4)]
    ki = ztile([128, SEQ], 'ki')
    wi = S.sb([128, 16, 4], F32, 'wi')
    absw = S.sb([128, 16, 4], F32, 'absw')
    sgnw = S.sb([128, 16, 4], F32, 'sgnw')
    scores = [S.sb([128, SEQ], F32, f'score{i}') for i in range(2)]
    rl = [S.sb([128, 512], F32, f'rl{i}') for i in range(2)]
    junkb = S.sb([128, SEQ], BF16, 'junkb')
    nmasks = [S.sb([128, SEQ], BF16, f'nmask{i}') for i in range(2)]
    nmTs = [S.sb([128, 16, 512], BF16, f'nmT{i}') for i in range(2)]
    kcT = ztile([128, SEQ], 'kcT')
    vcx = S.sb([128, 16, 65], BF16, 'vcx')
    vdxs = [S.sb([128, 16, 65], BF16, f'vdx{i}') for i in range(2)]
    S.pool(lambda e: e.memset(vcx[:], 1.0), r=[], w=[vcx])
    for v_ in vdxs:
        S.pool(lambda e, v_=v_: e.memset(v_[:], 1.0), r=[], w=[v_])
    qcall = [ztile([128, SEQ], f'qcall{h}') for h in range(8)]
    qds = [ztile([128, SEQ], f'qd{i}') for i in range(2)]
    kds = [ztile([128, SEQ], f'kd{i}') for i in range(2)]
    for kd_ in kds:
        S.dve(lambda e, kd_=kd_: e.tensor_copy(out=kd_[64:72, :], in_=E8[0:8, :]), r=[E8], w=[(kd_, 'e')])
    kmf = S.sb([64, 8], F32, 'kmf')
    kmbs = [ztile([128, 8], f'kmb{i}') for i in range(2)]
    gsb = S.sb([128, 128], F32, 'gsb')
    ns8all = S.sb([128, 16, 32], BF16, 'ns8all')
    otok = S.sb([128, 16, 1024], BF16, 'otok')
    PT = [S.sb([128, 512], BF16, f'PT{i}') for i in range(4)]
    st5 = [S.sb([128, 8], F32, f'st5{i}') for i in range(2)]
    gs = [S.sb([128, 8], F32, f'gs{i}') for i in range(2)]
    m8 = [S.sb([128, 8], F32, f'm8{i}') for i in range(2)]
    ns8 = [S.sb([128, 8], BF16, f'ns8{i}') for i in range(2)]
    fsc = [S.sb([128, 1], F32, f'fsc{i}') for i in range(4)]
    pl = S.ps([128, 512], F32, 'pl')
    STps = [S.ps([128, 512], F32, f'st{i}') for i in range(2)]
    Oacc = [S.ps([128, 512], F32, f'oa{i}') for i in range(4)]
    ptb = S.ps([128, 8, 128], BF16, 'ptb')
    fcount = [0]

    def make_fin(col0):
        def fin(c, j, O, off):
            i = 4 * c + j
            f = fsc[fcount[0] % 4]
            fcount[0] += 1
            S.dve(lambda e: e.reciprocal(out=f[:], in_=O[:, off + 64:off + 65]), r=[O], w=[f])
            S.dve(lambda e: e.tensor_scalar(out=otok[:, i, col0:col0 + 64], in0=O[:, off:off + 64], scalar1=f[:, 0:1],
                                            scalar2=None, op0=ALU.mult), r=[O, f], w=[(otok, (i, col0))])
        return fin

    def index_steps(c):
        nmT = nmTs[c % 2]
        steps = []
        chains = {}
        for j in range(4):
            i = 4 * c + j
            W = 128 * (i + 1)
            score = scores[j % 2]
            nmask = nmasks[j % 2]
            steps = chains.setdefault(j, [])
            if i < 2:
                def trivial(i=i, j=j):
                    for st_ in range(i + 1):
                        if st_ == i:
                            S.pool(lambda e, st_=st_: e.tensor_copy(out=nmT[:, st_, j * 128:(j + 1) * 128], in_=trib[:]),
                                   r=[trib], w=[(nmT, (st_, j))])
                        else:
                            S.pool(lambda e, st_=st_: e.memset(nmT[:, st_, j * 128:(j + 1) * 128], 0.0),
                                   r=[], w=[(nmT, (st_, j))])
                steps.append(trivial)
                continue
            s5 = st5[i % 2]
            for h in range(4):
                def logits(h=h, i=i, W=W, score=score):
                    for sc in range((W + 511) // 512):
                        n = min(512, W - 512 * sc)
                        rb = rl[(h * 4 + sc) % 2]
                        S.pe(lambda e, sc=sc, n=n: e.matmul(pl[:, 0:n], lhsT=qi[h][:, i * 128:(i + 1) * 128],
                                                            rhs=ki[:, sc * 512:sc * 512 + n], start=True, stop=True),
                             r=[qi[h], ki], w=[pl])
                        S.act(lambda e, rb=rb, n=n: e.activation(out=rb[:, 0:n], in_=pl[:, 0:n], func=AF.Relu,
                                                                 scale=absw[:, i, h:h + 1]), r=[pl, absw], w=[rb])
                        if h == 0:
                            S.dve(lambda e, rb=rb, n=n, sc=sc: e.tensor_scalar(
                                out=score[:, sc * 512:sc * 512 + n], in0=rb[:, 0:n], scalar1=sgnw[:, i, h:h + 1],
                                scalar2=None, op0=ALU.mult), r=[rb, sgnw], w=[(score, sc)])
                        else:
                            S.dve(lambda e, rb=rb, n=n, sc=sc: e.scalar_tensor_tensor(
                                out=score[:, sc * 512:sc * 512 + n], in0=rb[:, 0:n], scalar=sgnw[:, i, h:h + 1],
                                in1=score[:, sc * 512:sc * 512 + n], op0=ALU.mult, op1=ALU.add),
                                r=[rb, sgnw, (score, sc)], w=[(score, sc)])
                steps.append(logits)

            def bounds(i=i, W=W, s5=s5, score=score):
                S.dve(lambda e: e.tensor_reduce(out=s5[:, 5:6], in_=score[:, 0:W], axis=AX.X, op=ALU.max),
                      r=[score], w=[(s5, 5)])
                S.dve(lambda e: e.tensor_reduce(out=s5[:, 0:1], in_=score[:, 0:W], axis=AX.X, op=ALU.min),
                      r=[score], w=[(s5, 0)])
                S.dve(lambda e: e.tensor_tensor(out=s5[:, 1:2], in0=s5[:, 5:6], in1=s5[:, 0:1], op=ALU.subtract),
                      r=[(s5, 5), (s5, 0)], w=[(s5, 1)])
                S.dve(lambda e: e.tensor_tensor(out=score[:, i * 128:(i + 1) * 128],
                                                in0=score[:, i * 128:(i + 1) * 128], in1=triqs[:], op=ALU.add),
                      r=[score, triqs], w=[score])
            steps.append(bounds)
            for it in range(NBIS):
                def bis(W=W, s5=s5, score=score):
                    S.dve(lambda e: e.tensor_scalar(out=s5[:, 1:2], in0=s5[:, 1:2], scalar1=0.5, scalar2=None,
                                                    op0=ALU.mult), r=[(s5, 1)], w=[(s5, 1)])
                    S.dve(lambda e: e.tensor_tensor(out=s5[:, 2:3], in0=s5[:, 0:1], in1=s5[:, 1:2], op=ALU.add),
                          r=[(s5, 0), (s5, 1)], w=[(s5, 2)])
                    S.dve(lambda e: e.tensor_scalar(out=junkb[:, 0:W], in0=score[:, 0:W], scalar1=s5[:, 2:3],
                                                    scalar2=0.0, op0=ALU.is_ge, op1=ALU.add, accum_out=s5[:, 3:4]),
                          r=[score, (s5, 2)], w=[junkb, (s5, 3)])
                    S.dve(lambda e: e.tensor_scalar(out=s5[:, 4:5], in0=s5[:, 3:4], scalar1=255.5, scalar2=None,
                                                    op0=ALU.is_ge), r=[(s5, 3)], w=[(s5, 4)])
                    S.dve(lambda e: e.scalar_tensor_tensor(out=s5[:, 0:1], in0=s5[:, 1:2], scalar=s5[:, 4:5],
                                                           in1=s5[:, 0:1], op0=ALU.mult, op1=ALU.add),
                          r=[(s5, 1), (s5, 4), (s5, 0)], w=[(s5, 0)])
                steps.append(bis)

            def fin_mask(i=i, j=j, W=W, s5=s5, score=score, nmask=nmask):
                S.dve(lambda e: e.tensor_scalar(out=nmask[:, 0:W], in0=score[:, 0:W], scalar1=s5[:, 0:1],
                                                scalar2=NEG, op0=ALU.is_lt, op1=ALU.mult),
                      r=[score, (s5, 0)], w=[nmask])
                for s0 in range(0, i + 1, 8):
                    n = min(8, i + 1 - s0)
                    for k in range(n):
                        S.pe(lambda e, s0=s0, k=k: e.transpose(out=ptb[:, k, :],
                                                               in_=nmask[:, (s0 + k) * 128:(s0 + k + 1) * 128],
                                                               identity=ident[:]), r=[nmask, ident], w=[(ptb, k)])
                    S.act(lambda e, s0=s0, n=n: e.copy(out=nmT[:, s0:s0 + n, j * 128:(j + 1) * 128],
                                                       in_=ptb[:, 0:n, :]), r=[ptb], w=[(nmT, ('b', s0, j))])
            steps.append(fin_mask)
        out = []
        for pair in ((0, 1), (2, 3)):
            la, lb = chains[pair[0]], chains[pair[1]]
            for k in range(max(len(la), len(lb))):
                if k < len(la):
                    out.append(la[k])
                if k < len(lb):
                    out.append(lb[k])
        return out

    for sq in range(nseq):
        tb = sq * SEQ
        for h in range(4):
            S.dma('sp', qi[h][0:32, :], dr['qi'][h * 32:(h + 1) * 32, tb:tb + SEQ], r=[], w=[(qi[h], 'q')])
        S.dma('sp', ki[0:32, :], dr['ki'][:, tb:tb + SEQ], r=[], w=[(ki, 'q')])
        S.dma('sp', wi[:], dr['wi'][tb:tb + SEQ, :].rearrange("(n p) c -> p n c", p=128), r=[], w=[wi])
        S.act(lambda e: e.activation(out=absw[:], in_=wi[:], func=AF.Abs), r=[wi], w=[absw])
        S.act(lambda e: e.activation(out=sgnw[:], in_=wi[:], func=AF.Sign), r=[wi], w=[sgnw])
        S.dma('sp', kcT[0:64, :], dr['kcd'][:, tb:tb + SEQ], r=[], w=[(kcT, 'q')])
        S.dma('sp', vcx[:, :, 0:64], dr['vcd'][tb:tb + SEQ, :].rearrange("(n p) c -> p n c", p=128), r=[], w=[vcx])
        for h in range(8):
            S.dma('sp', qcall[h][0:64, :], dr['qc'][h * 64:(h + 1) * 64, tb:tb + SEQ], r=[], w=[(qcall[h], 'q')])
        import os
        STOP = int(os.environ.get('STOP', '0'))
        if STOP == 1:
            break
        for st in index_steps(0):
            st()
        if STOP == 2:
            break
        jobs = []
        for c in range(4):
            nmT = nmTs[c % 2]
            nxt = index_steps(c + 1) if c < 3 else []
            per = (len(nxt) + 7) // 8
            for h in range(8):
                sl = nxt[h * per:(h + 1) * per]
                if os.environ.get('NOPRE'):
                    for st in sl:
                        st()
                    sl = []
                jobs.append(dict(c=c, tiles=causal_tiles(c, None), qT=qcall[h], kT=kcT, vext=vcx,
                                 extra=(lambda kt: ident[:], lambda kt, c, lo, hi, nmT=nmT: nmT[:, kt, lo:hi],
                                        [ident, nmT]),
                                 fin=make_fin(64 * h),
                                 pre=((lambda sl=sl: [st() for st in sl]) if sl else None)))
        import os
        if not os.environ.get('SKIP_DSA'):
            attn_run(S, jobs, STps, PT, Oacc, LA=1)

        def moba_load(h, tb=tb):
            b = h % 2
            S.dma('sp', qds[b][0:64, :], dr['qd'][h * 64:(h + 1) * 64, tb:tb + SEQ], r=[], w=[(qds[b], 'q')])
            S.dma('sp', kds[b][0:64, :], dr['kd'][h * 64:(h + 1) * 64, tb:tb + SEQ], r=[], w=[(kds[b], 'q')])
            S.dma('sp', vdxs[b][:, :, 0:64], dr['vdd'][tb:tb + SEQ, h * 64:(h + 1) * 64].rearrange(
                "(n p) c -> p n c", p=128), r=[], w=[vdxs[b]])

        def gate_a(h):
            b = h % 2
            qd, kd, kmb = qds[b], kds[b], kmbs[b]
            S.dve(lambda e: e.tensor_reduce(out=kmf[:], in_=kd[0:64, :].rearrange("p (j k) -> p j k", k=256),
                                            axis=AX.X, op=ALU.add), r=[(kd, 'q')], w=[kmf])
            S.dve(lambda e: e.tensor_scalar(out=kmb[0:64, :], in0=kmf[:], scalar1=1.0 / 256, scalar2=None,
                                            op0=ALU.mult), r=[kmf], w=[(kmb, 'm')])
            for i in range(16):
                S.pe(lambda e, i=i: e.matmul(pl[:, i * 8:(i + 1) * 8], lhsT=qd[:, i * 128:(i + 1) * 128], rhs=kmb[:],
                                             start=True, stop=True), r=[qd, kmb], w=[(pl, i)])
            S.dve(lambda e: e.tensor_tensor(out=gsb[:], in0=pl[:, 0:128], in1=mbias[:], op=ALU.add),
                  r=[pl, mbias], w=[gsb])
            for i in range(16):
                m = m8[i % 2]
                S.dve(lambda e, i=i, m=m: e.max(out=m[:], in_=gsb[:, i * 8:(i + 1) * 8]), r=[(gsb, i)], w=[m])
                S.dve(lambda e, i=i, m=m: e.tensor_scalar(out=gsb[:, i * 8:(i + 1) * 8], in0=gsb[:, i * 8:(i + 1) * 8],
                                                          scalar1=m[:, 2:3], scalar2=None, op0=ALU.is_ge),
                      r=[(gsb, i), m], w=[(gsb, i)])
            S.dve(lambda e: e.tensor_tensor(out=gsb[:], in0=gsb[:], in1=mpast[:], op=ALU.mult), r=[gsb, mpast], w=[gsb])
            S.dve(lambda e: e.tensor_tensor(out=gsb[:], in0=gsb[:], in1=mown[:], op=ALU.add), r=[gsb, mown], w=[gsb])
            S.dve(lambda e: e.tensor_scalar(out=ns8all[:, :, 0:8], in0=gsb[:].rearrange("p (i k) -> p i k", k=8),
                                            scalar1=-NEG, scalar2=NEG, op0=ALU.mult, op1=ALU.add),
                  r=[gsb], w=[ns8all])

        def gate_b(h):
            qd = qds[h % 2]
            for half in range(2):
                for k in range(8):
                    i = half * 8 + k
                    S.pe(lambda e, k=k, i=i: e.transpose(out=ptb[0:8, k, :], in_=ns8all[:, i, 0:8],
                                                         identity=ident[:]), r=[ns8all, ident], w=[(ptb, k)])
                S.dve(lambda e, half=half: e.tensor_copy(
                    out=qd[64:72, half * 1024:(half + 1) * 1024].rearrange("p (k q) -> p k q", q=128),
                    in_=ptb[0:8, :, :]), r=[ptb], w=[(qd, ('n', half))])

        if STOP == 3:
            break
        moba_load(0)
        gate_a(0)
        if STOP == 4:
            break
        gate_b(0)
        if STOP == 5:
            break
        NH_ = int(os.environ.get('MOBA_H', '8'))
        for h in range(0 if not os.environ.get('SKIP_MOBA') else 8, NH_):
            b = h % 2
            if h + 1 < 8:
                moba_load(h + 1)
            jobs = []
            for c in range(4):
                jobs.append(dict(c=c, tiles=causal_tiles(c, tri), qT=qds[b], kT=kds[b], vext=vdxs[b],
                                 fin=make_fin(512 + 64 * h),
                                 pre=((lambda h=h: gate_a(h + 1)) if (c == 1 and h + 1 < 8 and not os.environ.get('NOGATE')) else None)))
            attn_run(S, jobs, STps, PT, Oacc, LA=1)
            if h + 1 < 8:
                gate_b(h + 1)
        S.dma('sp', dr['o_tok'][tb:tb + SEQ, :].rearrange("(n p) c -> p n c", p=128), otok[:],
              r=[otok], w=[DW(S, dr['o_tok'])])
    S.flush()


def attn_chunk_v1(S, c, tiles, qT, kT, extra, vext, ident, STps, PT, Oacc, finalize, tag, qdep=None):
    cover = {}
    for n, (kt, lo, hi, bt, blo) in enumerate(tiles):
        for j in range(lo // 128, hi // 128):
            cover.setdefault(j, []).append(n)

    qd_ = qdep if qdep is not None else qT

    def qk(n):
        kt, lo, hi, bt, blo = tiles[n]
        ps = STps[n % 2]
        nterm = 1 + (1 if extra else 0) + (1 if bt is not None else 0)
        S.pe(lambda e: e.matmul(ps[:, lo:hi], lhsT=kT[:, kt * 128:(kt + 1) * 128],
                                rhs=qT[:, c * 512 + lo:c * 512 + hi], start=True, stop=(nterm == 1)),
             r=[kT, qd_], w=[ps])
        k = 1
        if extra:
            k += 1
            S.pe(lambda e: e.matmul(ps[:, lo:hi], lhsT=extra[0](kt), rhs=extra[1](kt, c, lo, hi),
                                    start=False, stop=(k == nterm), skip_group_check=True),
                 r=list(extra[2]), w=[ps])
        if bt is not None:
            S.pe(lambda e: e.matmul(ps[:, blo:blo + 128], lhsT=ident[:], rhs=bt[:], start=False, stop=True,
                                    skip_group_check=True), r=[ident, bt], w=[ps])

    qk(0)
    for n, (kt, lo, hi, bt, blo) in enumerate(tiles):
        if n + 1 < len(tiles):
            qk(n + 1)
        ps, p = STps[n % 2], PT[n % 2]
        S.act(lambda e, ps=ps, p=p, lo=lo, hi=hi: e.activation(out=p[:, lo:hi], in_=ps[:, lo:hi], func=AF.Exp),
              r=[ps], w=[p])
        for j in range(lo // 128, hi // 128):
            S.pe(lambda e, p=p, j=j, kt=kt, n=n: e.matmul(
                Oacc[j][:, 0:65], lhsT=p[:, j * 128:(j + 1) * 128], rhs=vext[:, kt, :],
                start=(cover[j][0] == n), stop=(cover[j][-1] == n), skip_group_check=True),
                r=[p, vext], w=[Oacc[j]])
            if cover[j][-1] == n:
                finalize(c, j, Oacc[j])


def causal_tiles_v1(c, tri):
    out = []
    for kt in range(4 * c + 4):
        if kt < 4 * c:
            out.append((kt, 0, 512, None, 0))
        else:
            lo = (kt - 4 * c) * 128
            out.append((kt, lo, 512, tri, lo))
    return out


def dsa_moba_stage_v1(S, prm, dr, nseq, cpack):
    C = Consts(S, cpack)
    ident = C.get('ident', BF16)
    tri = C.get('tri_ge', BF16)
    E8 = C.get('E8', BF16)
    triqs = C.get('tri_qs', F32)
    mbias, mpast, mown = C.get('mbias'), C.get('mpast'), C.get('mown')
    qi = [S.sb([32, SEQ], BF16, f'qi{h}') for h in range(4)]
    ki = S.sb([32, SEQ], BF16, 'ki')
    wi = S.sb([128, 16, 4], F32, 'wi')
    absw = S.sb([128, 16, 4], F32, 'absw')
    sgnw = S.sb([128, 16, 4], F32, 'sgnw')
    score = S.sb([128, SEQ], F32, 'score')
    rl = [S.sb([128, 512], F32, f'rl{i}') for i in range(2)]
    junkb = S.sb([128, SEQ], BF16, 'junkb')
    nmask = S.sb([128, SEQ], BF16, 'nmask')
    nmT = S.sb([128, 16, 512], BF16, 'nmT')
    kcT = S.sb([64, SEQ], BF16, 'kcT')
    vcx = S.sb([128, 16, 65], BF16, 'vcx')
    vdx = S.sb([128, 16, 65], BF16, 'vdx')
    S.pool(lambda e: e.memset(vcx[:], 1.0), r=[], w=[vcx])
    S.pool(lambda e: e.memset(vdx[:], 1.0), r=[], w=[vdx])
    qcall = [S.sb([64, SEQ], BF16, f'qcall{h}') for h in range(8)]
    qd = S.sb([64, SEQ], BF16, 'qd')
    kd = S.sb([64, SEQ], BF16, 'kd')
    kmf = S.sb([64, 8], F32, 'kmf')
    kmb = S.sb([64, 8], BF16, 'kmb')
    negsel8 = S.sb([8, SEQ], BF16, 'negsel8')
    otok = S.sb([128, 16, 1024], BF16, 'otok')
    PT = [S.sb([128, 512], BF16, f'PT{i}') for i in range(2)]
    st5 = [S.sb([128, 8], F32, f'st5{i}') for i in range(2)]
    gs = [S.sb([128, 8], F32, f'gs{i}') for i in range(2)]
    m8 = [S.sb([128, 8], F32, f'm8{i}') for i in range(2)]
    ns8 = [S.sb([128, 8], BF16, f'ns8{i}') for i in range(2)]
    fsc = [S.sb([128, 1], F32, f'fsc{i}') for i in range(4)]
    STps = [S.ps([128, 512], F32, f'st{i}') for i in range(2)]
    Oacc = [S.ps([128, 512], F32, f'oa{i}') for i in range(4)]
    ptb = S.ps([128, 8, 128], BF16, 'ptb')
    pl = S.ps([128, 512], F32, 'pl')
    fcount = [0]

    def make_fin(col0):
        def fin(c, j, O):
            i = 4 * c + j
            f = fsc[fcount[0] % 4]
            fcount[0] += 1
            S.dve(lambda e: e.tensor_scalar(out=f[:], in0=O[:, 64:65], scalar1=1e-30, scalar2=None, op0=ALU.max),
                  r=[O], w=[f])
            S.dve(lambda e: e.reciprocal(out=f[:], in_=f[:]), r=[f], w=[f])
            S.dve(lambda e: e.tensor_scalar(out=otok[:, i, col0:col0 + 64], in0=O[:, 0:64], scalar1=f[:, 0:1],
                                            scalar2=None, op0=ALU.mult), r=[O, f], w=[(otok, (i, col0))])
        return fin

    for sq in range(nseq):
        tb = sq * SEQ
        for h in range(4):
            S.dma('sp', qi[h][:], dr['qi'][h * 32:(h + 1) * 32, tb:tb + SEQ], r=[], w=[qi[h]])
        S.dma('sp', ki[:], dr['ki'][:, tb:tb + SEQ], r=[], w=[ki])
        S.dma('sp', wi[:], dr['wi'][tb:tb + SEQ, :].rearrange("(n p) c -> p n c", p=128), r=[], w=[wi])
        S.act(lambda e: e.activation(out=absw[:], in_=wi[:], func=AF.Abs), r=[wi], w=[absw])
        S.act(lambda e: e.activation(out=sgnw[:], in_=wi[:], func=AF.Sign), r=[wi], w=[sgnw])
        S.dma('sp', kcT[:], dr['kcd'][:, tb:tb + SEQ], r=[], w=[kcT])
        S.dma('sp', vcx[:, :, 0:64], dr['vcd'][tb:tb + SEQ, :].rearrange("(n p) c -> p n c", p=128), r=[], w=[vcx])
        for h in range(8):
            S.dma('sp', qcall[h][:], dr['qc'][h * 64:(h + 1) * 64, tb:tb + SEQ], r=[], w=[qcall[h]])
        for c in range(4):
            for j in range(4):
                i = 4 * c + j
                W = 128 * (i + 1)
                if i < 2:
                    for st_ in range(i + 1):
                        if st_ == i:
                            S.pool(lambda e, st_=st_, j=j: e.tensor_copy(out=nmT[:, st_, j * 128:(j + 1) * 128],
                                                                         in_=tri[:]), r=[tri], w=[(nmT, (st_, j))])
                        else:
                            S.pool(lambda e, st_=st_, j=j: e.memset(nmT[:, st_, j * 128:(j + 1) * 128], 0.0),
                                   r=[], w=[(nmT, (st_, j))])
                    continue
                for h in range(4):
                    for sc in range((W + 511) // 512):
                        n = min(512, W - 512 * sc)
                        rb = rl[(h * 4 + sc) % 2]
                        S.pe(lambda e, h=h, sc=sc, n=n, i=i: e.matmul(pl[:, 0:n], lhsT=qi[h][:, i * 128:(i + 1) * 128],
                                                                       rhs=ki[:, sc * 512:sc * 512 + n], start=True,
                                                                       stop=True), r=[qi[h], ki], w=[pl])
                        S.act(lambda e, rb=rb, n=n, i=i, h=h: e.activation(out=rb[:, 0:n], in_=pl[:, 0:n], func=AF.Relu,
                                                                           scale=absw[:, i, h:h + 1]),
                              r=[pl, absw], w=[rb])
                        if h == 0:
                            S.dve(lambda e, rb=rb, n=n, sc=sc, i=i, h=h: e.tensor_scalar(
                                out=score[:, sc * 512:sc * 512 + n], in0=rb[:, 0:n], scalar1=sgnw[:, i, h:h + 1],
                                scalar2=None, op0=ALU.mult), r=[rb, sgnw], w=[(score, sc)])
                        else:
                            S.dve(lambda e, rb=rb, n=n, sc=sc, i=i, h=h: e.scalar_tensor_tensor(
                                out=score[:, sc * 512:sc * 512 + n], in0=rb[:, 0:n], scalar=sgnw[:, i, h:h + 1],
                                in1=score[:, sc * 512:sc * 512 + n], op0=ALU.mult, op1=ALU.add),
                                r=[rb, sgnw, (score, sc)], w=[(score, sc)])
                s5 = st5[i % 2]
                S.dve(lambda e, s5=s5, W=W: e.tensor_reduce(out=s5[:, 5:6], in_=score[:, 0:W], axis=AX.X, op=ALU.max),
                      r=[score], w=[(s5, 5)])
                S.dve(lambda e, s5=s5, W=W: e.tensor_reduce(out=s5[:, 0:1], in_=score[:, 0:W], axis=AX.X, op=ALU.min),
                      r=[score], w=[(s5, 0)])
                S.dve(lambda e, s5=s5: e.tensor_tensor(out=s5[:, 1:2], in0=s5[:, 5:6], in1=s5[:, 0:1], op=ALU.subtract),
                      r=[(s5, 5), (s5, 0)], w=[(s5, 1)])
                S.dve(lambda e, i=i: e.tensor_tensor(out=score[:, i * 128:(i + 1) * 128],
                                                     in0=score[:, i * 128:(i + 1) * 128], in1=triqs[:], op=ALU.add),
                      r=[score, triqs], w=[score])
                for it in range(NBIS):
                    S.dve(lambda e, s5=s5: e.tensor_scalar(out=s5[:, 1:2], in0=s5[:, 1:2], scalar1=0.5, scalar2=None,
                                                           op0=ALU.mult), r=[(s5, 1)], w=[(s5, 1)])
                    S.dve(lambda e, s5=s5: e.tensor_tensor(out=s5[:, 2:3], in0=s5[:, 0:1], in1=s5[:, 1:2], op=ALU.add),
                          r=[(s5, 0), (s5, 1)], w=[(s5, 2)])
                    S.dve(lambda e, s5=s5, W=W: e.tensor_scalar(out=junkb[:, 0:W], in0=score[:, 0:W],
                                                                scalar1=s5[:, 2:3], scalar2=0.0, op0=ALU.is_ge,
                                                                op1=ALU.add, accum_out=s5[:, 3:4]),
                          r=[score, (s5, 2)], w=[junkb, (s5, 3)])
                    S.dve(lambda e, s5=s5: e.tensor_scalar(out=s5[:, 4:5], in0=s5[:, 3:4], scalar1=255.5, scalar2=None,
                                                           op0=ALU.is_ge), r=[(s5, 3)], w=[(s5, 4)])
                    S.dve(lambda e, s5=s5: e.scalar_tensor_tensor(out=s5[:, 0:1], in0=s5[:, 1:2], scalar=s5[:, 4:5],
                                                                  in1=s5[:, 0:1], op0=ALU.mult, op1=ALU.add),
                          r=[(s5, 1), (s5, 4), (s5, 0)], w=[(s5, 0)])
                S.dve(lambda e, s5=s5, W=W: e.tensor_scalar(out=nmask[:, 0:W], in0=score[:, 0:W], scalar1=s5[:, 0:1],
                                                            scalar2=NEG, op0=ALU.is_lt, op1=ALU.mult),
                      r=[score, (s5, 0)], w=[nmask])
                for s0 in range(0, i + 1, 8):
                    n = min(8, i + 1 - s0)
                    for k in range(n):
                        S.pe(lambda e, s0=s0, k=k: e.transpose(out=ptb[:, k, :],
                                                               in_=nmask[:, (s0 + k) * 128:(s0 + k + 1) * 128],
                                                               identity=ident[:]), r=[nmask, ident], w=[(ptb, k)])
                    S.act(lambda e, s0=s0, n=n, j=j: e.copy(out=nmT[:, s0:s0 + n, j * 128:(j + 1) * 128],
                                                            in_=ptb[:, 0:n, :]), r=[ptb], w=[(nmT, ('b', s0, j))])
            for h in range(8):
                attn_chunk_v1(S, c, causal_tiles_v1(c, None), qcall[h], kcT,
                           (lambda kt: ident[:], lambda kt, c, lo, hi: nmT[:, kt, lo:hi], [ident, nmT]),
                           vcx, ident, STps, PT, Oacc, make_fin(64 * h), 'dsa')
        for h in range(8):
            S.dma('sp', qd[:], dr['qd'][h * 64:(h + 1) * 64, tb:tb + SEQ], r=[], w=[qd])
            S.dma('sp', kd[:], dr['kd'][h * 64:(h + 1) * 64, tb:tb + SEQ], r=[], w=[kd])
            S.dma('sp', vdx[:, :, 0:64], dr['vdd'][tb:tb + SEQ, h * 64:(h + 1) * 64].rearrange(
                "(n p) c -> p n c", p=128), r=[], w=[vdx])
            S.dve(lambda e: e.tensor_reduce(out=kmf[:], in_=kd[:].rearrange("p (j k) -> p j k", k=256), axis=AX.X,
                                            op=ALU.add), r=[kd], w=[kmf])
            S.dve(lambda e: e.tensor_scalar(out=kmb[:], in0=kmf[:], scalar1=1.0 / 256, scalar2=None, op0=ALU.mult),
                  r=[kmf], w=[kmb])
            for i in range(16):
                b = i % 2
                S.pe(lambda e, i=i: e.matmul(pl[:, 0:8], lhsT=qd[:, i * 128:(i + 1) * 128], rhs=kmb[:], start=True,
                                             stop=True), r=[qd, kmb], w=[pl])
                g_ = gs[b]
                S.dve(lambda e, g_=g_, i=i: e.tensor_tensor(out=g_[:], in0=pl[:, 0:8], in1=mbias[:, i * 8:(i + 1) * 8],
                                                           op=ALU.add), r=[pl, mbias], w=[g_])
                S.dve(lambda e, g_=g_, b=b: e.max(out=m8[b][:], in_=g_[:]), r=[g_], w=[m8[b]])
                S.dve(lambda e, g_=g_, b=b: e.tensor_scalar(out=g_[:], in0=g_[:], scalar1=m8[b][:, 2:3], scalar2=None,
                                                            op0=ALU.is_ge), r=[g_, m8[b]], w=[g_])
                S.dve(lambda e, g_=g_, i=i: e.tensor_tensor(out=g_[:], in0=g_[:], in1=mpast[:, i * 8:(i + 1) * 8],
                                                           op=ALU.mult), r=[g_, mpast], w=[g_])
                S.dve(lambda e, g_=g_, i=i: e.tensor_tensor(out=g_[:], in0=g_[:], in1=mown[:, i * 8:(i + 1) * 8],
                                                           op=ALU.add), r=[g_, mown], w=[g_])
                S.dve(lambda e, g_=g_, b=b: e.tensor_scalar(out=ns8[b][:], in0=g_[:], scalar1=-NEG, scalar2=NEG,
                                                            op0=ALU.mult, op1=ALU.add), r=[g_], w=[ns8[b]])
                S.pe(lambda e, b=b: e.transpose(out=ptb[0:8, b, :], in_=ns8[b][:], identity=ident[:]),
                     r=[ns8[b], ident], w=[(ptb, b)])
                S.act(lambda e, b=b, i=i: e.copy(out=negsel8[:, i * 128:(i + 1) * 128], in_=ptb[0:8, b, :]),
                      r=[(ptb, b)], w=[(negsel8, i // 4)])
            for c in range(4):
                attn_chunk_v1(S, c, causal_tiles_v1(c, tri), qd, kd,
                           (lambda kt: E8[0:8, kt * 128:(kt + 1) * 128], lambda kt, c, lo, hi: negsel8[:, c * 512 + lo:c * 512 + hi],
                            [E8, negsel8]),
                           vdx, ident, STps, PT, Oacc, make_fin(512 + 64 * h), 'moba')
        S.dma('sp', dr['o_tok'][tb:tb + SEQ, :].rearrange("(n p) c -> p n c", p=128), otok[:],
              r=[otok], w=[DW(S, dr['o_tok'])])
    S.flush()


NCORES = 8
TPC = 2 * SEQ


def build_program(stages=None):
    nc = bass.Bass("TRN2", target_bir_lowering=False)
    ins = {}

    def din(name, shape, dt=F32):
        ins[name] = nc.dram_tensor(name, list(shape), dt, kind="ExternalInput").ap()
        return ins[name]

    def scr(name, shape, dt=BF16):
        return nc.dram_tensor(name, list(shape), dt, kind="Internal").ap()

    x = din('x', [TPC, D])
    pos = din('pos', [TPC], I32)
    cp = din('cpack', [128, CP_N])
    P = {}
    for L in range(2):
        for f in ('ffn1', 'ffn2'):
            P[f'{f}_norm{L}'] = din(f'{f}_norm{L}', [D])
            P[f'{f}_wg{L}'] = din(f'{f}_wg{L}', [D, DFF])
            P[f'{f}_wu{L}'] = din(f'{f}_wu{L}', [D, DFF])
            P[f'{f}_wd{L}'] = din(f'{f}_wd{L}', [DFF, D])
        P[f'mix_norm{L}'] = din(f'mix_norm{L}', [D])
    ev = {'w_in': din('ev_w_in', [D, 2468]), 'mix_norm': P['mix_norm0'], 'pos': pos,
          'sgu_norm': din('ev_sgu_norm', [256]), 'sgu_wT': din('ev_sgu_wT', [4, 128, 128]),
          'sgu_bT': din('ev_sgu_bT', [128, 4]),
          'cmp_w1_k': din('ev_w1k', [2048, 256]), 'cmp_w2_k': din('ev_w2k', [256, 64]),
          'cmp_posT_k': din('ev_pk', [64, 32]),
          'cmp_w1_v': din('ev_w1v', [2048, 256]), 'cmp_w2_v': din('ev_w2v', [256, 64]),
          'cmp_posT_v': din('ev_pv', [64, 32])}
    ev_w_out = din('ev_w_out', [D, D])
    od = {'w_in': din('od_w_in', [D, 2340]), 'mix_norm': P['mix_norm1'], 'pos': pos}
    od_w_out = din('od_w_out', [D, D])
    fin_g = din('final_norm', [D])
    y = nc.dram_tensor('y', [TPC, D], F32, kind="ExternalOutput").ap()
    xa = scr('xa', [TPC, D], F32)
    xb = scr('xb', [TPC, D], F32)
    T_ = TPC
    dre = {'a_tok': scr('a_tok', [T_, 512]), 'qraw': scr('qraw', [768, T_]), 'qrot': scr('qrot', [768, T_]),
           'kc': scr('kc', [192, T_]), 'vc': scr('vc', [192, T_]), 'ksl': scr('ksl', [192, T_]),
           'kw': scr('kw', [192, T_]), 'vsl': scr('vsl', [T_, 192]), 'vw': scr('vw', [T_, 192]),
           'gate': scr('gate', [T_, 36], F32), 'o_tok': scr('o_tok0', [T_, 1024])}
    dro = {'qc': scr('qc', [512, T_]), 'kcd': scr('kcd', [64, T_]), 'vcd': scr('vcd', [T_, 64]),
           'qi': scr('qi', [128, T_]), 'ki': scr('ki', [32, T_]), 'wi': scr('wi', [T_, 4], F32),
           'qd': scr('qd', [512, T_]), 'kd': scr('kd', [512, T_]), 'vdd': scr('vdd', [T_, 512]),
           'o_tok': scr('o_tok1', [T_, 1024])}
    S = Sched(nc)
    w16 = {'wg': scr('wg16', [128, DFF // 256, 8, 256]), 'wu': scr('wu16', [128, DFF // 256, 8, 256]),
           'wd': scr('wd16', [128, NFC, D])}

    def ffn(xi, xo, f, L, fg=None):
        ffn_stage(S, xi, xo, P[f'{f}_norm{L}'], P[f'{f}_wg{L}'], P[f'{f}_wu{L}'], P[f'{f}_wd{L}'], TPC, cp_d, fg, w16)
    cp_d = {'ident': cp[:, CP_OFF['ident'][0]:CP_OFF['ident'][0] + 128]}
    ffn(x, xa, 'ffn1', 0)
    even_proj(S, xa, ev, dre, TPC, cp)
    nsa_stage(S, ev, dre, 2, cp)
    outproj_stage(S, xa, xb, dre['o_tok'], ev_w_out, TPC, cp)
    ffn(xb, xa, 'ffn2', 0)
    ffn(xa, xb, 'ffn1', 1)
    odd_proj(S, xb, od, dro, TPC, cp)
    (dsa_moba_stage if USE_NEW_ODD else dsa_moba_stage_v1)(S, od, dro, 2, cp)
    outproj_stage(S, xb, xa, dro['o_tok'], od_w_out, TPC, cp)
    ffn(xa, y, 'ffn2', 1, fin_g)
    return nc, S


def kernel(**inp):
    inp = {k: np.asarray(v) for k, v in inp.items()}
    nc, S = build_program()
    cpk = host_consts()
    c = np.ascontiguousarray
    shared = {'cpack': cpk}
    for L in range(2):
        for f in ('ffn1', 'ffn2'):
            shared[f'{f}_norm{L}'] = c(inp[f'{f}_norm'][L])
            shared[f'{f}_wg{L}'] = c(inp[f'{f}_w_gate'][L])
            shared[f'{f}_wu{L}'] = c(inp[f'{f}_w_up'][L])
            shared[f'{f}_wd{L}'] = c(inp[f'{f}_w_down'][L])
        shared[f'mix_norm{L}'] = c(inp['mix_norm'][L])
    shared.update({
        'ev_w_in': c(inp['ev_w_in'][0]), 'ev_sgu_norm': c(inp['ev_sgu_norm'][0]),
        'ev_sgu_wT': c(inp['ev_sgu_w'][0].transpose(0, 2, 1)), 'ev_sgu_bT': c(inp['ev_sgu_b'][0].T),
        'ev_w1k': c(inp['ev_cmp_w1_k'][0]), 'ev_w2k': c(inp['ev_cmp_w2_k'][0]), 'ev_pk': c(inp['ev_cmp_pos_k'][0].T),
        'ev_w1v': c(inp['ev_cmp_w1_v'][0]), 'ev_w2v': c(inp['ev_cmp_w2_v'][0]), 'ev_pv': c(inp['ev_cmp_pos_v'][0].T),
        'ev_w_out': c(inp['ev_w_out'][0]), 'od_w_in': c(inp['od_w_in'][0]), 'od_w_out': c(inp['od_w_out'][0]),
        'final_norm': c(inp['final_norm'])})
    in_maps = []
    for k in range(NCORES):
        m = dict(shared)
        m['x'] = c(inp['x'][2 * k:2 * k + 2].reshape(TPC, D))
        m['pos'] = c(inp['positions'][2 * k:2 * k + 2].reshape(TPC).astype(np.int32))
        in_maps.append(m)
    res = run_bass_kernel_spmd(nc, in_maps, core_ids=list(range(NCORES)))
    out = np.stack([np.asarray(r['y']).reshape(2, SEQ, D) for r in res.results], axis=0)
    return out.reshape(16, SEQ, D).astype(np.float32)
```

```python
from contextlib import ExitStack
import numpy as np
import concourse.bass as bass
import concourse.mybir as mybir
from concourse.bass_utils import run_bass_kernel_spmd

F32 = mybir.dt.float32
BF16 = mybir.dt.bfloat16
I32 = mybir.dt.int32
AF = mybir.ActivationFunctionType
ALU = mybir.AluOpType
AX = mybir.AxisListType

ENGS = ['pe', 'act', 'dve', 'pool', 'sp']
import os
BANKDEP = False
USE_NEW_ODD = True
NDSEM = 66
NSWSEM = 26


class T:
    _n = 0

    def __init__(self, t, name=None):
        self.t = t
        T._n += 1
        self.id = T._n
        self.name = name

    def __getitem__(self, idx):
        return self.t[idx]


class Op:
    __slots__ = ('eng', 'fn', 'deps', 'isdma', 'sem', 'cnt', 'signal', 'waits', 'vc')


class Sched:
    def __init__(self, nc):
        self.nc = nc
        self.esem = {e: nc.alloc_semaphore(name=f'es_{e}') for e in ENGS}
        self.ecnt = {e: 0 for e in ENGS}
        self.free_dsems = {False: [nc.alloc_semaphore(name=f'ds_{i}') for i in range(NDSEM)],
                           True: [nc.alloc_semaphore(name=f'dw_{i}') for i in range(NSWSEM)]}
        self.dcnt = {}
        self.n_inst = 0
        self.base = {}
        self._reset()

    def _reset(self):
        self.ops = []
        self.state = {}
        self.buf_dsem = {}
        self.stack = ExitStack()

    def sb(self, shape, dtype, name=None):
        t = self.stack.enter_context(self.nc.sbuf_tensor(f'{name or "sb"}_{T._n}', list(shape), dtype))
        return T(t, name)

    def ps(self, shape, dtype, name=None):
        t = self.stack.enter_context(self.nc.psum_tensor(f'{name or "ps"}_{T._n}', list(shape), dtype))
        return T(t, name)

    @staticmethod
    def _norm(item):
        if isinstance(item, T):
            return item.id, None
        return item[0].id, item[1]

    def _track(self, r, w, opi):
        deps = {}
        for item in r:
            tid, key = self._norm(item)
            st = self.state.setdefault(tid, {})
            for k, ent in st.items():
                if k == key or k is None or key is None:
                    if ent[0] is not None:
                        deps[ent[0]] = True
            st.setdefault(key, [None, []])[1].append(opi)
        for item in w:
            tid, key = self._norm(item)
            st = self.state.setdefault(tid, {})
            for k, ent in st.items():
                if k == key or k is None or key is None:
                    if ent[0] is not None:
                        deps[ent[0]] = True
                    for x in ent[1]:
                        deps.setdefault(x, False)
            if key is None:
                st.clear()
            st[key] = [opi, []]
        deps.pop(opi, None)
        return deps

    @staticmethod
    def _skip(p, o, raw):
        if p.isdma or o.isdma or p.eng != o.eng:
            return False
        return p.eng == 'pe' or not raw

    def op(self, eng, fn, r=(), w=()):
        o = Op()
        o.eng, o.fn, o.isdma, o.signal = eng, fn, False, False
        o.sem, o.cnt, o.waits, o.vc = None, 0, None, None
        o.deps = self._track(r, w, len(self.ops))
        self.ops.append(o)
        return o

    def dma(self, eng, out_ap, in_ap, r, w, **kw):
        o = self.op(eng, lambda e: e.dma_start(out=out_ap, in_=in_ap, **kw), r, w)
        o.isdma = True
        it = w[0] if isinstance(w[0], T) else w[0][0]
        if it.t is None and len(r) > 0:
            it = r[0] if isinstance(r[0], T) else r[0][0]
        tid = (it.id, eng == 'pool')
        if tid not in self.buf_dsem:
            self.buf_dsem[tid] = self.free_dsems[eng == 'pool'].pop()
            if not hasattr(self, 'sem_names'):
                self.sem_names = {}
            self.sem_names[self.buf_dsem[tid]] = it.name
        o.sem = self.buf_dsem[tid]
        self.dcnt[o.sem] = self.dcnt.get(o.sem, 0) + 16
        o.cnt = self.dcnt[o.sem]
        return o

    def pe(self, fn, r=(), w=()):
        return self.op('pe', fn, r, w)

    def act(self, fn, r=(), w=()):
        return self.op('act', fn, r, w)

    def dve(self, fn, r=(), w=()):
        return self.op('dve', fn, r, w)

    def pool(self, fn, r=(), w=()):
        return self.op('pool', fn, r, w)

    def flush(self):
        nc, ops = self.nc, self.ops
        for o in ops:
            for d, raw in o.deps.items():
                p = ops[d]
                if p.isdma or self._skip(p, o, raw):
                    continue
                p.signal = True
        for o in ops:
            if not o.isdma and o.signal:
                self.ecnt[o.eng] += 1
                o.cnt = self.ecnt[o.eng]
                o.sem = self.esem[o.eng]
        known = {e: dict(self.base) for e in ENGS}
        for o in ops:
            kn = known[o.eng]
            waits = {}
            for d in sorted(o.deps, reverse=True):
                p = ops[d]
                if self._skip(p, o, o.deps[d]):
                    continue
                if kn.get(p.sem, 0) >= p.cnt:
                    continue
                if waits.get(p.sem, 0) < p.cnt:
                    waits[p.sem] = p.cnt
                for s, c in p.vc.items():
                    if kn.get(s, 0) < c:
                        kn[s] = c
                kn[p.sem] = p.cnt
            o.waits = list(waits.items())
            if o.isdma and o.cnt > 16 and kn.get(o.sem, 0) < o.cnt - 16 and getattr(self, 'diag', False):
                print('DMA overlap on sem', self.sem_names.get(o.sem), 'cnt', o.cnt, 'known', kn.get(o.sem, 0))
            if o.isdma or o.signal:
                o.vc = dict(kn)
        by = {e: [o for o in ops if o.eng == e] for e in ENGS}
        final_d = [(s, self.dcnt[s]) for s in set(self.buf_dsem.values())]
        self.n_inst += len(ops)

        def emit(e, lst):
            for o in lst:
                for s, c in o.waits:
                    e.wait_ge(s, c)
                ins = o.fn(e)
                if o.isdma:
                    ins.then_inc(o.sem, 16)
                elif o.signal:
                    ins.then_inc(o.sem, 1)

        with nc.Block() as block:
            @block.tensor
            def _(e):
                emit(e, by['pe'])

            @block.scalar
            def _(e):
                emit(e, by['act'])

            @block.vector
            def _(e):
                emit(e, by['dve'])

            @block.gpsimd
            def _(e):
                emit(e, by['pool'])

            @block.sync
            def _(e):
                emit(e, by['sp'])
                for s, c in final_d:
                    e.wait_ge(s, c)
        for (tid_, sw), s in self.buf_dsem.items():
            self.free_dsems[sw].append(s)
        self.base = dict(self.dcnt)
        for e in ENGS:
            self.base[self.esem[e]] = self.ecnt[e]
        self.stack.close()
        self._reset()


D = 1024
DFF = 2816
NFC = DFF // 128
SEQ = 2048
EPS = 1e-6


def load_consts(S, cpack):
    c = {}
    idf = S.sb([128, 128], F32, 'idf')
    S.dma('sp', idf[:], cpack['ident'], r=[], w=[idf])
    idb = S.sb([128, 128], BF16, 'idb')
    S.dve(lambda e: e.tensor_copy(out=idb[:], in_=idf[:]), r=[idf], w=[idb])
    c['ident'] = idb
    return c


def rms_rstd(S, xt, junk, ss, rstd, width, key=None):
    S.act(lambda e: e.activation(out=junk[:], in_=xt[:], func=AF.Square, accum_out=ss[:]),
          r=[xt], w=[junk, ss])
    S.act(lambda e: e.activation(out=rstd[:], in_=ss[:], func=AF.Sqrt, bias=EPS, scale=1.0 / width),
          r=[ss], w=[rstd])
    S.dve(lambda e: e.reciprocal(out=rstd[:], in_=rstd[:]), r=[rstd], w=[rstd])


def ffn_stage(S, x_in, x_out, g_ap, wg, wu, wd, ntok, cpack, final_g=None, w16=None):
    CH = 1024
    NT = CH // 128
    consts = load_consts(S, cpack)
    ident = consts['ident']
    gb = S.sb([128, D], F32, 'gb')
    S.dma('sp', gb[:], g_ap.partition_broadcast(128), r=[], w=[gb])
    if final_g is not None:
        fgb = S.sb([128, D], F32, 'fgb')
        S.dma('sp', fgb[:], final_g.partition_broadcast(128), r=[], w=[fgb])
    hT = S.sb([128, 8, CH], BF16, 'hT')
    actT = S.sb([128, NFC, CH], BF16, 'actT')
    wdb = S.sb([128, NFC, D], BF16, 'wdb')
    xt = [S.sb([128, D], F32, f'xt{i}') for i in range(2)]
    hb = [S.sb([128, D], BF16, f'hb{i}') for i in range(2)]
    junk = S.sb([128, D], BF16, 'junk')
    ss = [S.sb([128, 1], F32, f'ss{i}') for i in range(2)]
    rstd = [S.sb([128, 1], F32, f'rstd{i}') for i in range(2)]
    FB = 256
    NB = DFF // FB
    wgs = [S.sb([128, 8, FB], F32, f'wgs{i}') for i in range(2)]
    wus = [S.sb([128, 8, FB], F32, f'wus{i}') for i in range(2)]
    wgb = [S.sb([128, 8, FB], BF16, f'wgb{i}') for i in range(2)]
    wub = [S.sb([128, 8, FB], BF16, f'wub{i}') for i in range(2)]
    wds = [S.sb([128, D], F32, f'wds{i}') for i in range(2)]
    sg = [S.sb([128, 512], F32, f'sg{i}') for i in range(2)]
    ot = [S.sb([128, D], F32, f'ot{i}') for i in range(2)]
    ptr = [S.ps([128, 8, 128], BF16, f'ptr{i}') for i in range(2)]
    pg = [S.ps([128, 512], F32, f'pg{i}') for i in range(2)]
    pu = [S.ps([128, 512], F32, f'pu{i}') for i in range(2)]
    py = [S.ps([128, 512], F32, f'py{i}') for i in range(2)]
    wg_v = wg.rearrange("(c p) f -> p c f", p=128)
    wu_v = wu.rearrange("(c p) f -> p c f", p=128)
    wd_v = wd.rearrange("(c p) m -> p c m", p=128)

    xt3 = [S.sb([128, D], F32, f'xt3{i}') for i in range(2)]

    def phase1_tile(ch, i):
        t0 = ch * CH
        b = i % 2
        x_t, h_b = xt[b], hb[b]
        S.dma('sp', x_t[:], x_in[t0 + i * 128:t0 + (i + 1) * 128, :], r=[], w=[x_t])
        rms_rstd(S, x_t, junk, ss[b], rstd[b], D)
        S.dve(lambda e: e.scalar_tensor_tensor(
            out=h_b[:], in0=x_t[:], scalar=rstd[b][:, 0:1], in1=gb[:], op0=ALU.mult, op1=ALU.mult),
            r=[x_t, rstd[b], gb], w=[h_b])
        p = ptr[b]
        for c in range(8):
            S.pe(lambda e, c=c: e.transpose(out=p[:, c, :], in_=h_b[:, c * 128:(c + 1) * 128], identity=ident[:]),
                 r=[h_b, ident], w=[(p, c)])
        S.act(lambda e: e.copy(out=hT[:, :, i * 128:(i + 1) * 128], in_=p[:]), r=[p], w=[(hT, i // 4)])

    wdT = T(None, 'wd16')

    def load_wd(ch, fc):
        if ch == 0 or w16 is None:
            s_ = wds[fc % 2]
            S.dma('sp', s_[:], wd_v[:, fc, :], r=[], w=[s_])
            S.act(lambda e: e.copy(out=wdb[:, fc, :], in_=s_[:]), r=[s_], w=[(wdb, fc)])
            if w16 is not None:
                S.dma('pool', w16['wd'][:, fc, :], wdb[:, fc, :], r=[(wdb, fc)], w=[(wdT, fc)])
        else:
            S.dma('sp', wdb[:, fc, :], w16['wd'][:, fc, :], r=[wdT], w=[(wdb, fc)])

    wgT = T(None, 'wg16')

    def phase2(ch):
        for fb in range(NB):
            b = fb % 2
            if ch == 0 or w16 is None:
                S.dma('sp', wgs[b][:], wg_v[:, :, fb * FB:(fb + 1) * FB], r=[], w=[wgs[b]])
                S.dma('sp', wus[b][:], wu_v[:, :, fb * FB:(fb + 1) * FB], r=[], w=[wus[b]])
                S.dve(lambda e, b=b: e.tensor_copy(out=wgb[b][:], in_=wgs[b][:]), r=[wgs[b]], w=[wgb[b]])
                S.dve(lambda e, b=b: e.tensor_copy(out=wub[b][:], in_=wus[b][:]), r=[wus[b]], w=[wub[b]])
                if w16 is not None:
                    S.dma('pool', w16['wg'][:, fb, :, :], wgb[b][:], r=[wgb[b]], w=[(wgT, ('g', fb))])
                    S.dma('pool', w16['wu'][:, fb, :, :], wub[b][:], r=[wub[b]], w=[(wgT, ('u', fb))])
            else:
                S.dma('sp', wgb[b][:], w16['wg'][:, fb, :, :], r=[wgT], w=[wgb[b]])
                S.dma('sp', wub[b][:], w16['wu'][:, fb, :, :], r=[wgT], w=[wub[b]])
            load_wd(ch, 2 * fb)
            load_wd(ch, 2 * fb + 1)
            for fs in range(FB // 128):
                fc = fb * (FB // 128) + fs
                for tb in range(CH // 512):
                    q = (fc * 2 + tb) % 2
                    for c in range(8):
                        S.pe(lambda e, q=q, b=b, c=c, fs=fs, tb=tb: e.matmul(
                            pg[q][:], lhsT=wgb[b][:, c, fs * 128:(fs + 1) * 128],
                            rhs=hT[:, c, tb * 512:(tb + 1) * 512], start=(c == 0), stop=(c == 7)),
                            r=[wgb[b], (hT, tb)], w=[pg[q]])
                    for c in range(8):
                        S.pe(lambda e, q=q, b=b, c=c, fs=fs, tb=tb: e.matmul(
                            pu[q][:], lhsT=wub[b][:, c, fs * 128:(fs + 1) * 128],
                            rhs=hT[:, c, tb * 512:(tb + 1) * 512], start=(c == 0), stop=(c == 7)),
                            r=[wub[b], (hT, tb)], w=[pu[q]])
                    S.act(lambda e, q=q: e.activation(out=sg[q][:], in_=pg[q][:], func=AF.Silu),
                          r=[pg[q]], w=[sg[q]])
                    S.dve(lambda e, q=q, fc=fc, tb=tb: e.tensor_tensor(
                        out=actT[:, fc, tb * 512:(tb + 1) * 512], in0=pu[q][:], in1=sg[q][:], op=ALU.mult),
                        r=[pu[q], sg[q]], w=[(actT, (fc, tb))])

    def phase3_tile(ch, i):
        t0 = ch * CH
        b = i % 2
        x_t, o_t = xt3[b], ot[b]
        S.dma('sp', x_t[:], x_in[t0 + i * 128:t0 + (i + 1) * 128, :], r=[], w=[x_t])
        for mh in range(2):
            p = py[mh]
            for fc in range(NFC):
                S.pe(lambda e, p=p, fc=fc, mh=mh: e.matmul(
                    p[:], lhsT=actT[:, fc, i * 128:(i + 1) * 128], rhs=wdb[:, fc, mh * 512:(mh + 1) * 512],
                    start=(fc == 0), stop=(fc == NFC - 1)),
                    r=[(actT, (fc, i // 4)), wdb], w=[p])
            S.dve(lambda e, p=p, mh=mh: e.scalar_tensor_tensor(
                out=o_t[:, mh * 512:(mh + 1) * 512], in0=p[:], scalar=0.5, in1=x_t[:, mh * 512:(mh + 1) * 512],
                op0=ALU.mult, op1=ALU.add), r=[p, x_t], w=[(o_t, mh)])
        if final_g is not None:
            rms_rstd(S, o_t, junk, ss3[b], rstd3[b], D)
            S.dve(lambda e: e.scalar_tensor_tensor(
                out=o_t[:], in0=o_t[:], scalar=rstd3[b][:, 0:1], in1=fgb[:], op0=ALU.mult, op1=ALU.mult),
                r=[o_t, rstd3[b], fgb], w=[o_t])
        S.dma('pool', x_out[t0 + i * 128:t0 + (i + 1) * 128, :], o_t[:], r=[o_t], w=[DW(S, x_out)])

    ss3 = [S.sb([128, 1], F32, f'ss3{i}') for i in range(2)]
    rstd3 = [S.sb([128, 1], F32, f'rstd3{i}') for i in range(2)]
    nch = ntok // CH
    for i in range(NT):
        phase1_tile(0, i)
    for ch in range(nch):
        phase2(ch)
        for i in range(NT):
            phase3_tile(ch, i)
            if ch + 1 < nch:
                phase1_tile(ch + 1, i)
    S.flush()


_dram_T = {}
_dkey = [0]


def x_out_T(S, ap):
    k = ap.name
    if k not in _dram_T:
        _dram_T[k] = T(None, k)
    return _dram_T[k]


def DW(S, ap):
    _dkey[0] += 1
    return (x_out_T(S, ap), _dkey[0])


THETA = 500000.0
NEG = -30000.0


def _cpack_layout():
    items = [('ident', 128), ('tri_ge', 128), ('band_lt', 128), ('tril_st', 128), ('ones', 128),
             ('invf64', 1), ('nsgn64', 1), ('P64', 128), ('invf32', 1), ('nsgn32', 1), ('P32', 128),
             ('cmpbias', 2048), ('overlap', 32), ('E32', 2048), ('E8', 2048),
             ('keep', 512), ('addm', 512), ('adm', 512), ('tri_qs', 128), ('tri01', 128), ('band01', 128), ('mbias', 128), ('mpast', 128), ('mown', 128)]
    off, o = {}, 0
    for k, n in items:
        off[k] = (o, n)
        o += n
    return off, o


CP_OFF, CP_N = _cpack_layout()


def host_consts():
    cp = np.zeros((128, CP_N), np.float32)

    def put(k, a):
        o, n = CP_OFF[k]
        a = np.asarray(a, np.float32)
        cp[:a.shape[0], o:o + a.shape[1]] = a
    p = np.arange(128)
    put('ident', np.eye(128))
    kk, qq = p[:, None], p[None, :]
    put('tri_ge', np.where(qq >= kk, 0.0, NEG))
    put('band_lt', np.where(qq < kk, 0.0, NEG))
    put('tri01', (qq >= kk).astype(np.float32))
    put('band01', (qq < kk).astype(np.float32))
    put('tril_st', (kk <= qq).astype(np.float32))
    put('tri_qs', np.where(qq <= kk, 0.0, -1e30))
    put('ones', np.ones((128, 128)))
    m64 = p % 64
    put('invf64', np.where(m64 < 16, THETA ** (-(2.0 * (m64 % 8)) / 16.0), 0.0)[:, None])
    put('nsgn64', np.where(m64 < 8, -1.0, np.where(m64 < 16, 1.0, 0.0))[:, None])
    P = np.zeros((128, 128))
    for m in range(128):
        if m % 64 < 8:
            P[m + 8, m] = 1
        elif m % 64 < 16:
            P[m - 8, m] = 1
    put('P64', P)
    m32 = p % 32
    put('invf32', np.where(m32 < 8, THETA ** (-(2.0 * (m32 % 4)) / 8.0), 0.0)[:, None])
    put('nsgn32', np.where(m32 < 4, -1.0, np.where(m32 < 8, 1.0, 0.0))[:, None])
    P = np.zeros((128, 128))
    for m in range(128):
        if m % 32 < 4:
            P[m + 4, m] = 1
        elif m % 32 < 8:
            P[m - 4, m] = 1
    put('P32', P)
    n = np.arange(127)
    t = np.arange(2048)
    put('cmpbias', np.where(16 * n[:, None] + 31 <= t[None, :], 0.0, NEG))
    c0 = n * 16
    s0 = np.arange(32) * 64
    put('overlap', ((c0[:, None] < s0[None, :] + 64) & (c0[:, None] + 32 > s0[None, :])).astype(np.float32))
    put('E32', (t[None, :] // 64 == np.arange(32)[:, None]).astype(np.float32))
    put('E8', (t[None, :] // 256 == np.arange(8)[:, None]).astype(np.float32))
    tt = (np.arange(16)[None, :, None] * 128 + p[:, None, None])
    j = np.arange(32)[None, None, :]
    adm = j * 64 <= tt
    forced = (j == 0) | (j == tt // 64)
    put('keep', (adm & ~forced).astype(np.float32).reshape(128, 512))
    put('addm', np.where(adm, np.where(forced, 1e4, 0.0), -1e30).reshape(128, 512))
    put('adm', adm.astype(np.float32).reshape(128, 512))
    own = (np.arange(16)[None, :, None] * 128 + p[:, None, None]) // 256
    j8 = np.arange(8)[None, None, :]
    put('mbias', np.where(j8 < own, 0.0, -1e30).reshape(128, 128))
    put('mpast', (j8 < own).astype(np.float32).reshape(128, 128))
    put('mown', (j8 == own).astype(np.float32).reshape(128, 128))
    return cp


class Consts:
    def __init__(self, S, cpack_ap):
        self.S, self.ap, self.cache = S, cpack_ap, {}

    def get(self, k, dtype=F32, rows=128):
        key = (k, dtype)
        if key in self.cache:
            return self.cache[key]
        S = self.S
        o, n = CP_OFF[k]
        if dtype == F32:
            f = S.sb([128, n], F32, 'c_' + k)
            S.dma('sp', f[:], self.ap[:, o:o + n], r=[], w=[f], allow_slow_non_contiguous=(n == 1))
            self.cache[key] = f
            return f
        if not hasattr(self, 'stg'):
            self.stg = S.sb([128, 2048], F32, 'c_stg')
        f = self.stg
        S.dma('sp', f[:, 0:n], self.ap[:, o:o + n], r=[], w=[f])
        b = S.sb([128, n], dtype, 'cb_' + k)
        S.dve(lambda e: e.tensor_copy(out=b[:], in_=f[:, 0:n]), r=[f], w=[b])
        self.cache[key] = b
        return b


def rope_tables(S, C, pos_ap, ntok, invk, sgnk, tmp):
    invf = C.get(invk)
    nsg = C.get(sgnk)
    if 'pi' not in tmp:
        tmp['pi'] = S.sb([128, 1024], I32, 'pos_i')
        tmp['ang'] = S.sb([128, 1024], F32, 'ang')
        tmp['kf'] = S.sb([128, 1024], F32, 'kf')
        tmp['ki'] = S.sb([128, 1024], I32, 'ki')
    pi_, ang, kf, ki = tmp['pi'], tmp['ang'], tmp['kf'], tmp['ki']
    ct = S.sb([128, ntok], F32, 'ropeC')
    st = S.sb([128, ntok], F32, 'ropeS')
    TWO_PI = 2.0 * np.pi
    for c0 in range(0, ntok, 1024):
        S.dma('sp', pi_[:], pos_ap[c0:c0 + 1024].partition_broadcast(128), r=[], w=[pi_])
        S.dve(lambda e: e.tensor_copy(out=ang[:], in_=pi_[:]), r=[pi_], w=[ang])
        S.dve(lambda e: e.tensor_scalar(out=ang[:], in0=ang[:], scalar1=invf[:, 0:1], scalar2=None, op0=ALU.mult),
              r=[ang, invf], w=[ang])

        def reduce_sin(dst, shift, post, c0=c0):
            S.dve(lambda e: e.tensor_scalar(out=kf[:], in0=ang[:], scalar1=shift, scalar2=1.0 / TWO_PI,
                                            op0=ALU.add, op1=ALU.mult), r=[ang], w=[kf])
            S.dve(lambda e: e.tensor_copy(out=ki[:], in_=kf[:]), r=[kf], w=[ki])
            S.dve(lambda e: e.tensor_copy(out=kf[:], in_=ki[:]), r=[ki], w=[kf])
            S.dve(lambda e: e.scalar_tensor_tensor(out=kf[:], in0=kf[:], scalar=-TWO_PI, in1=ang[:],
                                                   op0=ALU.mult, op1=ALU.add), r=[kf, ang], w=[kf])
            S.dve(lambda e: e.tensor_scalar(out=kf[:], in0=kf[:], scalar1=shift, scalar2=3.14159, op0=ALU.add,
                                            op1=ALU.min), r=[kf], w=[kf])
            S.dve(lambda e: e.tensor_scalar(out=kf[:], in0=kf[:], scalar1=-3.14159, scalar2=None, op0=ALU.max),
                  r=[kf], w=[kf])
            S.act(lambda e: e.activation(out=dst[:, c0:c0 + 1024], in_=kf[:], func=AF.Sin), r=[kf], w=[(dst, c0)])
            if post is not None:
                S.dve(lambda e: e.tensor_scalar(out=dst[:, c0:c0 + 1024], in0=dst[:, c0:c0 + 1024],
                                                scalar1=post[:, 0:1], scalar2=None, op0=ALU.mult),
                      r=[(dst, c0), post], w=[(dst, c0)])
        reduce_sin(ct, np.pi / 2.0, None)
        reduce_sin(st, 0.0, nsg)
    return ct, st


def proj_stage(S, x, w_in, nin, g_ap, pos, ntok, cpack, fm_specs, tm_groups, tm_post, use_idx=False):
    C = Consts(S, cpack)
    ident = C.get('ident', BF16)
    gb = S.sb([128, D], F32, 'gb')
    S.dma('sp', gb[:], g_ap.partition_broadcast(128), r=[], w=[gb])
    winb = S.sb([128, 8, nin], BF16, 'winb')
    wv = w_in.rearrange("(c p) f -> p c f", p=128)
    wst = [S.sb([128, 8, 256], F32, f'wst{i}') for i in range(2)]
    for bi, c0 in enumerate(range(0, nin, 256)):
        n = min(256, nin - c0)
        st = wst[bi % 2]
        S.dma('sp', st[:, :, 0:n], wv[:, :, c0:c0 + n], r=[], w=[st])
        S.dve(lambda e, st=st, c0=c0, n=n: e.tensor_copy(out=winb[:, :, c0:c0 + n], in_=st[:, :, 0:n]),
              r=[st], w=[(winb, bi)])
    ropes = {}
    rtmp = {}
    if any(sp[3] == 'r64' for sp in fm_specs):
        ct, sn = rope_tables(S, C, pos, ntok, 'invf64', 'nsgn64', rtmp)
        ropes['r64'] = (ct, sn, C.get('P64', BF16))
    if use_idx:
        ct, sn = rope_tables(S, C, pos, ntok, 'invf32', 'nsgn32', rtmp)
        ropes['r32'] = (ct, sn, C.get('P32', BF16))
    xt = [S.sb([128, D], F32, f'xt{i}') for i in range(2)]
    hb = [S.sb([128, D], BF16, f'hb{i}') for i in range(2)]
    junk = S.sb([128, D], F32, 'junk')
    ss = [S.sb([128, 1], F32, f'ss{i}') for i in range(2)]
    rstd = [S.sb([128, 1], F32, f'rstd{i}') for i in range(2)]
    hT = [S.sb([128, 8, 512], BF16, f'hT{i}') for i in range(2)]
    xh = [S.sb([128, 512], BF16, f'xh{i}') for i in range(2)]
    t1 = [S.sb([128, 512], F32, f't1{i}') for i in range(2)]
    t2 = [S.sb([128, 512], F32, f't2{i}') for i in range(2)]
    xr = [S.sb([128, 512], BF16, f'xr{i}') for i in range(2)]
    ptr = [S.ps([128, 8, 128], BF16, f'ptr{i}') for i in range(2)]
    pf = [S.ps([128, 512], F32, f'pf{i}') for i in range(2)]
    p2 = [S.ps([128, 512], F32, f'p2{i}') for i in range(2)]
    pt = [S.ps([128, 512], F32, f'pt{i}') for i in range(2)]
    nfm = 0
    ntm = 0
    for blk in range(ntok // 512):
        t0 = blk * 512
        hTb = hT[blk % 2]
        for i in range(4):
            b = i % 2
            x_t, h_b = xt[b], hb[b]
            S.dma('sp', x_t[:], x[t0 + i * 128:t0 + (i + 1) * 128, :], r=[], w=[x_t])
            rms_rstd(S, x_t, junk, ss[b], rstd[b], D)
            S.dve(lambda e, x_t=x_t, h_b=h_b, b=b: e.scalar_tensor_tensor(
                out=h_b[:], in0=x_t[:], scalar=rstd[b][:, 0:1], in1=gb[:], op0=ALU.mult, op1=ALU.mult),
                r=[x_t, rstd[b], gb], w=[h_b])
            p = ptr[b]
            for c in range(8):
                S.pe(lambda e, p=p, h_b=h_b, c=c: e.transpose(out=p[:, c, :], in_=h_b[:, c * 128:(c + 1) * 128],
                                                               identity=ident[:]), r=[h_b, ident], w=[(p, c)])
            S.act(lambda e, p=p, i=i, hTb=hTb: e.copy(out=hTb[:, :, i * 128:(i + 1) * 128], in_=p[:]),
                  r=[p], w=[(hTb, i)])
        for (col0, M, scale, rope, raw_dst, rot_dst) in fm_specs:
            q = nfm % 2
            nfm += 1
            pp = pf[q]
            for c in range(8):
                S.pe(lambda e, pp=pp, c=c, col0=col0, M=M, hTb=hTb: e.matmul(
                    pp[0:M, :], lhsT=winb[:, c, col0:col0 + M], rhs=hTb[:, c, :], start=(c == 0), stop=(c == 7)),
                    r=[winb, hTb], w=[pp])
            xq = xh[q]
            S.act(lambda e, xq=xq, pp=pp, M=M, scale=scale: e.mul(out=xq[0:M, :], in_=pp[0:M, :], mul=scale),
                  r=[pp], w=[xq])
            if raw_dst is not None:
                S.dma('pool', raw_dst[:, t0:t0 + 512], xq[0:M, :], r=[xq], w=[DW(S, raw_dst)])
            if rope is not None:
                ct, sn, Pm = ropes[rope]
                pq = p2[q]
                S.pe(lambda e, pq=pq, xq=xq, M=M, Pm=Pm: e.matmul(pq[0:M, :], lhsT=Pm[0:M, 0:M], rhs=xq[0:M, :],
                                                                  start=True, stop=True), r=[xq, Pm], w=[pq])
                S.dve(lambda e, q=q, xq=xq, M=M, ct=ct, t0=t0: e.tensor_tensor(
                    out=t1[q][0:M, :], in0=xq[0:M, :], in1=ct[0:M, t0:t0 + 512], op=ALU.mult),
                    r=[xq, ct], w=[t1[q]])
                S.dve(lambda e, q=q, pq=pq, M=M, sn=sn, t0=t0: e.tensor_tensor(
                    out=t2[q][0:M, :], in0=pq[0:M, :], in1=sn[0:M, t0:t0 + 512], op=ALU.mult),
                    r=[pq, sn], w=[t2[q]])
                S.dve(lambda e, q=q, M=M: e.tensor_tensor(out=xr[q][0:M, :], in0=t1[q][0:M, :], in1=t2[q][0:M, :],
                                                         op=ALU.add), r=[t1[q], t2[q]], w=[xr[q]])
                S.dma('pool', rot_dst[:, t0:t0 + 512], xr[q][0:M, :], r=[xr[q]], w=[DW(S, rot_dst)])
        for i in range(4):
            for gi, (col0, N) in enumerate(tm_groups):
                q = ntm % 2
                ntm += 1
                pp = pt[q]
                for c in range(8):
                    S.pe(lambda e, pp=pp, c=c, col0=col0, N=N, i=i, hTb=hTb: e.matmul(
                        pp[:, 0:N], lhsT=hTb[:, c, i * 128:(i + 1) * 128], rhs=winb[:, c, col0:col0 + N],
                        start=(c == 0), stop=(c == 7)), r=[winb, (hTb, i)], w=[pp])
                tm_post(gi, pp, t0 + i * 128, ntm)
    S.flush()


def even_proj(S, x, prm, dr, ntok, cpack):
    at = [S.sb([128, 512], BF16, f'at{i}') for i in range(2)]
    g1 = [S.sb([128, 512], F32, f'g1{i}') for i in range(2)]
    vb = [S.sb([128, 192], BF16, f'vb{i}') for i in range(2)]
    vb2 = [S.sb([128, 192], BF16, f'vb2{i}') for i in range(2)]
    gt = [S.sb([128, 36], F32, f'gt{i}') for i in range(2)]
    sgb = S.sb([128, 256], F32, 'sgb')
    S.dma('sp', sgb[:], prm['sgu_norm'].partition_broadcast(128), r=[], w=[sgb])
    junk = S.sb([128, 256], F32, 'junk2')
    ss = [S.sb([128, 1], F32, f'ssv{i}') for i in range(2)]
    rs = [S.sb([128, 1], F32, f'rsv{i}') for i in range(2)]
    cnt = [0]

    def tm_post(gi, pp, tok0, n):
        if gi == 0:
            b = cnt[0] % 2
            cnt[0] += 1
            g, a = g1[b], at[b]
            S.act(lambda e: e.activation(out=g[:], in_=pp[:], func=AF.Gelu_apprx_tanh), r=[pp], w=[g])
            S.pool(lambda e: e.tensor_copy(out=a[:, 0:256], in_=g[:, 0:256]), r=[g], w=[(a, 0)])
            S.act(lambda e: e.activation(out=junk[:], in_=g[:, 256:512], func=AF.Square, accum_out=ss[b][:]),
                  r=[g], w=[junk, ss[b]])
            S.act(lambda e: e.activation(out=rs[b][:], in_=ss[b][:], func=AF.Sqrt, bias=EPS, scale=1.0 / 256),
                  r=[ss[b]], w=[rs[b]])
            S.dve(lambda e: e.reciprocal(out=rs[b][:], in_=rs[b][:]), r=[rs[b]], w=[rs[b]])
            S.dve(lambda e: e.scalar_tensor_tensor(out=a[:, 256:512], in0=g[:, 256:512], scalar=rs[b][:, 0:1],
                                                   in1=sgb[:], op0=ALU.mult, op1=ALU.mult),
                  r=[g, rs[b], sgb], w=[(a, 1)])
            S.dma('pool', dr['a_tok'][tok0:tok0 + 128, :], a[:], r=[a], w=[DW(S, dr['a_tok'])])
        elif gi == 1:
            v = vb[(tok0 // 128) % 2]
            S.act(lambda e: e.copy(out=v[:], in_=pp[:, 0:192]), r=[pp], w=[v])
            S.dma('pool', dr['vsl'][tok0:tok0 + 128, :], v[:], r=[v], w=[DW(S, dr['vsl'])])
        else:
            v = vb2[(tok0 // 128) % 2]
            g = gt[(tok0 // 128) % 2]
            S.act(lambda e: e.copy(out=v[:], in_=pp[:, 0:192]), r=[pp], w=[v])
            S.act(lambda e: e.activation(out=g[:], in_=pp[:, 192:228], func=AF.Sigmoid), r=[pp], w=[g])
            S.dma('pool', dr['vw'][tok0:tok0 + 128, :], v[:], r=[v], w=[DW(S, dr['vw'])])
            S.dma('pool', dr['gate'][tok0:tok0 + 128, :], g[:], r=[g], w=[DW(S, dr['gate'])])
    fm = []
    for i in range(6):
        fm.append((512 + 128 * i, 128, 0.125, 'r64', dr['qraw'][128 * i:128 * (i + 1), :],
                   dr['qrot'][128 * i:128 * (i + 1), :]))
    for nm, c0, rope in (('kc', 1280, None), ('vc', 1472, None), ('ksl', 1664, 'r64'), ('kw', 2048, 'r64')):
        for (o, M) in ((0, 128), (128, 64)):
            dst = dr[nm][o:o + M, :]
            fm.append((c0 + o, M, 1.0, rope, dst if rope is None else None, dst if rope else None))
    proj_stage(S, x, prm['w_in'], 2468, prm['mix_norm'], prm['pos'], ntok, cpack, fm,
               [(0, 512), (1856, 192), (2240, 228)], tm_post)


def attn_chunk(S, c, tiles, qT, kT, extra, vext, STps, PT, Oacc, finalize):
    cover = {}
    for n, (kt, lo, hi, bt, blo) in enumerate(tiles):
        for j in range(lo // 128, hi // 128):
            cover.setdefault(j, []).append(n)
    NP = len(PT)

    def qk(n):
        kt, lo, hi, bt, blo = tiles[n]
        ps = STps[n % 2]
        S.pe(lambda e: e.matmul(ps[:, lo:hi], lhsT=kT[:, kt * 128:(kt + 1) * 128],
                                rhs=qT[:, c * 512 + lo:c * 512 + hi], start=True, stop=(extra is None)),
             r=[kT, qT], w=[ps])
        if extra:
            S.pe(lambda e: e.matmul(ps[:, lo:hi], lhsT=extra[0](kt), rhs=extra[1](kt, c, lo, hi),
                                    start=False, stop=True, skip_group_check=True), r=list(extra[2]), w=[ps])

    qk(0)
    for n, (kt, lo, hi, bt, blo) in enumerate(tiles):
        if n + 1 < len(tiles):
            qk(n + 1)
        ps, p = STps[n % 2], PT[n % NP]
        S.act(lambda e, ps=ps, p=p, lo=lo, hi=hi: e.activation(out=p[:, lo:hi], in_=ps[:, lo:hi], func=AF.Exp),
              r=[ps], w=[p])
        if bt is not None:
            S.dve(lambda e, p=p, bt=bt, blo=blo: e.tensor_tensor(out=p[:, blo:blo + 128], in0=p[:, blo:blo + 128],
                                                                 in1=bt[:], op=ALU.mult), r=[p, bt], w=[p])
        for j in range(lo // 128, hi // 128):
            S.pe(lambda e, p=p, j=j, kt=kt, n=n: e.matmul(
                Oacc[j][:, 0:65], lhsT=p[:, j * 128:(j + 1) * 128], rhs=vext[:, kt, :],
                start=(cover[j][0] == n), stop=(cover[j][-1] == n), skip_group_check=True),
                r=[p, vext], w=[Oacc[j]])
            if cover[j][-1] == n:
                finalize(c, j, Oacc[j])


def attn_run(S, jobs, STps, PT, Oacc, LA=2):
    flat = []
    for ji, jb in enumerate(jobs):
        cover = {}
        for n, (kt, lo, hi, bt, blo) in enumerate(jb['tiles']):
            for j in range(lo // 128, hi // 128):
                cover.setdefault(j, []).append(n)
        jb['cover'] = cover
        for n in range(len(jb['tiles'])):
            flat.append((ji, n))
    NS, NP = len(STps), len(PT)

    def qk(f):
        ji, n = flat[f]
        jb = jobs[ji]
        if n == 0 and jb.get('pre') is not None:
            jb['pre']()
        kt, lo, hi, bt, blo = jb['tiles'][n]
        c, qT, kT, extra = jb['c'], jb['qT'], jb['kT'], jb.get('extra')
        ps = STps[f % NS]
        S.pe(lambda e: e.matmul(ps[:, lo:hi], lhsT=kT[:, kt * 128:(kt + 1) * 128],
                                rhs=qT[:, c * 512 + lo:c * 512 + hi], start=True, stop=(extra is None)),
             r=[kT, qT], w=[ps])
        if extra:
            S.pe(lambda e: e.matmul(ps[:, lo:hi], lhsT=extra[0](kt), rhs=extra[1](kt, c, lo, hi),
                                    start=False, stop=True), r=list(extra[2]), w=[ps])

    for f in range(min(LA, len(flat))):
        qk(f)
    for f, (ji, n) in enumerate(flat):
        if f + LA < len(flat):
            qk(f + LA)
        jb = jobs[ji]
        kt, lo, hi, bt, blo = jb['tiles'][n]
        cover, vext = jb['cover'], jb['vext']
        ps, p = STps[f % NS], PT[f % NP]
        S.act(lambda e, ps=ps, p=p, lo=lo, hi=hi: e.activation(out=p[:, lo:hi], in_=ps[:, lo:hi], func=AF.Exp),
              r=[ps], w=[p])
        if bt is not None:
            S.pool(lambda e, p=p, bt=bt, blo=blo: e.tensor_tensor(out=p[:, blo:blo + 128], in0=p[:, blo:blo + 128],
                                                                  in1=bt[:], op=ALU.mult), r=[p, bt], w=[p])
        for j in range(lo // 128, hi // 128):
            O = Oacc[j]
            S.pe(lambda e, p=p, j=j, kt=kt, O=O, vext=vext, first=(cover[j][0] == n), last=(cover[j][-1] == n):
                 e.matmul(O[:, 0:65], lhsT=p[:, j * 128:(j + 1) * 128], rhs=vext[:, kt, :], start=first, stop=last),
                 r=[p, vext], w=[O])
            if cover[j][-1] == n:
                jb['fin'](jb['c'], j, O, 0)


def causal_tiles(c, tri):
    out = []
    for kt in range(4 * c + 4):
        if kt < 4 * c:
            out.append((kt, 0, 512, None, 0))
        else:
            lo = (kt - 4 * c) * 128
            out.append((kt, lo, 512, tri, lo))
    return out


def window_tiles(c, tri, band):
    out = []
    if c >= 1:
        out.append((4 * c - 1, 0, 512, band, 384))
        for i in range(3):
            out.append((4 * c - 4 + i, 0, 128 * (i + 1), band, 128 * i))
    for i in range(4):
        out.append((4 * c + i, 128 * i, 512, tri, 128 * i))
    return out


def nsa_stage(S, prm, dr, nseq, cpack):
    C = Consts(S, cpack)
    ident = C.get('ident', BF16)
    tri = C.get('tri01', BF16)
    band = C.get('band01', BF16)
    ones = C.get('ones', BF16)
    cmpb = C.get('cmpbias', BF16)
    ovl = C.get('overlap', F32)
    E32 = C.get('E32', F32)
    keep, addm, adm = C.get('keep'), C.get('addm'), C.get('adm')
    tril = C.get('tril_st', F32)
    wTf = S.sb([128, 4, 128], F32, 'wTf')
    S.dma('sp', wTf[:], prm['sgu_wT'].rearrange("g s t -> s g t"), r=[], w=[wTf])
    wTb = S.sb([128, 4, 128], BF16, 'wTb')
    for g in range(4):
        S.dve(lambda e, g=g: e.tensor_tensor(out=wTb[:, g, :], in0=wTf[:, g, :], in1=tril[:], op=ALU.mult),
              r=[wTf, tril], w=[(wTb, g)])
    bT = S.sb([128, 4], F32, 'bT')
    S.dma('sp', bT[:], prm['sgu_bT'], r=[], w=[bT])
    cw = {}
    stg = [S.sb([64, 4, 256], F32, f'w1s{i}') for i in range(2)]
    w2s = S.sb([128, 2, 64], F32, 'w2s')
    pst = S.sb([64, 32], F32, 'pst')
    pm0 = S.ps([128, 512], F32, 'pm0')
    pmisc = [pm0, pm0]
    ptb = S.ps([128, 1024], BF16, 'ptb')
    k = 0
    for kv in ('k', 'v'):
        w1b = S.sb([64, 32, 256], BF16, 'w1b' + kv)
        w1v = prm['cmp_w1_' + kv].rearrange("(l e) c -> e l c", e=64)
        for q4 in range(8):
            st = stg[k % 2]
            k += 1
            S.dma('sp', st[:], w1v[:, q4 * 4:(q4 + 1) * 4, :], r=[], w=[st])
            S.dve(lambda e, st=st, w1b=w1b, q4=q4: e.tensor_copy(out=w1b[:, q4 * 4:(q4 + 1) * 4, :], in_=st[:]),
                  r=[st], w=[(w1b, q4)])
        w2b = S.sb([128, 2, 64], BF16, 'w2b' + kv)
        S.dma('sp', w2s[:], prm['cmp_w2_' + kv].rearrange("(c p) e -> p c e", p=128), r=[], w=[w2s])
        S.dve(lambda e, w2b=w2b: e.tensor_copy(out=w2b[:], in_=w2s[:]), r=[w2s], w=[w2b])
        posb = S.sb([64, 32], BF16, 'posb' + kv)
        S.dma('sp', pst[:], prm['cmp_posT_' + kv], r=[], w=[pst])
        S.dve(lambda e, posb=posb: e.tensor_copy(out=posb[:], in_=pst[:]), r=[pst], w=[posb])
        pbias = S.sb([128, 2], F32, 'pbias' + kv)
        for cc in range(2):
            pp = pmisc[cc]
            for l in range(32):
                S.pe(lambda e, pp=pp, w1b=w1b, posb=posb, l=l, cc=cc: e.matmul(
                    pp[:, 0:1], lhsT=w1b[:, l, cc * 128:(cc + 1) * 128], rhs=posb[:, l:l + 1],
                    start=(l == 0), stop=(l == 31)), r=[w1b, posb], w=[pp])
            S.dve(lambda e, pp=pp, pbias=pbias, cc=cc: e.tensor_copy(out=pbias[:, cc:cc + 1], in_=pp[:, 0:1]),
                  r=[pp], w=[(pbias, cc)])
        cw[kv] = (w1b, w2b, pbias)
    atok = S.sb([128, 16, 512], BF16, 'atok')
    gates = S.sb([128, 16, 36], F32, 'gates')
    otok = S.sb([128, 16, 1024], BF16, 'otok')
    accg = S.sb([128, 16, 256], F32, 'accg')
    kcT = S.sb([64, SEQ], BF16, 'kcT')
    vcT = S.sb([64, SEQ], BF16, 'vcT')
    kslT = S.sb([128, SEQ], BF16, 'kslT')
    kwT = S.sb([128, SEQ], BF16, 'kwT')
    S.dve(lambda e: e.memset(kslT[:], 0.0), r=[], w=[kslT])
    S.pool(lambda e: e.memset(kwT[:], 0.0), r=[], w=[kwT])
    S.dve(lambda e: e.tensor_copy(out=kslT[64:96, :], in_=E32[0:32, :]), r=[E32], w=[(kslT, 'e')])
    vsl = S.sb([128, 16, 65], BF16, 'vslx')
    vw = S.sb([128, 16, 65], BF16, 'vwx')
    S.pool(lambda e: e.memset(vsl[:], 1.0), r=[], w=[vsl])
    S.pool(lambda e: e.memset(vw[:], 1.0), r=[], w=[vw])
    qraw = S.sb([128, SEQ], BF16, 'qrawT')
    qrots = [S.sb([128, SEQ], BF16, f'qrotT{i}') for i in range(2)]
    S.pool(lambda e: e.memset(qraw[:], 0.0), r=[], w=[qraw])
    S.dve(lambda e: e.memset(qrots[0][:], 0.0), r=[], w=[qrots[0]])
    S.pool(lambda e: e.memset(qrots[1][:], 0.0), r=[], w=[qrots[1]])
    ghT = S.sb([128, 2, 128], BF16, 'ghT')
    kcmpT = S.sb([128, 128], BF16, 'kcmpT')
    S.dve(lambda e: e.memset(kcmpT[:], 0.0), r=[], w=[kcmpT])
    vcmpx = S.sb([128, 65], BF16, 'vcmpx')
    S.dve(lambda e: e.memset(vcmpx[:], 1.0), r=[], w=[vcmpx])
    impacc = S.sb([128, 16, 32], F32, 'impacc')
    pcf = [S.sb([128, 512], BF16, f'pcf{i}') for i in range(2)]
    ovlb = C.get('overlap', BF16)
    PT = [S.sb([128, 512], BF16, f'PT{i}') for i in range(4)]
    gm = [S.sb([128, 256], F32, f'gm{i}') for i in range(2)]
    sel = [S.sb([128, 32], F32, f'sel{i}') for i in range(2)]
    m8 = [S.sb([128, 8], F32, f'm8{i}') for i in range(2)]
    nsel = [S.sb([128, 32], BF16, f'nsel{i}') for i in range(2)]
    fsc = [S.sb([128, 1], F32, f'fsc{i}') for i in range(4)]
    ftmp = [S.sb([128, 64], F32, f'ftmp{i}') for i in range(4)]
    STps = [S.ps([128, 512], F32, f'st{i}') for i in range(2)] + [pm0]
    Oacc = [S.ps([128, 512], F32, f'oa{i}') for i in range(4)]
    fcount = [0]

    def make_finalize(r, gidx, first):
        def fin(c, j, O, off):
            i = 4 * c + j
            f = fsc[fcount[0] % 4]
            fcount[0] += 1
            tm = ftmp[fcount[0] % 4]
            S.dve(lambda e: e.reciprocal(out=f[:], in_=O[:, off + 64:off + 65]), r=[O], w=[f])
            S.dve(lambda e: e.tensor_scalar(out=tm[:], in0=O[:, off:off + 64], scalar1=f[:, 0:1],
                                            scalar2=gates[:, i, gidx:gidx + 1], op0=ALU.mult, op1=ALU.mult),
                  r=[O, f, gates], w=[tm])
            S.pool(lambda e: e.tensor_tensor(out=accg[:, i, r * 64:(r + 1) * 64], in0=accg[:, i, r * 64:(r + 1) * 64],
                                             in1=tm[:], op=ALU.add), r=[tm, (accg, (i, r))], w=[(accg, (i, r))])
        return fin

    for sq in range(nseq):
        tb = sq * SEQ
        S.dma('sp', atok[:], dr['a_tok'][tb:tb + SEQ, :].rearrange("(n p) c -> p n c", p=128), r=[], w=[atok])
        S.dma('sp', gates[:], dr['gate'][tb:tb + SEQ, :].rearrange("(n p) c -> p n c", p=128), r=[], w=[gates])
        for i in range(16):
            pp = pmisc[i % 2]
            g_ = gm[i % 2]
            for g in range(4):
                S.pe(lambda e, pp=pp, g=g, i=i: e.matmul(pp[:, g * 64:(g + 1) * 64], lhsT=wTb[:, g, :],
                                                          rhs=atok[:, i, 256 + g * 64:256 + (g + 1) * 64],
                                                          start=True, stop=True), r=[wTb, atok], w=[pp])
                S.dve(lambda e, pp=pp, g_=g_, g=g: e.tensor_scalar(
                    out=g_[:, g * 64:(g + 1) * 64], in0=pp[:, g * 64:(g + 1) * 64], scalar1=bT[:, g:g + 1],
                    scalar2=None, op0=ALU.add), r=[pp, bT], w=[(g_, g)])
            S.dve(lambda e, g_=g_, i=i: e.tensor_tensor(out=otok[:, i, 0:256], in0=g_[:], in1=atok[:, i, 0:256],
                                                         op=ALU.mult), r=[g_, atok], w=[(otok, (i, 0))])
        for g in range(3):
            for nm, tl in (('kc', kcT), ('vc', vcT)):
                S.dma('sp', tl[:], dr[nm][g * 64:(g + 1) * 64, tb:tb + SEQ], r=[], w=[tl])
            for nm, tl in (('ksl', kslT), ('kw', kwT)):
                S.dma('sp', tl[0:64, :], dr[nm][g * 64:(g + 1) * 64, tb:tb + SEQ], r=[], w=[(tl, 'k')])
            for nm, tl in (('vsl', vsl), ('vw', vw)):
                S.dma('sp', tl[:, :, 0:64], dr[nm][tb:tb + SEQ, g * 64:(g + 1) * 64].rearrange(
                    "(n p) c -> p n c", p=128), r=[], w=[tl])
            for kv, src in (('k', kcT), ('v', vcT)):
                w1b, w2b, pbias = cw[kv]
                for cc in range(2):
                    pp = pmisc[cc]
                    for l in range(32):
                        S.pe(lambda e, pp=pp, w1b=w1b, src=src, l=l, cc=cc: e.matmul(
                            pp[:, 0:127], lhsT=w1b[:, l, cc * 128:(cc + 1) * 128], rhs=src[:, l:l + 2017:16],
                            start=(l == 0), stop=(l == 31)), r=[w1b, src], w=[pp])
                    S.act(lambda e, pp=pp, cc=cc, pbias=pbias: e.activation(
                        out=ghT[:, cc, 0:127], in_=pp[:, 0:127], func=AF.Gelu_apprx_tanh, bias=pbias[:, cc:cc + 1]),
                        r=[pp, pbias], w=[(ghT, cc)])
                pp = pmisc[0]
                if kv == 'k':
                    for cc in range(2):
                        S.pe(lambda e, pp=pp, cc=cc, w2b=w2b: e.matmul(pp[0:64, 0:127], lhsT=w2b[:, cc, :],
                                                                     rhs=ghT[:, cc, 0:127], start=(cc == 0),
                                                                     stop=(cc == 1)), r=[w2b, ghT], w=[pp])
                    S.act(lambda e, pp=pp: e.copy(out=kcmpT[0:64, 0:127], in_=pp[0:64, 0:127]), r=[pp], w=[(kcmpT, 'k')])
                else:
                    for cc in range(2):
                        S.pe(lambda e, pp=pp, cc=cc, w2b=w2b: e.matmul(pp[0:127, 0:64], lhsT=ghT[:, cc, 0:127],
                                                                     rhs=w2b[:, cc, :], start=(cc == 0),
                                                                     stop=(cc == 1)), r=[w2b, ghT], w=[pp])
                    S.act(lambda e, pp=pp: e.copy(out=vcmpx[0:127, 0:64], in_=pp[0:127, 0:64]), r=[pp], w=[(vcmpx, 'v')])
            for r in range(4):
                h = 4 * g + r
                S.dma('sp', qraw[0:64, :], dr['qraw'][h * 64:(h + 1) * 64, tb:tb + SEQ], r=[], w=[(qraw, 'q')])
                for c in range(4):
                    b = c % 2
                    ps = STps[b]
                    S.pe(lambda e, ps=ps, c=c: e.matmul(ps[0:127, :], lhsT=kcmpT[:, 0:127],
                                                        rhs=qraw[:, c * 512:(c + 1) * 512], start=True, stop=False),
                         r=[kcmpT, qraw], w=[ps])
                    S.pe(lambda e, ps=ps, c=c: e.matmul(ps[0:127, :], lhsT=ident[0:127, 0:127],
                                                        rhs=cmpb[0:127, c * 512:(c + 1) * 512], start=False, stop=True),
                         r=[ident, cmpb], w=[ps])
                    S.act(lambda e, ps=ps, b=b: e.activation(out=pcf[b][0:127, :], in_=ps[0:127, :], func=AF.Exp),
                          r=[ps], w=[pcf[b]])
                    for j in range(4):
                        i = 4 * c + j
                        O = Oacc[j]
                        S.pe(lambda e, O=O, b=b, j=j: e.matmul(O[:, 0:65], lhsT=pcf[b][0:127, j * 128:(j + 1) * 128],
                                                               rhs=vcmpx[0:127, :], start=True, stop=True),
                             r=[pcf[b], vcmpx], w=[O])
                        S.pe(lambda e, O=O, b=b, j=j: e.matmul(O[:, 128:160], lhsT=pcf[b][0:127, j * 128:(j + 1) * 128],
                                                               rhs=ovlb[0:127, 0:32], start=True, stop=True),
                             r=[pcf[b], ovlb], w=[O])
                        f = fsc[fcount[0] % 4]
                        fcount[0] += 1
                        S.dve(lambda e, O=O, f=f: e.tensor_scalar(out=f[:], in0=O[:, 64:65], scalar1=1e-30, scalar2=None,
                                                                  op0=ALU.max), r=[O], w=[f])
                        S.dve(lambda e, f=f: e.reciprocal(out=f[:], in_=f[:]), r=[f], w=[f])
                        S.dve(lambda e, O=O, i=i, r=r, h=h, f=f: e.tensor_scalar(
                            out=accg[:, i, r * 64:(r + 1) * 64], in0=O[:, 0:64], scalar1=f[:, 0:1],
                            scalar2=gates[:, i, 3 * h:3 * h + 1], op0=ALU.mult, op1=ALU.mult),
                            r=[O, f, gates], w=[(accg, (i, r))])
                        if r == 0:
                            S.dve(lambda e, O=O, i=i, f=f: e.tensor_scalar(out=impacc[:, i, :], in0=O[:, 128:160],
                                                                           scalar1=f[:, 0:1], scalar2=None, op0=ALU.mult),
                                  r=[O, f], w=[(impacc, i)])
                        else:
                            S.dve(lambda e, O=O, i=i, f=f: e.scalar_tensor_tensor(
                                out=impacc[:, i, :], in0=O[:, 128:160], scalar=f[:, 0:1], in1=impacc[:, i, :],
                                op0=ALU.mult, op1=ALU.add), r=[O, f, (impacc, i)], w=[(impacc, i)])
            for i in range(16):
                b = i % 2
                sl = sel[b]
                S.dve(lambda e, sl=sl, i=i: e.tensor_tensor(out=sl[:], in0=impacc[:, i, :],
                                                           in1=keep[:, i * 32:(i + 1) * 32], op=ALU.mult),
                      r=[(impacc, i), keep], w=[sl])
                S.dve(lambda e, sl=sl, i=i: e.tensor_tensor(out=sl[:], in0=sl[:], in1=addm[:, i * 32:(i + 1) * 32],
                                                           op=ALU.add), r=[sl, addm], w=[sl])
                S.dve(lambda e, sl=sl, b=b: e.max(out=m8[b][:], in_=sl[:]), r=[sl], w=[m8[b]])
                S.dve(lambda e, sl=sl, b=b: e.tensor_scalar(out=sl[:], in0=sl[:], scalar1=m8[b][:, 7:8], scalar2=None,
                                                            op0=ALU.is_ge), r=[sl, m8[b]], w=[sl])
                S.dve(lambda e, sl=sl, i=i: e.tensor_tensor(out=sl[:], in0=sl[:], in1=adm[:, i * 32:(i + 1) * 32],
                                                           op=ALU.mult), r=[sl, adm], w=[sl])
                S.dve(lambda e, sl=sl, b=b: e.tensor_scalar(out=nsel[b][:], in0=sl[:], scalar1=-NEG, scalar2=NEG,
                                                            op0=ALU.mult, op1=ALU.add), r=[sl], w=[nsel[b]])
                S.pe(lambda e, b=b: e.transpose(out=ptb[0:32, b * 128:(b + 1) * 128], in_=nsel[b][:],
                                                identity=ident[:]), r=[nsel[b], ident], w=[(ptb, b)])
                for qb in qrots:
                    S.dve(lambda e, i=i, b=b, qb=qb: e.tensor_copy(out=qb[64:96, i * 128:(i + 1) * 128],
                                                                   in_=ptb[0:32, b * 128:(b + 1) * 128]),
                          r=[(ptb, b)], w=[(qb, ('n', i))])
            def load_q(r, g=g, tb=tb):
                h = 4 * g + r
                qb = qrots[r % 2]
                S.dma('sp', qb[0:64, :], dr['qrot'][h * 64:(h + 1) * 64, tb:tb + SEQ], r=[], w=[(qb, 'q')])
            load_q(0)
            jobs = []
            for r in range(4):
                h = 4 * g + r
                qb = qrots[r % 2]
                for c in range(4):
                    jobs.append(dict(c=c, tiles=causal_tiles(c, tri), qT=qb, kT=kslT, vext=vsl,
                                     fin=make_finalize(r, 3 * h + 1, False),
                                     pre=((lambda r=r: load_q(r + 1)) if (c == 0 and r < 3) else None)))
                for c in range(4):
                    jobs.append(dict(c=c, tiles=window_tiles(c, tri, band), qT=qb, kT=kwT, vext=vw,
                                     fin=make_finalize(r, 3 * h + 2, False)))
            attn_run(S, jobs, STps, PT, Oacc)
            S.act(lambda e, g=g: e.copy(out=otok[:, :, 256 + 256 * g:256 + 256 * (g + 1)], in_=accg[:]),
                  r=[accg], w=[(otok, ('g', g))])
        S.dma('sp', dr['o_tok'][tb:tb + SEQ, :].rearrange("(n p) c -> p n c", p=128), otok[:],
              r=[otok], w=[DW(S, dr['o_tok'])])
    S.flush()


def outproj_stage(S, x_in, x_out, o_tok, w_out, ntok, cpack):
    C = Consts(S, cpack)
    ident = C.get('ident', BF16)
    wob = S.sb([128, 8, D], BF16, 'wob')
    wst = [S.sb([128, D], F32, f'wos{i}') for i in range(2)]
    wv = w_out.rearrange("(c p) m -> p c m", p=128)
    for c in range(8):
        st = wst[c % 2]
        S.dma('sp', st[:], wv[:, c, :], r=[], w=[st])
        S.dve(lambda e, st=st, c=c: e.tensor_copy(out=wob[:, c, :], in_=st[:]), r=[st], w=[(wob, c)])
    ot = [S.sb([128, D], BF16, f'oo{i}') for i in range(2)]
    oT = [S.sb([128, 8, 128], BF16, f'oT{i}') for i in range(2)]
    xt = [S.sb([128, D], F32, f'xo{i}') for i in range(2)]
    xo = [S.sb([128, D], F32, f'xn{i}') for i in range(2)]
    ptr = [S.ps([128, 8, 128], BF16, f'ptr{i}') for i in range(2)]
    py = [S.ps([128, 512], F32, f'py{i}') for i in range(4)]
    for i in range(ntok // 128):
        b = i % 2
        S.dma('sp', ot[b][:], o_tok[i * 128:(i + 1) * 128, :], r=[], w=[ot[b]])
        S.dma('sp', xt[b][:], x_in[i * 128:(i + 1) * 128, :], r=[], w=[xt[b]])
        p = ptr[b]
        for c in range(8):
            S.pe(lambda e, p=p, b=b, c=c: e.transpose(out=p[:, c, :], in_=ot[b][:, c * 128:(c + 1) * 128],
                                                      identity=ident[:]), r=[ot[b], ident], w=[(p, c)])
        S.act(lambda e, p=p, b=b: e.copy(out=oT[b][:], in_=p[:]), r=[p], w=[oT[b]])
        for mh in range(2):
            pp = py[b * 2 + mh]
            for c in range(8):
                S.pe(lambda e, pp=pp, b=b, c=c, mh=mh: e.matmul(pp[:], lhsT=oT[b][:, c, :],
                                                                rhs=wob[:, c, mh * 512:(mh + 1) * 512],
                                                                start=(c == 0), stop=(c == 7)),
                     r=[oT[b], wob], w=[pp])
            S.dve(lambda e, pp=pp, b=b, mh=mh: e.tensor_tensor(out=xo[b][:, mh * 512:(mh + 1) * 512], in0=pp[:],
                                                               in1=xt[b][:, mh * 512:(mh + 1) * 512], op=ALU.add),
                  r=[pp, xt[b]], w=[(xo[b], mh)])
        S.dma('pool', x_out[i * 128:(i + 1) * 128, :], xo[b][:], r=[xo[b]], w=[DW(S, x_out)])
    S.flush()


def odd_proj(S, x, prm, dr, ntok, cpack):
    vb = [S.sb([128, 64], BF16, f'vb{i}') for i in range(2)]
    wb = [S.sb([128, 4], F32, f'wb{i}') for i in range(2)]
    vd = [S.sb([128, 512], BF16, f'vd{i}') for i in range(2)]

    def tm_post(gi, pp, tok0, n):
        b = (tok0 // 128) % 2
        if gi == 0:
            S.act(lambda e: e.copy(out=vb[b][:], in_=pp[:, 0:64]), r=[pp], w=[vb[b]])
            S.dma('pool', dr['vcd'][tok0:tok0 + 128, :], vb[b][:], r=[vb[b]], w=[DW(S, dr['vcd'])])
        elif gi == 1:
            S.act(lambda e: e.copy(out=wb[b][:], in_=pp[:, 0:4]), r=[pp], w=[wb[b]])
            S.dma('pool', dr['wi'][tok0:tok0 + 128, :], wb[b][:], r=[wb[b]], w=[DW(S, dr['wi'])])
        else:
            S.act(lambda e: e.copy(out=vd[b][:], in_=pp[:, 0:512]), r=[pp], w=[vd[b]])
            S.dma('pool', dr['vdd'][tok0:tok0 + 128, :], vd[b][:], r=[vd[b]], w=[DW(S, dr['vdd'])])
    fm = []
    for i in range(4):
        fm.append((128 * i, 128, 0.125, 'r64', None, dr['qc'][128 * i:128 * (i + 1), :]))
    fm.append((512, 64, 1.0, 'r64', None, dr['kcd'][:, :]))
    fm.append((640, 128, 1.0, 'r32', None, dr['qi'][:, :]))
    fm.append((768, 32, 1.0, 'r32', None, dr['ki'][:, :]))
    for i in range(4):
        fm.append((804 + 128 * i, 128, 0.125, 'r64', None, dr['qd'][128 * i:128 * (i + 1), :]))
    for i in range(4):
        fm.append((1316 + 128 * i, 128, 1.0, 'r64', None, dr['kd'][128 * i:128 * (i + 1), :]))
    proj_stage(S, x, prm['w_in'], 2340, prm['mix_norm'], prm['pos'], ntok, cpack, fm,
               [(576, 64), (800, 4), (1828, 512)], tm_post, use_idx=True)


NBIS = 12


def dsa_moba_stage(S, prm, dr, nseq, cpack):
    C = Consts(S, cpack)
    ident = C.get('ident', BF16)
    tri = C.get('tri01', BF16)
    trib = C.get('tri_ge', BF16)
    E8 = C.get('E8', F32)
    triqs = C.get('tri_qs', F32)
    mbias, mpast, mown = C.get('mbias'), C.get('mpast'), C.get('mown')
    zc = [0]

    def ztile(shape, name):
        t = S.sb(shape, BF16, name)
        zc[0] += 1
        if zc[0] % 2:
            S.dve(lambda e: e.memset(t[:], 0.0), r=[], w=[t])
        else:
            S.pool(lambda e: e.memset(t[:], 0.0), r=[], w=[t])
        return t
    qi = [ztile([128, SEQ], f'qi{h}') for h in range(4)]
    ki = ztile([128, SEQ], 'ki')
    wi = S.sb([128, 16, 4], F32, 'wi')
    absw = S.sb([128, 16, 4], F32, 'absw')
    sgnw = S.sb([128, 16, 4], F32, 'sgnw')
    scores = [S.sb([128, SEQ], F32, f'score{i}') for i in range(2)]
    rl = [S.sb([128, 512], F32, f'rl{i}') for i in range(2)]
    junkb = S.sb([128, SEQ], BF16, 'junkb')
    nmasks = [S.sb([128, SEQ], BF16, f'nmask{i}') for i in range(2)]
    nmTs = [S.sb([128, 16, 512], BF16, f'nmT{i}') for i in range(2)]
    kcT = ztile([128, SEQ], 'kcT')
    vcx = S.sb([128, 16, 65], BF16, 'vcx')
    vdxs = [S.sb([128, 16, 65], BF16, f'vdx{i}') for i in range(2)]
    S.pool(lambda e: e.memset(vcx[:], 1.0), r=[], w=[vcx])
    for v_ in vdxs:
        S.pool(lambda e, v_=v_: e.memset(v_[:], 1.0), r=[], w=[v_])
    qcall = [ztile([128, SEQ], f'qcall{h}') for h in range(8)]
    qds = [ztile([128, SEQ], f'qd{i}') for i in range(2)]
    kds = [ztile([128, SEQ], f'kd{i}') for i in range(2)]
    for kd_ in kds:
        S.dve(lambda e, kd_=kd_: e.tensor_copy(out=kd_[64:72, :], in_=E8[0:8, :]), r=[E8], w=[(kd_, 'e')])
    kmf = S.sb([64, 8], F32, 'kmf')
    kmbs = [ztile([128, 8], f'kmb{i}') for i in range(2)]
    gsb = S.sb([128, 128], F32, 'gsb')
    ns8all = S.sb([128, 16, 32], BF16, 'ns8all')
    otok = S.sb([128, 16, 1024], BF16, 'otok')
    PT = [S.sb([128, 512], BF16, f'PT{i}') for i in range(4)]
    st5 = [S.sb([128, 8], F32, f'st5{i}') for i in range(2)]
    gs = [S.sb([128, 8], F32, f'gs{i}') for i in range(2)]
    m8 = [S.sb([128, 8], F32, f'm8{i}') for i in range(2)]
    ns8 = [S.sb([128, 8], BF16, f'ns8{i}') for i in range(2)]
    fsc = [S.sb([128, 1], F32, f'fsc{i}') for i in range(4)]
    pl = S.ps([128, 512], F32, 'pl')
    STps = [S.ps([128, 512], F32, f'st{i}') for i in range(2)]
    Oacc = [S.ps([128, 512], F32, f'oa{i}') for i in range(4)]
    ptb = S.ps([128, 8, 128], BF16, 'ptb')
    fcount = [0]

    def make_fin(col0):
        def fin(c, j, O, off):
            i = 4 * c + j
            f = fsc[fcount[0] % 4]
            fcount[0] += 1
            S.dve(lambda e: e.reciprocal(out=f[:], in_=O[:, off + 64:off + 65]), r=[O], w=[f])
            S.dve(lambda e: e.tensor_scalar(out=otok[:, i, col0:col0 + 64], in0=O[:, off:off + 64], scalar1=f[:, 0:1],
                                            scalar2=None, op0=ALU.mult), r=[O, f], w=[(otok, (i, col0))])
        return fin

    def index_steps(c):
        nmT = nmTs[c % 2]
        steps = []
        chains = {}
        for j in range(4):
            i = 4 * c + j
            W = 128 * (i + 1)
            score = scores[j % 2]
            nmask = nmasks[j % 2]
            steps = chains.setdefault(j, [])
            if i < 2:
                def trivial(i=i, j=j):
                    for st_ in range(i + 1):
                        if st_ == i:
                            S.pool(lambda e, st_=st_: e.tensor_copy(out=nmT[:, st_, j * 128:(j + 1) * 128], in_=trib[:]),
                                   r=[trib], w=[(nmT, (st_, j))])
                        else:
                            S.pool(lambda e, st_=st_: e.memset(nmT[:, st_, j * 128:(j + 1) * 128], 0.0),
                                   r=[], w=[(nmT, (st_, j))])
                steps.append(trivial)
                continue
            s5 = st5[i % 2]
            for h in range(4):
                def logits(h=h, i=i, W=W, score=score):
                    for sc in range((W + 511) // 512):
                        n = min(512, W - 512 * sc)
                        rb = rl[(h * 4 + sc) % 2]
                        S.pe(lambda e, sc=sc, n=n: e.matmul(pl[:, 0:n], lhsT=qi[h][:, i * 128:(i + 1) * 128],
                                                            rhs=ki[:, sc * 512:sc * 512 + n], start=True, stop=True),
                             r=[qi[h], ki], w=[pl])
                        S.act(lambda e, rb=rb, n=n: e.activation(out=rb[:, 0:n], in_=pl[:, 0:n], func=AF.Relu,
                                                                 scale=absw[:, i, h:h + 1]), r=[pl, absw], w=[rb])
                        if h == 0:
                            S.dve(lambda e, rb=rb, n=n, sc=sc: e.tensor_scalar(
                                out=score[:, sc * 512:sc * 512 + n], in0=rb[:, 0:n], scalar1=sgnw[:, i, h:h + 1],
                                scalar2=None, op0=ALU.mult), r=[rb, sgnw], w=[(score, sc)])
                        else:
                            S.dve(lambda e, rb=rb, n=n, sc=sc: e.scalar_tensor_tensor(
                                out=score[:, sc * 512:sc * 512 + n], in0=rb[:, 0:n], scalar=sgnw[:, i, h:h + 1],
                                in1=score[:, sc * 512:sc * 512 + n], op0=ALU.mult, op1=ALU.add),
                                r=[rb, sgnw, (score, sc)], w=[(score, sc)])
                steps.append(logits)

            def bounds(i=i, W=W, s5=s5, score=score):
                S.dve(lambda e: e.tensor_reduce(out=s5[:, 5:6], in_=score[:, 0:W], axis=AX.X, op=ALU.max),
                      r=[score], w=[(s5, 5)])
                S.dve(lambda e: e.tensor_reduce(out=s5[:, 0:1], in_=score[:, 0:W], axis=AX.X, op=ALU.min),
                      r=[score], w=[(s5, 0)])
                S.dve(lambda e: e.tensor_tensor(out=s5[:, 1:2], in0=s5[:, 5:6], in1=s5[:, 0:1], op=ALU.subtract),
                      r=[(s5, 5), (s5, 0)], w=[(s5, 1)])
                S.dve(lambda e: e.tensor_tensor(out=score[:, i * 128:(i + 1) * 128],
                                                in0=score[:, i * 128:(i + 1) * 128], in1=triqs[:], op=ALU.add),
                      r=[score, triqs], w=[score])
            steps.append(bounds)
            for it in range(NBIS):
                def bis(W=W, s5=s5, score=score):
                    S.dve(lambda e: e.tensor_scalar(out=s5[:, 1:2], in0=s5[:, 1:2], scalar1=0.5, scalar2=None,
                                                    op0=ALU.mult), r=[(s5, 1)], w=[(s5, 1)])
                    S.dve(lambda e: e.tensor_tensor(out=s5[:, 2:3], in0=s5[:, 0:1], in1=s5[:, 1:2], op=ALU.add),
                          r=[(s5, 0), (s5, 1)], w=[(s5, 2)])
                    S.dve(lambda e: e.tensor_scalar(out=junkb[:, 0:W], in0=score[:, 0:W], scalar1=s5[:, 2:3],
                                                    scalar2=0.0, op0=ALU.is_ge, op1=ALU.add, accum_out=s5[:, 3:4]),
                          r=[score, (s5, 2)], w=[junkb, (s5, 3)])
                    S.dve(lambda e: e.tensor_scalar(out=s5[:, 4:5], in0=s5[:, 3:4], scalar1=255.5, scalar2=None,
                                                    op0=ALU.is_ge), r=[(s5, 3)], w=[(s5, 4)])
                    S.dve(lambda e: e.scalar_tensor_tensor(out=s5[:, 0:1], in0=s5[:, 1:2], scalar=s5[:, 4:5],
                                                           in1=s5[:, 0:1], op0=ALU.mult, op1=ALU.add),
                          r=[(s5, 1), (s5, 4), (s5, 0)], w=[(s5, 0)])
                steps.append(bis)

            def fin_mask(i=i, j=j, W=W, s5=s5, score=score, nmask=nmask):
                S.dve(lambda e: e.tensor_scalar(out=nmask[:, 0:W], in0=score[:, 0:W], scalar1=s5[:, 0:1],
                                                scalar2=NEG, op0=ALU.is_lt, op1=ALU.mult),
                      r=[score, (s5, 0)], w=[nmask])
                for s0 in range(0, i + 1, 8):
                    n = min(8, i + 1 - s0)
                    for k in range(n):
                        S.pe(lambda e, s0=s0, k=k: e.transpose(out=ptb[:, k, :],
                                                               in_=nmask[:, (s0 + k) * 128:(s0 + k + 1) * 128],
                                                               identity=ident[:]), r=[nmask, ident], w=[(ptb, k)])
                    S.act(lambda e, s0=s0, n=n: e.copy(out=nmT[:, s0:s0 + n, j * 128:(j + 1) * 128],
                                                       in_=ptb[:, 0:n, :]), r=[ptb], w=[(nmT, ('b', s0, j))])
            steps.append(fin_mask)
        out = []
        for pair in ((0, 1), (2, 3)):
            la, lb = chains[pair[0]], chains[pair[1]]
            for k in range(max(len(la), len(lb))):
                if k < len(la):
                    out.append(la[k])
                if k < len(lb):
                    out.append(lb[k])
        return out

    for sq in range(nseq):
        tb = sq * SEQ
        for h in range(4):
            S.dma('sp', qi[h][0:32, :], dr['qi'][h * 32:(h + 1) * 32, tb:tb + SEQ], r=[], w=[(qi[h], 'q')])
        S.dma('sp', ki[0:32, :], dr['ki'][:, tb:tb + SEQ], r=[], w=[(ki, 'q')])
        S.dma('sp', wi[:], dr['wi'][tb:tb + SEQ, :].rearrange("(n p) c -> p n c", p=128), r=[], w=[wi])
        S.act(lambda e: e.activation(out=absw[:], in_=wi[:], func=AF.Abs), r=[wi], w=[absw])
        S.act(lambda e: e.activation(out=sgnw[:], in_=wi[:], func=AF.Sign), r=[wi], w=[sgnw])
        S.dma('sp', kcT[0:64, :], dr['kcd'][:, tb:tb + SEQ], r=[], w=[(kcT, 'q')])
        S.dma('sp', vcx[:, :, 0:64], dr['vcd'][tb:tb + SEQ, :].rearrange("(n p) c -> p n c", p=128), r=[], w=[vcx])
        for h in range(8):
            S.dma('sp', qcall[h][0:64, :], dr['qc'][h * 64:(h + 1) * 64, tb:tb + SEQ], r=[], w=[(qcall[h], 'q')])
        import os
        STOP = int(os.environ.get('STOP', '0'))
        if STOP == 1:
            break
        for st in index_steps(0):
            st()
        if STOP == 2:
            break
        jobs = []
        for c in range(4):
            nmT = nmTs[c % 2]
            nxt = index_steps(c + 1) if c < 3 else []
            per = (len(nxt) + 7) // 8
            for h in range(8):
                sl = nxt[h * per:(h + 1) * per]
                if os.environ.get('NOPRE'):
                    for st in sl:
                        st()
                    sl = []
                jobs.append(dict(c=c, tiles=causal_tiles(c, None), qT=qcall[h], kT=kcT, vext=vcx,
                                 extra=(lambda kt: ident[:], lambda kt, c, lo, hi, nmT=nmT: nmT[:, kt, lo:hi],
                                        [ident, nmT]),
                                 fin=make_fin(64 * h),
                                 pre=((lambda sl=sl: [st() for st in sl]) if sl else None)))
        import os
        if not os.environ.get('SKIP_DSA'):
            attn_run(S, jobs, STps, PT, Oacc, LA=1)

        def moba_load(h, tb=tb):
            b = h % 2
            S.dma('sp', qds[b][0:64, :], dr['qd'][h * 64:(h + 1) * 64, tb:tb + SEQ], r=[], w=[(qds[b], 'q')])
            S.dma('sp', kds[b][0:64, :], dr['kd'][h * 64:(h + 1) * 64, tb:tb + SEQ], r=[], w=[(kds[b], 'q')])
            S.dma('sp', vdxs[b][:, :, 0:64], dr['vdd'][tb:tb + SEQ, h * 64:(h + 1) * 64].rearrange(
                "(n p) c -> p n c", p=128), r=[], w=[vdxs[b]])

        def gate_a(h):
            b = h % 2
            qd, kd, kmb = qds[b], kds[b], kmbs[b]
            S.dve(lambda e: e.tensor_reduce(out=kmf[:], in_=kd[0:64, :].rearrange("p (j k) -> p j k", k=256),
                                            axis=AX.X, op=ALU.add), r=[(kd, 'q')], w=[kmf])
            S.dve(lambda e: e.tensor_scalar(out=kmb[0:64, :], in0=kmf[:], scalar1=1.0 / 256, scalar2=None,
                                            op0=ALU.mult), r=[kmf], w=[(kmb, 'm')])
            for i in range(16):
                S.pe(lambda e, i=i: e.matmul(pl[:, i * 8:(i + 1) * 8], lhsT=qd[:, i * 128:(i + 1) * 128], rhs=kmb[:],
                                             start=True, stop=True), r=[qd, kmb], w=[(pl, i)])
            S.dve(lambda e: e.tensor_tensor(out=gsb[:], in0=pl[:, 0:128], in1=mbias[:], op=ALU.add),
                  r=[pl, mbias], w=[gsb])
            for i in range(16):
                m = m8[i % 2]
                S.dve(lambda e, i=i, m=m: e.max(out=m[:], in_=gsb[:, i * 8:(i + 1) * 8]), r=[(gsb, i)], w=[m])
                S.dve(lambda e, i=i, m=m: e.tensor_scalar(out=gsb[:, i * 8:(i + 1) * 8], in0=gsb[:, i * 8:(i + 1) * 8],
                                                          scalar1=m[:, 2:3], scalar2=None, op0=ALU.is_ge),
                      r=[(gsb, i), m], w=[(gsb, i)])
            S.dve(lambda e: e.tensor_tensor(out=gsb[:], in0=gsb[:], in1=mpast[:], op=ALU.mult), r=[gsb, mpast], w=[gsb])
            S.dve(lambda e: e.tensor_tensor(out=gsb[:], in0=gsb[:], in1=mown[:], op=ALU.add), r=[gsb, mown], w=[gsb])
            S.dve(lambda e: e.tensor_scalar(out=ns8all[:, :, 0:8], in0=gsb[:].rearrange("p (i k) -> p i k", k=8),
                                            scalar1=-NEG, scalar2=NEG, op0=ALU.mult, op1=ALU.add),
                  r=[gsb], w=[ns8all])

        def gate_b(h):
            qd = qds[h % 2]
            for half in range(2):
                for k in range(8):
                    i = half * 8 + k
                    S.pe(lambda e, k=k, i=i: e.transpose(out=ptb[0:8, k, :], in_=ns8all[:, i, 0:8],
                                                         identity=ident[:]), r=[ns8all, ident], w=[(ptb, k)])
                S.dve(lambda e, half=half: e.tensor_copy(
                    out=qd[64:72, half * 1024:(half + 1) * 1024].rearrange("p (k q) -> p k q", q=128),
                    in_=ptb[0:8, :, :]), r=[ptb], w=[(qd, ('n', half))])

        if STOP == 3:
            break
        moba_load(0)
        gate_a(0)
        if STOP == 4:
            break
        gate_b(0)
        if STOP == 5:
            break
        NH_ = int(os.environ.get('MOBA_H', '8'))
        for h in range(0 if not os.environ.get('SKIP_MOBA') else 8, NH_):
            b = h % 2
            if h + 1 < 8:
                moba_load(h + 1)
            jobs = []
            for c in range(4):
                jobs.append(dict(c=c, tiles=causal_tiles(c, tri), qT=qds[b], kT=kds[b], vext=vdxs[b],
                                 fin=make_fin(512 + 64 * h),
                                 pre=((lambda h=h: gate_a(h + 1)) if (c == 1 and h + 1 < 8 and not os.environ.get('NOGATE')) else None)))
            attn_run(S, jobs, STps, PT, Oacc, LA=1)
            if h + 1 < 8:
                gate_b(h + 1)
        S.dma('sp', dr['o_tok'][tb:tb + SEQ, :].rearrange("(n p) c -> p n c", p=128), otok[:],
              r=[otok], w=[DW(S, dr['o_tok'])])
    S.flush()


def attn_chunk_v1(S, c, tiles, qT, kT, extra, vext, ident, STps, PT, Oacc, finalize, tag, qdep=None):
    cover = {}
    for n, (kt, lo, hi, bt, blo) in enumerate(tiles):
        for j in range(lo // 128, hi // 128):
            cover.setdefault(j, []).append(n)

    qd_ = qdep if qdep is not None else qT

    def qk(n):
        kt, lo, hi, bt, blo = tiles[n]
        ps = STps[n % 2]
        nterm = 1 + (1 if extra else 0) + (1 if bt is not None else 0)
        S.pe(lambda e: e.matmul(ps[:, lo:hi], lhsT=kT[:, kt * 128:(kt + 1) * 128],
                                rhs=qT[:, c * 512 + lo:c * 512 + hi], start=True, stop=(nterm == 1)),
             r=[kT, qd_], w=[ps])
        k = 1
        if extra:
            k += 1
            S.pe(lambda e: e.matmul(ps[:, lo:hi], lhsT=extra[0](kt), rhs=extra[1](kt, c, lo, hi),
                                    start=False, stop=(k == nterm), skip_group_check=True),
                 r=list(extra[2]), w=[ps])
        if bt is not None:
            S.pe(lambda e: e.matmul(ps[:, blo:blo + 128], lhsT=ident[:], rhs=bt[:], start=False, stop=True,
                                    skip_group_check=True), r=[ident, bt], w=[ps])

    qk(0)
    for n, (kt, lo, hi, bt, blo) in enumerate(tiles):
        if n + 1 < len(tiles):
            qk(n + 1)
        ps, p = STps[n % 2], PT[n % 2]
        S.act(lambda e, ps=ps, p=p, lo=lo, hi=hi: e.activation(out=p[:, lo:hi], in_=ps[:, lo:hi], func=AF.Exp),
              r=[ps], w=[p])
        for j in range(lo // 128, hi // 128):
            S.pe(lambda e, p=p, j=j, kt=kt, n=n: e.matmul(
                Oacc[j][:, 0:65], lhsT=p[:, j * 128:(j + 1) * 128], rhs=vext[:, kt, :],
                start=(cover[j][0] == n), stop=(cover[j][-1] == n), skip_group_check=True),
                r=[p, vext], w=[Oacc[j]])
            if cover[j][-1] == n:
                finalize(c, j, Oacc[j])


def causal_tiles_v1(c, tri):
    out = []
    for kt in range(4 * c + 4):
        if kt < 4 * c:
            out.append((kt, 0, 512, None, 0))
        else:
            lo = (kt - 4 * c) * 128
            out.append((kt, lo, 512, tri, lo))
    return out


def dsa_moba_stage_v1(S, prm, dr, nseq, cpack):
    C = Consts(S, cpack)
    ident = C.get('ident', BF16)
    tri = C.get('tri_ge', BF16)
    E8 = C.get('E8', BF16)
    triqs = C.get('tri_qs', F32)
    mbias, mpast, mown = C.get('mbias'), C.get('mpast'), C.get('mown')
    qi = [S.sb([32, SEQ], BF16, f'qi{h}') for h in range(4)]
    ki = S.sb([32, SEQ], BF16, 'ki')
    wi = S.sb([128, 16, 4], F32, 'wi')
    absw = S.sb([128, 16, 4], F32, 'absw')
    sgnw = S.sb([128, 16, 4], F32, 'sgnw')
    score = S.sb([128, SEQ], F32, 'score')
    rl = [S.sb([128, 512], F32, f'rl{i}') for i in range(2)]
    junkb = S.sb([128, SEQ], BF16, 'junkb')
    nmask = S.sb([128, SEQ], BF16, 'nmask')
    nmT = S.sb([128, 16, 512], BF16, 'nmT')
    kcT = S.sb([64, SEQ], BF16, 'kcT')
    vcx = S.sb([128, 16, 65], BF16, 'vcx')
    vdx = S.sb([128, 16, 65], BF16, 'vdx')
    S.pool(lambda e: e.memset(vcx[:], 1.0), r=[], w=[vcx])
    S.pool(lambda e: e.memset(vdx[:], 1.0), r=[], w=[vdx])
    qcall = [S.sb([64, SEQ], BF16, f'qcall{h}') for h in range(8)]
    qd = S.sb([64, SEQ], BF16, 'qd')
    kd = S.sb([64, SEQ], BF16, 'kd')
    kmf = S.sb([64, 8], F32, 'kmf')
    kmb = S.sb([64, 8], BF16, 'kmb')
    negsel8 = S.sb([8, SEQ], BF16, 'negsel8')
    otok = S.sb([128, 16, 1024], BF16, 'otok')
    PT = [S.sb([128, 512], BF16, f'PT{i}') for i in range(2)]
    st5 = [S.sb([128, 8], F32, f'st5{i}') for i in range(2)]
    gs = [S.sb([128, 8], F32, f'gs{i}') for i in range(2)]
    m8 = [S.sb([128, 8], F32, f'm8{i}') for i in range(2)]
    ns8 = [S.sb([128, 8], BF16, f'ns8{i}') for i in range(2)]
    fsc = [S.sb([128, 1], F32, f'fsc{i}') for i in range(4)]
    STps = [S.ps([128, 512], F32, f'st{i}') for i in range(2)]
    Oacc = [S.ps([128, 512], F32, f'oa{i}') for i in range(4)]
    ptb = S.ps([128, 8, 128], BF16, 'ptb')
    pl = S.ps([128, 512], F32, 'pl')
    fcount = [0]

    def make_fin(col0):
        def fin(c, j, O):
            i = 4 * c + j
            f = fsc[fcount[0] % 4]
            fcount[0] += 1
            S.dve(lambda e: e.tensor_scalar(out=f[:], in0=O[:, 64:65], scalar1=1e-30, scalar2=None, op0=ALU.max),
                  r=[O], w=[f])
            S.dve(lambda e: e.reciprocal(out=f[:], in_=f[:]), r=[f], w=[f])
            S.dve(lambda e: e.tensor_scalar(out=otok[:, i, col0:col0 + 64], in0=O[:, 0:64], scalar1=f[:, 0:1],
                                            scalar2=None, op0=ALU.mult), r=[O, f], w=[(otok, (i, col0))])
        return fin

    for sq in range(nseq):
        tb = sq * SEQ
        for h in range(4):
            S.dma('sp', qi[h][:], dr['qi'][h * 32:(h + 1) * 32, tb:tb + SEQ], r=[], w=[qi[h]])
        S.dma('sp', ki[:], dr['ki'][:, tb:tb + SEQ], r=[], w=[ki])
        S.dma('sp', wi[:], dr['wi'][tb:tb + SEQ, :].rearrange("(n p) c -> p n c", p=128), r=[], w=[wi])
        S.act(lambda e: e.activation(out=absw[:], in_=wi[:], func=AF.Abs), r=[wi], w=[absw])
        S.act(lambda e: e.activation(out=sgnw[:], in_=wi[:], func=AF.Sign), r=[wi], w=[sgnw])
        S.dma('sp', kcT[:], dr['kcd'][:, tb:tb + SEQ], r=[], w=[kcT])
        S.dma('sp', vcx[:, :, 0:64], dr['vcd'][tb:tb + SEQ, :].rearrange("(n p) c -> p n c", p=128), r=[], w=[vcx])
        for h in range(8):
            S.dma('sp', qcall[h][:], dr['qc'][h * 64:(h + 1) * 64, tb:tb + SEQ], r=[], w=[qcall[h]])
        for c in range(4):
            for j in range(4):
                i = 4 * c + j
                W = 128 * (i + 1)
                if i < 2:
                    for st_ in range(i + 1):
                        if st_ == i:
                            S.pool(lambda e, st_=st_, j=j: e.tensor_copy(out=nmT[:, st_, j * 128:(j + 1) * 128],
                                                                         in_=tri[:]), r=[tri], w=[(nmT, (st_, j))])
                        else:
                            S.pool(lambda e, st_=st_, j=j: e.memset(nmT[:, st_, j * 128:(j + 1) * 128], 0.0),
                                   r=[], w=[(nmT, (st_, j))])
                    continue
                for h in range(4):
                    for sc in range((W + 511) // 512):
                        n = min(512, W - 512 * sc)
                        rb = rl[(h * 4 + sc) % 2]
                        S.pe(lambda e, h=h, sc=sc, n=n, i=i: e.matmul(pl[:, 0:n], lhsT=qi[h][:, i * 128:(i + 1) * 128],
                                                                       rhs=ki[:, sc * 512:sc * 512 + n], start=True,
                                                                       stop=True), r=[qi[h], ki], w=[pl])
                        S.act(lambda e, rb=rb, n=n, i=i, h=h: e.activation(out=rb[:, 0:n], in_=pl[:, 0:n], func=AF.Relu,
                                                                           scale=absw[:, i, h:h + 1]),
                              r=[pl, absw], w=[rb])
                        if h == 0:
                            S.dve(lambda e, rb=rb, n=n, sc=sc, i=i, h=h: e.tensor_scalar(
                                out=score[:, sc * 512:sc * 512 + n], in0=rb[:, 0:n], scalar1=sgnw[:, i, h:h + 1],
                                scalar2=None, op0=ALU.mult), r=[rb, sgnw], w=[(score, sc)])
                        else:
                            S.dve(lambda e, rb=rb, n=n, sc=sc, i=i, h=h: e.scalar_tensor_tensor(
                                out=score[:, sc * 512:sc * 512 + n], in0=rb[:, 0:n], scalar=sgnw[:, i, h:h + 1],
                                in1=score[:, sc * 512:sc * 512 + n], op0=ALU.mult, op1=ALU.add),
                                r=[rb, sgnw, (score, sc)], w=[(score, sc)])
                s5 = st5[i % 2]
                S.dve(lambda e, s5=s5, W=W: e.tensor_reduce(out=s5[:, 5:6], in_=score[:, 0:W], axis=AX.X, op=ALU.max),
                      r=[score], w=[(s5, 5)])
                S.dve(lambda e, s5=s5, W=W: e.tensor_reduce(out=s5[:, 0:1], in_=score[:, 0:W], axis=AX.X, op=ALU.min),
                      r=[score], w=[(s5, 0)])
                S.dve(lambda e, s5=s5: e.tensor_tensor(out=s5[:, 1:2], in0=s5[:, 5:6], in1=s5[:, 0:1], op=ALU.subtract),
                      r=[(s5, 5), (s5, 0)], w=[(s5, 1)])
                S.dve(lambda e, i=i: e.tensor_tensor(out=score[:, i * 128:(i + 1) * 128],
                                                     in0=score[:, i * 128:(i + 1) * 128], in1=triqs[:], op=ALU.add),
                      r=[score, triqs], w=[score])
                for it in range(NBIS):
                    S.dve(lambda e, s5=s5: e.tensor_scalar(out=s5[:, 1:2], in0=s5[:, 1:2], scalar1=0.5, scalar2=None,
                                                           op0=ALU.mult), r=[(s5, 1)], w=[(s5, 1)])
                    S.dve(lambda e, s5=s5: e.tensor_tensor(out=s5[:, 2:3], in0=s5[:, 0:1], in1=s5[:, 1:2], op=ALU.add),
                          r=[(s5, 0), (s5, 1)], w=[(s5, 2)])
                    S.dve(lambda e, s5=s5, W=W: e.tensor_scalar(out=junkb[:, 0:W], in0=score[:, 0:W],
                                                                scalar1=s5[:, 2:3], scalar2=0.0, op0=ALU.is_ge,
                                                                op1=ALU.add, accum_out=s5[:, 3:4]),
                          r=[score, (s5, 2)], w=[junkb, (s5, 3)])
                    S.dve(lambda e, s5=s5: e.tensor_scalar(out=s5[:, 4:5], in0=s5[:, 3:4], scalar1=255.5, scalar2=None,
                                                           op0=ALU.is_ge), r=[(s5, 3)], w=[(s5, 4)])
                    S.dve(lambda e, s5=s5: e.scalar_tensor_tensor(out=s5[:, 0:1], in0=s5[:, 1:2], scalar=s5[:, 4:5],
                                                                  in1=s5[:, 0:1], op0=ALU.mult, op1=ALU.add),
                          r=[(s5, 1), (s5, 4), (s5, 0)], w=[(s5, 0)])
                S.dve(lambda e, s5=s5, W=W: e.tensor_scalar(out=nmask[:, 0:W], in0=score[:, 0:W], scalar1=s5[:, 0:1],
                                                            scalar2=NEG, op0=ALU.is_lt, op1=ALU.mult),
                      r=[score, (s5, 0)], w=[nmask])
                for s0 in range(0, i + 1, 8):
                    n = min(8, i + 1 - s0)
                    for k in range(n):
                        S.pe(lambda e, s0=s0, k=k: e.transpose(out=ptb[:, k, :],
                                                               in_=nmask[:, (s0 + k) * 128:(s0 + k + 1) * 128],
                                                               identity=ident[:]), r=[nmask, ident], w=[(ptb, k)])
                    S.act(lambda e, s0=s0, n=n, j=j: e.copy(out=nmT[:, s0:s0 + n, j * 128:(j + 1) * 128],
                                                            in_=ptb[:, 0:n, :]), r=[ptb], w=[(nmT, ('b', s0, j))])
            for h in range(8):
                attn_chunk_v1(S, c, causal_tiles_v1(c, None), qcall[h], kcT,
                           (lambda kt: ident[:], lambda kt, c, lo, hi: nmT[:, kt, lo:hi], [ident, nmT]),
                           vcx, ident, STps, PT, Oacc, make_fin(64 * h), 'dsa')
        for h in range(8):
            S.dma('sp', qd[:], dr['qd'][h * 64:(h + 1) * 64, tb:tb + SEQ], r=[], w=[qd])
            S.dma('sp', kd[:], dr['kd'][h * 64:(h + 1) * 64, tb:tb + SEQ], r=[], w=[kd])
            S.dma('sp', vdx[:, :, 0:64], dr['vdd'][tb:tb + SEQ, h * 64:(h + 1) * 64].rearrange(
                "(n p) c -> p n c", p=128), r=[], w=[vdx])
            S.dve(lambda e: e.tensor_reduce(out=kmf[:], in_=kd[:].rearrange("p (j k) -> p j k", k=256), axis=AX.X,
                                            op=ALU.add), r=[kd], w=[kmf])
            S.dve(lambda e: e.tensor_scalar(out=kmb[:], in0=kmf[:], scalar1=1.0 / 256, scalar2=None, op0=ALU.mult),
                  r=[kmf], w=[kmb])
            for i in range(16):
                b = i % 2
                S.pe(lambda e, i=i: e.matmul(pl[:, 0:8], lhsT=qd[:, i * 128:(i + 1) * 128], rhs=kmb[:], start=True,
                                             stop=True), r=[qd, kmb], w=[pl])
                g_ = gs[b]
                S.dve(lambda e, g_=g_, i=i: e.tensor_tensor(out=g_[:], in0=pl[:, 0:8], in1=mbias[:, i * 8:(i + 1) * 8],
                                                           op=ALU.add), r=[pl, mbias], w=[g_])
                S.dve(lambda e, g_=g_, b=b: e.max(out=m8[b][:], in_=g_[:]), r=[g_], w=[m8[b]])
                S.dve(lambda e, g_=g_, b=b: e.tensor_scalar(out=g_[:], in0=g_[:], scalar1=m8[b][:, 2:3], scalar2=None,
                                                            op0=ALU.is_ge), r=[g_, m8[b]], w=[g_])
                S.dve(lambda e, g_=g_, i=i: e.tensor_tensor(out=g_[:], in0=g_[:], in1=mpast[:, i * 8:(i + 1) * 8],
                                                           op=ALU.mult), r=[g_, mpast], w=[g_])
                S.dve(lambda e, g_=g_, i=i: e.tensor_tensor(out=g_[:], in0=g_[:], in1=mown[:, i * 8:(i + 1) * 8],
                                                           op=ALU.add), r=[g_, mown], w=[g_])
                S.dve(lambda e, g_=g_, b=b: e.tensor_scalar(out=ns8[b][:], in0=g_[:], scalar1=-NEG, scalar2=NEG,
                                                            op0=ALU.mult, op1=ALU.add), r=[g_], w=[ns8[b]])
                S.pe(lambda e, b=b: e.transpose(out=ptb[0:8, b, :], in_=ns8[b][:], identity=ident[:]),
                     r=[ns8[b], ident], w=[(ptb, b)])
                S.act(lambda e, b=b, i=i: e.copy(out=negsel8[:, i * 128:(i + 1) * 128], in_=ptb[0:8, b, :]),
                      r=[(ptb, b)], w=[(negsel8, i // 4)])
            for c in range(4):
                attn_chunk_v1(S, c, causal_tiles_v1(c, tri), qd, kd,
                           (lambda kt: E8[0:8, kt * 128:(kt + 1) * 128], lambda kt, c, lo, hi: negsel8[:, c * 512 + lo:c * 512 + hi],
                            [E8, negsel8]),
                           vdx, ident, STps, PT, Oacc, make_fin(512 + 64 * h), 'moba')
        S.dma('sp', dr['o_tok'][tb:tb + SEQ, :].rearrange("(n p) c -> p n c", p=128), otok[:],
              r=[otok], w=[DW(S, dr['o_tok'])])
    S.flush()


NCORES = 8
TPC = 2 * SEQ


def build_program(stages=None):
    nc = bass.Bass("TRN2", target_bir_lowering=False)
    ins = {}

    def din(name, shape, dt=F32):
        ins[name] = nc.dram_tensor(name, list(shape), dt, kind="ExternalInput").ap()
        return ins[name]

    def scr(name, shape, dt=BF16):
        return nc.dram_tensor(name, list(shape), dt, kind="Internal").ap()

    x = din('x', [TPC, D])
    pos = din('pos', [TPC], I32)
    cp = din('cpack', [128, CP_N])
    P = {}
    for L in range(2):
        for f in ('ffn1', 'ffn2'):
            P[f'{f}_norm{L}'] = din(f'{f}_norm{L}', [D])
            P[f'{f}_wg{L}'] = din(f'{f}_wg{L}', [D, DFF])
            P[f'{f}_wu{L}'] = din(f'{f}_wu{L}', [D, DFF])
            P[f'{f}_wd{L}'] = din(f'{f}_wd{L}', [DFF, D])
        P[f'mix_norm{L}'] = din(f'mix_norm{L}', [D])
    ev = {'w_in': din('ev_w_in', [D, 2468]), 'mix_norm': P['mix_norm0'], 'pos': pos,
          'sgu_norm': din('ev_sgu_norm', [256]), 'sgu_wT': din('ev_sgu_wT', [4, 128, 128]),
          'sgu_bT': din('ev_sgu_bT', [128, 4]),
          'cmp_w1_k': din('ev_w1k', [2048, 256]), 'cmp_w2_k': din('ev_w2k', [256, 64]),
          'cmp_posT_k': din('ev_pk', [64, 32]),
          'cmp_w1_v': din('ev_w1v', [2048, 256]), 'cmp_w2_v': din('ev_w2v', [256, 64]),
          'cmp_posT_v': din('ev_pv', [64, 32])}
    ev_w_out = din('ev_w_out', [D, D])
    od = {'w_in': din('od_w_in', [D, 2340]), 'mix_norm': P['mix_norm1'], 'pos': pos}
    od_w_out = din('od_w_out', [D, D])
    fin_g = din('final_norm', [D])
    y = nc.dram_tensor('y', [TPC, D], F32, kind="ExternalOutput").ap()
    xa = scr('xa', [TPC, D], F32)
    xb = scr('xb', [TPC, D], F32)
    T_ = TPC
    dre = {'a_tok': scr('a_tok', [T_, 512]), 'qraw': scr('qraw', [768, T_]), 'qrot': scr('qrot', [768, T_]),
           'kc': scr('kc', [192, T_]), 'vc': scr('vc', [192, T_]), 'ksl': scr('ksl', [192, T_]),
           'kw': scr('kw', [192, T_]), 'vsl': scr('vsl', [T_, 192]), 'vw': scr('vw', [T_, 192]),
           'gate': scr('gate', [T_, 36], F32), 'o_tok': scr('o_tok0', [T_, 1024])}
    dro = {'qc': scr('qc', [512, T_]), 'kcd': scr('kcd', [64, T_]), 'vcd': scr('vcd', [T_, 64]),
           'qi': scr('qi', [128, T_]), 'ki': scr('ki', [32, T_]), 'wi': scr('wi', [T_, 4], F32),
           'qd': scr('qd', [512, T_]), 'kd': scr('kd', [512, T_]), 'vdd': scr('vdd', [T_, 512]),
           'o_tok': scr('o_tok1', [T_, 1024])}
    S = Sched(nc)
    w16 = {'wg': scr('wg16', [128, DFF // 256, 8, 256]), 'wu': scr('wu16', [128, DFF // 256, 8, 256]),
           'wd': scr('wd16', [128, NFC, D])}

    def ffn(xi, xo, f, L, fg=None):
        ffn_stage(S, xi, xo, P[f'{f}_norm{L}'], P[f'{f}_wg{L}'], P[f'{f}_wu{L}'], P[f'{f}_wd{L}'], TPC, cp_d, fg, w16)
    cp_d = {'ident': cp[:, CP_OFF['ident'][0]:CP_OFF['ident'][0] + 128]}
    ffn(x, xa, 'ffn1', 0)
    even_proj(S, xa, ev, dre, TPC, cp)
    nsa_stage(S, ev, dre, 2, cp)
    outproj_stage(S, xa, xb, dre['o_tok'], ev_w_out, TPC, cp)
    ffn(xb, xa, 'ffn2', 0)
    ffn(xa, xb, 'ffn1', 1)
    odd_proj(S, xb, od, dro, TPC, cp)
    (dsa_moba_stage if USE_NEW_ODD else dsa_moba_stage_v1)(S, od, dro, 2, cp)
    outproj_stage(S, xb, xa, dro['o_tok'], od_w_out, TPC, cp)
    ffn(xa, y, 'ffn2', 1, fin_g)
    return nc, S


def kernel(**inp):
    inp = {k: np.asarray(v) for k, v in inp.items()}
    nc, S = build_program()
    cpk = host_consts()
    c = np.ascontiguousarray
    shared = {'cpack': cpk}
    for L in range(2):
        for f in ('ffn1', 'ffn2'):
            shared[f'{f}_norm{L}'] = c(inp[f'{f}_norm'][L])
            shared[f'{f}_wg{L}'] = c(inp[f'{f}_w_gate'][L])
            shared[f'{f}_wu{L}'] = c(inp[f'{f}_w_up'][L])
            shared[f'{f}_wd{L}'] = c(inp[f'{f}_w_down'][L])
        shared[f'mix_norm{L}'] = c(inp['mix_norm'][L])
    shared.update({
        'ev_w_in': c(inp['ev_w_in'][0]), 'ev_sgu_norm': c(inp['ev_sgu_norm'][0]),
        'ev_sgu_wT': c(inp['ev_sgu_w'][0].transpose(0, 2, 1)), 'ev_sgu_bT': c(inp['ev_sgu_b'][0].T),
        'ev_w1k': c(inp['ev_cmp_w1_k'][0]), 'ev_w2k': c(inp['ev_cmp_w2_k'][0]), 'ev_pk': c(inp['ev_cmp_pos_k'][0].T),
        'ev_w1v': c(inp['ev_cmp_w1_v'][0]), 'ev_w2v': c(inp['ev_cmp_w2_v'][0]), 'ev_pv': c(inp['ev_cmp_pos_v'][0].T),
        'ev_w_out': c(inp['ev_w_out'][0]), 'od_w_in': c(inp['od_w_in'][0]), 'od_w_out': c(inp['od_w_out'][0]),
        'final_norm': c(inp['final_norm'])})
    in_maps = []
    for k in range(NCORES):
        m = dict(shared)
        m['x'] = c(inp['x'][2 * k:2 * k + 2].reshape(TPC, D))
        m['pos'] = c(inp['positions'][2 * k:2 * k + 2].reshape(TPC).astype(np.int32))
        in_maps.append(m)
    res = run_bass_kernel_spmd(nc, in_maps, core_ids=list(range(NCORES)))
    out = np.stack([np.asarray(r['y']).reshape(2, SEQ, D) for r in res.results], axis=0)
    return out.reshape(16, SEQ, D).astype(np.float32)
```
